# Optimizing a Trainium2 kernel written in Bass

```python
import math
import jax, jax.numpy as jnp
from jax import lax
import numpy as np


D_MODEL = 1024
BATCH = 2
SEQ = 8192
DEPTH = 2

CHUNK = 64
N_EVEN = (DEPTH + 1) // 2
N_ODD = DEPTH // 2
CONV_W = 4
NORM_EPS = 1e-6
F32 = jnp.float32

W_MIX = D_MODEL
W_A = D_MODEL // 2
NH_A = 8
HW_A = W_A // NH_A
RG_C = 8.0
NH_B = 4
DK_B = 64
DV_B = 128
K_B = NH_B * DK_B
V_B = NH_B * DV_B
R_GATE = 16
GATE_NORM = 16.0
P_AB = 2 * W_A + 2 * K_B + 2 * V_B + R_GATE

NH_C = 8
DH_C = 128
W_C = NH_C * DH_C
P_C = 4 * W_C + 2 * NH_C

N_GROUPS = 4
EXP_PER_GROUP = 8
N_EXPERTS = N_GROUPS * EXP_PER_GROUP
TOP_K = 2
D_EXPERT = 512
ROW_BLOCK = 128

kernel_name = 'hybrid_rglru_gla_gdn_hmoe_adaln'


def _offsets(sizes):
    out, acc = [], 0
    for s in sizes[:-1]:
        acc += s
        out.append(acc)
    return out


def rms_norm(x, g):
    x32 = x.astype(F32)
    y = x32 * lax.rsqrt(jnp.mean(x32 * x32, axis=-1, keepdims=True) + NORM_EPS)
    return (y * g.astype(F32)).astype(x.dtype)


def head_rms_norm(o, g):
    return o * lax.rsqrt(jnp.mean(o * o, axis=-1, keepdims=True) + NORM_EPS) * g.astype(F32)


def l2_norm(x):
    return x * lax.rsqrt(jnp.sum(x * x, axis=-1, keepdims=True) + NORM_EPS)


def causal_conv(x, w):
    return lax.conv_general_dilated(
        x, w[:, None, :].astype(x.dtype), window_strides=(1,), padding=[(CONV_W - 1, 0)],
        dimension_numbers=('NWC', 'WIO', 'NWC'), feature_group_count=x.shape[-1])


def to_chunks(t):
    B, S, H = t.shape[:3]
    t = t.reshape((B, S // CHUNK, CHUNK, H) + t.shape[3:])
    return jnp.moveaxis(t, (1, 3), (0, 2))


def from_chunks(t):
    t = jnp.moveaxis(t, (0, 2), (1, 3))
    B, N, C, H = t.shape[:4]
    return t.reshape((B, N * C, H) + t.shape[4:])


def _linear_recurrence_combine(left, right):
    a_l, b_l = left
    a_r, b_r = right
    return a_l * a_r, a_r * b_l + b_r


def rg_lru(x, wa, ba, wx, bx, lam):
    B, S, _ = x.shape
    xh = x.reshape(B, S, NH_A, HW_A)
    r = jax.nn.sigmoid((jnp.einsum('bshi,hij->bshj', xh, wa).reshape(B, S, W_A) + ba).astype(F32))
    i = jax.nn.sigmoid((jnp.einsum('bshi,hij->bshj', xh, wx).reshape(B, S, W_A) + bx).astype(F32))
    log_a = -RG_C * r * jax.nn.softplus(-lam.astype(F32))
    a = jnp.exp(log_a)
    b = jnp.sqrt(-jnp.expm1(2.0 * log_a)) * (i * x.astype(F32))
    _, h = lax.associative_scan(_linear_recurrence_combine, (a, b), axis=1)
    return h


def gla_chunked(q, k, v, lg):
    B, H = q.shape[0], q.shape[2]
    qc, kc, vc = to_chunks(q), to_chunks(k), to_chunks(v)
    gcum = jnp.cumsum(to_chunks(lg), axis=3)
    causal = jnp.tril(jnp.ones((CHUNK, CHUNK), dtype=bool))

    def step(state, inp):
        qb, kb, vb, gb = inp
        g_last = gb[:, :, -1:, :]
        o_inter = jnp.einsum('bhcd,bhde->bhce', qb * jnp.exp(gb), state)
        diff = jnp.where(causal[:, :, None], gb[:, :, :, None, :] - gb[:, :, None, :, :], -jnp.inf)
        att = jnp.einsum('bhid,bhjd,bhijd->bhij', qb, kb, jnp.exp(diff))
        o = o_inter + jnp.einsum('bhij,bhje->bhie', att, vb)
        state = state * jnp.exp(g_last[:, :, 0, :, None]) + jnp.einsum(
            'bhcd,bhce->bhde', kb * jnp.exp(g_last - gb), vb)
        return state, o

    s0 = jnp.zeros((B, H, DK_B, DV_B), F32)
    _, o = lax.scan(step, s0, (qc, kc, vc, gcum))
    return from_chunks(o)


def gated_delta_chunked(q, k, v, beta, g):
    B, H, DV = q.shape[0], q.shape[2], v.shape[-1]
    qc, kc, vc = to_chunks(q), to_chunks(k), to_chunks(v)
    bc = to_chunks(beta)
    gcum = jnp.cumsum(to_chunks(g), axis=-1)
    idx = jnp.arange(CHUNK)
    incl = idx[:, None] >= idx[None, :]
    strict = idx[:, None] > idx[None, :]
    decay = jnp.exp(jnp.where(incl, gcum[..., :, None] - gcum[..., None, :], -jnp.inf))
    k_beta = kc * bc[..., None]
    lower = jnp.where(strict, jnp.einsum('...id,...jd->...ij', k_beta, kc) * decay, 0.0)
    rhs = jnp.concatenate([vc * bc[..., None], k_beta * jnp.exp(gcum)[..., None]], axis=-1)
    sol = lax.linalg.triangular_solve(lower, rhs, left_side=True, lower=True, unit_diagonal=True)
    u_c, w_c = sol[..., :DV], sol[..., DV:]

    def step(state, inp):
        qb, kb, gb, ub, wb, db = inp
        v_new = ub - jnp.einsum('bhck,bhkv->bhcv', wb, state)
        att = jnp.einsum('bhik,bhjk->bhij', qb, kb) * db
        o = jnp.einsum('bhck,bhkv->bhcv', qb * jnp.exp(gb)[..., None], state) + jnp.einsum(
            'bhij,bhjv->bhiv', att, v_new)
        g_last = gb[..., -1:]
        state = state * jnp.exp(g_last)[..., None] + jnp.einsum(
            'bhck,bhcv->bhkv', kb * jnp.exp(g_last - gb)[..., None], v_new)
        return state, o

    s0 = jnp.zeros((B, H, q.shape[-1], DV), F32)
    _, o = lax.scan(step, s0, (qc, kc, gcum, u_c, w_c, decay))
    return from_chunks(o)


def mixer_ab(u, w_in, conv_w, conv_b, rg_wa, rg_ba, rg_wx, rg_bx, rg_lam, gla_wg2, gla_bg2, gla_norm, w_out):
    B, S, _ = u.shape
    xa, ga, q, k, v, og, gl = jnp.split(
        u @ w_in, _offsets([W_A, W_A, K_B, K_B, V_B, V_B, R_GATE]), axis=-1)
    xa = causal_conv(xa, conv_w) + conv_b
    ya = jax.nn.gelu(ga.astype(F32)) * rg_lru(xa, rg_wa, rg_ba, rg_wx, rg_bx, rg_lam)
    q = q.astype(F32).reshape(B, S, NH_B, DK_B) * DK_B ** -0.5
    k = k.astype(F32).reshape(B, S, NH_B, DK_B)
    v = v.astype(F32).reshape(B, S, NH_B, DV_B)
    lg = jax.nn.log_sigmoid((gl @ gla_wg2 + gla_bg2).astype(F32)).reshape(B, S, NH_B, DK_B) / GATE_NORM
    ob = head_rms_norm(gla_chunked(q, k, v, lg), gla_norm) * jax.nn.silu(og.astype(F32)).reshape(B, S, NH_B, DV_B)
    y = jnp.concatenate([ya, ob.reshape(B, S, V_B)], axis=-1).astype(u.dtype)
    return y @ w_out


def mixer_c(u, w_in, conv_w, a_log, dt_bias, norm_g, w_out):
    B, S, _ = u.shape
    qkv, z, b_logit, a_logit = jnp.split(u @ w_in, _offsets([3 * W_C, W_C, NH_C, NH_C]), axis=-1)
    qkv = jax.nn.silu(causal_conv(qkv, conv_w).astype(F32))
    q, k, v = jnp.split(qkv, 3, axis=-1)
    q = l2_norm(q.reshape(B, S, NH_C, DH_C)) * DH_C ** -0.5
    k = l2_norm(k.reshape(B, S, NH_C, DH_C))
    v = v.reshape(B, S, NH_C, DH_C)
    beta = jax.nn.sigmoid(b_logit.astype(F32))
    g = -jnp.exp(a_log.astype(F32)) * jax.nn.softplus(a_logit.astype(F32) + dt_bias.astype(F32))
    o = gated_delta_chunked(q, k, v, beta, g)
    o = head_rms_norm(o, norm_g) * jax.nn.silu(z.astype(F32)).reshape(B, S, NH_C, DH_C)
    return o.reshape(B, S, W_C).astype(u.dtype) @ w_out


def hier_moe(u, w_grp, b_grp, w_rt, b_rt, w1, w3, w2):
    B, S, D = u.shape
    T = B * S
    TK = T * TOP_K
    xt = u.reshape(T, D)
    pg = jax.nn.softmax((xt @ w_grp + b_grp).astype(F32), axis=-1)
    pg_top, g_idx = lax.top_k(pg, 1)
    le = (xt @ w_rt + b_rt).astype(F32).reshape(T, N_GROUPS, EXP_PER_GROUP)
    le = jnp.einsum('tge,tg->te', le, jax.nn.one_hot(g_idx[:, 0], N_GROUPS, dtype=F32))
    pe_top, e_idx = lax.top_k(jax.nn.softmax(le, axis=-1), TOP_K)
    w_tok = pg_top * pe_top / jnp.sum(pe_top, axis=-1, keepdims=True)
    eid = (g_idx * EXP_PER_GROUP + e_idx).reshape(TK)
    tok = jnp.repeat(jnp.arange(T, dtype=jnp.int32), TOP_K)
    wts = w_tok.reshape(TK)
    order = jnp.argsort(eid)
    se = eid[order]
    counts = jnp.bincount(eid, length=N_EXPERTS)
    padded = (counts + ROW_BLOCK - 1) // ROW_BLOCK * ROW_BLOCK
    start_s = jnp.cumsum(counts) - counts
    end_p = jnp.cumsum(padded)
    start_p = end_p - padded
    dest = start_p[se] + jnp.arange(TK) - start_s[se]
    n_blocks = (TK + ROW_BLOCK - 1) // ROW_BLOCK + N_EXPERTS
    n_rows = n_blocks * ROW_BLOCK
    buf_tok = jnp.full((n_rows,), T, jnp.int32).at[dest].set(tok[order])
    buf_w = jnp.zeros((n_rows,), F32).at[dest].set(wts[order])
    blk_e = jnp.minimum(
        jnp.sum(jnp.arange(n_blocks)[:, None] * ROW_BLOCK >= end_p[None, :], axis=1), N_EXPERTS - 1)
    xpad = jnp.concatenate([xt, jnp.zeros((1, D), xt.dtype)], axis=0)
    xb = xpad[buf_tok].reshape(n_blocks, ROW_BLOCK, D)

    def expert_block(args):
        xr, e = args
        return (jax.nn.silu(xr @ w1[e]) * (xr @ w3[e])) @ w2[e]

    yb = lax.map(expert_block, (xb, blk_e))
    y = jnp.zeros((T + 1, D), F32).at[buf_tok].add(yb.reshape(n_rows, D).astype(F32) * buf_w[:, None])
    return y[:T].reshape(B, S, D).astype(u.dtype)


def setup_inputs(seed: int = 0) -> dict:
    key = jax.random.key(seed)
    k = jax.random.split(key, 40)
    D = D_MODEL

    def nrm(i, shape, scale):
        return jax.random.normal(k[i], shape, jnp.float32) * scale

    def unif(i, shape, lo, hi):
        return jax.random.uniform(k[i], shape, jnp.float32, lo, hi)

    a_rg = unif(13, (N_EVEN, W_A), 0.9, 0.999) ** (1.0 / RG_C)
    dt = jnp.exp(unif(21, (N_ODD, NH_C), math.log(1e-3), math.log(1e-1)))
    return {
        'x': nrm(0, (BATCH, SEQ, D), 1.0),
        'c': nrm(1, (BATCH, D), 1.0),
        'norm1': 1.0 + nrm(2, (DEPTH, D), 0.02),
        'norm2': 1.0 + nrm(3, (DEPTH, D), 0.02),
        'w_ada': nrm(4, (DEPTH, D, 6 * D), 0.5 * D ** -0.5),
        'b_ada': nrm(5, (DEPTH, 6 * D), 0.02),
        'w_in_ab': nrm(6, (N_EVEN, D, P_AB), D ** -0.5),
        'conv_a_w': nrm(7, (N_EVEN, CONV_W, W_A), CONV_W ** -0.5),
        'conv_a_b': nrm(8, (N_EVEN, W_A), 0.02),
        'rg_wa': nrm(9, (N_EVEN, NH_A, HW_A, HW_A), HW_A ** -0.5),
        'rg_ba': nrm(10, (N_EVEN, W_A), 0.02),
        'rg_wx': nrm(11, (N_EVEN, NH_A, HW_A, HW_A), HW_A ** -0.5),
        'rg_bx': nrm(12, (N_EVEN, W_A), 0.02),
        'rg_lam': jnp.log(a_rg) - jnp.log1p(-a_rg),
        'gla_wg2': nrm(14, (N_EVEN, R_GATE, K_B), R_GATE ** -0.5),
        'gla_bg2': nrm(15, (N_EVEN, K_B), 0.02),
        'gla_norm': 1.0 + nrm(16, (N_EVEN, DV_B), 0.02),
        'w_out_ab': nrm(17, (N_EVEN, W_MIX, D), W_MIX ** -0.5),
        'w_in_c': nrm(18, (N_ODD, D, P_C), D ** -0.5),
        'conv_c_w': nrm(19, (N_ODD, CONV_W, 3 * W_C), CONV_W ** -0.5),
        'dn_a_log': jnp.log(unif(20, (N_ODD, NH_C), 1.0, 16.0)),
        'dn_dt_bias': dt + jnp.log(-jnp.expm1(-dt)),
        'dn_norm': 1.0 + nrm(22, (N_ODD, DH_C), 0.02),
        'w_out_c': nrm(23, (N_ODD, W_C, D), W_C ** -0.5),
        'moe_w_grp': nrm(24, (DEPTH, D, N_GROUPS), D ** -0.5),
        'moe_b_grp': nrm(25, (DEPTH, N_GROUPS), 0.01),
        'moe_w_rt': nrm(26, (DEPTH, D, N_EXPERTS), D ** -0.5),
        'moe_b_rt': nrm(27, (DEPTH, N_EXPERTS), 0.01),
        'moe_w1': nrm(28, (DEPTH, N_EXPERTS, D, D_EXPERT), D ** -0.5),
        'moe_w3': nrm(29, (DEPTH, N_EXPERTS, D, D_EXPERT), D ** -0.5),
        'moe_w2': nrm(30, (DEPTH, N_EXPERTS, D_EXPERT, D), D_EXPERT ** -0.5),
        'final_norm': 1.0 + nrm(31, (D,), 0.02),
    }


def reference(x, c, norm1, norm2, w_ada, b_ada, w_in_ab, conv_a_w, conv_a_b, rg_wa, rg_ba, rg_wx, rg_bx,
              rg_lam, gla_wg2, gla_bg2, gla_norm, w_out_ab, w_in_c, conv_c_w, dn_a_log, dn_dt_bias, dn_norm,
              w_out_c, moe_w_grp, moe_b_grp, moe_w_rt, moe_b_rt, moe_w1, moe_w3, moe_w2, final_norm):
    cond = jax.nn.silu(c)
    h = x
    for layer in range(DEPTH):
        mod = cond @ w_ada[layer] + b_ada[layer]
        sh1, sc1, gt1, sh2, sc2, gt2 = jnp.split(mod[:, None, :], 6, axis=-1)
        u = rms_norm(h, norm1[layer]) * (1.0 + sc1) + sh1
        j = layer // 2
        if layer % 2 == 0:
            m = mixer_ab(u, w_in_ab[j], conv_a_w[j], conv_a_b[j], rg_wa[j], rg_ba[j], rg_wx[j], rg_bx[j],
                         rg_lam[j], gla_wg2[j], gla_bg2[j], gla_norm[j], w_out_ab[j])
        else:
            m = mixer_c(u, w_in_c[j], conv_c_w[j], dn_a_log[j], dn_dt_bias[j], dn_norm[j], w_out_c[j])
        h = h + gt1 * m
        u = rms_norm(h, norm2[layer]) * (1.0 + sc2) + sh2
        h = h + gt2 * hier_moe(u, moe_w_grp[layer], moe_b_grp[layer], moe_w_rt[layer], moe_b_rt[layer],
                               moe_w1[layer], moe_w3[layer], moe_w2[layer])
    return rms_norm(h, final_norm)
```

```python
import numpy as np
import ml_dtypes
import concourse.bass as bass
import concourse.mybir as mybir
from concourse.bass_utils import run_bass_kernel_spmd

F32 = mybir.dt.float32
BF16 = mybir.dt.bfloat16
AF = mybir.ActivationFunctionType
ALU = mybir.AluOpType
AX = mybir.AxisListType

D = 1024
S = 8192
EPS = 1e-6
NCORES = 8


class T:
    __slots__ = ("h", "w", "r", "name")

    def __init__(self, h, name=""):
        self.h = h
        self.w = None
        self.r = {}
        self.name = name

    def __getitem__(self, idx):
        return self.h[idx]


class KB:
    NDMA_SEM = 6

    def __init__(self, nc):
        self.nc = nc
        self.eng = {"pe": nc.tensor, "act": nc.scalar, "dve": nc.vector, "pool": nc.gpsimd, "sp": nc.sync}
        self.csem = {e: nc.alloc_semaphore("cs_" + e) for e in ("pe", "act", "dve", "pool")}
        self.cnt = {e: 0 for e in self.csem}
        self.pending = {e: False for e in self.csem}
        self.dsem = {}
        self.dcnt = {}
        for q in ("sp", "pool", "act"):
            self.dsem[q] = [nc.alloc_semaphore("ds_%s%d" % (q, i)) for i in range(self.NDMA_SEM)]
            self.dcnt[q] = 0
        self.seen = {e: {} for e in self.eng}
        self.ninst = 0
        self.stack = []
        self.tiles = []
        self.freed = {}
        self.uid = 0

    def sb(self, shape, dt=F32, name=None):
        self.uid += 1
        nm = "%s_%d" % (name or "t", self.uid)
        g = self.nc.sbuf_tensor(nm, list(shape), dt)
        h = g.__enter__()
        self.stack.append(g)
        t = T(h, nm)
        t.r = dict(self.freed)
        self.tiles.append(t)
        return t

    def ps(self, shape, dt=F32, name=None):
        self.uid += 1
        nm = "%s_%d" % (name or "p", self.uid)
        g = self.nc.psum_tensor(nm, list(shape), dt)
        h = g.__enter__()
        self.stack.append(g)
        t = T(h, nm)
        t.r = dict(self.freed)
        self.tiles.append(t)
        return t

    def mark(self):
        return len(self.stack)

    def release(self, mark):
        while len(self.stack) > mark:
            g = self.stack.pop()
            t = self.tiles.pop()
            toks = list(t.r.values()) + ([t.w] if t.w is not None else [])
            for tok in toks:
                o = self.freed.get(tok[0])
                if o is None or o[2] < tok[2]:
                    self.freed[tok[0]] = tok
            g.__exit__(None, None, None)

    def _deps(self, e, reads, writes):
        need = {}

        def add(tok):
            if tok is None:
                return
            key, sem, val = tok
            if self.seen[e].get(key, 0) >= val:
                return
            if key not in need or need[key][1] < val:
                need[key] = (sem, val)

        for t in reads:
            add(t.w)
        for t in writes:
            add(t.w)
            for tok in t.r.values():
                add(tok)
        for key, (sem, val) in need.items():
            self.eng[e].wait_ge(sem, val)
            self.seen[e][key] = val

    def _commit(self, tok, reads, writes):
        key = tok[0]
        for t in reads:
            o = t.r.get(key)
            if o is None or o[2] < tok[2]:
                t.r[key] = tok
        for t in writes:
            t.w = tok
            t.r = {}

    def op(self, e, fn, reads=(), writes=(), inc=True):
        inc = True
        self._deps(e, reads, writes)
        ins = fn(self.eng[e])
        if inc:
            self.cnt[e] += 1
            ins.then_inc(self.csem[e], 1)
            tok = (e, self.csem[e], self.cnt[e])
            self.pending[e] = False
        else:
            tok = (e, self.csem[e], self.cnt[e] + 1)
            self.pending[e] = True
        self._commit(tok, reads, writes)
        self.ninst += 1
        return tok

    def dma(self, q, out, in_, reads=(), writes=(), **kw):
        i = self.dcnt[q]
        self.dcnt[q] += 1
        slot = i % self.NDMA_SEM
        rnd = i // self.NDMA_SEM
        sem = self.dsem[q][slot]
        key = ("d", q, slot)
        if rnd > 0 and self.seen[q].get(key, 0) < 16 * rnd:
            self.eng[q].wait_ge(sem, 16 * rnd)
            self.seen[q][key] = 16 * rnd
        self._deps(q, reads, writes)
        ins = self.eng[q].dma_start(out=out, in_=in_, **kw)
        ins.then_inc(sem, 16)
        tok = (key, sem, 16 * (rnd + 1))
        self._commit(tok, reads, writes)
        self.ninst += 1
        return tok

    def finish(self, toks):
        for e in self.pending:
            assert not self.pending[e], "engine %s ends with a non-incrementing instruction" % e
        toks = list(toks)
        for e in self.csem:
            if self.cnt[e] > 0:
                toks.append((e, self.csem[e], self.cnt[e]))
        for q in self.dsem:
            n = self.dcnt[q]
            for slot in range(self.NDMA_SEM):
                if n > slot:
                    rounds = (n - slot + self.NDMA_SEM - 1) // self.NDMA_SEM
                    toks.append((("d", q, slot), self.dsem[q][slot], 16 * rounds))
        for tok in toks:
            key, sem, val = tok
            if self.seen["sp"].get(key, 0) < val:
                self.eng["sp"].wait_ge(sem, val)
                self.seen["sp"][key] = val


def _bc(ap, shape):
    return ap.to_broadcast(list(shape))


def load_consts(k, nc, consts_ap):
    c = k.sb([128, NCONST, 128], F32, "consts")
    k.dma("sp", c[:], consts_ap, writes=[c])
    cb = k.sb([128, NCONST, 128], BF16, "consts_bf")
    k.op("dve", lambda e: e.tensor_copy(out=cb[:], in_=c[:]), reads=[c], writes=[cb])
    return c, cb


NCONST = 10
C_ID = 0
C_TRI = 1
C_SU = 2
C_BLK = 3
C_M16 = 4
C_MC1 = 5
C_MC2 = 6
C_ONES = 7
C_UT64 = 8
C_CH0 = 9


def make_consts():
    p = np.arange(128)
    i = p[:, None]
    j = p[None, :]
    c = np.zeros((128, NCONST, 128), np.float32)
    same64 = (i // 64) == (j // 64)
    same32 = (i // 32) == (j // 32)
    same16 = (i // 16) == (j // 16)
    c[:, C_ID] = (i == j)
    c[:, C_TRI] = same64 & (i <= j)
    c[:, C_SU] = same64 & (j < i)
    c[:, C_BLK] = same64
    c[:, C_M16] = same16 & (j < i)
    c[:, C_MC1] = same32 & (~same16) & (j < i)
    c[:, C_MC2] = same64 & (~same32) & (j < i)
    c[:, C_ONES] = 1.0
    c[:, C_UT64] = same64 & (i <= j)
    c[:, C_CH0, 0] = (p < 64)
    c[:, C_CH0, 1] = (p >= 64)
    return c


def compute_mod(k, nc, condT_ap, w_ada_ap, b_ada_ap, ncols, outs, ps_pool):
    m0 = k.mark()
    ct = k.sb([128, 8], F32, "ct")
    k.dma("sp", ct[:], condT_ap, writes=[ct])
    sg = k.sb([128, 8], F32, "sg")
    k.op("act", lambda e: e.activation(out=sg[:], in_=ct[:], func=AF.Sigmoid), reads=[ct], writes=[sg])
    cond = k.sb([128, 8], F32, "cond")
    k.op("dve", lambda e: e.tensor_tensor(out=cond[:], in0=ct[:], in1=sg[:], op=ALU.mult), reads=[ct, sg], writes=[cond])
    cbc = k.sb([128, 8, 128], BF16, "cond_bc")
    k.op("dve", lambda e: e.tensor_copy(out=cbc[:], in_=_bc(cond[:].unsqueeze(2), [128, 8, 128])), reads=[cond], writes=[cbc])
    wv = w_ada_ap.rearrange("(k p) n -> p k n", p=128)
    nch = ncols // 512
    wbuf = [k.sb([128, 8, 512], BF16, "wada%d" % i) for i in range(2)]
    bbuf = [k.sb([128, 512], F32, "bada%d" % i) for i in range(2)]
    for j in range(nch):
        wb = wbuf[j % 2]
        bb = bbuf[j % 2]
        k.dma("pool", wb[:], wv[:, :, j * 512:(j + 1) * 512], writes=[wb])
        k.dma("sp", bb[:], b_ada_ap[j * 512:(j + 1) * 512].partition_broadcast(128), writes=[bb])
        pt = ps_pool[j % len(ps_pool)]
        for kk in range(8):
            k.op("pe", lambda e, kk=kk: e.matmul(pt[:, 0:512], lhsT=cbc[:, kk, :], rhs=wb[:, kk, :], start=(kk == 0), stop=(kk == 7)),
                 reads=[cbc, wb], writes=[pt], inc=(kk == 7))
        o = outs[j // 2]
        c0 = (j % 2) * 512
        k.op("dve", lambda e: e.tensor_tensor(out=o[:, c0:c0 + 512], in0=pt[:, 0:512], in1=bb[:], op=ALU.add),
             reads=[pt, bb], writes=[o])
    k.release(m0)


def rstd_from_ss(k, ss, rstd, n, scale):
    k.op("dve", lambda e: e.tensor_scalar(out=rstd[:, 0:n], in0=ss[:, 0:n], scalar1=scale, scalar2=EPS, op0=ALU.mult, op1=ALU.add),
         reads=[ss], writes=[rstd])
    k.op("act", lambda e: e.activation(out=rstd[:, 0:n], in_=rstd[:, 0:n], func=AF.Sqrt), reads=[rstd], writes=[rstd])
    k.op("dve", lambda e: e.reciprocal(out=rstd[:, 0:n], in_=rstd[:, 0:n]), reads=[rstd], writes=[rstd])


NTB = 2048
NE = 32


def build_phaseB(final, upto=99):
    nc = bass.Bass("TRN2", target_bir_lowering=False)
    dt = nc.dram_tensor
    h_in = dt("h_in", [NTB, D], F32, kind="ExternalInput").ap()
    yT = dt("yT", [D, NTB], BF16, kind="ExternalInput").ap()
    w_out = dt("w_out", [D, D], F32, kind="ExternalInput").ap()
    condT = dt("condT", [128, 8], F32, kind="ExternalInput").ap()
    w_ada = dt("w_ada", [D, 4096], F32, kind="ExternalInput").ap()
    b_ada = dt("b_ada", [4096], F32, kind="ExternalInput").ap()
    norm2 = dt("norm2", [D], F32, kind="ExternalInput").ap()
    w_r = dt("w_r", [D, 36], F32, kind="ExternalInput").ap()
    b_r = dt("b_r", [36], F32, kind="ExternalInput").ap()
    if upto >= 5:
        w1 = dt("w1", [NE, D, 512], F32, kind="ExternalInput").ap()
        w3 = dt("w3", [NE, D, 512], F32, kind="ExternalInput").ap()
        w2 = dt("w2", [NE, 512, D], F32, kind="ExternalInput").ap()
    fnorm = dt("fnorm", [D], F32, kind="ExternalInput").ap()
    consts = dt("consts", [128, NCONST, 128], F32, kind="ExternalInput").ap()
    out = dt("out", [NTB, D], F32, kind="ExternalOutput").ap()

    k = KB(nc)
    NT = NTB // 128
    cst, cstb = load_consts(k, nc, consts)
    ident = cst

    hres = [k.sb([128, D], F32, "hres%d" % t) for t in range(NT)]
    h_v = h_in.rearrange("(t p) d -> t p d", p=128)
    for t in range(NT):
        k.dma("sp", hres[t][:], h_v[t], writes=[hres[t]])
    gt2 = k.sb([128, D], F32, "gt2")
    u2T = k.sb([128, 8, NTB], BF16, "u2T")
    logits = k.sb([128, NT, 36], F32, "logits")
    Wd = k.sb([128, NT, NE], F32, "Wd")
    m_mod = k.mark()
    gt1 = k.sb([128, D], F32, "gt1")
    sh2 = k.sb([128, D], F32, "sh2")
    sc2 = k.sb([128, D], F32, "sc2")

    if upto < 1:
        return _finB(k, out, hres, NT)
    m1 = k.mark()
    pp = [k.ps([128, 512], F32, "modps%d" % i) for i in range(2)]
    compute_mod(k, nc, condT, w_ada, b_ada, 4096, [gt1, sh2, sc2, gt2], pp)
    k.release(m1)
    g2 = k.sb([128, D], F32, "g2")
    k.dma("sp", g2[:], norm2.partition_broadcast(128), writes=[g2])
    k.op("dve", lambda e: e.scalar_tensor_tensor(out=g2[:], in0=sc2[:], scalar=1.0, in1=g2[:], op0=ALU.add, op1=ALU.mult),
         reads=[sc2, g2], writes=[g2])

    if upto < 2:
        return _finB(k, out, hres, NT)
    m2 = k.mark()
    yTs = k.sb([128, 8, NTB], BF16, "yTs")
    yv = yT.rearrange("(k p) t -> p k t", p=128)
    for kk in range(8):
        k.dma("sp", yTs[:, kk, :], yv[:, kk, :], writes=[yTs])
    wo = k.sb([128, 8, D], BF16, "wo")
    wov = w_out.rearrange("(k p) n -> p k n", p=128)
    for kk in range(0, 8, 2):
        k.dma("pool", wo[:, kk:kk + 2, :], wov[:, kk:kk + 2, :], writes=[wo])
    k.op("pool", lambda e: e.tensor_tensor(out=wo[:], in0=wo[:], in1=_bc(gt1[:].unsqueeze(1), [128, 8, D]), op=ALU.mult),
         reads=[wo, gt1], writes=[wo])
    psy = [k.ps([128, D], F32, "psy%d" % i) for i in range(2)]
    for t in range(NT):
        p = psy[t % 2]
        for half in range(2):
            for kk in range(8):
                k.op("pe", lambda e, kk=kk, half=half: e.matmul(p[:, half * 512:(half + 1) * 512], lhsT=yTs[:, kk, t * 128:(t + 1) * 128],
                                                                 rhs=wo[:, kk, half * 512:(half + 1) * 512], start=(kk == 0), stop=(kk == 7)),
                     reads=[yTs, wo], writes=[p], inc=(kk == 7))
        k.op("dve", lambda e: e.tensor_tensor(out=hres[t][:], in0=p[:], in1=hres[t][:], op=ALU.add), reads=[p, hres[t]], writes=[hres[t]])
    k.release(m2)

    if upto < 3:
        return _finB(k, out, hres, NT)
    m3 = k.mark()
    ss = k.sb([128, NT], F32, "ss")
    rstd = k.sb([128, NT], F32, "rstd")
    junk = [k.sb([128, D], BF16, "junk%d" % i) for i in range(2)]
    for t in range(NT):
        jk = junk[t % 2]
        k.op("act", lambda e: e.activation(out=jk[:], in_=hres[t][:], func=AF.Square, accum_out=ss[:, t:t + 1]),
             reads=[hres[t]], writes=[jk, ss])
    rstd_from_ss(k, ss, rstd, NT, 1.0 / D)
    wr = k.sb([128, 8, 36], F32, "wr")
    k.dma("sp", wr[:], w_r.rearrange("(k p) n -> p k n", p=128), writes=[wr])
    brb = k.sb([128, 36], F32, "brb")
    k.dma("sp", brb[:], b_r.partition_broadcast(128), writes=[brb])
    t1b = [k.sb([128, D], F32, "t1b%d" % i) for i in range(2)]
    u32 = [k.sb([128, D], F32, "u32_%d" % i) for i in range(2)]
    uT32 = [k.sb([128, 8, 128], F32, "uT32_%d" % i) for i in range(2)]
    pst = [k.ps([128, 8, 128], F32, "pst%d" % i) for i in range(2)]
    psr = [k.ps([128, 36], F32, "psr%d" % i) for i in range(2)]
    for t in range(NT):
        a = t1b[t % 2]
        u = u32[t % 2]
        ut = uT32[t % 2]
        pt = pst[t % 2]
        pr = psr[t % 2]
        k.op("dve", lambda e: e.scalar_tensor_tensor(out=a[:], in0=hres[t][:], scalar=rstd[:, t:t + 1], in1=g2[:], op0=ALU.mult, op1=ALU.mult),
             reads=[hres[t], rstd, g2], writes=[a])
        k.op("pool", lambda e: e.tensor_tensor(out=u[:], in0=a[:], in1=sh2[:], op=ALU.add), reads=[a, sh2], writes=[u])
        for kk in range(8):
            k.op("pe", lambda e, kk=kk: e.transpose(out=pt[:, kk, :], in_=u[:, kk * 128:(kk + 1) * 128], identity=cst[:, C_ID, :]),
                 reads=[u, cst], writes=[pt], inc=(kk == 7))
        k.op("act", lambda e: e.activation(out=ut[:], in_=pt[:], func=AF.Copy), reads=[pt], writes=[ut])
        k.op("pool", lambda e: e.tensor_copy(out=u2T[:, :, t * 128:(t + 1) * 128], in_=ut[:]), reads=[ut], writes=[u2T])
        for kk in range(8):
            k.op("pe", lambda e, kk=kk: e.matmul(pr[:], lhsT=ut[:, kk, :], rhs=wr[:, kk, :], start=(kk == 0), stop=(kk == 7)),
                 reads=[ut, wr], writes=[pr], inc=(kk == 7))
        k.op("dve", lambda e: e.tensor_tensor(out=logits[:, t, :], in0=pr[:], in1=brb[:], op=ALU.add), reads=[pr, brb], writes=[logits])
    k.release(m3)

    if upto < 4:
        return _finB(k, out, hres, NT)
    m4 = k.mark()
    BIG = 1.0e30

    def dve(fn, reads, writes):
        k.op("dve", fn, reads=reads, writes=writes)

    lg = logits[:, :, 0:4]
    le = logits[:, :, 4:36]
    gmax = k.sb([128, NT], F32, "gmax")
    dve(lambda e: e.tensor_reduce(out=gmax[:], in_=lg, axis=AX.X, op=ALU.max), [logits], [gmax])
    eg = k.sb([128, NT, 4], F32, "eg")
    dve(lambda e: e.tensor_tensor(out=eg[:], in0=lg, in1=_bc(gmax[:].unsqueeze(2), [128, NT, 4]), op=ALU.subtract), [logits, gmax], [eg])
    k.op("act", lambda e: e.activation(out=eg[:], in_=eg[:], func=AF.Exp), reads=[eg], writes=[eg])
    gsum = k.sb([128, NT], F32, "gsum")
    dve(lambda e: e.tensor_reduce(out=gsum[:], in_=eg[:], axis=AX.X, op=ALU.add), [eg], [gsum])
    pgt = k.sb([128, NT], F32, "pgt")
    dve(lambda e: e.reciprocal(out=pgt[:], in_=gsum[:]), [gsum], [pgt])
    pen = k.sb([128, NT, 4], F32, "pen")
    dve(lambda e: e.tensor_tensor(out=pen[:], in0=lg, in1=_bc(gmax[:].unsqueeze(2), [128, NT, 4]), op=ALU.is_equal), [logits, gmax], [pen])
    dve(lambda e: e.tensor_scalar(out=pen[:], in0=pen[:], scalar1=1.0, scalar2=BIG, op0=ALU.subtract, op1=ALU.mult), [pen], [pen])
    lem = k.sb([128, NT, NE], F32, "lem")
    dve(lambda e: e.tensor_tensor(out=lem[:].rearrange("p t (g x) -> p t g x", g=4), in0=le.rearrange("p t (g x) -> p t g x", g=4),
                                  in1=_bc(pen[:].unsqueeze(3), [128, NT, 4, 8]), op=ALU.add), [logits, pen], [lem])
    mx1 = k.sb([128, NT], F32, "mx1")
    dve(lambda e: e.tensor_reduce(out=mx1[:], in_=lem[:], axis=AX.X, op=ALU.max), [lem], [mx1])
    oh1 = k.sb([128, NT, NE], F32, "oh1")
    dve(lambda e: e.tensor_tensor(out=oh1[:], in0=lem[:], in1=_bc(mx1[:].unsqueeze(2), [128, NT, NE]), op=ALU.is_equal), [lem, mx1], [oh1])
    lem2 = k.sb([128, NT, NE], F32, "lem2")
    dve(lambda e: e.scalar_tensor_tensor(out=lem2[:], in0=oh1[:], scalar=-BIG, in1=lem[:], op0=ALU.mult, op1=ALU.add), [oh1, lem], [lem2])
    mx2 = k.sb([128, NT], F32, "mx2")
    dve(lambda e: e.tensor_reduce(out=mx2[:], in_=lem2[:], axis=AX.X, op=ALU.max), [lem2], [mx2])
    oh2 = k.sb([128, NT, NE], F32, "oh2")
    dve(lambda e: e.tensor_tensor(out=oh2[:], in0=lem2[:], in1=_bc(mx2[:].unsqueeze(2), [128, NT, NE]), op=ALU.is_equal), [lem2, mx2], [oh2])
    rr = k.sb([128, NT], F32, "rr")
    dve(lambda e: e.tensor_tensor(out=rr[:], in0=mx2[:], in1=mx1[:], op=ALU.subtract), [mx2, mx1], [rr])
    k.op("act", lambda e: e.activation(out=rr[:], in_=rr[:], func=AF.Exp), reads=[rr], writes=[rr])
    den = k.sb([128, NT], F32, "den")
    dve(lambda e: e.tensor_scalar(out=den[:], in0=rr[:], scalar1=1.0, scalar2=None, op0=ALU.add), [rr], [den])
    dve(lambda e: e.reciprocal(out=den[:], in_=den[:]), [den], [den])
    wt1 = k.sb([128, NT], F32, "wt1")
    dve(lambda e: e.tensor_tensor(out=wt1[:], in0=pgt[:], in1=den[:], op=ALU.mult), [pgt, den], [wt1])
    wt2 = k.sb([128, NT], F32, "wt2")
    dve(lambda e: e.tensor_tensor(out=wt2[:], in0=wt1[:], in1=rr[:], op=ALU.mult), [wt1, rr], [wt2])
    dve(lambda e: e.tensor_tensor(out=Wd[:], in0=oh1[:], in1=_bc(wt1[:].unsqueeze(2), [128, NT, NE]), op=ALU.mult), [oh1, wt1], [Wd])
    dve(lambda e: e.tensor_tensor(out=oh2[:], in0=oh2[:], in1=_bc(wt2[:].unsqueeze(2), [128, NT, NE]), op=ALU.mult), [oh2, wt2], [oh2])
    dve(lambda e: e.tensor_tensor(out=Wd[:], in0=Wd[:], in1=oh2[:], op=ALU.add), [Wd, oh2], [Wd])
    k.release(m4)

    if upto < 5:
        return _finB(k, out, hres, NT)
    k.release(m_mod)
    m5 = k.mark()
    w1b = [k.sb([128, 8, 512], BF16, "w1b%d" % i) for i in range(2)]
    w3b = [k.sb([128, 8, 512], BF16, "w3b%d" % i) for i in range(2)]
    w2b = [k.sb([128, 4, D], BF16, "w2b%d" % i) for i in range(2)]
    stg = [k.sb([128, 4096], F32, "stg%d" % i) for i in range(2)]
    actT = [[k.sb([128, 512], BF16, "actT%d_%d" % (i, f)) for f in range(4)] for i in range(2)]
    slb = [k.sb([128, 512], F32, "slb%d" % i) for i in range(2)]
    ps1 = [k.ps([128, 512], F32, "ps1_%d" % i) for i in range(2)]
    ps3 = [k.ps([128, 512], F32, "ps3_%d" % i) for i in range(2)]
    psy = [k.ps([128, D], F32, "psye%d" % i) for i in range(2)]
    cnt_f = 0
    cnt_y = 0
    for ex in range(NE):
        b = ex % 2
        s1 = stg[(3 * ex) % 2]
        k.dma("sp", s1[:].rearrange("p (k f) -> p k f", k=8), w1[ex].rearrange("(k p) f -> p k f", p=128), writes=[s1])
        k.op("pool", lambda e: e.tensor_copy(out=w1b[b][:].rearrange("p k f -> p (k f)"), in_=s1[:]), reads=[s1], writes=[w1b[b]])
        s3 = stg[(3 * ex + 1) % 2]
        k.dma("sp", s3[:].rearrange("p (k f) -> p k f", k=8), w3[ex].rearrange("(k p) f -> p k f", p=128), writes=[s3])
        k.op("pool", lambda e: e.tensor_copy(out=w3b[b][:].rearrange("p k f -> p (k f)"), in_=s3[:]), reads=[s3], writes=[w3b[b]])
        s2 = stg[(3 * ex + 2) % 2]
        k.dma("sp", s2[:].rearrange("p (c d) -> p c d", c=4), w2[ex].rearrange("(c p) d -> p c d", p=128), writes=[s2])
        k.op("pool", lambda e: e.tensor_tensor(out=w2b[b][:], in0=s2[:].rearrange("p (c d) -> p c d", c=4), in1=_bc(gt2[:].unsqueeze(1), [128, 4, D]), op=ALU.mult),
             reads=[s2, gt2], writes=[w2b[b]])
        for blk in range(NTB // 512):
            ab = actT[blk % 2]
            for fc in range(4):
                p1 = ps1[cnt_f % 2]
                p3 = ps3[cnt_f % 2]
                sl = slb[cnt_f % 2]
                cnt_f += 1
                for kk in range(8):
                    k.op("pe", lambda e, kk=kk: e.matmul(p1[:], lhsT=w1b[b][:, kk, fc * 128:(fc + 1) * 128], rhs=u2T[:, kk, blk * 512:(blk + 1) * 512],
                                                         start=(kk == 0), stop=(kk == 7)), reads=[w1b[b], u2T], writes=[p1], inc=(kk == 7))
                for kk in range(8):
                    k.op("pe", lambda e, kk=kk: e.matmul(p3[:], lhsT=w3b[b][:, kk, fc * 128:(fc + 1) * 128], rhs=u2T[:, kk, blk * 512:(blk + 1) * 512],
                                                         start=(kk == 0), stop=(kk == 7)), reads=[w3b[b], u2T], writes=[p3], inc=(kk == 7))
                k.op("act", lambda e: e.activation(out=sl[:], in_=p1[:], func=AF.Silu), reads=[p1], writes=[sl])
                k.op("dve", lambda e: e.tensor_tensor(out=ab[fc][:], in0=p3[:], in1=sl[:], op=ALU.mult), reads=[p3, sl], writes=[ab[fc]])
            for ti in range(4):
                t = blk * 4 + ti
                py = psy[cnt_y % 2]
                cnt_y += 1
                for half in range(2):
                    for fc in range(4):
                        k.op("pe", lambda e, fc=fc, half=half: e.matmul(py[:, half * 512:(half + 1) * 512], lhsT=ab[fc][:, ti * 128:(ti + 1) * 128],
                                                                         rhs=w2b[b][:, fc, half * 512:(half + 1) * 512], start=(fc == 0), stop=(fc == 3)),
                             reads=[ab[fc], w2b[b]], writes=[py], inc=(fc == 3))
                k.op("dve", lambda e: e.scalar_tensor_tensor(out=hres[t][:], in0=py[:], scalar=Wd[:, t, ex:ex + 1], in1=hres[t][:],
                                                             op0=ALU.mult, op1=ALU.add), reads=[py, Wd, hres[t]], writes=[hres[t]])
    k.release(m5)

    toks = []
    o_v = out.rearrange("(t p) d -> t p d", p=128)
    if final:
        fn = k.sb([128, D], F32, "fn")
        k.dma("sp", fn[:], fnorm.partition_broadcast(128), writes=[fn])
        ss2 = k.sb([128, NT], F32, "ss2")
        rs2 = k.sb([128, NT], F32, "rs2")
        junk2 = [k.sb([128, D], BF16, "junkf%d" % i) for i in range(2)]
        for t in range(NT):
            jk = junk2[t % 2]
            k.op("act", lambda e: e.activation(out=jk[:], in_=hres[t][:], func=AF.Square, accum_out=ss2[:, t:t + 1]),
                 reads=[hres[t]], writes=[jk, ss2])
        rstd_from_ss(k, ss2, rs2, NT, 1.0 / D)
        for t in range(NT):
            k.op("dve", lambda e: e.scalar_tensor_tensor(out=hres[t][:], in0=hres[t][:], scalar=rs2[:, t:t + 1], in1=fn[:], op0=ALU.mult, op1=ALU.mult),
                 reads=[hres[t], rs2, fn], writes=[hres[t]])
    for t in range(NT):
        toks.append(k.dma("sp", o_v[t], hres[t][:], reads=[hres[t]]))
    k.finish(toks)
    k.release(0)
    return nc, k


def _finB(k, out, hres, NT):
    o_v = out.rearrange("(t p) d -> t p d", p=128)
    toks = [k.dma("sp", o_v[t], hres[t][:], reads=[hres[t]]) for t in range(NT)]
    k.finish(toks)
    k.release(0)
    return k.nc, k


def phaseB_inmaps(layer, h, yT_full, inp, final):
    maps = []
    wa = np.ascontiguousarray(inp["w_ada"][layer][:, 2048:6144])
    ba = np.ascontiguousarray(inp["b_ada"][layer][2048:6144])
    w_out = inp["w_out_ab"][0] if layer == 0 else inp["w_out_c"][0]
    w_r = np.ascontiguousarray(np.concatenate([inp["moe_w_grp"][layer], inp["moe_w_rt"][layer]], axis=1))
    b_r = np.ascontiguousarray(np.concatenate([inp["moe_b_grp"][layer], inp["moe_b_rt"][layer]], axis=0))
    consts = make_consts()
    for core in range(NCORES):
        b, q = core // 4, core % 4
        sl = slice(q * NTB, (q + 1) * NTB)
        maps.append({
            "h_in": np.ascontiguousarray(h[b, sl]),
            "yT": np.ascontiguousarray(yT_full[b][:, sl]),
            "w_out": np.ascontiguousarray(w_out),
            "condT": np.ascontiguousarray(inp["c"][b].reshape(8, 128).T),
            "w_ada": wa, "b_ada": ba,
            "norm2": np.ascontiguousarray(inp["norm2"][layer]),
            "w_r": w_r, "b_r": b_r,
            "w1": inp["moe_w1"][layer], "w3": inp["moe_w3"][layer], "w2": inp["moe_w2"][layer],
            "fnorm": np.ascontiguousarray(inp["final_norm"]),
            "consts": consts,
        })
    return maps


class UMaker:
    def __init__(self, k, nc, h_ap, condT, w_ada, b_ada, norm_ap, cstb):
        self.k = k
        self.cstb = cstb
        self.h_v = h_ap.rearrange("(t p) d -> t p d", p=128)
        self.sh = k.sb([128, D], F32, "sh1")
        self.sc = k.sb([128, D], F32, "sc1")
        m = k.mark()
        pp = [k.ps([128, 512], F32, "modps%d" % i) for i in range(2)]
        compute_mod(k, nc, condT, w_ada, b_ada, 2048, [self.sh, self.sc], pp)
        k.release(m)
        self.g = k.sb([128, D], F32, "g1")
        k.dma("sp", self.g[:], norm_ap.partition_broadcast(128), writes=[self.g])
        k.op("dve", lambda e: e.scalar_tensor_tensor(out=self.g[:], in0=self.sc[:], scalar=1.0, in1=self.g[:], op0=ALU.add, op1=ALU.mult),
             reads=[self.sc, self.g], writes=[self.g])
        self.ht = [k.sb([128, D], F32, "ht%d" % i) for i in range(4)]
        self.junk = k.sb([128, D], BF16, "junk")
        self.ss = k.sb([128, 4], F32, "ss")
        self.rstd = k.sb([128, 4], F32, "rstd")
        self.a = [k.sb([128, D], F32, "ua%d" % i) for i in range(2)]
        self.ub = [k.sb([128, D], BF16, "ub%d" % i) for i in range(2)]
        self.pst = [k.ps([128, 8, 128], BF16, "pst%d" % i) for i in range(1)]

    def block(self, blk, uT):
        k = self.k
        for ti in range(4):
            ht = self.ht[ti]
            k.dma("sp", ht[:], self.h_v[blk * 4 + ti], writes=[ht])
            k.op("act", lambda e: e.activation(out=self.junk[:], in_=ht[:], func=AF.Square, accum_out=self.ss[:, ti:ti + 1]),
                 reads=[ht], writes=[self.junk, self.ss])
        rstd_from_ss(k, self.ss, self.rstd, 4, 1.0 / D)
        for ti in range(4):
            ht = self.ht[ti]
            a = self.a[ti % 2]
            ub = self.ub[ti % 2]
            pt = self.pst[0]
            k.op("dve", lambda e: e.scalar_tensor_tensor(out=a[:], in0=ht[:], scalar=self.rstd[:, ti:ti + 1], in1=self.g[:], op0=ALU.mult, op1=ALU.mult),
                 reads=[ht, self.rstd, self.g], writes=[a])
            k.op("pool", lambda e: e.tensor_tensor(out=ub[:], in0=a[:], in1=self.sh[:], op=ALU.add), reads=[a, self.sh], writes=[ub])
            for kk in range(8):
                k.op("pe", lambda e: e.transpose(out=pt[:, kk, :], in_=ub[:, kk * 128:(kk + 1) * 128], identity=self.cstb[:, C_ID, :]),
                     reads=[ub, self.cstb], writes=[pt])
            k.op("act", lambda e: e.activation(out=uT[:, :, ti * 128:(ti + 1) * 128], in_=pt[:], func=AF.Copy), reads=[pt], writes=[uT])


class Banks:
    def __init__(self, k, n):
        self.t = [k.ps([128, 512], F32, "bank%d" % i) for i in range(n)]
        self.i = 0

    def get(self):
        b = self.t[self.i % len(self.t)]
        self.i += 1
        return b


NCOL_A0 = 656


def build_phaseA0(nblk=16):
    nc = bass.Bass("TRN2", target_bir_lowering=False)
    dt = nc.dram_tensor
    h_in = dt("h_in", [S, D], F32, kind="ExternalInput").ap()
    condT = dt("condT", [128, 8], F32, kind="ExternalInput").ap()
    w_ada = dt("w_ada", [D, 2048], F32, kind="ExternalInput").ap()
    b_ada = dt("b_ada", [2048], F32, kind="ExternalInput").ap()
    norm1 = dt("norm1", [D], F32, kind="ExternalInput").ap()
    w_in = dt("w_in", [D, NCOL_A0], F32, kind="ExternalInput").ap()
    WAd = dt("WA", [128, 128], F32, kind="ExternalInput").ap()
    WXd = dt("WX", [128, 128], F32, kind="ExternalInput").ap()
    wg2d = dt("wg2", [16, 64], F32, kind="ExternalInput").ap()
    pcold = dt("pcol", [128, 16], F32, kind="ExternalInput").ap()
    consts = dt("consts", [128, NCONST, 128], F32, kind="ExternalInput").ap()
    yT = dt("yT", [256, S], BF16, kind="ExternalOutput").ap()

    k = KB(nc)
    cst, cstb = load_consts(k, nc, consts)
    um = UMaker(k, nc, h_in, condT, w_ada, b_ada, norm1, cstb)
    win = k.sb([128, 8, NCOL_A0], BF16, "win")
    k.dma("pool", win[:], w_in.rearrange("(k p) n -> p k n", p=128), writes=[win])
    WA = k.sb([128, 128], BF16, "WA")
    WX = k.sb([128, 128], BF16, "WX")
    wg2 = k.sb([16, 64], BF16, "wg2")
    k.dma("pool", WA[:], WAd, writes=[WA])
    k.dma("pool", WX[:], WXd, writes=[WX])
    k.dma("pool", wg2[:], wg2d, writes=[wg2])
    pc = k.sb([128, 16], F32, "pcol")
    k.dma("sp", pc[:], pcold, writes=[pc])
    dc = k.sb([128, 4], F32, "dc")
    tl = k.sb([128, 1], F32, "tl")
    k.op("act", lambda e: e.activation(out=tl[:], in_=pc[:, 7:8], func=AF.Exp, scale=-1.0), reads=[pc], writes=[tl])
    k.op("act", lambda e: e.activation(out=tl[:], in_=tl[:], func=AF.Ln, bias=1.0), reads=[tl], writes=[tl])
    k.op("dve", lambda e: e.tensor_scalar(out=dc[:, 0:1], in0=tl[:], scalar1=-8.0, scalar2=None, op0=ALU.mult), reads=[tl], writes=[dc])
    k.op("dve", lambda e: e.tensor_scalar(out=dc[:, 1:2], in0=tl[:], scalar1=-16.0, scalar2=None, op0=ALU.mult), reads=[tl], writes=[dc])
    k.op("dve", lambda e: e.tensor_scalar(out=dc[:, 2:3], in0=pc[:, 9:10], scalar1=-1.0, scalar2=None, op0=ALU.mult), reads=[pc], writes=[dc])
    rmask = k.sb([64, 8, 64], F32, "rmask")
    k.op("dve", lambda e: e.memset(rmask[:], 1.0), writes=[rmask])
    k.op("dve", lambda e: e.memset(rmask[:, :, 0:1], 0.0), writes=[rmask])
    rmask2 = rmask[:].rearrange("p a b -> p (a b)")

    banks = Banks(k, 5)
    pkg = k.ps([128, 4, 64], BF16, "pkg")
    uT = [k.sb([128, 8, 512], BF16, "uT%d" % i) for i in range(2)]
    xabuf = [k.sb([128, 515], F32, "xabuf%d" % i) for i in range(2)]
    k.op("dve", lambda e: e.memset(xabuf[0][:, 0:3], 0.0), writes=[xabuf[0]])
    hs = [k.sb([128, 512], F32, "hs%d" % i) for i in range(2)]
    Sst = k.sb([64, 128], F32, "Sst")
    k.op("dve", lambda e: e.memset(Sst[:], 0.0), writes=[Sst])
    Sbt = [k.sb([64, 128], BF16, "Sbt%d" % i) for i in range(8)]

    def sbt(shape, dtp, name):
        return k.sb(shape, dtp, name)

    ga = sbt([128, 512], F32, "ga")
    q_sb = sbt([64, 512], F32, "q_sb")
    k_sb = sbt([64, 512], F32, "k_sb")
    sog = sbt([128, 512], F32, "sog")
    gl_bf = sbt([16, 512], BF16, "gl_bf")
    v_tok = sbt([128, 4, 128], BF16, "v_tok")
    xc = sbt([128, 512], F32, "xc")
    xcb = sbt([128, 512], BF16, "xcb")
    r_sb = sbt([128, 512], F32, "r_sb")
    i_sb = sbt([128, 512], F32, "i_sb")
    a_sb = sbt([128, 512], F32, "a_sb")
    a2_sb = sbt([128, 512], F32, "a2_sb")
    t_sb = sbt([128, 512], F32, "t_sb")
    b_sb = sbt([128, 512], F32, "b_sb")
    g2_sb = sbt([128, 512], F32, "g2_sb")
    inner = sbt([128, 512], F32, "inner")
    ge = sbt([128, 512], F32, "ge")
    ya = [sbt([128, 512], BF16, "ya%d" % i) for i in range(2)]
    e1 = sbt([64, 512], F32, "e1")
    sp_ = sbt([64, 512], F32, "sp")
    cum = sbt([64, 512], F32, "cum")
    E1 = sbt([64, 512], F32, "E1")
    E2 = sbt([64, 512], F32, "E2")
    qg = sbt([64, 512], BF16, "qg")
    kg = sbt([64, 512], BF16, "kg")
    kg_tok = sbt([128, 4, 64], BF16, "kg_tok")
    attm = sbt([128, 4, 128], BF16, "attm")
    tS = sbt([64, 128], F32, "tS")
    osb = sbt([128, 512], F32, "osb")
    o2 = sbt([128, 512], F32, "o2")
    rs = sbt([128, 512], F32, "rs")
    t1 = sbt([128, 512], F32, "t1")
    ob = [sbt([128, 512], BF16, "ob%d" % i) for i in range(2)]

    def op(e, fn, r, w):
        return k.op(e, fn, reads=r, writes=w)

    out_toks = []
    for blk in range(nblk):
        u = uT[blk % 2]
        xb = xabuf[blk % 2]
        xbn = xabuf[(blk + 1) % 2]
        um.block(blk, u)

        def proj(c0, c1):
            p = banks.get()
            m = c1 - c0
            for kk in range(8):
                op("pe", lambda e: e.matmul(p[0:m, :], lhsT=win[:, kk, c0:c1], rhs=u[:, kk, :], start=(kk == 0), stop=(kk == 7)), [win, u], [p])
            return p

        p = proj(0, 128)
        op("act", lambda e: e.activation(out=xb[:, 3:515], in_=p[:], func=AF.Copy), [p], [xb])
        p = proj(128, 256)
        op("act", lambda e: e.activation(out=ga[:], in_=p[:], func=AF.Copy), [p], [ga])
        p = proj(256, 320)
        op("dve", lambda e: e.tensor_scalar(out=q_sb[:], in0=p[0:64, :], scalar1=0.125, scalar2=None, op0=ALU.mult), [p], [q_sb])
        p = proj(320, 384)
        op("act", lambda e: e.activation(out=k_sb[:], in_=p[0:64, :], func=AF.Copy), [p], [k_sb])
        p = proj(512, 640)
        op("act", lambda e: e.activation(out=sog[:], in_=p[:], func=AF.Silu), [p], [sog])
        p = proj(640, 656)
        op("dve", lambda e: e.tensor_copy(out=gl_bf[:], in_=p[0:16, :]), [p], [gl_bf])
        p = banks.get()
        for ti in range(4):
            for kk in range(8):
                op("pe", lambda e: e.matmul(p[:, ti * 128:(ti + 1) * 128], lhsT=u[:, kk, ti * 128:(ti + 1) * 128], rhs=win[:, kk, 384:512],
                                            start=(kk == 0), stop=(kk == 7)), [win, u], [p])
        op("dve", lambda e: e.tensor_copy(out=v_tok[:].rearrange("p a b -> p (a b)"), in_=p[:]), [p], [v_tok])

        op("act", lambda e: e.activation(out=xc[:], in_=xb[:, 3:515], func=AF.Identity, bias=pc[:, 4:5], scale=pc[:, 3:4]), [xb, pc], [xc])
        for w in range(3):
            op("dve", lambda e: e.scalar_tensor_tensor(out=xc[:], in0=xb[:, w:w + 512], scalar=pc[:, w:w + 1], in1=xc[:], op0=ALU.mult, op1=ALU.add),
               [xb, pc, xc], [xc])
        op("pool", lambda e: e.tensor_copy(out=xbn[:, 0:3], in_=xb[:, 512:515]), [xb], [xbn])
        op("act", lambda e: e.activation(out=xcb[:], in_=xc[:], func=AF.Copy), [xc], [xcb])
        p = banks.get()
        op("pe", lambda e: e.matmul(p[:], lhsT=WA[:], rhs=xcb[:], start=True, stop=True), [WA, xcb], [p])
        op("act", lambda e: e.activation(out=r_sb[:], in_=p[:], func=AF.Sigmoid, bias=pc[:, 5:6]), [p, pc], [r_sb])
        p = banks.get()
        op("pe", lambda e: e.matmul(p[:], lhsT=WX[:], rhs=xcb[:], start=True, stop=True), [WX, xcb], [p])
        op("act", lambda e: e.activation(out=i_sb[:], in_=p[:], func=AF.Sigmoid, bias=pc[:, 6:7]), [p, pc], [i_sb])
        op("act", lambda e: e.activation(out=a_sb[:], in_=r_sb[:], func=AF.Exp, scale=dc[:, 0:1]), [r_sb, dc], [a_sb])
        op("act", lambda e: e.activation(out=a2_sb[:], in_=r_sb[:], func=AF.Exp, scale=dc[:, 1:2]), [r_sb, dc], [a2_sb])
        op("act", lambda e: e.activation(out=a2_sb[:], in_=a2_sb[:], func=AF.Sqrt, bias=1.0, scale=-1.0), [a2_sb], [a2_sb])
        op("dve", lambda e: e.tensor_tensor(out=t_sb[:], in0=i_sb[:], in1=xc[:], op=ALU.mult), [i_sb, xc], [t_sb])
        op("pool", lambda e: e.tensor_tensor(out=b_sb[:], in0=t_sb[:], in1=a2_sb[:], op=ALU.mult), [t_sb, a2_sb], [b_sb])
        hcur = hs[blk % 2]
        hprev = hs[(blk + 1) % 2]
        if blk == 0:
            op("dve", lambda e: e.tensor_tensor_scan(out=hcur[:], data0=a_sb[:], data1=b_sb[:], initial=0.0, op0=ALU.mult, op1=ALU.add),
               [a_sb, b_sb], [hcur])
        else:
            op("dve", lambda e: e.tensor_tensor_scan(out=hcur[:], data0=a_sb[:], data1=b_sb[:], initial=hprev[:, 511:512], op0=ALU.mult, op1=ALU.add),
               [a_sb, b_sb, hprev], [hcur])
        op("act", lambda e: e.activation(out=g2_sb[:], in_=ga[:], func=AF.Square), [ga], [g2_sb])
        op("dve", lambda e: e.tensor_scalar(out=g2_sb[:], in0=g2_sb[:], scalar1=0.044715, scalar2=1.0, op0=ALU.mult, op1=ALU.add), [g2_sb], [g2_sb])
        op("pool", lambda e: e.tensor_tensor(out=inner[:], in0=g2_sb[:], in1=ga[:], op=ALU.mult), [g2_sb, ga], [inner])
        op("act", lambda e: e.activation(out=inner[:], in_=inner[:], func=AF.Sigmoid, scale=1.5957691216), [inner], [inner])
        op("dve", lambda e: e.tensor_tensor(out=ge[:], in0=ga[:], in1=inner[:], op=ALU.mult), [ga, inner], [ge])
        yab = ya[blk % 2]
        op("pool", lambda e: e.tensor_tensor(out=yab[:], in0=ge[:], in1=hcur[:], op=ALU.mult), [ge, hcur], [yab])
        out_toks.append(k.dma("sp", yT[0:128, blk * 512:(blk + 1) * 512], yab[:], reads=[yab]))

        p = banks.get()
        op("pe", lambda e: e.matmul(p[0:64, :], lhsT=wg2[:], rhs=gl_bf[:], start=True, stop=True), [wg2, gl_bf], [p])
        op("act", lambda e: e.activation(out=e1[:], in_=p[0:64, :], func=AF.Exp, bias=dc[0:64, 2:3], scale=-1.0), [p, dc], [e1])
        op("act", lambda e: e.activation(out=sp_[:], in_=e1[:], func=AF.Ln, bias=1.0), [e1], [sp_])
        op("dve", lambda e: e.tensor_tensor_scan(out=cum[:], data0=rmask2, data1=sp_[:], initial=0.0, op0=ALU.mult, op1=ALU.add), [rmask, sp_], [cum])
        op("act", lambda e: e.activation(out=E1[:], in_=cum[:], func=AF.Exp, scale=-1.0 / 16.0), [cum], [E1])
        op("act", lambda e: e.activation(out=E2[:], in_=cum[:], func=AF.Exp, scale=1.0 / 16.0), [cum], [E2])
        op("dve", lambda e: e.tensor_tensor(out=qg[:], in0=q_sb[:], in1=E1[:], op=ALU.mult), [q_sb, E1], [qg])
        op("pool", lambda e: e.tensor_tensor(out=kg[:], in0=k_sb[:], in1=E2[:], op=ALU.mult), [k_sb, E2], [kg])
        for pr in range(4):
            op("pe", lambda e: e.transpose(out=pkg[:, pr, :], in_=kg[:, pr * 128:(pr + 1) * 128], identity=cstb[0:64, C_ID, 0:64]), [kg, cstb], [pkg])
        op("act", lambda e: e.activation(out=kg_tok[:], in_=pkg[:], func=AF.Copy), [pkg], [kg_tok])
        p = banks.get()
        for pr in range(4):
            op("pe", lambda e: e.matmul(p[:, pr * 128:(pr + 1) * 128], lhsT=kg[:, pr * 128:(pr + 1) * 128], rhs=qg[:, pr * 128:(pr + 1) * 128],
                                        start=True, stop=True), [kg, qg], [p])
        op("dve", lambda e: e.tensor_tensor(out=attm[:], in0=p[:].rearrange("p (a b) -> p a b", a=4),
                                            in1=_bc(cst[:, C_UT64:C_UT64 + 1, :], [128, 4, 128]), op=ALU.mult), [p, cst], [attm])
        kva = banks.get()
        kvb = banks.get()
        for c in range(8):
            pr, half = c // 2, c % 2
            kvp = kva if c < 4 else kvb
            op("pe", lambda e: e.matmul(kvp[0:64, (c % 4) * 128:(c % 4 + 1) * 128], lhsT=kg_tok[half * 64:(half + 1) * 64, pr, :],
                                        rhs=v_tok[half * 64:(half + 1) * 64, pr, :], start=True, stop=True), [kg_tok, v_tok], [kvp])
        for c in range(8):
            kvp = kva if c < 4 else kvb
            op("act", lambda e: e.activation(out=Sbt[c][:], in_=Sst[:], func=AF.Copy), [Sst], [Sbt[c]])
            op("dve", lambda e: e.tensor_tensor(out=tS[:], in0=kvp[0:64, (c % 4) * 128:(c % 4 + 1) * 128], in1=Sst[:], op=ALU.add), [kvp, Sst], [tS])
            op("dve", lambda e: e.tensor_scalar(out=Sst[:], in0=tS[:], scalar1=E1[:, c * 64 + 63:c * 64 + 64], scalar2=None, op0=ALU.mult),
               [tS, E1], [Sst])
        po = banks.get()
        for pr in range(4):
            op("pe", lambda e: e.matmul(po[:, pr * 128:(pr + 1) * 128], lhsT=v_tok[:, pr, :], rhs=attm[:, pr, :], start=True, stop=False),
               [v_tok, attm], [po])
            for half in range(2):
                c = 2 * pr + half
                op("pe", lambda e: e.matmul(po[:, c * 64:(c + 1) * 64], lhsT=Sbt[c][:], rhs=qg[:, c * 64:(c + 1) * 64], start=False, stop=(half == 1)),
                   [Sbt[c], qg], [po])
        op("act", lambda e: e.activation(out=osb[:], in_=po[:], func=AF.Copy), [po], [osb])
        op("act", lambda e: e.activation(out=o2[:], in_=osb[:], func=AF.Square), [osb], [o2])
        p = banks.get()
        op("pe", lambda e: e.matmul(p[:], lhsT=cst[:, C_ONES, :], rhs=o2[:], start=True, stop=True), [cst, o2], [p])
        op("act", lambda e: e.activation(out=rs[:], in_=p[:], func=AF.Sqrt, bias=EPS, scale=1.0 / 128.0), [p], [rs])
        op("dve", lambda e: e.reciprocal(out=rs[:], in_=rs[:]), [rs], [rs])
        op("dve", lambda e: e.scalar_tensor_tensor(out=t1[:], in0=osb[:], scalar=pc[:, 8:9], in1=rs[:], op0=ALU.mult, op1=ALU.mult), [osb, pc, rs], [t1])
        obb = ob[blk % 2]
        op("pool", lambda e: e.tensor_tensor(out=obb[:], in0=t1[:], in1=sog[:], op=ALU.mult), [t1, sog], [obb])
        out_toks.append(k.dma("sp", yT[128:256, blk * 512:(blk + 1) * 512], obb[:], reads=[obb]))
    k.finish(out_toks)
    k.release(0)
    return nc, k


def phaseA0_inmaps(h, inp):
    maps = []
    consts = make_consts()
    w_in = inp["w_in_ab"][0]
    wa = np.ascontiguousarray(inp["w_ada"][0][:, 0:2048])
    ba = np.ascontiguousarray(inp["b_ada"][0][0:2048])
    for core in range(NCORES):
        b, hg = core // 4, core % 4
        ch = slice(hg * 128, (hg + 1) * 128)
        cols = np.concatenate([
            np.arange(hg * 128, (hg + 1) * 128),
            512 + np.arange(hg * 128, (hg + 1) * 128),
            1024 + np.arange(hg * 64, (hg + 1) * 64),
            1280 + np.arange(hg * 64, (hg + 1) * 64),
            1536 + np.arange(hg * 128, (hg + 1) * 128),
            2048 + np.arange(hg * 128, (hg + 1) * 128),
            2560 + np.arange(16),
        ])
        WA = np.zeros((128, 128), np.float32)
        WX = np.zeros((128, 128), np.float32)
        for j in range(2):
            WA[j * 64:(j + 1) * 64, j * 64:(j + 1) * 64] = inp["rg_wa"][0][hg * 2 + j]
            WX[j * 64:(j + 1) * 64, j * 64:(j + 1) * 64] = inp["rg_wx"][0][hg * 2 + j]
        pcol = np.zeros((128, 16), np.float32)
        pcol[:, 0:4] = inp["conv_a_w"][0][:, ch].T
        pcol[:, 4] = inp["conv_a_b"][0][ch]
        pcol[:, 5] = inp["rg_ba"][0][ch]
        pcol[:, 6] = inp["rg_bx"][0][ch]
        pcol[:, 7] = inp["rg_lam"][0][ch]
        pcol[:, 8] = inp["gla_norm"][0]
        pcol[0:64, 9] = inp["gla_bg2"][0][hg * 64:(hg + 1) * 64]
        maps.append({
            "h_in": np.ascontiguousarray(h[b]),
            "condT": np.ascontiguousarray(inp["c"][b].reshape(8, 128).T),
            "w_ada": wa, "b_ada": ba,
            "norm1": np.ascontiguousarray(inp["norm1"][0]),
            "w_in": np.ascontiguousarray(w_in[:, cols]),
            "WA": WA, "WX": WX,
            "wg2": np.ascontiguousarray(inp["gla_wg2"][0][:, hg * 64:(hg + 1) * 64]),
            "pcol": pcol, "consts": consts,
        })
    return maps


def assemble_yT_A0(results):
    yT = np.zeros((2, D, S), ml_dtypes.bfloat16)
    for core in range(NCORES):
        b, hg = core // 4, core % 4
        r = results[core]["yT"]
        yT[b, hg * 128:(hg + 1) * 128] = r[0:128]
        yT[b, 512 + hg * 128:512 + (hg + 1) * 128] = r[128:256]
    return yT


NCOL_A1 = 1028


def build_phaseA1(nblk=16, stop=99):
    nc = bass.Bass("TRN2", target_bir_lowering=False)
    dt = nc.dram_tensor
    h_in = dt("h_in", [S, D], F32, kind="ExternalInput").ap()
    condT = dt("condT", [128, 8], F32, kind="ExternalInput").ap()
    w_ada = dt("w_ada", [D, 2048], F32, kind="ExternalInput").ap()
    b_ada = dt("b_ada", [2048], F32, kind="ExternalInput").ap()
    norm1 = dt("norm1", [D], F32, kind="ExternalInput").ap()
    w_in = dt("w_in", [D, NCOL_A1], F32, kind="ExternalInput").ap()
    pcold = dt("pcol", [128, 24], F32, kind="ExternalInput").ap()
    prmd = dt("prm", [128, 4], F32, kind="ExternalInput").ap()
    dnd = dt("dnorm", [128], F32, kind="ExternalInput").ap()
    alogd = dt("a_log", [128, 2], F32, kind="ExternalInput").ap()
    consts = dt("consts", [128, NCONST, 128], F32, kind="ExternalInput").ap()
    yT = dt("yT", [256, S], BF16, kind="ExternalOutput").ap()

    k = KB(nc)
    cst, cstb = load_consts(k, nc, consts)
    um = UMaker(k, nc, h_in, condT, w_ada, b_ada, norm1, cstb)
    win = k.sb([128, 8, NCOL_A1], BF16, "win")
    wv = w_in.rearrange("(k p) n -> p k n", p=128)
    for kk in range(8):
        k.dma("pool", win[:, kk, :], wv[:, kk, :], writes=[win])
    pc = k.sb([128, 24], F32, "pcol")
    k.dma("sp", pc[:], pcold, writes=[pc])
    prm = k.sb([128, 4], F32, "prm")
    k.dma("sp", prm[:], prmd, writes=[prm])
    dnb = k.sb([128, 128], F32, "dnb")
    k.dma("sp", dnb[:], dnd.partition_broadcast(128), writes=[dnb])
    alg = k.sb([128, 2], F32, "alg")
    k.dma("sp", alg[:], alogd, writes=[alg])
    k.op("act", lambda e: e.activation(out=alg[:], in_=alg[:], func=AF.Exp), reads=[alg], writes=[alg])
    k.op("dve", lambda e: e.tensor_scalar(out=prm[:, 2:4], in0=alg[:], scalar1=-1.0, scalar2=None, op0=ALU.mult), reads=[alg, prm], writes=[prm])

    banks = Banks(k, 6)
    ptr = k.ps([128, 128], BF16, "ptr")
    uT = [k.sb([128, 8, 512], BF16, "uT%d" % i) for i in range(2)]
    cbuf = [[k.sb([128, 515], F32, "cbuf%d_%d" % (j, i)) for i in range(2)] for j in range(6)]
    for j in range(6):
        k.op("dve", lambda e: e.memset(cbuf[j][0][:, 0:3], 0.0), writes=[cbuf[j][0]])
    Sst = [k.sb([128, 128], F32, "Sst%d" % h) for h in range(2)]
    Sb = [k.sb([128, 128], BF16, "Sb%d" % h) for h in range(2)]
    for h in range(2):
        k.op("dve", lambda e: e.memset(Sst[h][:], 0.0), writes=[Sst[h]])
        k.op("dve", lambda e: e.memset(Sb[h][:], 0.0), writes=[Sb[h]])

    sb = k.sb
    sj = [sb([128, 512], F32, "sj%d" % j) for j in range(6)]
    sq = sb([128, 512], F32, "sq")
    rs = sb([128, 512], F32, "rs")
    nT = [sb([128, 512], BF16, "nT%d" % j) for j in range(4)]
    vTb = [sb([128, 512], BF16, "vTb%d" % h) for h in range(2)]
    sz = [sb([128, 256], F32, "sz%d" % t) for t in range(4)]
    g4 = sb([128, 4, 4], F32, "g4")
    beta = sb([128, 4, 2], F32, "beta")
    nbeta = sb([128, 4, 2], F32, "nbeta")
    gx = sb([128, 4, 2], F32, "gx")
    gg = sb([128, 4, 2], F32, "gg")
    gch = sb([128, 4], F32, "gch")
    gcl = sb([128, 8], F32, "gcl")
    eg = sb([128, 2], F32, "eg")
    ed = sb([128, 2], F32, "ed")
    be = sb([128, 2], F32, "be")
    egl = sb([128, 4], F32, "egl")
    gm = sb([128, 128], F32, "gm")
    DTm = sb([128, 128], F32, "DTm")
    Dm = sb([128, 128], F32, "Dm")
    A_ = sb([128, 128], F32, "A_")
    B_ = sb([128, 128], F32, "B_")
    Tt = sb([128, 128], F32, "Tt")
    Ttb = sb([128, 128], BF16, "Ttb")
    attT = sb([128, 128], BF16, "attT")
    bv = sb([128, 128], BF16, "bv")
    kbg = sb([128, 128], BF16, "kbg")
    kd = sb([128, 128], BF16, "kd")
    U = sb([128, 128], F32, "U")
    WT = sb([128, 128], BF16, "WT")
    dg = sb([128, 128], F32, "dg")
    qg = sb([128, 128], BF16, "qg")
    vnew = sb([128, 128], BF16, "vnew")
    o_tok = [sb([128, 4, 128], F32, "o_tok%d" % h) for h in range(2)]
    ss = sb([128, 4], F32, "oss")
    rstd = sb([128, 4], F32, "orstd")
    junk = sb([128, 128], F32, "ojunk")
    on = sb([128, 128], F32, "on")
    ytok = sb([128, 128], BF16, "ytok")
    yTs = [sb([128, 512], BF16, "yTs%d" % i) for i in range(2)]

    def op(e, fn, r, w):
        return k.op(e, fn, reads=r, writes=w)

    ident = cst[:, C_ID, :]
    TRI = cst[:, C_TRI, :]
    BLK = cst[:, C_BLK, :]
    SU = cst[:, C_SU, :]
    ONES = cst[:, C_ONES, :]
    UT64 = cst[:, C_UT64, :]
    out_toks = []
    cnt_y = 0
    for blk in range(nblk):
        u = uT[blk % 2]
        if stop <= -3:
            break
        um.block(blk, u)
        for j in range(6):
            if stop <= -2:
                break
            cb = cbuf[j][blk % 2]
            cbn = cbuf[j][(blk + 1) % 2]
            p = banks.get()
            for kk in range(8):
                op("pe", lambda e: e.matmul(p[:], lhsT=win[:, kk, j * 128:(j + 1) * 128], rhs=u[:, kk, :], start=(kk == 0), stop=(kk == 7)), [win, u], [p])
            op("act", lambda e: e.activation(out=cb[:, 3:515], in_=p[:], func=AF.Copy), [p], [cb])
            op("act", lambda e: e.activation(out=sj[j][:], in_=cb[:, 3:515], func=AF.Copy, scale=pc[:, j * 4 + 3:j * 4 + 4]), [cb, pc], [sj[j]])
            for w in range(3):
                op("dve", lambda e: e.scalar_tensor_tensor(out=sj[j][:], in0=cb[:, w:w + 512], scalar=pc[:, j * 4 + w:j * 4 + w + 1], in1=sj[j][:],
                                                           op0=ALU.mult, op1=ALU.add), [cb, pc, sj[j]], [sj[j]])
            op("pool", lambda e: e.tensor_copy(out=cbn[:, 0:3], in_=cb[:, 512:515]), [cb], [cbn])
            op("act", lambda e: e.activation(out=sj[j][:], in_=sj[j][:], func=AF.Silu), [sj[j]], [sj[j]])
        if stop <= -1:
            break
        for j in range(4):
            op("act", lambda e: e.activation(out=sq[:], in_=sj[j][:], func=AF.Square), [sj[j]], [sq])
            p = banks.get()
            op("pe", lambda e: e.matmul(p[:], lhsT=ONES, rhs=sq[:], start=True, stop=True), [cst, sq], [p])
            op("act", lambda e: e.activation(out=rs[:], in_=p[:], func=AF.Sqrt, bias=EPS), [p], [rs])
            op("dve", lambda e: e.reciprocal(out=rs[:], in_=rs[:]), [rs], [rs])
            scl = 128.0 ** -0.5 if j < 2 else 1.0
            op("dve", lambda e: e.scalar_tensor_tensor(out=nT[j][:], in0=sj[j][:], scalar=scl, in1=rs[:], op0=ALU.mult, op1=ALU.mult), [sj[j], rs], [nT[j]])
        for h in range(2):
            op("act", lambda e: e.activation(out=vTb[h][:], in_=sj[4 + h][:], func=AF.Copy), [sj[4 + h]], [vTb[h]])
        if stop <= 0:
            break
        for ti in range(4):
            p = banks.get()
            for kk in range(8):
                op("pe", lambda e: e.matmul(p[:, 0:256], lhsT=u[:, kk, ti * 128:(ti + 1) * 128], rhs=win[:, kk, 768:1024], start=(kk == 0), stop=(kk == 7)),
                   [win, u], [p])
            for kk in range(8):
                op("pe", lambda e: e.matmul(p[:, 256:260], lhsT=u[:, kk, ti * 128:(ti + 1) * 128], rhs=win[:, kk, 1024:1028], start=(kk == 0), stop=(kk == 7)),
                   [win, u], [p])
            op("act", lambda e: e.activation(out=sz[ti][:], in_=p[:, 0:256], func=AF.Silu), [p], [sz[ti]])
            op("act", lambda e: e.activation(out=g4[:, ti, :], in_=p[:, 256:260], func=AF.Copy), [p], [g4])
        if stop <= 0.2:
            break
        op("act", lambda e: e.activation(out=beta[:], in_=g4[:, :, 0:2], func=AF.Sigmoid), [g4], [beta])
        op("dve", lambda e: e.tensor_scalar(out=nbeta[:], in0=beta[:], scalar1=-1.0, scalar2=None, op0=ALU.mult), [beta], [nbeta])
        if stop <= 0.4:
            break
        op("dve", lambda e: e.tensor_tensor(out=gx[:], in0=g4[:, :, 2:4], in1=_bc(prm[:, 0:2].unsqueeze(1), [128, 4, 2]), op=ALU.add), [g4, prm], [gx])
        if stop <= 0.6:
            break
        op("act", lambda e: e.activation(out=gx[:], in_=gx[:], func=AF.Exp), [gx], [gx])
        op("act", lambda e: e.activation(out=gx[:], in_=gx[:], func=AF.Ln, bias=1.0), [gx], [gx])
        if stop <= 0.8:
            break
        op("dve", lambda e: e.tensor_tensor(out=gg[:], in0=gx[:], in1=_bc(prm[:, 2:4].unsqueeze(1), [128, 4, 2]), op=ALU.mult), [gx, prm], [gg])

        if stop <= 1:
            break
        for ti in range(4):
            tsl = slice(ti * 128, (ti + 1) * 128)
            op("dve", lambda e: e.tensor_tensor(out=gch[:].rearrange("p (a b) -> p a b", a=2), in0=_bc(gg[:, ti, :].unsqueeze(1), [128, 2, 2]),
                                                in1=_bc(cst[:, C_CH0, 0:2].unsqueeze(2), [128, 2, 2]), op=ALU.mult), [gg, cst], [gch])
            pg = banks.get()
            op("pe", lambda e: e.matmul(pg[:, 0:2], lhsT=TRI, rhs=gg[:, ti, :], start=True, stop=True), [cst, gg], [pg])
            op("pe", lambda e: e.matmul(pg[:, 2:4], lhsT=BLK, rhs=gg[:, ti, :], start=True, stop=True), [cst, gg], [pg])
            op("pe", lambda e: e.matmul(pg[:, 4:8], lhsT=ONES, rhs=gch[:], start=True, stop=True), [cst, gch], [pg])
            op("dve", lambda e: e.tensor_copy(out=gcl[:], in_=pg[:, 0:8]), [pg], [gcl])
            op("act", lambda e: e.activation(out=eg[:], in_=gcl[:, 0:2], func=AF.Exp), [gcl], [eg])
            op("dve", lambda e: e.tensor_tensor(out=ed[:], in0=gcl[:, 2:4], in1=gcl[:, 0:2], op=ALU.subtract), [gcl], [ed])
            op("act", lambda e: e.activation(out=ed[:], in_=ed[:], func=AF.Exp), [ed], [ed])
            op("act", lambda e: e.activation(out=egl[:], in_=gcl[:, 4:8], func=AF.Exp), [gcl], [egl])
            op("dve", lambda e: e.tensor_tensor(out=be[:], in0=beta[:, ti, :], in1=eg[:], op=ALU.mult), [beta, eg], [be])
            for h in range(2):
                if stop <= 2:
                    break
                qT = nT[h]
                kT = nT[2 + h]
                op("dve", lambda e: e.tensor_scalar(out=gm[:], in0=SU, scalar1=gg[:, ti, h:h + 1], scalar2=None, op0=ALU.mult), [cst, gg], [gm])
                p = banks.get()
                op("pe", lambda e: e.matmul(p[:, 0:128], lhsT=gm[:], rhs=TRI, start=True, stop=True), [gm, cst], [p])
                op("act", lambda e: e.activation(out=DTm[:], in_=p[:, 0:128], func=AF.Exp), [p], [DTm])
                op("pool", lambda e: e.tensor_tensor(out=DTm[:], in0=DTm[:], in1=UT64, op=ALU.mult), [DTm, cst], [DTm])
                p = banks.get()
                op("pe", lambda e: e.matmul(p[:, 0:128], lhsT=TRI, rhs=gm[:], start=True, stop=True), [gm, cst], [p])
                op("act", lambda e: e.activation(out=Dm[:], in_=p[:, 0:128], func=AF.Exp), [p], [Dm])
                op("pool", lambda e: e.tensor_tensor(out=Dm[:], in0=Dm[:], in1=SU, op=ALU.mult), [Dm, cst], [Dm])
                p = banks.get()
                op("pe", lambda e: e.matmul(p[:, 0:128], lhsT=kT[:, tsl], rhs=kT[:, tsl], start=True, stop=True), [kT], [p])
                op("dve", lambda e: e.scalar_tensor_tensor(out=A_[:], in0=p[:, 0:128], scalar=nbeta[:, ti, h:h + 1], in1=Dm[:], op0=ALU.mult, op1=ALU.mult),
                   [p, nbeta, Dm], [A_])
                p = banks.get()
                op("pe", lambda e: e.transpose(out=p[:, 0:128], in_=A_[:], identity=ident), [A_, cst], [p])
                op("act", lambda e: e.activation(out=B_[:], in_=p[:, 0:128], func=AF.Copy), [p], [B_])
                p = banks.get()
                op("pe", lambda e: e.matmul(p[:, 0:128], lhsT=kT[:, tsl], rhs=qT[:, tsl], start=True, stop=True), [kT, qT], [p])
                op("dve", lambda e: e.tensor_tensor(out=attT[:], in0=p[:, 0:128], in1=DTm[:], op=ALU.mult), [p, DTm], [attT])
                op("pool", lambda e: e.tensor_tensor(out=Tt[:], in0=B_[:], in1=ident, op=ALU.add), [B_, cst], [Tt])
                for lvl in range(1, 6):
                    pa = banks.get()
                    op("pe", lambda e: e.matmul(pa[:, 0:128], lhsT=B_[:], rhs=A_[:], start=True, stop=True), [A_, B_], [pa])
                    if lvl < 5:
                        pb = banks.get()
                        op("pe", lambda e: e.matmul(pb[:, 0:128], lhsT=A_[:], rhs=B_[:], start=True, stop=True), [A_, B_], [pb])
                    op("act", lambda e: e.activation(out=A_[:], in_=pa[:, 0:128], func=AF.Copy), [pa], [A_])
                    if lvl < 5:
                        op("dve", lambda e: e.tensor_copy(out=B_[:], in_=pb[:, 0:128]), [pb], [B_])
                    pt = banks.get()
                    op("pe", lambda e: e.matmul(pt[:, 0:128], lhsT=A_[:], rhs=Tt[:], start=True, stop=True), [A_, Tt], [pt])
                    op("dve", lambda e: e.tensor_tensor(out=Tt[:], in0=pt[:, 0:128], in1=Tt[:], op=ALU.add), [pt, Tt], [Tt])
                if stop <= 3:
                    continue
                op("act", lambda e: e.activation(out=Ttb[:], in_=Tt[:], func=AF.Copy), [Tt], [Ttb])
                op("pe", lambda e: e.transpose(out=ptr[:], in_=kT[:, tsl], identity=cstb[:, C_ID, :]), [kT, cstb], [ptr])
                op("dve", lambda e: e.tensor_scalar(out=kbg[:], in0=ptr[:], scalar1=be[:, h:h + 1], scalar2=None, op0=ALU.mult), [ptr, be], [kbg])
                op("dve", lambda e: e.tensor_scalar(out=kd[:], in0=ptr[:], scalar1=ed[:, h:h + 1], scalar2=None, op0=ALU.mult), [ptr, ed], [kd])
                op("pe", lambda e: e.transpose(out=ptr[:], in_=vTb[h][:, tsl], identity=cstb[:, C_ID, :]), [vTb[h], cstb], [ptr])
                op("dve", lambda e: e.tensor_scalar(out=bv[:], in0=ptr[:], scalar1=beta[:, ti, h:h + 1], scalar2=None, op0=ALU.mult), [ptr, beta], [bv])
                p = banks.get()
                op("pe", lambda e: e.matmul(p[:, 0:128], lhsT=Ttb[:], rhs=bv[:], start=True, stop=True), [Ttb, bv], [p])
                op("act", lambda e: e.activation(out=U[:], in_=p[:, 0:128], func=AF.Copy), [p], [U])
                p = banks.get()
                op("pe", lambda e: e.matmul(p[:, 0:128], lhsT=kbg[:], rhs=Ttb[:], start=True, stop=True), [kbg, Ttb], [p])
                op("act", lambda e: e.activation(out=WT[:], in_=p[:, 0:128], func=AF.Copy), [p], [WT])
                op("dve", lambda e: e.tensor_scalar(out=dg[:], in0=ident, scalar1=eg[:, h:h + 1], scalar2=None, op0=ALU.mult), [cst, eg], [dg])
                p = banks.get()
                op("pe", lambda e: e.matmul(p[:, 0:128], lhsT=ONES, rhs=dg[:], start=True, stop=True), [cst, dg], [p])
                op("dve", lambda e: e.tensor_tensor(out=qg[:], in0=p[:, 0:128], in1=qT[:, tsl], op=ALU.mult), [p, qT], [qg])
                for half in range(2):
                    if stop <= 4:
                        break
                    rows = slice(half * 64, (half + 1) * 64)
                    pw = banks.get()
                    op("pe", lambda e: e.matmul(pw[rows, 0:128], lhsT=WT[:, rows], rhs=Sb[h][:], start=True, stop=True), [WT, Sb[h]], [pw])
                    op("dve", lambda e: e.tensor_tensor(out=vnew[rows, :], in0=U[rows, :], in1=pw[rows, 0:128], op=ALU.subtract), [U, pw], [vnew])
                    po = banks.get()
                    op("pe", lambda e: e.matmul(po[rows, 0:128], lhsT=qg[:, rows], rhs=Sb[h][:], start=True, stop=False), [qg, Sb[h]], [po])
                    op("pe", lambda e: e.matmul(po[rows, 0:128], lhsT=attT[rows, rows], rhs=vnew[rows, :], start=False, stop=True), [attT, vnew], [po])
                    pk = banks.get()
                    op("pe", lambda e: e.matmul(pk[:, 0:128], lhsT=kd[rows, :], rhs=vnew[rows, :], start=True, stop=True), [kd, vnew], [pk])
                    op("dve", lambda e: e.scalar_tensor_tensor(out=Sst[h][:], in0=Sst[h][:], scalar=egl[:, half * 2 + h:half * 2 + h + 1], in1=pk[:, 0:128],
                                                               op0=ALU.mult, op1=ALU.add), [Sst[h], egl, pk], [Sst[h]])
                    op("act", lambda e: e.activation(out=Sb[h][:], in_=Sst[h][:], func=AF.Copy), [Sst[h]], [Sb[h]])
                    op("act", lambda e: e.activation(out=o_tok[h][rows, ti, :], in_=po[rows, 0:128], func=AF.Copy), [po], [o_tok[h]])
        for h in range(2):
            if stop <= 5:
                break
            for ti in range(4):
                op("act", lambda e: e.activation(out=junk[:], in_=o_tok[h][:, ti, :], func=AF.Square, accum_out=ss[:, ti:ti + 1]), [o_tok[h]], [junk, ss])
            rstd_from_ss(k, ss, rstd, 4, 1.0 / 128.0)
            ys = yTs[cnt_y % 2]
            cnt_y += 1
            for ti in range(4):
                op("dve", lambda e: e.scalar_tensor_tensor(out=on[:], in0=o_tok[h][:, ti, :], scalar=rstd[:, ti:ti + 1], in1=dnb[:], op0=ALU.mult, op1=ALU.mult),
                   [o_tok[h], rstd, dnb], [on])
                op("pool", lambda e: e.tensor_tensor(out=ytok[:], in0=on[:], in1=sz[ti][:, h * 128:(h + 1) * 128], op=ALU.mult), [on, sz[ti]], [ytok])
                op("pe", lambda e: e.transpose(out=ptr[:], in_=ytok[:], identity=cstb[:, C_ID, :]), [ytok, cstb], [ptr])
                op("act", lambda e: e.activation(out=ys[:, ti * 128:(ti + 1) * 128], in_=ptr[:], func=AF.Copy), [ptr], [ys])
            out_toks.append(k.dma("sp", yT[h * 128:(h + 1) * 128, blk * 512:(blk + 1) * 512], ys[:], reads=[ys]))
    k.finish(out_toks)
    k.release(0)
    return nc, k


def phaseA1_inmaps(h, inp):
    maps = []
    consts = make_consts()
    w_in = inp["w_in_c"][0]
    wa = np.ascontiguousarray(inp["w_ada"][1][:, 0:2048])
    ba = np.ascontiguousarray(inp["b_ada"][1][0:2048])
    cw = inp["conv_c_w"][0]
    for core in range(NCORES):
        b, hg = core // 4, core % 4
        hs = [2 * hg, 2 * hg + 1]
        cols = []
        for base in (0, 1024, 2048):
            for hh in hs:
                cols.append(base + np.arange(hh * 128, (hh + 1) * 128))
        for hh in hs:
            cols.append(3072 + np.arange(hh * 128, (hh + 1) * 128))
        cols.append(np.array([4096 + hs[0], 4096 + hs[1], 4104 + hs[0], 4104 + hs[1]]))
        cols = np.concatenate(cols)
        pcol = np.zeros((128, 6, 4), np.float32)
        j = 0
        for base in (0, 1024, 2048):
            for hh in hs:
                pcol[:, j, :] = cw[:, base + hh * 128:base + (hh + 1) * 128].T
                j += 1
        prm = np.zeros((128, 4), np.float32)
        prm[:, 0] = inp["dn_dt_bias"][0][hs[0]]
        prm[:, 1] = inp["dn_dt_bias"][0][hs[1]]
        maps.append({
            "h_in": np.ascontiguousarray(h[b]),
            "condT": np.ascontiguousarray(inp["c"][b].reshape(8, 128).T),
            "w_ada": wa, "b_ada": ba,
            "norm1": np.ascontiguousarray(inp["norm1"][1]),
            "w_in": np.ascontiguousarray(w_in[:, cols]),
            "pcol": np.ascontiguousarray(pcol.reshape(128, 24)),
            "prm": prm,
            "a_log": np.ascontiguousarray(np.tile(inp["dn_a_log"][0][hs][None, :], (128, 1))),
            "dnorm": np.ascontiguousarray(inp["dn_norm"][0]),
            "consts": consts,
        })
    return maps


def assemble_yT_A1(results):
    yT = np.zeros((2, D, S), ml_dtypes.bfloat16)
    for core in range(NCORES):
        b, hg = core // 4, core % 4
        yT[b, hg * 256:(hg + 1) * 256] = results[core]["yT"]
    return yT


_PROGS = {}


def _prog(name):
    if name not in _PROGS:
        if name == "A0":
            _PROGS[name] = build_phaseA0()[0]
        elif name == "A1":
            _PROGS[name] = build_phaseA1()[0]
        elif name == "B0":
            _PROGS[name] = build_phaseB(False)[0]
        else:
            _PROGS[name] = build_phaseB(True)[0]
    return _PROGS[name]


def _gather_B(results):
    return np.stack([np.concatenate([results[b * 4 + q]["out"] for q in range(4)], 0) for b in range(2)])


def kernel(**inputs):
    inp = {k_: np.ascontiguousarray(np.asarray(v, dtype=np.float32)) for k_, v in inputs.items()}
    cores = list(range(NCORES))
    x = inp["x"]
    r = run_bass_kernel_spmd(_prog("A0"), phaseA0_inmaps(x, inp), core_ids=cores)
    yT0 = assemble_yT_A0(r.results)
    r = run_bass_kernel_spmd(_prog("B0"), phaseB_inmaps(0, x, yT0, inp, False), core_ids=cores)
    h0 = _gather_B(r.results)
    r = run_bass_kernel_spmd(_prog("A1"), phaseA1_inmaps(h0, inp), core_ids=cores)
    yT1 = assemble_yT_A1(r.results)
    r = run_bass_kernel_spmd(_prog("B1"), phaseB_inmaps(1, h0, yT1, inp, True), core_ids=cores)
    return _gather_B(r.results).astype(np.float32)
```

```python
import numpy as np
import ml_dtypes
import concourse.bass as bass
import concourse.mybir as mybir
from concourse.bass_utils import run_bass_kernel_spmd

F32 = mybir.dt.float32
BF16 = mybir.dt.bfloat16
AF = mybir.ActivationFunctionType
ALU = mybir.AluOpType
AX = mybir.AxisListType

D = 1024
S = 8192
EPS = 1e-6
NCORES = 8


class T:
    __slots__ = ("h", "w", "r", "name")

    def __init__(self, h, name=""):
        self.h = h
        self.w = None
        self.r = {}
        self.name = name

    def __getitem__(self, idx):
        return self.h[idx]


class KB:
    NDMA_SEM = 6

    def __init__(self, nc):
        self.nc = nc
        self.eng = {"pe": nc.tensor, "act": nc.scalar, "dve": nc.vector, "pool": nc.gpsimd, "sp": nc.sync}
        self.csem = {e: nc.alloc_semaphore("cs_" + e) for e in ("pe", "act", "dve", "pool")}
        self.cnt = {e: 0 for e in self.csem}
        self.pending = {e: False for e in self.csem}
        self.dsem = {}
        self.dcnt = {}
        for q in ("sp", "pool", "act"):
            self.dsem[q] = [nc.alloc_semaphore("ds_%s%d" % (q, i)) for i in range(self.NDMA_SEM)]
            self.dcnt[q] = 0
        self.seen = {e: {} for e in self.eng}
        self.fast_pe = False
        self.ninst = 0
        self.stack = []
        self.tiles = []
        self.freed = {}
        self.uid = 0

    def sb(self, shape, dt=F32, name=None):
        self.uid += 1
        nm = "%s_%d" % (name or "t", self.uid)
        g = self.nc.sbuf_tensor(nm, list(shape), dt)
        h = g.__enter__()
        self.stack.append(g)
        t = T(h, nm)
        t.r = dict(self.freed)
        self.tiles.append(t)
        return t

    def ps(self, shape, dt=F32, name=None):
        self.uid += 1
        nm = "%s_%d" % (name or "p", self.uid)
        g = self.nc.psum_tensor(nm, list(shape), dt)
        h = g.__enter__()
        self.stack.append(g)
        t = T(h, nm)
        t.r = dict(self.freed)
        self.tiles.append(t)
        return t

    def mark(self):
        return len(self.stack)

    def release(self, mark):
        while len(self.stack) > mark:
            g = self.stack.pop()
            t = self.tiles.pop()
            toks = list(t.r.values()) + ([t.w] if t.w is not None else [])
            for tok in toks:
                o = self.freed.get(tok[0])
                if o is None or o[2] < tok[2]:
                    self.freed[tok[0]] = tok
            g.__exit__(None, None, None)

    def _deps(self, e, reads, writes):
        need = {}

        def add(tok):
            if tok is None:
                return
            key, sem, val = tok
            if key == "pe" and e == "pe" and self.fast_pe:
                return
            if self.seen[e].get(key, 0) >= val:
                return
            if key not in need or need[key][1] < val:
                need[key] = (sem, val)

        for t in reads:
            add(t.w)
        for t in writes:
            add(t.w)
            for tok in t.r.values():
                add(tok)
        for key, (sem, val) in need.items():
            self.eng[e].wait_ge(sem, val)
            self.seen[e][key] = val

    def _commit(self, tok, reads, writes):
        key = tok[0]
        for t in reads:
            o = t.r.get(key)
            if o is None or o[2] < tok[2]:
                t.r[key] = tok
        for t in writes:
            t.w = tok
            t.r = {}

    def op(self, e, fn, reads=(), writes=(), inc=True, fast=False):
        inc = True
        self.fast_pe = fast
        self._deps(e, reads, writes)
        self.fast_pe = False
        ins = fn(self.eng[e])
        if inc:
            self.cnt[e] += 1
            ins.then_inc(self.csem[e], 1)
            tok = (e, self.csem[e], self.cnt[e])
            self.pending[e] = False
        else:
            tok = (e, self.csem[e], self.cnt[e] + 1)
            self.pending[e] = True
        self._commit(tok, reads, writes)
        self.ninst += 1
        return tok

    def dma(self, q, out, in_, reads=(), writes=(), **kw):
        i = self.dcnt[q]
        self.dcnt[q] += 1
        slot = i % self.NDMA_SEM
        rnd = i // self.NDMA_SEM
        sem = self.dsem[q][slot]
        key = ("d", q, slot)
        if rnd > 0 and self.seen[q].get(key, 0) < 16 * rnd:
            self.eng[q].wait_ge(sem, 16 * rnd)
            self.seen[q][key] = 16 * rnd
        self._deps(q, reads, writes)
        ins = self.eng[q].dma_start(out=out, in_=in_, **kw)
        ins.then_inc(sem, 16)
        tok = (key, sem, 16 * (rnd + 1))
        self._commit(tok, reads, writes)
        self.ninst += 1
        return tok

    def finish(self, toks):
        for e in self.pending:
            assert not self.pending[e], "engine %s ends with a non-incrementing instruction" % e
        toks = list(toks)
        for e in self.csem:
            if self.cnt[e] > 0:
                toks.append((e, self.csem[e], self.cnt[e]))
        for q in self.dsem:
            n = self.dcnt[q]
            for slot in range(self.NDMA_SEM):
                if n > slot:
                    rounds = (n - slot + self.NDMA_SEM - 1) // self.NDMA_SEM
                    toks.append((("d", q, slot), self.dsem[q][slot], 16 * rounds))
        for tok in toks:
            key, sem, val = tok
            if self.seen["sp"].get(key, 0) < val:
                self.eng["sp"].wait_ge(sem, val)
                self.seen["sp"][key] = val


def _bc(ap, shape):
    return ap.to_broadcast(list(shape))


def load_consts(k, nc, consts_ap):
    c = k.sb([128, NCONST, 128], F32, "consts")
    k.dma("sp", c[:], consts_ap, writes=[c])
    cb = k.sb([128, NCONST, 128], BF16, "consts_bf")
    k.op("dve", lambda e: e.tensor_copy(out=cb[:], in_=c[:]), reads=[c], writes=[cb])
    return c, cb


NCONST = 10
C_ID = 0
C_TRI = 1
C_SU = 2
C_BLK = 3
C_M16 = 4
C_MC1 = 5
C_MC2 = 6
C_ONES = 7
C_UT64 = 8
C_CH0 = 9


def make_consts():
    p = np.arange(128)
    i = p[:, None]
    j = p[None, :]
    c = np.zeros((128, NCONST, 128), np.float32)
    same64 = (i // 64) == (j // 64)
    same32 = (i // 32) == (j // 32)
    same16 = (i // 16) == (j // 16)
    c[:, C_ID] = (i == j)
    c[:, C_TRI] = same64 & (i <= j)
    c[:, C_SU] = same64 & (j < i)
    c[:, C_BLK] = same64
    c[:, C_M16] = same16 & (j < i)
    c[:, C_MC1] = same32 & (~same16) & (j < i)
    c[:, C_MC2] = same64 & (~same32) & (j < i)
    c[:, C_ONES] = 1.0
    c[:, C_UT64] = same64 & (i <= j)
    c[:, C_CH0, 0] = (p < 64)
    c[:, C_CH0, 1] = (p >= 64)
    return c


def compute_mod(k, nc, condT_ap, w_ada_ap, b_ada_ap, ncols, outs, ps_pool):
    m0 = k.mark()
    ct = k.sb([128, 8], F32, "ct")
    k.dma("sp", ct[:], condT_ap, writes=[ct])
    sg = k.sb([128, 8], F32, "sg")
    k.op("act", lambda e: e.activation(out=sg[:], in_=ct[:], func=AF.Sigmoid), reads=[ct], writes=[sg])
    cond = k.sb([128, 8], F32, "cond")
    k.op("dve", lambda e: e.tensor_tensor(out=cond[:], in0=ct[:], in1=sg[:], op=ALU.mult), reads=[ct, sg], writes=[cond])
    cbc = k.sb([128, 8, 128], BF16, "cond_bc")
    k.op("dve", lambda e: e.tensor_copy(out=cbc[:], in_=_bc(cond[:].unsqueeze(2), [128, 8, 128])), reads=[cond], writes=[cbc])
    wv = w_ada_ap.rearrange("(k p) n -> p k n", p=128)
    nch = ncols // 512
    wbuf = [k.sb([128, 8, 512], BF16, "wada%d" % i) for i in range(2)]
    bbuf = [k.sb([128, 512], F32, "bada%d" % i) for i in range(2)]
    for j in range(nch):
        wb = wbuf[j % 2]
        bb = bbuf[j % 2]
        k.dma("pool", wb[:], wv[:, :, j * 512:(j + 1) * 512], writes=[wb])
        k.dma("sp", bb[:], b_ada_ap[j * 512:(j + 1) * 512].partition_broadcast(128), writes=[bb])
        pt = ps_pool[j % len(ps_pool)]
        for kk in range(8):
            k.op("pe", lambda e, kk=kk: e.matmul(pt[:, 0:512], lhsT=cbc[:, kk, :], rhs=wb[:, kk, :], start=(kk == 0), stop=(kk == 7)),
                 reads=[cbc, wb], writes=[pt], fast=True)
        o = outs[j // 2]
        c0 = (j % 2) * 512
        k.op("dve", lambda e: e.tensor_tensor(out=o[:, c0:c0 + 512], in0=pt[:, 0:512], in1=bb[:], op=ALU.add),
             reads=[pt, bb], writes=[o])
    k.release(m0)


def rstd_from_ss(k, ss, rstd, n, scale):
    k.op("dve", lambda e: e.tensor_scalar(out=rstd[:, 0:n], in0=ss[:, 0:n], scalar1=scale, scalar2=EPS, op0=ALU.mult, op1=ALU.add),
         reads=[ss], writes=[rstd])
    k.op("act", lambda e: e.activation(out=rstd[:, 0:n], in_=rstd[:, 0:n], func=AF.Sqrt), reads=[rstd], writes=[rstd])
    k.op("dve", lambda e: e.reciprocal(out=rstd[:, 0:n], in_=rstd[:, 0:n]), reads=[rstd], writes=[rstd])


NTB = 2048
NE = 32


def build_phaseB(final, upto=99, ne=NE):
    nc = bass.Bass("TRN2", target_bir_lowering=False)
    dt = nc.dram_tensor
    h_in = dt("h_in", [NTB, D], F32, kind="ExternalInput").ap()
    yT = dt("yT", [D, NTB], BF16, kind="ExternalInput").ap()
    w_out = dt("w_out", [D, D], F32, kind="ExternalInput").ap()
    condT = dt("condT", [128, 8], F32, kind="ExternalInput").ap()
    w_ada = dt("w_ada", [D, 4096], F32, kind="ExternalInput").ap()
    b_ada = dt("b_ada", [4096], F32, kind="ExternalInput").ap()
    norm2 = dt("norm2", [D], F32, kind="ExternalInput").ap()
    w_r = dt("w_r", [D, 36], F32, kind="ExternalInput").ap()
    b_r = dt("b_r", [36], F32, kind="ExternalInput").ap()
    if upto >= 5:
        w1 = dt("w1", [ne, D, 512], F32, kind="ExternalInput").ap()
        w3 = dt("w3", [ne, D, 512], F32, kind="ExternalInput").ap()
        w2 = dt("w2", [ne, 512, D], F32, kind="ExternalInput").ap()
    fnorm = dt("fnorm", [D], F32, kind="ExternalInput").ap()
    consts = dt("consts", [128, NCONST, 128], F32, kind="ExternalInput").ap()
    out = dt("out", [NTB, D], F32, kind="ExternalOutput").ap()

    k = KB(nc)
    NT = NTB // 128
    cst, cstb = load_consts(k, nc, consts)
    ident = cst

    hres = [k.sb([128, D], F32, "hres%d" % t) for t in range(NT)]
    h_v = h_in.rearrange("(t p) d -> t p d", p=128)
    for t in range(NT):
        k.dma("sp", hres[t][:], h_v[t], writes=[hres[t]])
    gt2 = k.sb([128, D], F32, "gt2")
    u2T = k.sb([128, 8, NTB], BF16, "u2T")
    logits = k.sb([128, NT, 36], F32, "logits")
    Wd = k.sb([128, NT, NE], F32, "Wd")
    m_mod = k.mark()
    gt1 = k.sb([128, D], F32, "gt1")
    sh2 = k.sb([128, D], F32, "sh2")
    sc2 = k.sb([128, D], F32, "sc2")

    if upto < 1:
        return _finB(k, out, hres, NT)
    m1 = k.mark()
    pp = [k.ps([128, 512], F32, "modps%d" % i) for i in range(2)]
    compute_mod(k, nc, condT, w_ada, b_ada, 4096, [gt1, sh2, sc2, gt2], pp)
    k.release(m1)
    g2 = k.sb([128, D], F32, "g2")
    k.dma("sp", g2[:], norm2.partition_broadcast(128), writes=[g2])
    k.op("dve", lambda e: e.scalar_tensor_tensor(out=g2[:], in0=sc2[:], scalar=1.0, in1=g2[:], op0=ALU.add, op1=ALU.mult),
         reads=[sc2, g2], writes=[g2])

    if upto < 2:
        return _finB(k, out, hres, NT)
    m2 = k.mark()
    yTs = k.sb([128, 8, NTB], BF16, "yTs")
    yv = yT.rearrange("(k p) t -> p k t", p=128)
    for kk in range(8):
        k.dma("sp", yTs[:, kk, :], yv[:, kk, :], writes=[yTs])
    wo = k.sb([128, 8, D], BF16, "wo")
    wov = w_out.rearrange("(k p) n -> p k n", p=128)
    for kk in range(0, 8, 2):
        k.dma("pool", wo[:, kk:kk + 2, :], wov[:, kk:kk + 2, :], writes=[wo])
    k.op("pool", lambda e: e.tensor_tensor(out=wo[:], in0=wo[:], in1=_bc(gt1[:].unsqueeze(1), [128, 8, D]), op=ALU.mult),
         reads=[wo, gt1], writes=[wo])
    psy = [k.ps([128, D], F32, "psy%d" % i) for i in range(2)]
    for t in range(NT):
        p = psy[t % 2]
        for half in range(2):
            for kk in range(8):
                k.op("pe", lambda e, kk=kk, half=half: e.matmul(p[:, half * 512:(half + 1) * 512], lhsT=yTs[:, kk, t * 128:(t + 1) * 128],
                                                                 rhs=wo[:, kk, half * 512:(half + 1) * 512], start=(kk == 0), stop=(kk == 7)),
                     reads=[yTs, wo], writes=[p], fast=True)
        k.op("dve", lambda e: e.tensor_tensor(out=hres[t][:], in0=p[:], in1=hres[t][:], op=ALU.add), reads=[p, hres[t]], writes=[hres[t]])
    k.release(m2)

    if upto < 3:
        return _finB(k, out, hres, NT)
    m3 = k.mark()
    ss = k.sb([128, NT], F32, "ss")
    rstd = k.sb([128, NT], F32, "rstd")
    junk = [k.sb([128, D], BF16, "junk%d" % i) for i in range(2)]
    for t in range(NT):
        jk = junk[t % 2]
        k.op("act", lambda e: e.activation(out=jk[:], in_=hres[t][:], func=AF.Square, accum_out=ss[:, t:t + 1]),
             reads=[hres[t]], writes=[jk, ss])
    rstd_from_ss(k, ss, rstd, NT, 1.0 / D)
    wr = k.sb([128, 8, 36], F32, "wr")
    k.dma("sp", wr[:], w_r.rearrange("(k p) n -> p k n", p=128), writes=[wr])
    brb = k.sb([128, 36], F32, "brb")
    k.dma("sp", brb[:], b_r.partition_broadcast(128), writes=[brb])
    t1b = [k.sb([128, D], F32, "t1b%d" % i) for i in range(2)]
    u32 = [k.sb([128, D], F32, "u32_%d" % i) for i in range(2)]
    uT32 = [k.sb([128, 8, 128], F32, "uT32_%d" % i) for i in range(2)]
    pst = [k.ps([128, 8, 128], F32, "pst%d" % i) for i in range(2)]
    psr = [k.ps([128, 36], F32, "psr%d" % i) for i in range(2)]
    for t in range(NT):
        a = t1b[t % 2]
        u = u32[t % 2]
        ut = uT32[t % 2]
        pt = pst[t % 2]
        pr = psr[t % 2]
        k.op("dve", lambda e: e.scalar_tensor_tensor(out=a[:], in0=hres[t][:], scalar=rstd[:, t:t + 1], in1=g2[:], op0=ALU.mult, op1=ALU.mult),
             reads=[hres[t], rstd, g2], writes=[a])
        k.op("pool", lambda e: e.tensor_tensor(out=u[:], in0=a[:], in1=sh2[:], op=ALU.add), reads=[a, sh2], writes=[u])
        for kk in range(8):
            k.op("pe", lambda e, kk=kk: e.transpose(out=pt[:, kk, :], in_=u[:, kk * 128:(kk + 1) * 128], identity=cst[:, C_ID, :]),
                 reads=[u, cst], writes=[pt], inc=(kk == 7))
        k.op("act", lambda e: e.activation(out=ut[:], in_=pt[:], func=AF.Copy), reads=[pt], writes=[ut])
        k.op("pool", lambda e: e.tensor_copy(out=u2T[:, :, t * 128:(t + 1) * 128], in_=ut[:]), reads=[ut], writes=[u2T])
        for kk in range(8):
            k.op("pe", lambda e, kk=kk: e.matmul(pr[:], lhsT=ut[:, kk, :], rhs=wr[:, kk, :], start=(kk == 0), stop=(kk == 7)),
                 reads=[ut, wr], writes=[pr], inc=(kk == 7))
        k.op("dve", lambda e: e.tensor_tensor(out=logits[:, t, :], in0=pr[:], in1=brb[:], op=ALU.add), reads=[pr, brb], writes=[logits])
    k.release(m3)

    if upto < 4:
        return _finB(k, out, hres, NT)
    m4 = k.mark()
    BIG = 1.0e30

    def dve(fn, reads, writes):
        k.op("dve", fn, reads=reads, writes=writes)

    lg = logits[:, :, 0:4]
    le = logits[:, :, 4:36]
    gmax = k.sb([128, NT], F32, "gmax")
    dve(lambda e: e.tensor_reduce(out=gmax[:], in_=lg, axis=AX.X, op=ALU.max), [logits], [gmax])
    eg = k.sb([128, NT, 4], F32, "eg")
    dve(lambda e: e.tensor_tensor(out=eg[:], in0=lg, in1=_bc(gmax[:].unsqueeze(2), [128, NT, 4]), op=ALU.subtract), [logits, gmax], [eg])
    k.op("act", lambda e: e.activation(out=eg[:], in_=eg[:], func=AF.Exp), reads=[eg], writes=[eg])
    gsum = k.sb([128, NT], F32, "gsum")
    dve(lambda e: e.tensor_reduce(out=gsum[:], in_=eg[:], axis=AX.X, op=ALU.add), [eg], [gsum])
    pgt = k.sb([128, NT], F32, "pgt")
    dve(lambda e: e.reciprocal(out=pgt[:], in_=gsum[:]), [gsum], [pgt])
    pen = k.sb([128, NT, 4], F32, "pen")
    dve(lambda e: e.tensor_tensor(out=pen[:], in0=lg, in1=_bc(gmax[:].unsqueeze(2), [128, NT, 4]), op=ALU.is_equal), [logits, gmax], [pen])
    dve(lambda e: e.tensor_scalar(out=pen[:], in0=pen[:], scalar1=1.0, scalar2=BIG, op0=ALU.subtract, op1=ALU.mult), [pen], [pen])
    lem = k.sb([128, NT, NE], F32, "lem")
    dve(lambda e: e.tensor_tensor(out=lem[:].rearrange("p t (g x) -> p t g x", g=4), in0=le.rearrange("p t (g x) -> p t g x", g=4),
                                  in1=_bc(pen[:].unsqueeze(3), [128, NT, 4, 8]), op=ALU.add), [logits, pen], [lem])
    mx1 = k.sb([128, NT], F32, "mx1")
    dve(lambda e: e.tensor_reduce(out=mx1[:], in_=lem[:], axis=AX.X, op=ALU.max), [lem], [mx1])
    oh1 = k.sb([128, NT, NE], F32, "oh1")
    dve(lambda e: e.tensor_tensor(out=oh1[:], in0=lem[:], in1=_bc(mx1[:].unsqueeze(2), [128, NT, NE]), op=ALU.is_equal), [lem, mx1], [oh1])
    lem2 = k.sb([128, NT, NE], F32, "lem2")
    dve(lambda e: e.scalar_tensor_tensor(out=lem2[:], in0=oh1[:], scalar=-BIG, in1=lem[:], op0=ALU.mult, op1=ALU.add), [oh1, lem], [lem2])
    mx2 = k.sb([128, NT], F32, "mx2")
    dve(lambda e: e.tensor_reduce(out=mx2[:], in_=lem2[:], axis=AX.X, op=ALU.max), [lem2], [mx2])
    oh2 = k.sb([128, NT, NE], F32, "oh2")
    dve(lambda e: e.tensor_tensor(out=oh2[:], in0=lem2[:], in1=_bc(mx2[:].unsqueeze(2), [128, NT, NE]), op=ALU.is_equal), [lem2, mx2], [oh2])
    rr = k.sb([128, NT], F32, "rr")
    dve(lambda e: e.tensor_tensor(out=rr[:], in0=mx2[:], in1=mx1[:], op=ALU.subtract), [mx2, mx1], [rr])
    k.op("act", lambda e: e.activation(out=rr[:], in_=rr[:], func=AF.Exp), reads=[rr], writes=[rr])
    den = k.sb([128, NT], F32, "den")
    dve(lambda e: e.tensor_scalar(out=den[:], in0=rr[:], scalar1=1.0, scalar2=None, op0=ALU.add), [rr], [den])
    dve(lambda e: e.reciprocal(out=den[:], in_=den[:]), [den], [den])
    wt1 = k.sb([128, NT], F32, "wt1")
    dve(lambda e: e.tensor_tensor(out=wt1[:], in0=pgt[:], in1=den[:], op=ALU.mult), [pgt, den], [wt1])
    wt2 = k.sb([128, NT], F32, "wt2")
    dve(lambda e: e.tensor_tensor(out=wt2[:], in0=wt1[:], in1=rr[:], op=ALU.mult), [wt1, rr], [wt2])
    dve(lambda e: e.tensor_tensor(out=Wd[:], in0=oh1[:], in1=_bc(wt1[:].unsqueeze(2), [128, NT, NE]), op=ALU.mult), [oh1, wt1], [Wd])
    dve(lambda e: e.tensor_tensor(out=oh2[:], in0=oh2[:], in1=_bc(wt2[:].unsqueeze(2), [128, NT, NE]), op=ALU.mult), [oh2, wt2], [oh2])
    dve(lambda e: e.tensor_tensor(out=Wd[:], in0=Wd[:], in1=oh2[:], op=ALU.add), [Wd, oh2], [Wd])
    k.release(m4)

    if upto < 5:
        return _finB(k, out, hres, NT)
    k.release(m_mod)
    m5 = k.mark()
    w1b = [k.sb([128, 8, 512], BF16, "w1b%d" % i) for i in range(2)]
    w3b = [k.sb([128, 8, 512], BF16, "w3b%d" % i) for i in range(2)]
    w2b = [k.sb([128, 4, D], BF16, "w2b%d" % i) for i in range(2)]
    stg = [k.sb([128, 4096], F32, "stg%d" % i) for i in range(2)]
    actT = [[k.sb([128, 512], BF16, "actT%d_%d" % (i, f)) for f in range(4)] for i in range(2)]
    slb = [k.sb([128, 512], F32, "slb%d" % i) for i in range(2)]
    ps1 = [k.ps([128, 512], F32, "ps1_%d" % i) for i in range(2)]
    ps3 = [k.ps([128, 512], F32, "ps3_%d" % i) for i in range(2)]
    psy = [k.ps([128, D], F32, "psye%d" % i) for i in range(2)]
    cnt_f = 0
    cnt_y = 0
    for ex in range(ne):
        b = ex % 2
        s1 = stg[(3 * ex) % 2]
        k.dma("sp", s1[:].rearrange("p (k f) -> p k f", k=8), w1[ex].rearrange("(k p) f -> p k f", p=128), writes=[s1])
        k.op("pool", lambda e: e.tensor_copy(out=w1b[b][:].rearrange("p k f -> p (k f)"), in_=s1[:]), reads=[s1], writes=[w1b[b]])
        s3 = stg[(3 * ex + 1) % 2]
        k.dma("sp", s3[:].rearrange("p (k f) -> p k f", k=8), w3[ex].rearrange("(k p) f -> p k f", p=128), writes=[s3])
        k.op("pool", lambda e: e.tensor_copy(out=w3b[b][:].rearrange("p k f -> p (k f)"), in_=s3[:]), reads=[s3], writes=[w3b[b]])
        s2 = stg[(3 * ex + 2) % 2]
        k.dma("sp", s2[:].rearrange("p (c d) -> p c d", c=4), w2[ex].rearrange("(c p) d -> p c d", p=128), writes=[s2])
        k.op("pool", lambda e: e.tensor_tensor(out=w2b[b][:], in0=s2[:].rearrange("p (c d) -> p c d", c=4), in1=_bc(gt2[:].unsqueeze(1), [128, 4, D]), op=ALU.mult),
             reads=[s2, gt2], writes=[w2b[b]])
        for blk in range(NTB // 512):
            ab = actT[blk % 2]
            for fc in range(4):
                p1 = ps1[cnt_f % 2]
                p3 = ps3[cnt_f % 2]
                sl = slb[cnt_f % 2]
                cnt_f += 1
                for kk in range(8):
                    k.op("pe", lambda e, kk=kk: e.matmul(p1[:], lhsT=w1b[b][:, kk, fc * 128:(fc + 1) * 128], rhs=u2T[:, kk, blk * 512:(blk + 1) * 512],
                                                         start=(kk == 0), stop=(kk == 7)), reads=[w1b[b], u2T], writes=[p1], fast=True)
                for kk in range(8):
                    k.op("pe", lambda e, kk=kk: e.matmul(p3[:], lhsT=w3b[b][:, kk, fc * 128:(fc + 1) * 128], rhs=u2T[:, kk, blk * 512:(blk + 1) * 512],
                                                         start=(kk == 0), stop=(kk == 7)), reads=[w3b[b], u2T], writes=[p3], fast=True)
                k.op("act", lambda e: e.activation(out=sl[:], in_=p1[:], func=AF.Silu), reads=[p1], writes=[sl])
                k.op("dve", lambda e: e.tensor_tensor(out=ab[fc][:], in0=p3[:], in1=sl[:], op=ALU.mult), reads=[p3, sl], writes=[ab[fc]])
            for ti in range(4):
                t = blk * 4 + ti
                py = psy[cnt_y % 2]
                cnt_y += 1
                for half in range(2):
                    for fc in range(4):
                        k.op("pe", lambda e, fc=fc, half=half: e.matmul(py[:, half * 512:(half + 1) * 512], lhsT=ab[fc][:, ti * 128:(ti + 1) * 128],
                                                                         rhs=w2b[b][:, fc, half * 512:(half + 1) * 512], start=(fc == 0), stop=(fc == 3)),
                             reads=[ab[fc], w2b[b]], writes=[py], fast=True)
                k.op("dve", lambda e: e.scalar_tensor_tensor(out=hres[t][:], in0=py[:], scalar=Wd[:, t, ex:ex + 1], in1=hres[t][:],
                                                             op0=ALU.mult, op1=ALU.add), reads=[py, Wd, hres[t]], writes=[hres[t]])
    k.release(m5)

    toks = []
    o_v = out.rearrange("(t p) d -> t p d", p=128)
    if final:
        fn = k.sb([128, D], F32, "fn")
        k.dma("sp", fn[:], fnorm.partition_broadcast(128), writes=[fn])
        ss2 = k.sb([128, NT], F32, "ss2")
        rs2 = k.sb([128, NT], F32, "rs2")
        junk2 = [k.sb([128, D], BF16, "junkf%d" % i) for i in range(2)]
        for t in range(NT):
            jk = junk2[t % 2]
            k.op("act", lambda e: e.activation(out=jk[:], in_=hres[t][:], func=AF.Square, accum_out=ss2[:, t:t + 1]),
                 reads=[hres[t]], writes=[jk, ss2])
        rstd_from_ss(k, ss2, rs2, NT, 1.0 / D)
        for t in range(NT):
            k.op("dve", lambda e: e.scalar_tensor_tensor(out=hres[t][:], in0=hres[t][:], scalar=rs2[:, t:t + 1], in1=fn[:], op0=ALU.mult, op1=ALU.mult),
                 reads=[hres[t], rs2, fn], writes=[hres[t]])
    for t in range(NT):
        toks.append(k.dma("sp", o_v[t], hres[t][:], reads=[hres[t]]))
    k.finish(toks)
    k.release(0)
    return nc, k


def _finB(k, out, hres, NT):
    o_v = out.rearrange("(t p) d -> t p d", p=128)
    toks = [k.dma("sp", o_v[t], hres[t][:], reads=[hres[t]]) for t in range(NT)]
    k.finish(toks)
    k.release(0)
    return k.nc, k


def phaseB_inmaps(layer, h, yT_full, inp, final):
    maps = []
    wa = np.ascontiguousarray(inp["w_ada"][layer][:, 2048:6144])
    ba = np.ascontiguousarray(inp["b_ada"][layer][2048:6144])
    w_out = inp["w_out_ab"][0] if layer == 0 else inp["w_out_c"][0]
    w_r = np.ascontiguousarray(np.concatenate([inp["moe_w_grp"][layer], inp["moe_w_rt"][layer]], axis=1))
    b_r = np.ascontiguousarray(np.concatenate([inp["moe_b_grp"][layer], inp["moe_b_rt"][layer]], axis=0))
    consts = make_consts()
    for core in range(NCORES):
        b, q = core // 4, core % 4
        sl = slice(q * NTB, (q + 1) * NTB)
        maps.append({
            "h_in": np.ascontiguousarray(h[b, sl]),
            "yT": np.ascontiguousarray(yT_full[b][:, sl]),
            "w_out": np.ascontiguousarray(w_out),
            "condT": np.ascontiguousarray(inp["c"][b].reshape(8, 128).T),
            "w_ada": wa, "b_ada": ba,
            "norm2": np.ascontiguousarray(inp["norm2"][layer]),
            "w_r": w_r, "b_r": b_r,
            "w1": inp["moe_w1"][layer], "w3": inp["moe_w3"][layer], "w2": inp["moe_w2"][layer],
            "fnorm": np.ascontiguousarray(inp["final_norm"]),
            "consts": consts,
        })
    return maps


class UMaker:
    def __init__(self, k, nc, h_ap, condT, w_ada, b_ada, norm_ap, cstb):
        self.k = k
        self.cstb = cstb
        self.h_v = h_ap.rearrange("(t p) d -> t p d", p=128)
        self.sh = k.sb([128, D], F32, "sh1")
        self.sc = k.sb([128, D], F32, "sc1")
        m = k.mark()
        pp = [k.ps([128, 512], F32, "modps%d" % i) for i in range(2)]
        compute_mod(k, nc, condT, w_ada, b_ada, 2048, [self.sh, self.sc], pp)
        k.release(m)
        self.g = k.sb([128, D], F32, "g1")
        k.dma("sp", self.g[:], norm_ap.partition_broadcast(128), writes=[self.g])
        k.op("dve", lambda e: e.scalar_tensor_tensor(out=self.g[:], in0=self.sc[:], scalar=1.0, in1=self.g[:], op0=ALU.add, op1=ALU.mult),
             reads=[self.sc, self.g], writes=[self.g])
        self.ht = [k.sb([128, D], F32, "ht%d" % i) for i in range(4)]
        self.junk = k.sb([128, D], BF16, "junk")
        self.ss = k.sb([128, 4], F32, "ss")
        self.rstd = k.sb([128, 4], F32, "rstd")
        self.a = [k.sb([128, D], F32, "ua%d" % i) for i in range(2)]
        self.ub = [k.sb([128, D], BF16, "ub%d" % i) for i in range(2)]
        self.pst = [k.ps([128, 8, 128], BF16, "pst%d" % i) for i in range(1)]

    def block(self, blk, uT):
        k = self.k
        for ti in range(4):
            ht = self.ht[ti]
            k.dma("sp", ht[:], self.h_v[blk * 4 + ti], writes=[ht])
            k.op("act", lambda e: e.activation(out=self.junk[:], in_=ht[:], func=AF.Square, accum_out=self.ss[:, ti:ti + 1]),
                 reads=[ht], writes=[self.junk, self.ss])
        rstd_from_ss(k, self.ss, self.rstd, 4, 1.0 / D)
        for ti in range(4):
            ht = self.ht[ti]
            a = self.a[ti % 2]
            ub = self.ub[ti % 2]
            pt = self.pst[0]
            k.op("dve", lambda e: e.scalar_tensor_tensor(out=a[:], in0=ht[:], scalar=self.rstd[:, ti:ti + 1], in1=self.g[:], op0=ALU.mult, op1=ALU.mult),
                 reads=[ht, self.rstd, self.g], writes=[a])
            k.op("pool", lambda e: e.tensor_tensor(out=ub[:], in0=a[:], in1=self.sh[:], op=ALU.add), reads=[a, self.sh], writes=[ub])
            for kk in range(8):
                k.op("pe", lambda e: e.transpose(out=pt[:, kk, :], in_=ub[:, kk * 128:(kk + 1) * 128], identity=self.cstb[:, C_ID, :]),
                     reads=[ub, self.cstb], writes=[pt])
            k.op("act", lambda e: e.activation(out=uT[:, :, ti * 128:(ti + 1) * 128], in_=pt[:], func=AF.Copy), reads=[pt], writes=[uT])


class Banks:
    def __init__(self, k, n):
        self.t = [k.ps([128, 512], F32, "bank%d" % i) for i in range(n)]
        self.i = 0

    def get(self):
        b = self.t[self.i % len(self.t)]
        self.i += 1
        return b


NCOL_A0 = 656


def build_phaseA0(nblk=16):
    nc = bass.Bass("TRN2", target_bir_lowering=False)
    dt = nc.dram_tensor
    h_in = dt("h_in", [S, D], F32, kind="ExternalInput").ap()
    condT = dt("condT", [128, 8], F32, kind="ExternalInput").ap()
    w_ada = dt("w_ada", [D, 2048], F32, kind="ExternalInput").ap()
    b_ada = dt("b_ada", [2048], F32, kind="ExternalInput").ap()
    norm1 = dt("norm1", [D], F32, kind="ExternalInput").ap()
    w_in = dt("w_in", [D, NCOL_A0], F32, kind="ExternalInput").ap()
    WAd = dt("WA", [128, 128], F32, kind="ExternalInput").ap()
    WXd = dt("WX", [128, 128], F32, kind="ExternalInput").ap()
    wg2d = dt("wg2", [16, 64], F32, kind="ExternalInput").ap()
    pcold = dt("pcol", [128, 16], F32, kind="ExternalInput").ap()
    consts = dt("consts", [128, NCONST, 128], F32, kind="ExternalInput").ap()
    yT = dt("yT", [256, S], BF16, kind="ExternalOutput").ap()

    k = KB(nc)
    cst, cstb = load_consts(k, nc, consts)
    um = UMaker(k, nc, h_in, condT, w_ada, b_ada, norm1, cstb)
    win = k.sb([128, 8, NCOL_A0], BF16, "win")
    k.dma("pool", win[:], w_in.rearrange("(k p) n -> p k n", p=128), writes=[win])
    WA = k.sb([128, 128], BF16, "WA")
    WX = k.sb([128, 128], BF16, "WX")
    wg2 = k.sb([16, 64], BF16, "wg2")
    k.dma("pool", WA[:], WAd, writes=[WA])
    k.dma("pool", WX[:], WXd, writes=[WX])
    k.dma("pool", wg2[:], wg2d, writes=[wg2])
    pc = k.sb([128, 16], F32, "pcol")
    k.dma("sp", pc[:], pcold, writes=[pc])
    dc = k.sb([128, 4], F32, "dc")
    tl = k.sb([128, 1], F32, "tl")
    k.op("act", lambda e: e.activation(out=tl[:], in_=pc[:, 7:8], func=AF.Exp, scale=-1.0), reads=[pc], writes=[tl])
    k.op("act", lambda e: e.activation(out=tl[:], in_=tl[:], func=AF.Ln, bias=1.0), reads=[tl], writes=[tl])
    k.op("dve", lambda e: e.tensor_scalar(out=dc[:, 0:1], in0=tl[:], scalar1=-8.0, scalar2=None, op0=ALU.mult), reads=[tl], writes=[dc])
    k.op("dve", lambda e: e.tensor_scalar(out=dc[:, 1:2], in0=tl[:], scalar1=-16.0, scalar2=None, op0=ALU.mult), reads=[tl], writes=[dc])
    k.op("dve", lambda e: e.tensor_scalar(out=dc[:, 2:3], in0=pc[:, 9:10], scalar1=-1.0, scalar2=None, op0=ALU.mult), reads=[pc], writes=[dc])
    rmask = k.sb([64, 8, 64], F32, "rmask")
    k.op("dve", lambda e: e.memset(rmask[:], 1.0), writes=[rmask])
    k.op("dve", lambda e: e.memset(rmask[:, :, 0:1], 0.0), writes=[rmask])
    rmask2 = rmask[:].rearrange("p a b -> p (a b)")

    banks = Banks(k, 5)
    pkg = k.ps([128, 4, 64], BF16, "pkg")
    uT = [k.sb([128, 8, 512], BF16, "uT%d" % i) for i in range(2)]
    xabuf = [k.sb([128, 515], F32, "xabuf%d" % i) for i in range(2)]
    k.op("dve", lambda e: e.memset(xabuf[0][:, 0:3], 0.0), writes=[xabuf[0]])
    hs = [k.sb([128, 512], F32, "hs%d" % i) for i in range(2)]
    Sst = k.sb([64, 128], F32, "Sst")
    k.op("dve", lambda e: e.memset(Sst[:], 0.0), writes=[Sst])
    Sbt = [k.sb([64, 128], BF16, "Sbt%d" % i) for i in range(8)]

    def sbt(shape, dtp, name):
        return k.sb(shape, dtp, name)

    ga = sbt([128, 512], F32, "ga")
    q_sb = sbt([64, 512], F32, "q_sb")
    k_sb = sbt([64, 512], F32, "k_sb")
    sog = sbt([128, 512], F32, "sog")
    gl_bf = sbt([16, 512], BF16, "gl_bf")
    v_tok = sbt([128, 4, 128], BF16, "v_tok")
    xc = sbt([128, 512], F32, "xc")
    xcb = sbt([128, 512], BF16, "xcb")
    r_sb = sbt([128, 512], F32, "r_sb")
    i_sb = sbt([128, 512], F32, "i_sb")
    a_sb = sbt([128, 512], F32, "a_sb")
    a2_sb = sbt([128, 512], F32, "a2_sb")
    t_sb = sbt([128, 512], F32, "t_sb")
    b_sb = sbt([128, 512], F32, "b_sb")
    g2_sb = sbt([128, 512], F32, "g2_sb")
    inner = sbt([128, 512], F32, "inner")
    ge = sbt([128, 512], F32, "ge")
    ya = [sbt([128, 512], BF16, "ya%d" % i) for i in range(2)]
    e1 = sbt([64, 512], F32, "e1")
    sp_ = sbt([64, 512], F32, "sp")
    cum = sbt([64, 512], F32, "cum")
    E1 = sbt([64, 512], F32, "E1")
    E2 = sbt([64, 512], F32, "E2")
    qg = sbt([64, 512], BF16, "qg")
    kg = sbt([64, 512], BF16, "kg")
    kg_tok = sbt([128, 4, 64], BF16, "kg_tok")
    attm = sbt([128, 4, 128], BF16, "attm")
    tS = sbt([64, 128], F32, "tS")
    osb = sbt([128, 512], F32, "osb")
    o2 = sbt([128, 512], F32, "o2")
    rs = sbt([128, 512], F32, "rs")
    t1 = sbt([128, 512], F32, "t1")
    ob = [sbt([128, 512], BF16, "ob%d" % i) for i in range(2)]

    def op(e, fn, r, w):
        return k.op(e, fn, reads=r, writes=w)

    out_toks = []
    for blk in range(nblk):
        u = uT[blk % 2]
        xb = xabuf[blk % 2]
        xbn = xabuf[(blk + 1) % 2]
        um.block(blk, u)

        def proj(c0, c1):
            p = banks.get()
            m = c1 - c0
            for kk in range(8):
                k.op("pe", lambda e: e.matmul(p[0:m, :], lhsT=win[:, kk, c0:c1], rhs=u[:, kk, :], start=(kk == 0), stop=(kk == 7)), reads=[win, u], writes=[p], fast=True)
            return p

        p = proj(0, 128)
        op("act", lambda e: e.activation(out=xb[:, 3:515], in_=p[:], func=AF.Copy), [p], [xb])
        p = proj(128, 256)
        op("act", lambda e: e.activation(out=ga[:], in_=p[:], func=AF.Copy), [p], [ga])
        p = proj(256, 320)
        op("dve", lambda e: e.tensor_scalar(out=q_sb[:], in0=p[0:64, :], scalar1=0.125, scalar2=None, op0=ALU.mult), [p], [q_sb])
        p = proj(320, 384)
        op("act", lambda e: e.activation(out=k_sb[:], in_=p[0:64, :], func=AF.Copy), [p], [k_sb])
        p = proj(512, 640)
        op("act", lambda e: e.activation(out=sog[:], in_=p[:], func=AF.Silu), [p], [sog])
        p = proj(640, 656)
        op("dve", lambda e: e.tensor_copy(out=gl_bf[:], in_=p[0:16, :]), [p], [gl_bf])
        p = banks.get()
        for ti in range(4):
            for kk in range(8):
                op("pe", lambda e: e.matmul(p[:, ti * 128:(ti + 1) * 128], lhsT=u[:, kk, ti * 128:(ti + 1) * 128], rhs=win[:, kk, 384:512],
                                            start=(kk == 0), stop=(kk == 7)), [win, u], [p])
        op("dve", lambda e: e.tensor_copy(out=v_tok[:].rearrange("p a b -> p (a b)"), in_=p[:]), [p], [v_tok])

        op("act", lambda e: e.activation(out=xc[:], in_=xb[:, 3:515], func=AF.Identity, bias=pc[:, 4:5], scale=pc[:, 3:4]), [xb, pc], [xc])
        for w in range(3):
            op("dve", lambda e: e.scalar_tensor_tensor(out=xc[:], in0=xb[:, w:w + 512], scalar=pc[:, w:w + 1], in1=xc[:], op0=ALU.mult, op1=ALU.add),
               [xb, pc, xc], [xc])
        op("pool", lambda e: e.tensor_copy(out=xbn[:, 0:3], in_=xb[:, 512:515]), [xb], [xbn])
        op("act", lambda e: e.activation(out=xcb[:], in_=xc[:], func=AF.Copy), [xc], [xcb])
        p = banks.get()
        op("pe", lambda e: e.matmul(p[:], lhsT=WA[:], rhs=xcb[:], start=True, stop=True), [WA, xcb], [p])
        op("act", lambda e: e.activation(out=r_sb[:], in_=p[:], func=AF.Sigmoid, bias=pc[:, 5:6]), [p, pc], [r_sb])
        p = banks.get()
        op("pe", lambda e: e.matmul(p[:], lhsT=WX[:], rhs=xcb[:], start=True, stop=True), [WX, xcb], [p])
        op("act", lambda e: e.activation(out=i_sb[:], in_=p[:], func=AF.Sigmoid, bias=pc[:, 6:7]), [p, pc], [i_sb])
        op("act", lambda e: e.activation(out=a_sb[:], in_=r_sb[:], func=AF.Exp, scale=dc[:, 0:1]), [r_sb, dc], [a_sb])
        op("act", lambda e: e.activation(out=a2_sb[:], in_=r_sb[:], func=AF.Exp, scale=dc[:, 1:2]), [r_sb, dc], [a2_sb])
        op("act", lambda e: e.activation(out=a2_sb[:], in_=a2_sb[:], func=AF.Sqrt, bias=1.0, scale=-1.0), [a2_sb], [a2_sb])
        op("dve", lambda e: e.tensor_tensor(out=t_sb[:], in0=i_sb[:], in1=xc[:], op=ALU.mult), [i_sb, xc], [t_sb])
        op("pool", lambda e: e.tensor_tensor(out=b_sb[:], in0=t_sb[:], in1=a2_sb[:], op=ALU.mult), [t_sb, a2_sb], [b_sb])
        hcur = hs[blk % 2]
        hprev = hs[(blk + 1) % 2]
        if blk == 0:
            op("dve", lambda e: e.tensor_tensor_scan(out=hcur[:], data0=a_sb[:], data1=b_sb[:], initial=0.0, op0=ALU.mult, op1=ALU.add),
               [a_sb, b_sb], [hcur])
        else:
            op("dve", lambda e: e.tensor_tensor_scan(out=hcur[:], data0=a_sb[:], data1=b_sb[:], initial=hprev[:, 511:512], op0=ALU.mult, op1=ALU.add),
               [a_sb, b_sb, hprev], [hcur])
        op("act", lambda e: e.activation(out=g2_sb[:], in_=ga[:], func=AF.Square), [ga], [g2_sb])
        op("dve", lambda e: e.tensor_scalar(out=g2_sb[:], in0=g2_sb[:], scalar1=0.044715, scalar2=1.0, op0=ALU.mult, op1=ALU.add), [g2_sb], [g2_sb])
        op("pool", lambda e: e.tensor_tensor(out=inner[:], in0=g2_sb[:], in1=ga[:], op=ALU.mult), [g2_sb, ga], [inner])
        op("act", lambda e: e.activation(out=inner[:], in_=inner[:], func=AF.Sigmoid, scale=1.5957691216), [inner], [inner])
        op("dve", lambda e: e.tensor_tensor(out=ge[:], in0=ga[:], in1=inner[:], op=ALU.mult), [ga, inner], [ge])
        yab = ya[blk % 2]
        op("pool", lambda e: e.tensor_tensor(out=yab[:], in0=ge[:], in1=hcur[:], op=ALU.mult), [ge, hcur], [yab])
        out_toks.append(k.dma("sp", yT[0:128, blk * 512:(blk + 1) * 512], yab[:], reads=[yab]))

        p = banks.get()
        op("pe", lambda e: e.matmul(p[0:64, :], lhsT=wg2[:], rhs=gl_bf[:], start=True, stop=True), [wg2, gl_bf], [p])
        op("act", lambda e: e.activation(out=e1[:], in_=p[0:64, :], func=AF.Exp, bias=dc[0:64, 2:3], scale=-1.0), [p, dc], [e1])
        op("act", lambda e: e.activation(out=sp_[:], in_=e1[:], func=AF.Ln, bias=1.0), [e1], [sp_])
        op("dve", lambda e: e.tensor_tensor_scan(out=cum[:], data0=rmask2, data1=sp_[:], initial=0.0, op0=ALU.mult, op1=ALU.add), [rmask, sp_], [cum])
        op("act", lambda e: e.activation(out=E1[:], in_=cum[:], func=AF.Exp, scale=-1.0 / 16.0), [cum], [E1])
        op("act", lambda e: e.activation(out=E2[:], in_=cum[:], func=AF.Exp, scale=1.0 / 16.0), [cum], [E2])
        op("dve", lambda e: e.tensor_tensor(out=qg[:], in0=q_sb[:], in1=E1[:], op=ALU.mult), [q_sb, E1], [qg])
        op("pool", lambda e: e.tensor_tensor(out=kg[:], in0=k_sb[:], in1=E2[:], op=ALU.mult), [k_sb, E2], [kg])
        for pr in range(4):
            op("pe", lambda e: e.transpose(out=pkg[:, pr, :], in_=kg[:, pr * 128:(pr + 1) * 128], identity=cstb[0:64, C_ID, 0:64]), [kg, cstb], [pkg])
        op("act", lambda e: e.activation(out=kg_tok[:], in_=pkg[:], func=AF.Copy), [pkg], [kg_tok])
        p = banks.get()
        for pr in range(4):
            op("pe", lambda e: e.matmul(p[:, pr * 128:(pr + 1) * 128], lhsT=kg[:, pr * 128:(pr + 1) * 128], rhs=qg[:, pr * 128:(pr + 1) * 128],
                                        start=True, stop=True), [kg, qg], [p])
        op("dve", lambda e: e.tensor_tensor(out=attm[:], in0=p[:].rearrange("p (a b) -> p a b", a=4),
                                            in1=_bc(cst[:, C_UT64:C_UT64 + 1, :], [128, 4, 128]), op=ALU.mult), [p, cst], [attm])
        kva = banks.get()
        kvb = banks.get()
        for c in range(8):
            pr, half = c // 2, c % 2
            kvp = kva if c < 4 else kvb
            op("pe", lambda e: e.matmul(kvp[0:64, (c % 4) * 128:(c % 4 + 1) * 128], lhsT=kg_tok[half * 64:(half + 1) * 64, pr, :],
                                        rhs=v_tok[half * 64:(half + 1) * 64, pr, :], start=True, stop=True), [kg_tok, v_tok], [kvp])
        for c in range(8):
            kvp = kva if c < 4 else kvb
            op("act", lambda e: e.activation(out=Sbt[c][:], in_=Sst[:], func=AF.Copy), [Sst], [Sbt[c]])
            op("dve", lambda e: e.tensor_tensor(out=tS[:], in0=kvp[0:64, (c % 4) * 128:(c % 4 + 1) * 128], in1=Sst[:], op=ALU.add), [kvp, Sst], [tS])
            op("dve", lambda e: e.tensor_scalar(out=Sst[:], in0=tS[:], scalar1=E1[:, c * 64 + 63:c * 64 + 64], scalar2=None, op0=ALU.mult),
               [tS, E1], [Sst])
        po = banks.get()
        for pr in range(4):
            op("pe", lambda e: e.matmul(po[:, pr * 128:(pr + 1) * 128], lhsT=v_tok[:, pr, :], rhs=attm[:, pr, :], start=True, stop=False),
               [v_tok, attm], [po])
            for half in range(2):
                c = 2 * pr + half
                op("pe", lambda e: e.matmul(po[:, c * 64:(c + 1) * 64], lhsT=Sbt[c][:], rhs=qg[:, c * 64:(c + 1) * 64], start=False, stop=(half == 1)),
                   [Sbt[c], qg], [po])
        op("act", lambda e: e.activation(out=osb[:], in_=po[:], func=AF.Copy), [po], [osb])
        op("act", lambda e: e.activation(out=o2[:], in_=osb[:], func=AF.Square), [osb], [o2])
        p = banks.get()
        op("pe", lambda e: e.matmul(p[:], lhsT=cst[:, C_ONES, :], rhs=o2[:], start=True, stop=True), [cst, o2], [p])
        op("act", lambda e: e.activation(out=rs[:], in_=p[:], func=AF.Sqrt, bias=EPS, scale=1.0 / 128.0), [p], [rs])
        op("dve", lambda e: e.reciprocal(out=rs[:], in_=rs[:]), [rs], [rs])
        op("dve", lambda e: e.scalar_tensor_tensor(out=t1[:], in0=osb[:], scalar=pc[:, 8:9], in1=rs[:], op0=ALU.mult, op1=ALU.mult), [osb, pc, rs], [t1])
        obb = ob[blk % 2]
        op("pool", lambda e: e.tensor_tensor(out=obb[:], in0=t1[:], in1=sog[:], op=ALU.mult), [t1, sog], [obb])
        out_toks.append(k.dma("sp", yT[128:256, blk * 512:(blk + 1) * 512], obb[:], reads=[obb]))
    k.finish(out_toks)
    k.release(0)
    return nc, k


def phaseA0_inmaps(h, inp):
    maps = []
    consts = make_consts()
    w_in = inp["w_in_ab"][0]
    wa = np.ascontiguousarray(inp["w_ada"][0][:, 0:2048])
    ba = np.ascontiguousarray(inp["b_ada"][0][0:2048])
    for core in range(NCORES):
        b, hg = core // 4, core % 4
        ch = slice(hg * 128, (hg + 1) * 128)
        cols = np.concatenate([
            np.arange(hg * 128, (hg + 1) * 128),
            512 + np.arange(hg * 128, (hg + 1) * 128),
            1024 + np.arange(hg * 64, (hg + 1) * 64),
            1280 + np.arange(hg * 64, (hg + 1) * 64),
            1536 + np.arange(hg * 128, (hg + 1) * 128),
            2048 + np.arange(hg * 128, (hg + 1) * 128),
            2560 + np.arange(16),
        ])
        WA = np.zeros((128, 128), np.float32)
        WX = np.zeros((128, 128), np.float32)
        for j in range(2):
            WA[j * 64:(j + 1) * 64, j * 64:(j + 1) * 64] = inp["rg_wa"][0][hg * 2 + j]
            WX[j * 64:(j + 1) * 64, j * 64:(j + 1) * 64] = inp["rg_wx"][0][hg * 2 + j]
        pcol = np.zeros((128, 16), np.float32)
        pcol[:, 0:4] = inp["conv_a_w"][0][:, ch].T
        pcol[:, 4] = inp["conv_a_b"][0][ch]
        pcol[:, 5] = inp["rg_ba"][0][ch]
        pcol[:, 6] = inp["rg_bx"][0][ch]
        pcol[:, 7] = inp["rg_lam"][0][ch]
        pcol[:, 8] = inp["gla_norm"][0]
        pcol[0:64, 9] = inp["gla_bg2"][0][hg * 64:(hg + 1) * 64]
        maps.append({
            "h_in": np.ascontiguousarray(h[b]),
            "condT": np.ascontiguousarray(inp["c"][b].reshape(8, 128).T),
            "w_ada": wa, "b_ada": ba,
            "norm1": np.ascontiguousarray(inp["norm1"][0]),
            "w_in": np.ascontiguousarray(w_in[:, cols]),
            "WA": WA, "WX": WX,
            "wg2": np.ascontiguousarray(inp["gla_wg2"][0][:, hg * 64:(hg + 1) * 64]),
            "pcol": pcol, "consts": consts,
        })
    return maps


def assemble_yT_A0(results):
    yT = np.zeros((2, D, S), ml_dtypes.bfloat16)
    for core in range(NCORES):
        b, hg = core // 4, core % 4
        r = results[core]["yT"]
        yT[b, hg * 128:(hg + 1) * 128] = r[0:128]
        yT[b, 512 + hg * 128:512 + (hg + 1) * 128] = r[128:256]
    return yT


NCOL_A1 = 1028


def build_phaseA1(nblk=16, stop=99):
    nc = bass.Bass("TRN2", target_bir_lowering=False)
    dt = nc.dram_tensor
    h_in = dt("h_in", [S, D], F32, kind="ExternalInput").ap()
    condT = dt("condT", [128, 8], F32, kind="ExternalInput").ap()
    w_ada = dt("w_ada", [D, 2048], F32, kind="ExternalInput").ap()
    b_ada = dt("b_ada", [2048], F32, kind="ExternalInput").ap()
    norm1 = dt("norm1", [D], F32, kind="ExternalInput").ap()
    w_in = dt("w_in", [D, NCOL_A1], F32, kind="ExternalInput").ap()
    pcold = dt("pcol", [128, 24], F32, kind="ExternalInput").ap()
    prmd = dt("prm", [128, 4], F32, kind="ExternalInput").ap()
    dnd = dt("dnorm", [128], F32, kind="ExternalInput").ap()
    alogd = dt("a_log", [128, 2], F32, kind="ExternalInput").ap()
    consts = dt("consts", [128, NCONST, 128], F32, kind="ExternalInput").ap()
    yT = dt("yT", [256, S], BF16, kind="ExternalOutput").ap()

    k = KB(nc)
    cst, cstb = load_consts(k, nc, consts)
    um = UMaker(k, nc, h_in, condT, w_ada, b_ada, norm1, cstb)
    win = k.sb([128, 8, NCOL_A1], BF16, "win")
    wv = w_in.rearrange("(k p) n -> p k n", p=128)
    for kk in range(8):
        k.dma("pool", win[:, kk, :], wv[:, kk, :], writes=[win])
    pc = k.sb([128, 24], F32, "pcol")
    k.dma("sp", pc[:], pcold, writes=[pc])
    prm = k.sb([128, 4], F32, "prm")
    k.dma("sp", prm[:], prmd, writes=[prm])
    dnb = k.sb([128, 128], F32, "dnb")
    k.dma("sp", dnb[:], dnd.partition_broadcast(128), writes=[dnb])
    alg = k.sb([128, 2], F32, "alg")
    k.dma("sp", alg[:], alogd, writes=[alg])
    k.op("act", lambda e: e.activation(out=alg[:], in_=alg[:], func=AF.Exp), reads=[alg], writes=[alg])
    k.op("dve", lambda e: e.tensor_scalar(out=prm[:, 2:4], in0=alg[:], scalar1=-1.0, scalar2=None, op0=ALU.mult), reads=[alg, prm], writes=[prm])

    banks = Banks(k, 6)
    ptr = k.ps([128, 128], BF16, "ptr")
    uT = [k.sb([128, 8, 512], BF16, "uT%d" % i) for i in range(2)]
    cbuf = [[k.sb([128, 515], F32, "cbuf%d_%d" % (j, i)) for i in range(2)] for j in range(6)]
    for j in range(6):
        k.op("dve", lambda e: e.memset(cbuf[j][0][:, 0:3], 0.0), writes=[cbuf[j][0]])
    Sst = [k.sb([128, 128], F32, "Sst%d" % h) for h in range(2)]
    Sb = [k.sb([128, 128], BF16, "Sb%d" % h) for h in range(2)]
    for h in range(2):
        k.op("dve", lambda e: e.memset(Sst[h][:], 0.0), writes=[Sst[h]])
        k.op("dve", lambda e: e.memset(Sb[h][:], 0.0), writes=[Sb[h]])

    sb = k.sb
    sj = [sb([128, 512], F32, "sj%d" % j) for j in range(6)]
    sq = sb([128, 512], F32, "sq")
    rs = sb([128, 512], F32, "rs")
    nT = [sb([128, 512], BF16, "nT%d" % j) for j in range(4)]
    vTb = [sb([128, 512], BF16, "vTb%d" % h) for h in range(2)]
    sz = [sb([128, 256], F32, "sz%d" % t) for t in range(4)]
    g4 = sb([128, 4, 4], F32, "g4")
    beta = sb([128, 4, 2], F32, "beta")
    nbeta = sb([128, 4, 2], F32, "nbeta")
    gx = sb([128, 4, 2], F32, "gx")
    gg = sb([128, 4, 2], F32, "gg")
    gch = sb([128, 4], F32, "gch")
    gcl = sb([128, 8], F32, "gcl")
    eg = sb([128, 2], F32, "eg")
    ed = sb([128, 2], F32, "ed")
    be = sb([128, 2], F32, "be")
    egl = sb([128, 4], F32, "egl")
    gm = sb([128, 128], F32, "gm")
    DTm = sb([128, 128], F32, "DTm")
    Dm = sb([128, 128], F32, "Dm")
    A_ = sb([128, 128], F32, "A_")
    B_ = sb([128, 128], F32, "B_")
    Tt = sb([128, 128], F32, "Tt")
    Ttb = sb([128, 128], BF16, "Ttb")
    attT = sb([128, 128], BF16, "attT")
    bv = sb([128, 128], BF16, "bv")
    kbg = sb([128, 128], BF16, "kbg")
    kd = sb([128, 128], BF16, "kd")
    U = sb([128, 128], F32, "U")
    WT = sb([128, 128], BF16, "WT")
    dg = sb([128, 128], F32, "dg")
    qg = sb([128, 128], BF16, "qg")
    vnew = sb([128, 128], BF16, "vnew")
    o_tok = [sb([128, 4, 128], F32, "o_tok%d" % h) for h in range(2)]
    ss = sb([128, 4], F32, "oss")
    rstd = sb([128, 4], F32, "orstd")
    junk = sb([128, 128], F32, "ojunk")
    on = sb([128, 128], F32, "on")
    ytok = sb([128, 128], BF16, "ytok")
    yTs = [sb([128, 512], BF16, "yTs%d" % i) for i in range(2)]

    def op(e, fn, r, w):
        return k.op(e, fn, reads=r, writes=w)

    ident = cst[:, C_ID, :]
    TRI = cst[:, C_TRI, :]
    BLK = cst[:, C_BLK, :]
    SU = cst[:, C_SU, :]
    ONES = cst[:, C_ONES, :]
    UT64 = cst[:, C_UT64, :]
    out_toks = []
    cnt_y = 0
    for blk in range(nblk):
        u = uT[blk % 2]
        if stop <= -3:
            break
        um.block(blk, u)
        for j in range(6):
            if stop <= -2:
                break
            cb = cbuf[j][blk % 2]
            cbn = cbuf[j][(blk + 1) % 2]
            p = banks.get()
            for kk in range(8):
                k.op("pe", lambda e: e.matmul(p[:], lhsT=win[:, kk, j * 128:(j + 1) * 128], rhs=u[:, kk, :], start=(kk == 0), stop=(kk == 7)), reads=[win, u], writes=[p], fast=True)
            op("act", lambda e: e.activation(out=cb[:, 3:515], in_=p[:], func=AF.Copy), [p], [cb])
            op("act", lambda e: e.activation(out=sj[j][:], in_=cb[:, 3:515], func=AF.Copy, scale=pc[:, j * 4 + 3:j * 4 + 4]), [cb, pc], [sj[j]])
            for w in range(3):
                op("dve", lambda e: e.scalar_tensor_tensor(out=sj[j][:], in0=cb[:, w:w + 512], scalar=pc[:, j * 4 + w:j * 4 + w + 1], in1=sj[j][:],
                                                           op0=ALU.mult, op1=ALU.add), [cb, pc, sj[j]], [sj[j]])
            op("pool", lambda e: e.tensor_copy(out=cbn[:, 0:3], in_=cb[:, 512:515]), [cb], [cbn])
            op("act", lambda e: e.activation(out=sj[j][:], in_=sj[j][:], func=AF.Silu), [sj[j]], [sj[j]])
        if stop <= -1:
            break
        for j in range(4):
            op("act", lambda e: e.activation(out=sq[:], in_=sj[j][:], func=AF.Square), [sj[j]], [sq])
            p = banks.get()
            op("pe", lambda e: e.matmul(p[:], lhsT=ONES, rhs=sq[:], start=True, stop=True), [cst, sq], [p])
            op("act", lambda e: e.activation(out=rs[:], in_=p[:], func=AF.Sqrt, bias=EPS), [p], [rs])
            op("dve", lambda e: e.reciprocal(out=rs[:], in_=rs[:]), [rs], [rs])
            scl = 128.0 ** -0.5 if j < 2 else 1.0
            op("dve", lambda e: e.scalar_tensor_tensor(out=nT[j][:], in0=sj[j][:], scalar=scl, in1=rs[:], op0=ALU.mult, op1=ALU.mult), [sj[j], rs], [nT[j]])
        for h in range(2):
            op("act", lambda e: e.activation(out=vTb[h][:], in_=sj[4 + h][:], func=AF.Copy), [sj[4 + h]], [vTb[h]])
        if stop <= 0:
            break
        for ti in range(4):
            p = banks.get()
            for kk in range(8):
                op("pe", lambda e: e.matmul(p[:, 0:256], lhsT=u[:, kk, ti * 128:(ti + 1) * 128], rhs=win[:, kk, 768:1024], start=(kk == 0), stop=(kk == 7)),
                   [win, u], [p])
            for kk in range(8):
                op("pe", lambda e: e.matmul(p[:, 256:260], lhsT=u[:, kk, ti * 128:(ti + 1) * 128], rhs=win[:, kk, 1024:1028], start=(kk == 0), stop=(kk == 7)),
                   [win, u], [p])
            op("act", lambda e: e.activation(out=sz[ti][:], in_=p[:, 0:256], func=AF.Silu), [p], [sz[ti]])
            op("act", lambda e: e.activation(out=g4[:, ti, :], in_=p[:, 256:260], func=AF.Copy), [p], [g4])
        if stop <= 0.2:
            break
        op("act", lambda e: e.activation(out=beta[:], in_=g4[:, :, 0:2], func=AF.Sigmoid), [g4], [beta])
        op("dve", lambda e: e.tensor_scalar(out=nbeta[:], in0=beta[:], scalar1=-1.0, scalar2=None, op0=ALU.mult), [beta], [nbeta])
        if stop <= 0.4:
            break
        op("dve", lambda e: e.tensor_tensor(out=gx[:], in0=g4[:, :, 2:4], in1=_bc(prm[:, 0:2].unsqueeze(1), [128, 4, 2]), op=ALU.add), [g4, prm], [gx])
        if stop <= 0.6:
            break
        op("act", lambda e: e.activation(out=gx[:], in_=gx[:], func=AF.Exp), [gx], [gx])
        op("act", lambda e: e.activation(out=gx[:], in_=gx[:], func=AF.Ln, bias=1.0), [gx], [gx])
        if stop <= 0.8:
            break
        op("dve", lambda e: e.tensor_tensor(out=gg[:], in0=gx[:], in1=_bc(prm[:, 2:4].unsqueeze(1), [128, 4, 2]), op=ALU.mult), [gx, prm], [gg])

        if stop <= 1:
            break
        for ti in range(4):
            tsl = slice(ti * 128, (ti + 1) * 128)
            op("dve", lambda e: e.tensor_tensor(out=gch[:].rearrange("p (a b) -> p a b", a=2), in0=_bc(gg[:, ti, :].unsqueeze(1), [128, 2, 2]),
                                                in1=_bc(cst[:, C_CH0, 0:2].unsqueeze(2), [128, 2, 2]), op=ALU.mult), [gg, cst], [gch])
            pg = banks.get()
            op("pe", lambda e: e.matmul(pg[:, 0:2], lhsT=TRI, rhs=gg[:, ti, :], start=True, stop=True), [cst, gg], [pg])
            op("pe", lambda e: e.matmul(pg[:, 2:4], lhsT=BLK, rhs=gg[:, ti, :], start=True, stop=True), [cst, gg], [pg])
            op("pe", lambda e: e.matmul(pg[:, 4:8], lhsT=ONES, rhs=gch[:], start=True, stop=True), [cst, gch], [pg])
            op("dve", lambda e: e.tensor_copy(out=gcl[:], in_=pg[:, 0:8]), [pg], [gcl])
            op("act", lambda e: e.activation(out=eg[:], in_=gcl[:, 0:2], func=AF.Exp), [gcl], [eg])
            op("dve", lambda e: e.tensor_tensor(out=ed[:], in0=gcl[:, 2:4], in1=gcl[:, 0:2], op=ALU.subtract), [gcl], [ed])
            op("act", lambda e: e.activation(out=ed[:], in_=ed[:], func=AF.Exp), [ed], [ed])
            op("act", lambda e: e.activation(out=egl[:], in_=gcl[:, 4:8], func=AF.Exp), [gcl], [egl])
            op("dve", lambda e: e.tensor_tensor(out=be[:], in0=beta[:, ti, :], in1=eg[:], op=ALU.mult), [beta, eg], [be])
            for h in range(2):
                if stop <= 2:
                    break
                qT = nT[h]
                kT = nT[2 + h]
                op("dve", lambda e: e.tensor_scalar(out=gm[:], in0=SU, scalar1=gg[:, ti, h:h + 1], scalar2=None, op0=ALU.mult), [cst, gg], [gm])
                p = banks.get()
                op("pe", lambda e: e.matmul(p[:, 0:128], lhsT=gm[:], rhs=TRI, start=True, stop=True), [gm, cst], [p])
                op("act", lambda e: e.activation(out=DTm[:], in_=p[:, 0:128], func=AF.Exp), [p], [DTm])
                op("pool", lambda e: e.tensor_tensor(out=DTm[:], in0=DTm[:], in1=UT64, op=ALU.mult), [DTm, cst], [DTm])
                p = banks.get()
                op("pe", lambda e: e.matmul(p[:, 0:128], lhsT=TRI, rhs=gm[:], start=True, stop=True), [gm, cst], [p])
                op("act", lambda e: e.activation(out=Dm[:], in_=p[:, 0:128], func=AF.Exp), [p], [Dm])
                op("pool", lambda e: e.tensor_tensor(out=Dm[:], in0=Dm[:], in1=SU, op=ALU.mult), [Dm, cst], [Dm])
                p = banks.get()
                op("pe", lambda e: e.matmul(p[:, 0:128], lhsT=kT[:, tsl], rhs=kT[:, tsl], start=True, stop=True), [kT], [p])
                op("dve", lambda e: e.scalar_tensor_tensor(out=A_[:], in0=p[:, 0:128], scalar=nbeta[:, ti, h:h + 1], in1=Dm[:], op0=ALU.mult, op1=ALU.mult),
                   [p, nbeta, Dm], [A_])
                p = banks.get()
                op("pe", lambda e: e.transpose(out=p[:, 0:128], in_=A_[:], identity=ident), [A_, cst], [p])
                op("act", lambda e: e.activation(out=B_[:], in_=p[:, 0:128], func=AF.Copy), [p], [B_])
                p = banks.get()
                op("pe", lambda e: e.matmul(p[:, 0:128], lhsT=kT[:, tsl], rhs=qT[:, tsl], start=True, stop=True), [kT, qT], [p])
                op("dve", lambda e: e.tensor_tensor(out=attT[:], in0=p[:, 0:128], in1=DTm[:], op=ALU.mult), [p, DTm], [attT])
                op("pool", lambda e: e.tensor_tensor(out=Tt[:], in0=B_[:], in1=ident, op=ALU.add), [B_, cst], [Tt])
                for lvl in range(1, 6):
                    pa = banks.get()
                    op("pe", lambda e: e.matmul(pa[:, 0:128], lhsT=B_[:], rhs=A_[:], start=True, stop=True), [A_, B_], [pa])
                    if lvl < 5:
                        pb = banks.get()
                        op("pe", lambda e: e.matmul(pb[:, 0:128], lhsT=A_[:], rhs=B_[:], start=True, stop=True), [A_, B_], [pb])
                    op("act", lambda e: e.activation(out=A_[:], in_=pa[:, 0:128], func=AF.Copy), [pa], [A_])
                    if lvl < 5:
                        op("dve", lambda e: e.tensor_copy(out=B_[:], in_=pb[:, 0:128]), [pb], [B_])
                    pt = banks.get()
                    op("pe", lambda e: e.matmul(pt[:, 0:128], lhsT=A_[:], rhs=Tt[:], start=True, stop=True), [A_, Tt], [pt])
                    op("dve", lambda e: e.tensor_tensor(out=Tt[:], in0=pt[:, 0:128], in1=Tt[:], op=ALU.add), [pt, Tt], [Tt])
                if stop <= 3:
                    continue
                op("act", lambda e: e.activation(out=Ttb[:], in_=Tt[:], func=AF.Copy), [Tt], [Ttb])
                op("pe", lambda e: e.transpose(out=ptr[:], in_=kT[:, tsl], identity=cstb[:, C_ID, :]), [kT, cstb], [ptr])
                op("dve", lambda e: e.tensor_scalar(out=kbg[:], in0=ptr[:], scalar1=be[:, h:h + 1], scalar2=None, op0=ALU.mult), [ptr, be], [kbg])
                op("dve", lambda e: e.tensor_scalar(out=kd[:], in0=ptr[:], scalar1=ed[:, h:h + 1], scalar2=None, op0=ALU.mult), [ptr, ed], [kd])
                op("pe", lambda e: e.transpose(out=ptr[:], in_=vTb[h][:, tsl], identity=cstb[:, C_ID, :]), [vTb[h], cstb], [ptr])
                op("dve", lambda e: e.tensor_scalar(out=bv[:], in0=ptr[:], scalar1=beta[:, ti, h:h + 1], scalar2=None, op0=ALU.mult), [ptr, beta], [bv])
                p = banks.get()
                op("pe", lambda e: e.matmul(p[:, 0:128], lhsT=Ttb[:], rhs=bv[:], start=True, stop=True), [Ttb, bv], [p])
                op("act", lambda e: e.activation(out=U[:], in_=p[:, 0:128], func=AF.Copy), [p], [U])
                p = banks.get()
                op("pe", lambda e: e.matmul(p[:, 0:128], lhsT=kbg[:], rhs=Ttb[:], start=True, stop=True), [kbg, Ttb], [p])
                op("act", lambda e: e.activation(out=WT[:], in_=p[:, 0:128], func=AF.Copy), [p], [WT])
                op("dve", lambda e: e.tensor_scalar(out=dg[:], in0=ident, scalar1=eg[:, h:h + 1], scalar2=None, op0=ALU.mult), [cst, eg], [dg])
                p = banks.get()
                op("pe", lambda e: e.matmul(p[:, 0:128], lhsT=ONES, rhs=dg[:], start=True, stop=True), [cst, dg], [p])
                op("dve", lambda e: e.tensor_tensor(out=qg[:], in0=p[:, 0:128], in1=qT[:, tsl], op=ALU.mult), [p, qT], [qg])
                for half in range(2):
                    if stop <= 4:
                        break
                    rows = slice(half * 64, (half + 1) * 64)
                    pw = banks.get()
                    op("pe", lambda e: e.matmul(pw[rows, 0:128], lhsT=WT[:, rows], rhs=Sb[h][:], start=True, stop=True), [WT, Sb[h]], [pw])
                    op("dve", lambda e: e.tensor_tensor(out=vnew[rows, :], in0=U[rows, :], in1=pw[rows, 0:128], op=ALU.subtract), [U, pw], [vnew])
                    po = banks.get()
                    op("pe", lambda e: e.matmul(po[rows, 0:128], lhsT=qg[:, rows], rhs=Sb[h][:], start=True, stop=False), [qg, Sb[h]], [po])
                    op("pe", lambda e: e.matmul(po[rows, 0:128], lhsT=attT[rows, rows], rhs=vnew[rows, :], start=False, stop=True), [attT, vnew], [po])
                    pk = banks.get()
                    op("pe", lambda e: e.matmul(pk[:, 0:128], lhsT=kd[rows, :], rhs=vnew[rows, :], start=True, stop=True), [kd, vnew], [pk])
                    op("dve", lambda e: e.scalar_tensor_tensor(out=Sst[h][:], in0=Sst[h][:], scalar=egl[:, half * 2 + h:half * 2 + h + 1], in1=pk[:, 0:128],
                                                               op0=ALU.mult, op1=ALU.add), [Sst[h], egl, pk], [Sst[h]])
                    op("act", lambda e: e.activation(out=Sb[h][:], in_=Sst[h][:], func=AF.Copy), [Sst[h]], [Sb[h]])
                    op("act", lambda e: e.activation(out=o_tok[h][rows, ti, :], in_=po[rows, 0:128], func=AF.Copy), [po], [o_tok[h]])
        for h in range(2):
            if stop <= 5:
                break
            for ti in range(4):
                op("act", lambda e: e.activation(out=junk[:], in_=o_tok[h][:, ti, :], func=AF.Square, accum_out=ss[:, ti:ti + 1]), [o_tok[h]], [junk, ss])
            rstd_from_ss(k, ss, rstd, 4, 1.0 / 128.0)
            ys = yTs[cnt_y % 2]
            cnt_y += 1
            for ti in range(4):
                op("dve", lambda e: e.scalar_tensor_tensor(out=on[:], in0=o_tok[h][:, ti, :], scalar=rstd[:, ti:ti + 1], in1=dnb[:], op0=ALU.mult, op1=ALU.mult),
                   [o_tok[h], rstd, dnb], [on])
                op("pool", lambda e: e.tensor_tensor(out=ytok[:], in0=on[:], in1=sz[ti][:, h * 128:(h + 1) * 128], op=ALU.mult), [on, sz[ti]], [ytok])
                op("pe", lambda e: e.transpose(out=ptr[:], in_=ytok[:], identity=cstb[:, C_ID, :]), [ytok, cstb], [ptr])
                op("act", lambda e: e.activation(out=ys[:, ti * 128:(ti + 1) * 128], in_=ptr[:], func=AF.Copy), [ptr], [ys])
            out_toks.append(k.dma("sp", yT[h * 128:(h + 1) * 128, blk * 512:(blk + 1) * 512], ys[:], reads=[ys]))
    k.finish(out_toks)
    k.release(0)
    return nc, k


def phaseA1_inmaps(h, inp):
    maps = []
    consts = make_consts()
    w_in = inp["w_in_c"][0]
    wa = np.ascontiguousarray(inp["w_ada"][1][:, 0:2048])
    ba = np.ascontiguousarray(inp["b_ada"][1][0:2048])
    cw = inp["conv_c_w"][0]
    for core in range(NCORES):
        b, hg = core // 4, core % 4
        hs = [2 * hg, 2 * hg + 1]
        cols = []
        for base in (0, 1024, 2048):
            for hh in hs:
                cols.append(base + np.arange(hh * 128, (hh + 1) * 128))
        for hh in hs:
            cols.append(3072 + np.arange(hh * 128, (hh + 1) * 128))
        cols.append(np.array([4096 + hs[0], 4096 + hs[1], 4104 + hs[0], 4104 + hs[1]]))
        cols = np.concatenate(cols)
        pcol = np.zeros((128, 6, 4), np.float32)
        j = 0
        for base in (0, 1024, 2048):
            for hh in hs:
                pcol[:, j, :] = cw[:, base + hh * 128:base + (hh + 1) * 128].T
                j += 1
        prm = np.zeros((128, 4), np.float32)
        prm[:, 0] = inp["dn_dt_bias"][0][hs[0]]
        prm[:, 1] = inp["dn_dt_bias"][0][hs[1]]
        maps.append({
            "h_in": np.ascontiguousarray(h[b]),
            "condT": np.ascontiguousarray(inp["c"][b].reshape(8, 128).T),
            "w_ada": wa, "b_ada": ba,
            "norm1": np.ascontiguousarray(inp["norm1"][1]),
            "w_in": np.ascontiguousarray(w_in[:, cols]),
            "pcol": np.ascontiguousarray(pcol.reshape(128, 24)),
            "prm": prm,
            "a_log": np.ascontiguousarray(np.tile(inp["dn_a_log"][0][hs][None, :], (128, 1))),
            "dnorm": np.ascontiguousarray(inp["dn_norm"][0]),
            "consts": consts,
        })
    return maps


def assemble_yT_A1(results):
    yT = np.zeros((2, D, S), ml_dtypes.bfloat16)
    for core in range(NCORES):
        b, hg = core // 4, core % 4
        yT[b, hg * 256:(hg + 1) * 256] = results[core]["yT"]
    return yT


_PROGS = {}


def _prog(name):
    if name not in _PROGS:
        if name == "A0":
            _PROGS[name] = build_phaseA0()[0]
        elif name == "A1":
            _PROGS[name] = build_phaseA1()[0]
        elif name == "B0":
            _PROGS[name] = build_phaseB(False)[0]
        else:
            _PROGS[name] = build_phaseB(True)[0]
    return _PROGS[name]


def _gather_B(results):
    return np.stack([np.concatenate([results[b * 4 + q]["out"] for q in range(4)], 0) for b in range(2)])


def kernel(**inputs):
    inp = {k_: np.ascontiguousarray(np.asarray(v, dtype=np.float32)) for k_, v in inputs.items()}
    cores = list(range(NCORES))
    x = inp["x"]
    r = run_bass_kernel_spmd(_prog("A0"), phaseA0_inmaps(x, inp), core_ids=cores)
    yT0 = assemble_yT_A0(r.results)
    r = run_bass_kernel_spmd(_prog("B0"), phaseB_inmaps(0, x, yT0, inp, False), core_ids=cores)
    h0 = _gather_B(r.results)
    r = run_bass_kernel_spmd(_prog("A1"), phaseA1_inmaps(h0, inp), core_ids=cores)
    yT1 = assemble_yT_A1(r.results)
    r = run_bass_kernel_spmd(_prog("B1"), phaseB_inmaps(1, h0, yT1, inp, True), core_ids=cores)
    return _gather_B(r.results).astype(np.float32)
```

```python
import numpy as np
import ml_dtypes
import concourse.bass as bass
import concourse.mybir as mybir
from concourse.bass_utils import run_bass_kernel_spmd

F32 = mybir.dt.float32
BF16 = mybir.dt.bfloat16
AF = mybir.ActivationFunctionType
ALU = mybir.AluOpType
AX = mybir.AxisListType

D = 1024
S = 8192
EPS = 1e-6
NCORES = 8


class T:
    __slots__ = ("h", "w", "r", "name")

    def __init__(self, h, name=""):
        self.h = h
        self.w = None
        self.r = {}
        self.name = name

    def __getitem__(self, idx):
        return self.h[idx]


class KB:
    NDMA_SEM = 6

    def __init__(self, nc):
        self.nc = nc
        self.eng = {"pe": nc.tensor, "act": nc.scalar, "dve": nc.vector, "pool": nc.gpsimd, "sp": nc.sync}
        self.csem = {e: nc.alloc_semaphore("cs_" + e) for e in ("pe", "act", "dve", "pool")}
        self.cnt = {e: 0 for e in self.csem}
        self.pending = {e: False for e in self.csem}
        self.dsem = {}
        self.dcnt = {}
        for q in ("sp", "pool", "act"):
            self.dsem[q] = [nc.alloc_semaphore("ds_%s%d" % (q, i)) for i in range(self.NDMA_SEM)]
            self.dcnt[q] = 0
        self.seen = {e: {} for e in self.eng}
        self.fast_pe = False
        self.ninst = 0
        self.stack = []
        self.tiles = []
        self.freed = {}
        self.uid = 0

    def sb(self, shape, dt=F32, name=None):
        self.uid += 1
        nm = "%s_%d" % (name or "t", self.uid)
        g = self.nc.sbuf_tensor(nm, list(shape), dt)
        h = g.__enter__()
        self.stack.append(g)
        t = T(h, nm)
        t.r = dict(self.freed)
        self.tiles.append(t)
        return t

    def ps(self, shape, dt=F32, name=None):
        self.uid += 1
        nm = "%s_%d" % (name or "p", self.uid)
        g = self.nc.psum_tensor(nm, list(shape), dt)
        h = g.__enter__()
        self.stack.append(g)
        t = T(h, nm)
        t.r = dict(self.freed)
        self.tiles.append(t)
        return t

    def mark(self):
        return len(self.stack)

    def release(self, mark):
        while len(self.stack) > mark:
            g = self.stack.pop()
            t = self.tiles.pop()
            toks = list(t.r.values()) + ([t.w] if t.w is not None else [])
            for tok in toks:
                o = self.freed.get(tok[0])
                if o is None or o[2] < tok[2]:
                    self.freed[tok[0]] = tok
            g.__exit__(None, None, None)

    def _deps(self, e, reads, writes):
        need = {}

        def add(tok):
            if tok is None:
                return
            key, sem, val = tok
            if key == "pe" and e == "pe" and self.fast_pe:
                return
            if self.seen[e].get(key, 0) >= val:
                return
            if key not in need or need[key][1] < val:
                need[key] = (sem, val)

        for t in reads:
            add(t.w)
        for t in writes:
            add(t.w)
            for tok in t.r.values():
                add(tok)
        for key, (sem, val) in need.items():
            self.eng[e].wait_ge(sem, val)
            self.seen[e][key] = val

    def _commit(self, tok, reads, writes):
        key = tok[0]
        for t in reads:
            o = t.r.get(key)
            if o is None or o[2] < tok[2]:
                t.r[key] = tok
        for t in writes:
            t.w = tok
            t.r = {}

    def op(self, e, fn, reads=(), writes=(), inc=True, fast=False):
        inc = True
        self.fast_pe = fast
        self._deps(e, reads, writes)
        self.fast_pe = False
        ins = fn(self.eng[e])
        if inc:
            self.cnt[e] += 1
            ins.then_inc(self.csem[e], 1)
            tok = (e, self.csem[e], self.cnt[e])
            self.pending[e] = False
        else:
            tok = (e, self.csem[e], self.cnt[e] + 1)
            self.pending[e] = True
        self._commit(tok, reads, writes)
        self.ninst += 1
        return tok

    def dma(self, q, out, in_, reads=(), writes=(), **kw):
        i = self.dcnt[q]
        self.dcnt[q] += 1
        slot = i % self.NDMA_SEM
        rnd = i // self.NDMA_SEM
        sem = self.dsem[q][slot]
        key = ("d", q, slot)
        if rnd > 0 and self.seen[q].get(key, 0) < 16 * rnd:
            self.eng[q].wait_ge(sem, 16 * rnd)
            self.seen[q][key] = 16 * rnd
        self._deps(q, reads, writes)
        ins = self.eng[q].dma_start(out=out, in_=in_, **kw)
        ins.then_inc(sem, 16)
        tok = (key, sem, 16 * (rnd + 1))
        self._commit(tok, reads, writes)
        self.ninst += 1
        return tok

    def finish(self, toks):
        for e in self.pending:
            assert not self.pending[e], "engine %s ends with a non-incrementing instruction" % e
        toks = list(toks)
        for e in self.csem:
            if self.cnt[e] > 0:
                toks.append((e, self.csem[e], self.cnt[e]))
        for q in self.dsem:
            n = self.dcnt[q]
            for slot in range(self.NDMA_SEM):
                if n > slot:
                    rounds = (n - slot + self.NDMA_SEM - 1) // self.NDMA_SEM
                    toks.append((("d", q, slot), self.dsem[q][slot], 16 * rounds))
        for tok in toks:
            key, sem, val = tok
            if self.seen["sp"].get(key, 0) < val:
                self.eng["sp"].wait_ge(sem, val)
                self.seen["sp"][key] = val


def _bc(ap, shape):
    return ap.to_broadcast(list(shape))


def load_consts(k, nc, consts_ap):
    c = k.sb([128, NCONST, 128], F32, "consts")
    k.dma("sp", c[:], consts_ap, writes=[c])
    cb = k.sb([128, NCONST, 128], BF16, "consts_bf")
    k.op("dve", lambda e: e.tensor_copy(out=cb[:], in_=c[:]), reads=[c], writes=[cb])
    return c, cb


NCONST = 10
C_ID = 0
C_TRI = 1
C_SU = 2
C_BLK = 3
C_M16 = 4
C_MC1 = 5
C_MC2 = 6
C_ONES = 7
C_UT64 = 8
C_CH0 = 9


def make_consts():
    p = np.arange(128)
    i = p[:, None]
    j = p[None, :]
    c = np.zeros((128, NCONST, 128), np.float32)
    same64 = (i // 64) == (j // 64)
    same32 = (i // 32) == (j // 32)
    same16 = (i // 16) == (j // 16)
    c[:, C_ID] = (i == j)
    c[:, C_TRI] = same64 & (i <= j)
    c[:, C_SU] = same64 & (j < i)
    c[:, C_BLK] = same64
    c[:, C_M16] = same16 & (j < i)
    c[:, C_MC1] = same32 & (~same16) & (j < i)
    c[:, C_MC2] = same64 & (~same32) & (j < i)
    c[:, C_ONES] = 1.0
    c[:, C_UT64] = same64 & (i <= j)
    c[:, C_CH0, 0] = (p < 64)
    c[:, C_CH0, 1] = (p >= 64)
    return c


def compute_mod(k, nc, condT_ap, w_ada_ap, b_ada_ap, ncols, outs, ps_pool):
    m0 = k.mark()
    ct = k.sb([128, 8], F32, "ct")
    k.dma("sp", ct[:], condT_ap, writes=[ct])
    sg = k.sb([128, 8], F32, "sg")
    k.op("act", lambda e: e.activation(out=sg[:], in_=ct[:], func=AF.Sigmoid), reads=[ct], writes=[sg])
    cond = k.sb([128, 8], F32, "cond")
    k.op("dve", lambda e: e.tensor_tensor(out=cond[:], in0=ct[:], in1=sg[:], op=ALU.mult), reads=[ct, sg], writes=[cond])
    cbc = k.sb([128, 8, 128], BF16, "cond_bc")
    k.op("dve", lambda e: e.tensor_copy(out=cbc[:], in_=_bc(cond[:].unsqueeze(2), [128, 8, 128])), reads=[cond], writes=[cbc])
    wv = w_ada_ap.rearrange("(k p) n -> p k n", p=128)
    nch = ncols // 512
    wbuf = [k.sb([128, 8, 512], BF16, "wada%d" % i) for i in range(2)]
    bbuf = [k.sb([128, 512], F32, "bada%d" % i) for i in range(2)]
    for j in range(nch):
        wb = wbuf[j % 2]
        bb = bbuf[j % 2]
        k.dma("pool", wb[:], wv[:, :, j * 512:(j + 1) * 512], writes=[wb])
        k.dma("sp", bb[:], b_ada_ap[j * 512:(j + 1) * 512].partition_broadcast(128), writes=[bb])
        pt = ps_pool[j % len(ps_pool)]
        for kk in range(8):
            k.op("pe", lambda e, kk=kk: e.matmul(pt[:, 0:512], lhsT=cbc[:, kk, :], rhs=wb[:, kk, :], start=(kk == 0), stop=(kk == 7)),
                 reads=[cbc, wb], writes=[pt], fast=True)
        o = outs[j // 2]
        c0 = (j % 2) * 512
        k.op("dve", lambda e: e.tensor_tensor(out=o[:, c0:c0 + 512], in0=pt[:, 0:512], in1=bb[:], op=ALU.add),
             reads=[pt, bb], writes=[o])
    k.release(m0)


def rstd_from_ss(k, ss, rstd, n, scale):
    k.op("dve", lambda e: e.tensor_scalar(out=rstd[:, 0:n], in0=ss[:, 0:n], scalar1=scale, scalar2=EPS, op0=ALU.mult, op1=ALU.add),
         reads=[ss], writes=[rstd])
    k.op("act", lambda e: e.activation(out=rstd[:, 0:n], in_=rstd[:, 0:n], func=AF.Sqrt), reads=[rstd], writes=[rstd])
    k.op("dve", lambda e: e.reciprocal(out=rstd[:, 0:n], in_=rstd[:, 0:n]), reads=[rstd], writes=[rstd])


NTB = 2048
NE = 32


def build_phaseB(final, upto=99, ne=NE):
    nc = bass.Bass("TRN2", target_bir_lowering=False)
    dt = nc.dram_tensor
    h_in = dt("h_in", [NTB, D], F32, kind="ExternalInput").ap()
    yT = dt("yT", [D, NTB], BF16, kind="ExternalInput").ap()
    w_out = dt("w_out", [D, D], F32, kind="ExternalInput").ap()
    condT = dt("condT", [128, 8], F32, kind="ExternalInput").ap()
    w_ada = dt("w_ada", [D, 4096], F32, kind="ExternalInput").ap()
    b_ada = dt("b_ada", [4096], F32, kind="ExternalInput").ap()
    norm2 = dt("norm2", [D], F32, kind="ExternalInput").ap()
    w_r = dt("w_r", [D, 36], F32, kind="ExternalInput").ap()
    b_r = dt("b_r", [36], F32, kind="ExternalInput").ap()
    if upto >= 5:
        w1 = dt("w1", [ne, D, 512], F32, kind="ExternalInput").ap()
        w3 = dt("w3", [ne, D, 512], F32, kind="ExternalInput").ap()
        w2 = dt("w2", [ne, 512, D], F32, kind="ExternalInput").ap()
    fnorm = dt("fnorm", [D], F32, kind="ExternalInput").ap()
    consts = dt("consts", [128, NCONST, 128], F32, kind="ExternalInput").ap()
    out = dt("out", [NTB, D], F32, kind="ExternalOutput").ap()

    k = KB(nc)
    NT = NTB // 128
    cst, cstb = load_consts(k, nc, consts)
    ident = cst

    hres = [k.sb([128, D], F32, "hres%d" % t) for t in range(NT)]
    h_v = h_in.rearrange("(t p) d -> t p d", p=128)
    for t in range(NT):
        k.dma("sp", hres[t][:], h_v[t], writes=[hres[t]])
    gt2 = k.sb([128, D], F32, "gt2")
    u2T = k.sb([128, 8, NTB], BF16, "u2T")
    logits = k.sb([128, NT, 36], F32, "logits")
    Wd = k.sb([128, NT, NE], F32, "Wd")
    m_mod = k.mark()
    gt1 = k.sb([128, D], F32, "gt1")
    sh2 = k.sb([128, D], F32, "sh2")
    sc2 = k.sb([128, D], F32, "sc2")

    if upto < 1:
        return _finB(k, out, hres, NT)
    m1 = k.mark()
    pp = [k.ps([128, 512], F32, "modps%d" % i) for i in range(2)]
    compute_mod(k, nc, condT, w_ada, b_ada, 4096, [gt1, sh2, sc2, gt2], pp)
    k.release(m1)
    g2 = k.sb([128, D], F32, "g2")
    k.dma("sp", g2[:], norm2.partition_broadcast(128), writes=[g2])
    k.op("dve", lambda e: e.scalar_tensor_tensor(out=g2[:], in0=sc2[:], scalar=1.0, in1=g2[:], op0=ALU.add, op1=ALU.mult),
         reads=[sc2, g2], writes=[g2])

    if upto < 2:
        return _finB(k, out, hres, NT)
    m2 = k.mark()
    yTs = k.sb([128, 8, NTB], BF16, "yTs")
    yv = yT.rearrange("(k p) t -> p k t", p=128)
    for kk in range(8):
        k.dma("sp", yTs[:, kk, :], yv[:, kk, :], writes=[yTs])
    wo = k.sb([128, 8, D], BF16, "wo")
    wov = w_out.rearrange("(k p) n -> p k n", p=128)
    for kk in range(0, 8, 2):
        k.dma("pool", wo[:, kk:kk + 2, :], wov[:, kk:kk + 2, :], writes=[wo])
    k.op("pool", lambda e: e.tensor_tensor(out=wo[:], in0=wo[:], in1=_bc(gt1[:].unsqueeze(1), [128, 8, D]), op=ALU.mult),
         reads=[wo, gt1], writes=[wo])
    psy = [k.ps([128, D], F32, "psy%d" % i) for i in range(2)]
    for t in range(NT):
        p = psy[t % 2]
        for half in range(2):
            for kk in range(8):
                k.op("pe", lambda e, kk=kk, half=half: e.matmul(p[:, half * 512:(half + 1) * 512], lhsT=yTs[:, kk, t * 128:(t + 1) * 128],
                                                                 rhs=wo[:, kk, half * 512:(half + 1) * 512], start=(kk == 0), stop=(kk == 7)),
                     reads=[yTs, wo], writes=[p], fast=True)
        k.op("dve", lambda e: e.tensor_tensor(out=hres[t][:], in0=p[:], in1=hres[t][:], op=ALU.add), reads=[p, hres[t]], writes=[hres[t]])
    k.release(m2)

    if upto < 3:
        return _finB(k, out, hres, NT)
    m3 = k.mark()
    ss = k.sb([128, NT], F32, "ss")
    rstd = k.sb([128, NT], F32, "rstd")
    junk = [k.sb([128, D], BF16, "junk%d" % i) for i in range(2)]
    for t in range(NT):
        jk = junk[t % 2]
        k.op("act", lambda e: e.activation(out=jk[:], in_=hres[t][:], func=AF.Square, accum_out=ss[:, t:t + 1]),
             reads=[hres[t]], writes=[jk, ss])
    rstd_from_ss(k, ss, rstd, NT, 1.0 / D)
    wr = k.sb([128, 8, 36], F32, "wr")
    k.dma("sp", wr[:], w_r.rearrange("(k p) n -> p k n", p=128), writes=[wr])
    brb = k.sb([128, 36], F32, "brb")
    k.dma("sp", brb[:], b_r.partition_broadcast(128), writes=[brb])
    t1b = [k.sb([128, D], F32, "t1b%d" % i) for i in range(2)]
    u32 = [k.sb([128, D], F32, "u32_%d" % i) for i in range(2)]
    uT32 = [k.sb([128, 8, 128], F32, "uT32_%d" % i) for i in range(2)]
    pst = [k.ps([128, 8, 128], F32, "pst%d" % i) for i in range(2)]
    psr = [k.ps([128, 36], F32, "psr%d" % i) for i in range(2)]
    for t in range(NT):
        a = t1b[t % 2]
        u = u32[t % 2]
        ut = uT32[t % 2]
        pt = pst[t % 2]
        pr = psr[t % 2]
        k.op("dve", lambda e: e.scalar_tensor_tensor(out=a[:], in0=hres[t][:], scalar=rstd[:, t:t + 1], in1=g2[:], op0=ALU.mult, op1=ALU.mult),
             reads=[hres[t], rstd, g2], writes=[a])
        k.op("pool", lambda e: e.tensor_tensor(out=u[:], in0=a[:], in1=sh2[:], op=ALU.add), reads=[a, sh2], writes=[u])
        for kk in range(8):
            k.op("pe", lambda e, kk=kk: e.transpose(out=pt[:, kk, :], in_=u[:, kk * 128:(kk + 1) * 128], identity=cst[:, C_ID, :]),
                 reads=[u, cst], writes=[pt], inc=(kk == 7))
        k.op("act", lambda e: e.activation(out=ut[:], in_=pt[:], func=AF.Copy), reads=[pt], writes=[ut])
        k.op("pool", lambda e: e.tensor_copy(out=u2T[:, :, t * 128:(t + 1) * 128], in_=ut[:]), reads=[ut], writes=[u2T])
        for kk in range(8):
            k.op("pe", lambda e, kk=kk: e.matmul(pr[:], lhsT=ut[:, kk, :], rhs=wr[:, kk, :], start=(kk == 0), stop=(kk == 7)),
                 reads=[ut, wr], writes=[pr], inc=(kk == 7))
        k.op("dve", lambda e: e.tensor_tensor(out=logits[:, t, :], in0=pr[:], in1=brb[:], op=ALU.add), reads=[pr, brb], writes=[logits])
    k.release(m3)

    if upto < 4:
        return _finB(k, out, hres, NT)
    m4 = k.mark()
    BIG = 1.0e30

    def dve(fn, reads, writes):
        k.op("dve", fn, reads=reads, writes=writes)

    lg = logits[:, :, 0:4]
    le = logits[:, :, 4:36]
    gmax = k.sb([128, NT], F32, "gmax")
    dve(lambda e: e.tensor_reduce(out=gmax[:], in_=lg, axis=AX.X, op=ALU.max), [logits], [gmax])
    eg = k.sb([128, NT, 4], F32, "eg")
    dve(lambda e: e.tensor_tensor(out=eg[:], in0=lg, in1=_bc(gmax[:].unsqueeze(2), [128, NT, 4]), op=ALU.subtract), [logits, gmax], [eg])
    k.op("act", lambda e: e.activation(out=eg[:], in_=eg[:], func=AF.Exp), reads=[eg], writes=[eg])
    gsum = k.sb([128, NT], F32, "gsum")
    dve(lambda e: e.tensor_reduce(out=gsum[:], in_=eg[:], axis=AX.X, op=ALU.add), [eg], [gsum])
    pgt = k.sb([128, NT], F32, "pgt")
    dve(lambda e: e.reciprocal(out=pgt[:], in_=gsum[:]), [gsum], [pgt])
    pen = k.sb([128, NT, 4], F32, "pen")
    dve(lambda e: e.tensor_tensor(out=pen[:], in0=lg, in1=_bc(gmax[:].unsqueeze(2), [128, NT, 4]), op=ALU.is_equal), [logits, gmax], [pen])
    dve(lambda e: e.tensor_scalar(out=pen[:], in0=pen[:], scalar1=1.0, scalar2=BIG, op0=ALU.subtract, op1=ALU.mult), [pen], [pen])
    lem = k.sb([128, NT, NE], F32, "lem")
    dve(lambda e: e.tensor_tensor(out=lem[:].rearrange("p t (g x) -> p t g x", g=4), in0=le.rearrange("p t (g x) -> p t g x", g=4),
                                  in1=_bc(pen[:].unsqueeze(3), [128, NT, 4, 8]), op=ALU.add), [logits, pen], [lem])
    mx1 = k.sb([128, NT], F32, "mx1")
    dve(lambda e: e.tensor_reduce(out=mx1[:], in_=lem[:], axis=AX.X, op=ALU.max), [lem], [mx1])
    oh1 = k.sb([128, NT, NE], F32, "oh1")
    dve(lambda e: e.tensor_tensor(out=oh1[:], in0=lem[:], in1=_bc(mx1[:].unsqueeze(2), [128, NT, NE]), op=ALU.is_equal), [lem, mx1], [oh1])
    lem2 = k.sb([128, NT, NE], F32, "lem2")
    dve(lambda e: e.scalar_tensor_tensor(out=lem2[:], in0=oh1[:], scalar=-BIG, in1=lem[:], op0=ALU.mult, op1=ALU.add), [oh1, lem], [lem2])
    mx2 = k.sb([128, NT], F32, "mx2")
    dve(lambda e: e.tensor_reduce(out=mx2[:], in_=lem2[:], axis=AX.X, op=ALU.max), [lem2], [mx2])
    oh2 = k.sb([128, NT, NE], F32, "oh2")
    dve(lambda e: e.tensor_tensor(out=oh2[:], in0=lem2[:], in1=_bc(mx2[:].unsqueeze(2), [128, NT, NE]), op=ALU.is_equal), [lem2, mx2], [oh2])
    rr = k.sb([128, NT], F32, "rr")
    dve(lambda e: e.tensor_tensor(out=rr[:], in0=mx2[:], in1=mx1[:], op=ALU.subtract), [mx2, mx1], [rr])
    k.op("act", lambda e: e.activation(out=rr[:], in_=rr[:], func=AF.Exp), reads=[rr], writes=[rr])
    den = k.sb([128, NT], F32, "den")
    dve(lambda e: e.tensor_scalar(out=den[:], in0=rr[:], scalar1=1.0, scalar2=None, op0=ALU.add), [rr], [den])
    dve(lambda e: e.reciprocal(out=den[:], in_=den[:]), [den], [den])
    wt1 = k.sb([128, NT], F32, "wt1")
    dve(lambda e: e.tensor_tensor(out=wt1[:], in0=pgt[:], in1=den[:], op=ALU.mult), [pgt, den], [wt1])
    wt2 = k.sb([128, NT], F32, "wt2")
    dve(lambda e: e.tensor_tensor(out=wt2[:], in0=wt1[:], in1=rr[:], op=ALU.mult), [wt1, rr], [wt2])
    dve(lambda e: e.tensor_tensor(out=Wd[:], in0=oh1[:], in1=_bc(wt1[:].unsqueeze(2), [128, NT, NE]), op=ALU.mult), [oh1, wt1], [Wd])
    dve(lambda e: e.tensor_tensor(out=oh2[:], in0=oh2[:], in1=_bc(wt2[:].unsqueeze(2), [128, NT, NE]), op=ALU.mult), [oh2, wt2], [oh2])
    dve(lambda e: e.tensor_tensor(out=Wd[:], in0=Wd[:], in1=oh2[:], op=ALU.add), [Wd, oh2], [Wd])
    k.release(m4)

    if upto < 5:
        return _finB(k, out, hres, NT)
    k.release(m_mod)
    m5 = k.mark()
    w1b = [k.sb([128, 8, 512], BF16, "w1b%d" % i) for i in range(2)]
    w3b = [k.sb([128, 8, 512], BF16, "w3b%d" % i) for i in range(2)]
    w2b = [k.sb([128, 4, D], BF16, "w2b%d" % i) for i in range(2)]
    stg = [k.sb([128, 4096], F32, "stg%d" % i) for i in range(2)]
    actT = [[k.sb([128, 512], BF16, "actT%d_%d" % (i, f)) for f in range(4)] for i in range(2)]
    slb = [k.sb([128, 512], F32, "slb%d" % i) for i in range(2)]
    ps1 = [k.ps([128, 512], F32, "ps1_%d" % i) for i in range(2)]
    ps3 = [k.ps([128, 512], F32, "ps3_%d" % i) for i in range(2)]
    psy = [k.ps([128, D], F32, "psye%d" % i) for i in range(2)]
    cnt_f = 0
    cnt_y = 0
    for ex in range(ne):
        b = ex % 2
        s1 = stg[(3 * ex) % 2]
        k.dma("sp", s1[:].rearrange("p (k f) -> p k f", k=8), w1[ex].rearrange("(k p) f -> p k f", p=128), writes=[s1])
        k.op("pool", lambda e: e.tensor_copy(out=w1b[b][:].rearrange("p k f -> p (k f)"), in_=s1[:]), reads=[s1], writes=[w1b[b]])
        s3 = stg[(3 * ex + 1) % 2]
        k.dma("sp", s3[:].rearrange("p (k f) -> p k f", k=8), w3[ex].rearrange("(k p) f -> p k f", p=128), writes=[s3])
        k.op("pool", lambda e: e.tensor_copy(out=w3b[b][:].rearrange("p k f -> p (k f)"), in_=s3[:]), reads=[s3], writes=[w3b[b]])
        s2 = stg[(3 * ex + 2) % 2]
        k.dma("sp", s2[:].rearrange("p (c d) -> p c d", c=4), w2[ex].rearrange("(c p) d -> p c d", p=128), writes=[s2])
        k.op("pool", lambda e: e.tensor_tensor(out=w2b[b][:], in0=s2[:].rearrange("p (c d) -> p c d", c=4), in1=_bc(gt2[:].unsqueeze(1), [128, 4, D]), op=ALU.mult),
             reads=[s2, gt2], writes=[w2b[b]])
        for blk in range(NTB // 512):
            ab = actT[blk % 2]
            for fc in range(4):
                p1 = ps1[cnt_f % 2]
                p3 = ps3[cnt_f % 2]
                sl = slb[cnt_f % 2]
                cnt_f += 1
                for kk in range(8):
                    k.op("pe", lambda e, kk=kk: e.matmul(p1[:], lhsT=w1b[b][:, kk, fc * 128:(fc + 1) * 128], rhs=u2T[:, kk, blk * 512:(blk + 1) * 512],
                                                         start=(kk == 0), stop=(kk == 7)), reads=[w1b[b], u2T], writes=[p1], fast=True)
                for kk in range(8):
                    k.op("pe", lambda e, kk=kk: e.matmul(p3[:], lhsT=w3b[b][:, kk, fc * 128:(fc + 1) * 128], rhs=u2T[:, kk, blk * 512:(blk + 1) * 512],
                                                         start=(kk == 0), stop=(kk == 7)), reads=[w3b[b], u2T], writes=[p3], fast=True)
                k.op("act", lambda e: e.activation(out=sl[:], in_=p1[:], func=AF.Silu), reads=[p1], writes=[sl])
                k.op("dve", lambda e: e.tensor_tensor(out=ab[fc][:], in0=p3[:], in1=sl[:], op=ALU.mult), reads=[p3, sl], writes=[ab[fc]])
            for ti in range(4):
                t = blk * 4 + ti
                py = psy[cnt_y % 2]
                cnt_y += 1
                for half in range(2):
                    for fc in range(4):
                        k.op("pe", lambda e, fc=fc, half=half: e.matmul(py[:, half * 512:(half + 1) * 512], lhsT=ab[fc][:, ti * 128:(ti + 1) * 128],
                                                                         rhs=w2b[b][:, fc, half * 512:(half + 1) * 512], start=(fc == 0), stop=(fc == 3)),
                             reads=[ab[fc], w2b[b]], writes=[py], fast=True)
                k.op("dve", lambda e: e.scalar_tensor_tensor(out=hres[t][:], in0=py[:], scalar=Wd[:, t, ex:ex + 1], in1=hres[t][:],
                                                             op0=ALU.mult, op1=ALU.add), reads=[py, Wd, hres[t]], writes=[hres[t]])
    k.release(m5)

    toks = []
    o_v = out.rearrange("(t p) d -> t p d", p=128)
    if final:
        fn = k.sb([128, D], F32, "fn")
        k.dma("sp", fn[:], fnorm.partition_broadcast(128), writes=[fn])
        ss2 = k.sb([128, NT], F32, "ss2")
        rs2 = k.sb([128, NT], F32, "rs2")
        junk2 = [k.sb([128, D], BF16, "junkf%d" % i) for i in range(2)]
        for t in range(NT):
            jk = junk2[t % 2]
            k.op("act", lambda e: e.activation(out=jk[:], in_=hres[t][:], func=AF.Square, accum_out=ss2[:, t:t + 1]),
                 reads=[hres[t]], writes=[jk, ss2])
        rstd_from_ss(k, ss2, rs2, NT, 1.0 / D)
        for t in range(NT):
            k.op("dve", lambda e: e.scalar_tensor_tensor(out=hres[t][:], in0=hres[t][:], scalar=rs2[:, t:t + 1], in1=fn[:], op0=ALU.mult, op1=ALU.mult),
                 reads=[hres[t], rs2, fn], writes=[hres[t]])
    for t in range(NT):
        toks.append(k.dma("sp", o_v[t], hres[t][:], reads=[hres[t]]))
    k.finish(toks)
    k.release(0)
    return nc, k


def _finB(k, out, hres, NT):
    o_v = out.rearrange("(t p) d -> t p d", p=128)
    toks = [k.dma("sp", o_v[t], hres[t][:], reads=[hres[t]]) for t in range(NT)]
    k.finish(toks)
    k.release(0)
    return k.nc, k


def phaseB_inmaps(layer, h, yT_full, inp, final):
    maps = []
    wa = np.ascontiguousarray(inp["w_ada"][layer][:, 2048:6144])
    ba = np.ascontiguousarray(inp["b_ada"][layer][2048:6144])
    w_out = inp["w_out_ab"][0] if layer == 0 else inp["w_out_c"][0]
    w_r = np.ascontiguousarray(np.concatenate([inp["moe_w_grp"][layer], inp["moe_w_rt"][layer]], axis=1))
    b_r = np.ascontiguousarray(np.concatenate([inp["moe_b_grp"][layer], inp["moe_b_rt"][layer]], axis=0))
    consts = make_consts()
    for core in range(NCORES):
        b, q = core // 4, core % 4
        sl = slice(q * NTB, (q + 1) * NTB)
        maps.append({
            "h_in": np.ascontiguousarray(h[b, sl]),
            "yT": np.ascontiguousarray(yT_full[b][:, sl]),
            "w_out": np.ascontiguousarray(w_out),
            "condT": np.ascontiguousarray(inp["c"][b].reshape(8, 128).T),
            "w_ada": wa, "b_ada": ba,
            "norm2": np.ascontiguousarray(inp["norm2"][layer]),
            "w_r": w_r, "b_r": b_r,
            "w1": inp["moe_w1"][layer], "w3": inp["moe_w3"][layer], "w2": inp["moe_w2"][layer],
            "fnorm": np.ascontiguousarray(inp["final_norm"]),
            "consts": consts,
        })
    return maps


class UMaker:
    def __init__(self, k, nc, h_ap, condT, w_ada, b_ada, norm_ap, cstb):
        self.k = k
        self.cstb = cstb
        self.h_v = h_ap.rearrange("(t p) d -> t p d", p=128)
        self.sh = k.sb([128, D], F32, "sh1")
        self.sc = k.sb([128, D], F32, "sc1")
        m = k.mark()
        pp = [k.ps([128, 512], F32, "modps%d" % i) for i in range(2)]
        compute_mod(k, nc, condT, w_ada, b_ada, 2048, [self.sh, self.sc], pp)
        k.release(m)
        self.g = k.sb([128, D], F32, "g1")
        k.dma("sp", self.g[:], norm_ap.partition_broadcast(128), writes=[self.g])
        k.op("dve", lambda e: e.scalar_tensor_tensor(out=self.g[:], in0=self.sc[:], scalar=1.0, in1=self.g[:], op0=ALU.add, op1=ALU.mult),
             reads=[self.sc, self.g], writes=[self.g])
        self.ht = [k.sb([128, D], F32, "ht%d" % i) for i in range(4)]
        self.junk = k.sb([128, D], BF16, "junk")
        self.ss = k.sb([128, 4], F32, "ss")
        self.rstd = k.sb([128, 4], F32, "rstd")
        self.a = [k.sb([128, D], F32, "ua%d" % i) for i in range(2)]
        self.ub = [k.sb([128, D], BF16, "ub%d" % i) for i in range(2)]
        self.pst = [k.ps([128, 8, 128], BF16, "pst%d" % i) for i in range(1)]

    def block(self, blk, uT):
        k = self.k
        for ti in range(4):
            ht = self.ht[ti]
            k.dma("sp", ht[:], self.h_v[blk * 4 + ti], writes=[ht])
            k.op("act", lambda e: e.activation(out=self.junk[:], in_=ht[:], func=AF.Square, accum_out=self.ss[:, ti:ti + 1]),
                 reads=[ht], writes=[self.junk, self.ss])
        rstd_from_ss(k, self.ss, self.rstd, 4, 1.0 / D)
        for ti in range(4):
            ht = self.ht[ti]
            a = self.a[ti % 2]
            ub = self.ub[ti % 2]
            pt = self.pst[0]
            k.op("dve", lambda e: e.scalar_tensor_tensor(out=a[:], in0=ht[:], scalar=self.rstd[:, ti:ti + 1], in1=self.g[:], op0=ALU.mult, op1=ALU.mult),
                 reads=[ht, self.rstd, self.g], writes=[a])
            k.op("pool", lambda e: e.tensor_tensor(out=ub[:], in0=a[:], in1=self.sh[:], op=ALU.add), reads=[a, self.sh], writes=[ub])
            for kk in range(8):
                k.op("pe", lambda e: e.transpose(out=pt[:, kk, :], in_=ub[:, kk * 128:(kk + 1) * 128], identity=self.cstb[:, C_ID, :]),
                     reads=[ub, self.cstb], writes=[pt])
            k.op("act", lambda e: e.activation(out=uT[:, :, ti * 128:(ti + 1) * 128], in_=pt[:], func=AF.Copy), reads=[pt], writes=[uT])


class Banks:
    def __init__(self, k, n):
        self.t = [k.ps([128, 512], F32, "bank%d" % i) for i in range(n)]
        self.i = 0

    def get(self):
        b = self.t[self.i % len(self.t)]
        self.i += 1
        return b


NCOL_A0 = 656


def build_phaseA0(nblk=16):
    nc = bass.Bass("TRN2", target_bir_lowering=False)
    dt = nc.dram_tensor
    h_in = dt("h_in", [S, D], F32, kind="ExternalInput").ap()
    condT = dt("condT", [128, 8], F32, kind="ExternalInput").ap()
    w_ada = dt("w_ada", [D, 2048], F32, kind="ExternalInput").ap()
    b_ada = dt("b_ada", [2048], F32, kind="ExternalInput").ap()
    norm1 = dt("norm1", [D], F32, kind="ExternalInput").ap()
    w_in = dt("w_in", [D, NCOL_A0], F32, kind="ExternalInput").ap()
    WAd = dt("WA", [128, 128], F32, kind="ExternalInput").ap()
    WXd = dt("WX", [128, 128], F32, kind="ExternalInput").ap()
    wg2d = dt("wg2", [16, 64], F32, kind="ExternalInput").ap()
    pcold = dt("pcol", [128, 16], F32, kind="ExternalInput").ap()
    consts = dt("consts", [128, NCONST, 128], F32, kind="ExternalInput").ap()
    yT = dt("yT", [256, S], BF16, kind="ExternalOutput").ap()

    k = KB(nc)
    cst, cstb = load_consts(k, nc, consts)
    um = UMaker(k, nc, h_in, condT, w_ada, b_ada, norm1, cstb)
    win = k.sb([128, 8, NCOL_A0], BF16, "win")
    k.dma("pool", win[:], w_in.rearrange("(k p) n -> p k n", p=128), writes=[win])
    WA = k.sb([128, 128], BF16, "WA")
    WX = k.sb([128, 128], BF16, "WX")
    wg2 = k.sb([16, 64], BF16, "wg2")
    k.dma("pool", WA[:], WAd, writes=[WA])
    k.dma("pool", WX[:], WXd, writes=[WX])
    k.dma("pool", wg2[:], wg2d, writes=[wg2])
    pc = k.sb([128, 16], F32, "pcol")
    k.dma("sp", pc[:], pcold, writes=[pc])
    dc = k.sb([128, 4], F32, "dc")
    tl = k.sb([128, 1], F32, "tl")
    k.op("act", lambda e: e.activation(out=tl[:], in_=pc[:, 7:8], func=AF.Exp, scale=-1.0), reads=[pc], writes=[tl])
    k.op("act", lambda e: e.activation(out=tl[:], in_=tl[:], func=AF.Ln, bias=1.0), reads=[tl], writes=[tl])
    k.op("dve", lambda e: e.tensor_scalar(out=dc[:, 0:1], in0=tl[:], scalar1=-8.0, scalar2=None, op0=ALU.mult), reads=[tl], writes=[dc])
    k.op("dve", lambda e: e.tensor_scalar(out=dc[:, 1:2], in0=tl[:], scalar1=-16.0, scalar2=None, op0=ALU.mult), reads=[tl], writes=[dc])
    k.op("dve", lambda e: e.tensor_scalar(out=dc[:, 2:3], in0=pc[:, 9:10], scalar1=-1.0, scalar2=None, op0=ALU.mult), reads=[pc], writes=[dc])
    rmask = k.sb([64, 8, 64], F32, "rmask")
    k.op("dve", lambda e: e.memset(rmask[:], 1.0), writes=[rmask])
    k.op("dve", lambda e: e.memset(rmask[:, :, 0:1], 0.0), writes=[rmask])
    rmask2 = rmask[:].rearrange("p a b -> p (a b)")

    banks = Banks(k, 5)
    pkg = k.ps([128, 4, 64], BF16, "pkg")
    uT = [k.sb([128, 8, 512], BF16, "uT%d" % i) for i in range(2)]
    xabuf = [k.sb([128, 515], F32, "xabuf%d" % i) for i in range(2)]
    k.op("dve", lambda e: e.memset(xabuf[0][:, 0:3], 0.0), writes=[xabuf[0]])
    hs = [k.sb([128, 512], F32, "hs%d" % i) for i in range(2)]
    Sst = k.sb([64, 128], F32, "Sst")
    k.op("dve", lambda e: e.memset(Sst[:], 0.0), writes=[Sst])
    Sbt = [k.sb([64, 128], BF16, "Sbt%d" % i) for i in range(8)]

    def sbt(shape, dtp, name):
        return k.sb(shape, dtp, name)

    ga = sbt([128, 512], F32, "ga")
    q_sb = sbt([64, 512], F32, "q_sb")
    k_sb = sbt([64, 512], F32, "k_sb")
    sog = sbt([128, 512], F32, "sog")
    gl_bf = sbt([16, 512], BF16, "gl_bf")
    v_tok = sbt([128, 4, 128], BF16, "v_tok")
    xc = sbt([128, 512], F32, "xc")
    xcb = sbt([128, 512], BF16, "xcb")
    r_sb = sbt([128, 512], F32, "r_sb")
    i_sb = sbt([128, 512], F32, "i_sb")
    a_sb = sbt([128, 512], F32, "a_sb")
    a2_sb = sbt([128, 512], F32, "a2_sb")
    t_sb = sbt([128, 512], F32, "t_sb")
    b_sb = sbt([128, 512], F32, "b_sb")
    g2_sb = sbt([128, 512], F32, "g2_sb")
    inner = sbt([128, 512], F32, "inner")
    ge = sbt([128, 512], F32, "ge")
    ya = [sbt([128, 512], BF16, "ya%d" % i) for i in range(2)]
    e1 = sbt([64, 512], F32, "e1")
    sp_ = sbt([64, 512], F32, "sp")
    cum = sbt([64, 512], F32, "cum")
    E1 = sbt([64, 512], F32, "E1")
    E2 = sbt([64, 512], F32, "E2")
    qg = sbt([64, 512], BF16, "qg")
    kg = sbt([64, 512], BF16, "kg")
    kg_tok = sbt([128, 4, 64], BF16, "kg_tok")
    attm = sbt([128, 4, 128], BF16, "attm")
    tS = sbt([64, 128], F32, "tS")
    osb = sbt([128, 512], F32, "osb")
    o2 = sbt([128, 512], F32, "o2")
    rs = sbt([128, 512], F32, "rs")
    t1 = sbt([128, 512], F32, "t1")
    ob = [sbt([128, 512], BF16, "ob%d" % i) for i in range(2)]

    def op(e, fn, r, w):
        return k.op(e, fn, reads=r, writes=w)

    out_toks = []
    for blk in range(nblk):
        u = uT[blk % 2]
        xb = xabuf[blk % 2]
        xbn = xabuf[(blk + 1) % 2]
        um.block(blk, u)

        def proj(c0, c1):
            p = banks.get()
            m = c1 - c0
            for kk in range(8):
                k.op("pe", lambda e: e.matmul(p[0:m, :], lhsT=win[:, kk, c0:c1], rhs=u[:, kk, :], start=(kk == 0), stop=(kk == 7)), reads=[win, u], writes=[p], fast=True)
            return p

        p = proj(0, 128)
        op("act", lambda e: e.activation(out=xb[:, 3:515], in_=p[:], func=AF.Copy), [p], [xb])
        p = proj(128, 256)
        op("act", lambda e: e.activation(out=ga[:], in_=p[:], func=AF.Copy), [p], [ga])
        p = proj(256, 320)
        op("dve", lambda e: e.tensor_scalar(out=q_sb[:], in0=p[0:64, :], scalar1=0.125, scalar2=None, op0=ALU.mult), [p], [q_sb])
        p = proj(320, 384)
        op("act", lambda e: e.activation(out=k_sb[:], in_=p[0:64, :], func=AF.Copy), [p], [k_sb])
        p = proj(512, 640)
        op("act", lambda e: e.activation(out=sog[:], in_=p[:], func=AF.Silu), [p], [sog])
        p = proj(640, 656)
        op("dve", lambda e: e.tensor_copy(out=gl_bf[:], in_=p[0:16, :]), [p], [gl_bf])
        p = banks.get()
        for ti in range(4):
            for kk in range(8):
                op("pe", lambda e: e.matmul(p[:, ti * 128:(ti + 1) * 128], lhsT=u[:, kk, ti * 128:(ti + 1) * 128], rhs=win[:, kk, 384:512],
                                            start=(kk == 0), stop=(kk == 7)), [win, u], [p])
        op("dve", lambda e: e.tensor_copy(out=v_tok[:].rearrange("p a b -> p (a b)"), in_=p[:]), [p], [v_tok])

        op("act", lambda e: e.activation(out=xc[:], in_=xb[:, 3:515], func=AF.Identity, bias=pc[:, 4:5], scale=pc[:, 3:4]), [xb, pc], [xc])
        for w in range(3):
            op("dve", lambda e: e.scalar_tensor_tensor(out=xc[:], in0=xb[:, w:w + 512], scalar=pc[:, w:w + 1], in1=xc[:], op0=ALU.mult, op1=ALU.add),
               [xb, pc, xc], [xc])
        op("pool", lambda e: e.tensor_copy(out=xbn[:, 0:3], in_=xb[:, 512:515]), [xb], [xbn])
        op("act", lambda e: e.activation(out=xcb[:], in_=xc[:], func=AF.Copy), [xc], [xcb])
        p = banks.get()
        op("pe", lambda e: e.matmul(p[:], lhsT=WA[:], rhs=xcb[:], start=True, stop=True), [WA, xcb], [p])
        op("act", lambda e: e.activation(out=r_sb[:], in_=p[:], func=AF.Sigmoid, bias=pc[:, 5:6]), [p, pc], [r_sb])
        p = banks.get()
        op("pe", lambda e: e.matmul(p[:], lhsT=WX[:], rhs=xcb[:], start=True, stop=True), [WX, xcb], [p])
        op("act", lambda e: e.activation(out=i_sb[:], in_=p[:], func=AF.Sigmoid, bias=pc[:, 6:7]), [p, pc], [i_sb])
        op("act", lambda e: e.activation(out=a_sb[:], in_=r_sb[:], func=AF.Exp, scale=dc[:, 0:1]), [r_sb, dc], [a_sb])
        op("act", lambda e: e.activation(out=a2_sb[:], in_=r_sb[:], func=AF.Exp, scale=dc[:, 1:2]), [r_sb, dc], [a2_sb])
        op("act", lambda e: e.activation(out=a2_sb[:], in_=a2_sb[:], func=AF.Sqrt, bias=1.0, scale=-1.0), [a2_sb], [a2_sb])
        op("dve", lambda e: e.tensor_tensor(out=t_sb[:], in0=i_sb[:], in1=xc[:], op=ALU.mult), [i_sb, xc], [t_sb])
        op("pool", lambda e: e.tensor_tensor(out=b_sb[:], in0=t_sb[:], in1=a2_sb[:], op=ALU.mult), [t_sb, a2_sb], [b_sb])
        hcur = hs[blk % 2]
        hprev = hs[(blk + 1) % 2]
        if blk == 0:
            op("dve", lambda e: e.tensor_tensor_scan(out=hcur[:], data0=a_sb[:], data1=b_sb[:], initial=0.0, op0=ALU.mult, op1=ALU.add),
               [a_sb, b_sb], [hcur])
        else:
            op("dve", lambda e: e.tensor_tensor_scan(out=hcur[:], data0=a_sb[:], data1=b_sb[:], initial=hprev[:, 511:512], op0=ALU.mult, op1=ALU.add),
               [a_sb, b_sb, hprev], [hcur])
        op("act", lambda e: e.activation(out=g2_sb[:], in_=ga[:], func=AF.Square), [ga], [g2_sb])
        op("dve", lambda e: e.tensor_scalar(out=g2_sb[:], in0=g2_sb[:], scalar1=0.044715, scalar2=1.0, op0=ALU.mult, op1=ALU.add), [g2_sb], [g2_sb])
        op("pool", lambda e: e.tensor_tensor(out=inner[:], in0=g2_sb[:], in1=ga[:], op=ALU.mult), [g2_sb, ga], [inner])
        op("act", lambda e: e.activation(out=inner[:], in_=inner[:], func=AF.Sigmoid, scale=1.5957691216), [inner], [inner])
        op("dve", lambda e: e.tensor_tensor(out=ge[:], in0=ga[:], in1=inner[:], op=ALU.mult), [ga, inner], [ge])
        yab = ya[blk % 2]
        op("pool", lambda e: e.tensor_tensor(out=yab[:], in0=ge[:], in1=hcur[:], op=ALU.mult), [ge, hcur], [yab])
        out_toks.append(k.dma("sp", yT[0:128, blk * 512:(blk + 1) * 512], yab[:], reads=[yab]))

        p = banks.get()
        op("pe", lambda e: e.matmul(p[0:64, :], lhsT=wg2[:], rhs=gl_bf[:], start=True, stop=True), [wg2, gl_bf], [p])
        op("act", lambda e: e.activation(out=e1[:], in_=p[0:64, :], func=AF.Exp, bias=dc[0:64, 2:3], scale=-1.0), [p, dc], [e1])
        op("act", lambda e: e.activation(out=sp_[:], in_=e1[:], func=AF.Ln, bias=1.0), [e1], [sp_])
        op("dve", lambda e: e.tensor_tensor_scan(out=cum[:], data0=rmask2, data1=sp_[:], initial=0.0, op0=ALU.mult, op1=ALU.add), [rmask, sp_], [cum])
        op("act", lambda e: e.activation(out=E1[:], in_=cum[:], func=AF.Exp, scale=-1.0 / 16.0), [cum], [E1])
        op("act", lambda e: e.activation(out=E2[:], in_=cum[:], func=AF.Exp, scale=1.0 / 16.0), [cum], [E2])
        op("dve", lambda e: e.tensor_tensor(out=qg[:], in0=q_sb[:], in1=E1[:], op=ALU.mult), [q_sb, E1], [qg])
        op("pool", lambda e: e.tensor_tensor(out=kg[:], in0=k_sb[:], in1=E2[:], op=ALU.mult), [k_sb, E2], [kg])
        for pr in range(4):
            op("pe", lambda e: e.transpose(out=pkg[:, pr, :], in_=kg[:, pr * 128:(pr + 1) * 128], identity=cstb[0:64, C_ID, 0:64]), [kg, cstb], [pkg])
        op("act", lambda e: e.activation(out=kg_tok[:], in_=pkg[:], func=AF.Copy), [pkg], [kg_tok])
        p = banks.get()
        for pr in range(4):
            op("pe", lambda e: e.matmul(p[:, pr * 128:(pr + 1) * 128], lhsT=kg[:, pr * 128:(pr + 1) * 128], rhs=qg[:, pr * 128:(pr + 1) * 128],
                                        start=True, stop=True), [kg, qg], [p])
        op("dve", lambda e: e.tensor_tensor(out=attm[:], in0=p[:].rearrange("p (a b) -> p a b", a=4),
                                            in1=_bc(cst[:, C_UT64:C_UT64 + 1, :], [128, 4, 128]), op=ALU.mult), [p, cst], [attm])
        kva = banks.get()
        kvb = banks.get()
        for c in range(8):
            pr, half = c // 2, c % 2
            kvp = kva if c < 4 else kvb
            op("pe", lambda e: e.matmul(kvp[0:64, (c % 4) * 128:(c % 4 + 1) * 128], lhsT=kg_tok[half * 64:(half + 1) * 64, pr, :],
                                        rhs=v_tok[half * 64:(half + 1) * 64, pr, :], start=True, stop=True), [kg_tok, v_tok], [kvp])
        for c in range(8):
            kvp = kva if c < 4 else kvb
            op("act", lambda e: e.activation(out=Sbt[c][:], in_=Sst[:], func=AF.Copy), [Sst], [Sbt[c]])
            op("dve", lambda e: e.tensor_tensor(out=tS[:], in0=kvp[0:64, (c % 4) * 128:(c % 4 + 1) * 128], in1=Sst[:], op=ALU.add), [kvp, Sst], [tS])
            op("dve", lambda e: e.tensor_scalar(out=Sst[:], in0=tS[:], scalar1=E1[:, c * 64 + 63:c * 64 + 64], scalar2=None, op0=ALU.mult),
               [tS, E1], [Sst])
        po = banks.get()
        for pr in range(4):
            op("pe", lambda e: e.matmul(po[:, pr * 128:(pr + 1) * 128], lhsT=v_tok[:, pr, :], rhs=attm[:, pr, :], start=True, stop=False),
               [v_tok, attm], [po])
            for half in range(2):
                c = 2 * pr + half
                op("pe", lambda e: e.matmul(po[:, c * 64:(c + 1) * 64], lhsT=Sbt[c][:], rhs=qg[:, c * 64:(c + 1) * 64], start=False, stop=(half == 1)),
                   [Sbt[c], qg], [po])
        op("act", lambda e: e.activation(out=osb[:], in_=po[:], func=AF.Copy), [po], [osb])
        op("act", lambda e: e.activation(out=o2[:], in_=osb[:], func=AF.Square), [osb], [o2])
        p = banks.get()
        op("pe", lambda e: e.matmul(p[:], lhsT=cst[:, C_ONES, :], rhs=o2[:], start=True, stop=True), [cst, o2], [p])
        op("act", lambda e: e.activation(out=rs[:], in_=p[:], func=AF.Sqrt, bias=EPS, scale=1.0 / 128.0), [p], [rs])
        op("dve", lambda e: e.reciprocal(out=rs[:], in_=rs[:]), [rs], [rs])
        op("dve", lambda e: e.scalar_tensor_tensor(out=t1[:], in0=osb[:], scalar=pc[:, 8:9], in1=rs[:], op0=ALU.mult, op1=ALU.mult), [osb, pc, rs], [t1])
        obb = ob[blk % 2]
        op("pool", lambda e: e.tensor_tensor(out=obb[:], in0=t1[:], in1=sog[:], op=ALU.mult), [t1, sog], [obb])
        out_toks.append(k.dma("sp", yT[128:256, blk * 512:(blk + 1) * 512], obb[:], reads=[obb]))
    k.finish(out_toks)
    k.release(0)
    return nc, k


def phaseA0_inmaps(h, inp):
    maps = []
    consts = make_consts()
    w_in = inp["w_in_ab"][0]
    wa = np.ascontiguousarray(inp["w_ada"][0][:, 0:2048])
    ba = np.ascontiguousarray(inp["b_ada"][0][0:2048])
    for core in range(NCORES):
        b, hg = core // 4, core % 4
        ch = slice(hg * 128, (hg + 1) * 128)
        cols = np.concatenate([
            np.arange(hg * 128, (hg + 1) * 128),
            512 + np.arange(hg * 128, (hg + 1) * 128),
            1024 + np.arange(hg * 64, (hg + 1) * 64),
            1280 + np.arange(hg * 64, (hg + 1) * 64),
            1536 + np.arange(hg * 128, (hg + 1) * 128),
            2048 + np.arange(hg * 128, (hg + 1) * 128),
            2560 + np.arange(16),
        ])
        WA = np.zeros((128, 128), np.float32)
        WX = np.zeros((128, 128), np.float32)
        for j in range(2):
            WA[j * 64:(j + 1) * 64, j * 64:(j + 1) * 64] = inp["rg_wa"][0][hg * 2 + j]
            WX[j * 64:(j + 1) * 64, j * 64:(j + 1) * 64] = inp["rg_wx"][0][hg * 2 + j]
        pcol = np.zeros((128, 16), np.float32)
        pcol[:, 0:4] = inp["conv_a_w"][0][:, ch].T
        pcol[:, 4] = inp["conv_a_b"][0][ch]
        pcol[:, 5] = inp["rg_ba"][0][ch]
        pcol[:, 6] = inp["rg_bx"][0][ch]
        pcol[:, 7] = inp["rg_lam"][0][ch]
        pcol[:, 8] = inp["gla_norm"][0]
        pcol[0:64, 9] = inp["gla_bg2"][0][hg * 64:(hg + 1) * 64]
        maps.append({
            "h_in": np.ascontiguousarray(h[b]),
            "condT": np.ascontiguousarray(inp["c"][b].reshape(8, 128).T),
            "w_ada": wa, "b_ada": ba,
            "norm1": np.ascontiguousarray(inp["norm1"][0]),
            "w_in": np.ascontiguousarray(w_in[:, cols]),
            "WA": WA, "WX": WX,
            "wg2": np.ascontiguousarray(inp["gla_wg2"][0][:, hg * 64:(hg + 1) * 64]),
            "pcol": pcol, "consts": consts,
        })
    return maps


def assemble_yT_A0(results):
    yT = np.zeros((2, D, S), ml_dtypes.bfloat16)
    for core in range(NCORES):
        b, hg = core // 4, core % 4
        r = results[core]["yT"]
        yT[b, hg * 128:(hg + 1) * 128] = r[0:128]
        yT[b, 512 + hg * 128:512 + (hg + 1) * 128] = r[128:256]
    return yT


NCOL_A1 = 1028
import os as _os
SEQ_DEBUG = bool(_os.environ.get('SEQ_DEBUG'))


def build_phaseA1(nblk=16, stop=99):
    nc = bass.Bass("TRN2", target_bir_lowering=False)
    dt = nc.dram_tensor
    h_in = dt("h_in", [S, D], F32, kind="ExternalInput").ap()
    condT = dt("condT", [128, 8], F32, kind="ExternalInput").ap()
    w_ada = dt("w_ada", [D, 2048], F32, kind="ExternalInput").ap()
    b_ada = dt("b_ada", [2048], F32, kind="ExternalInput").ap()
    norm1 = dt("norm1", [D], F32, kind="ExternalInput").ap()
    w_in = dt("w_in", [D, NCOL_A1], F32, kind="ExternalInput").ap()
    pcold = dt("pcol", [128, 24], F32, kind="ExternalInput").ap()
    prmd = dt("prm", [128, 4], F32, kind="ExternalInput").ap()
    dnd = dt("dnorm", [128], F32, kind="ExternalInput").ap()
    alogd = dt("a_log", [128, 2], F32, kind="ExternalInput").ap()
    consts = dt("consts", [128, NCONST, 128], F32, kind="ExternalInput").ap()
    yT = dt("yT", [256, S], BF16, kind="ExternalOutput").ap()

    k = KB(nc)
    cst, cstb = load_consts(k, nc, consts)
    um = UMaker(k, nc, h_in, condT, w_ada, b_ada, norm1, cstb)
    win = k.sb([128, 8, NCOL_A1], BF16, "win")
    wv = w_in.rearrange("(k p) n -> p k n", p=128)
    for kk in range(8):
        k.dma("pool", win[:, kk, :], wv[:, kk, :], writes=[win])
    pc = k.sb([128, 24], F32, "pcol")
    k.dma("sp", pc[:], pcold, writes=[pc])
    prm = k.sb([128, 4], F32, "prm")
    k.dma("sp", prm[:], prmd, writes=[prm])
    dnb = k.sb([128, 128], F32, "dnb")
    k.dma("sp", dnb[:], dnd.partition_broadcast(128), writes=[dnb])
    alg = k.sb([128, 2], F32, "alg")
    k.dma("sp", alg[:], alogd, writes=[alg])
    k.op("act", lambda e: e.activation(out=alg[:], in_=alg[:], func=AF.Exp), reads=[alg], writes=[alg])
    k.op("dve", lambda e: e.tensor_scalar(out=prm[:, 2:4], in0=alg[:], scalar1=-1.0, scalar2=None, op0=ALU.mult), reads=[alg, prm], writes=[prm])

    banks = Banks(k, 6)
    ptr = k.ps([128, 2, 128], BF16, "ptr")
    uT = [k.sb([128, 8, 512], BF16, "uT%d" % i) for i in range(2)]
    cbuf = [[k.sb([128, 515], F32, "cbuf%d_%d" % (j, i)) for i in range(2)] for j in range(6)]
    for j in range(6):
        k.op("dve", lambda e: e.memset(cbuf[j][0][:, 0:3], 0.0), writes=[cbuf[j][0]])
    Sst = [k.sb([128, 128], F32, "Sst%d" % h) for h in range(2)]
    Sb = [k.sb([128, 128], BF16, "Sb%d" % h) for h in range(2)]
    for h in range(2):
        k.op("dve", lambda e: e.memset(Sst[h][:], 0.0), writes=[Sst[h]])
        k.op("dve", lambda e: e.memset(Sb[h][:], 0.0), writes=[Sb[h]])

    sb = k.sb
    sj = [sb([128, 512], F32, "sj%d" % j) for j in range(6)]
    sq = sb([128, 512], F32, "sq")
    rs = sb([128, 512], F32, "rs")
    nT = [sb([128, 512], BF16, "nT%d" % j) for j in range(4)]
    vTb = [sb([128, 512], BF16, "vTb%d" % h) for h in range(2)]
    sz = [sb([128, 256], F32, "sz%d" % t) for t in range(4)]
    g4 = sb([128, 4, 4], F32, "g4")
    beta = sb([128, 4, 2], F32, "beta")
    nbeta = sb([128, 4, 2], F32, "nbeta")
    gx = sb([128, 4, 2], F32, "gx")
    gg = sb([128, 4, 2], F32, "gg")
    TST = []
    for a in range(2):
        TST.append({"gch": sb([128, 4], F32, "gch%d" % a), "gcl": sb([128, 8], F32, "gcl%d" % a), "eg": sb([128, 2], F32, "eg%d" % a),
                    "ed": sb([128, 2], F32, "ed%d" % a), "be": sb([128, 2], F32, "be%d" % a), "egl": sb([128, 4], F32, "egl%d" % a)})
    GB = []
    for g_ in range(4):
        d_ = {"ps": banks.t[g_], "ptr": ptr}
        for nm in ("gm", "DTm", "Dm", "Tt", "U", "dg"):
            d_[nm] = sb([128, 128], F32, "%s%d" % (nm, g_))
        d_["AB"] = sb([128, 2, 128], F32, "AB%d" % g_)
        for nm in ("Ttb", "attT", "bv", "kbg", "kd", "WT", "qg", "vnew"):
            d_[nm] = sb([128, 128], BF16, "%s%d" % (nm, g_))
        GB.append(d_)
    o_tok = [sb([128, 4, 128], F32, "o_tok%d" % h) for h in range(2)]
    ss = sb([128, 4], F32, "oss")
    rstd = sb([128, 4], F32, "orstd")
    junk = sb([128, 128], F32, "ojunk")
    on = sb([128, 128], F32, "on")
    ytok = sb([128, 128], BF16, "ytok")
    yTs = [sb([128, 512], BF16, "yTs%d" % i) for i in range(2)]

    def op(e, fn, r, w):
        return k.op(e, fn, reads=r, writes=w)

    ident = cst[:, C_ID, :]
    TRI = cst[:, C_TRI, :]
    BLK = cst[:, C_BLK, :]
    SU = cst[:, C_SU, :]
    ONES = cst[:, C_ONES, :]
    UT64 = cst[:, C_UT64, :]
    out_toks = []
    cnt_y = 0
    for blk in range(nblk):
        u = uT[blk % 2]
        if stop <= -3:
            break
        um.block(blk, u)
        for j in range(6):
            if stop <= -2:
                break
            cb = cbuf[j][blk % 2]
            cbn = cbuf[j][(blk + 1) % 2]
            p = banks.get()
            for kk in range(8):
                k.op("pe", lambda e: e.matmul(p[:], lhsT=win[:, kk, j * 128:(j + 1) * 128], rhs=u[:, kk, :], start=(kk == 0), stop=(kk == 7)), reads=[win, u], writes=[p], fast=True)
            op("act", lambda e: e.activation(out=cb[:, 3:515], in_=p[:], func=AF.Copy), [p], [cb])
            op("act", lambda e: e.activation(out=sj[j][:], in_=cb[:, 3:515], func=AF.Copy, scale=pc[:, j * 4 + 3:j * 4 + 4]), [cb, pc], [sj[j]])
            for w in range(3):
                op("dve", lambda e: e.scalar_tensor_tensor(out=sj[j][:], in0=cb[:, w:w + 512], scalar=pc[:, j * 4 + w:j * 4 + w + 1], in1=sj[j][:],
                                                           op0=ALU.mult, op1=ALU.add), [cb, pc, sj[j]], [sj[j]])
            op("pool", lambda e: e.tensor_copy(out=cbn[:, 0:3], in_=cb[:, 512:515]), [cb], [cbn])
            op("act", lambda e: e.activation(out=sj[j][:], in_=sj[j][:], func=AF.Silu), [sj[j]], [sj[j]])
        if stop <= -1:
            break
        for j in range(4):
            op("act", lambda e: e.activation(out=sq[:], in_=sj[j][:], func=AF.Square), [sj[j]], [sq])
            p = banks.get()
            op("pe", lambda e: e.matmul(p[:], lhsT=ONES, rhs=sq[:], start=True, stop=True), [cst, sq], [p])
            op("act", lambda e: e.activation(out=rs[:], in_=p[:], func=AF.Sqrt, bias=EPS), [p], [rs])
            op("dve", lambda e: e.reciprocal(out=rs[:], in_=rs[:]), [rs], [rs])
            scl = 128.0 ** -0.5 if j < 2 else 1.0
            op("dve", lambda e: e.scalar_tensor_tensor(out=nT[j][:], in0=sj[j][:], scalar=scl, in1=rs[:], op0=ALU.mult, op1=ALU.mult), [sj[j], rs], [nT[j]])
        for h in range(2):
            op("act", lambda e: e.activation(out=vTb[h][:], in_=sj[4 + h][:], func=AF.Copy), [sj[4 + h]], [vTb[h]])
        if stop <= 0:
            break
        for ti in range(4):
            p = banks.get()
            for kk in range(8):
                op("pe", lambda e: e.matmul(p[:, 0:256], lhsT=u[:, kk, ti * 128:(ti + 1) * 128], rhs=win[:, kk, 768:1024], start=(kk == 0), stop=(kk == 7)),
                   [win, u], [p])
            for kk in range(8):
                op("pe", lambda e: e.matmul(p[:, 256:260], lhsT=u[:, kk, ti * 128:(ti + 1) * 128], rhs=win[:, kk, 1024:1028], start=(kk == 0), stop=(kk == 7)),
                   [win, u], [p])
            op("act", lambda e: e.activation(out=sz[ti][:], in_=p[:, 0:256], func=AF.Silu), [p], [sz[ti]])
            op("act", lambda e: e.activation(out=g4[:, ti, :], in_=p[:, 256:260], func=AF.Copy), [p], [g4])
        if stop <= 0.2:
            break
        op("act", lambda e: e.activation(out=beta[:], in_=g4[:, :, 0:2], func=AF.Sigmoid), [g4], [beta])
        op("dve", lambda e: e.tensor_scalar(out=nbeta[:], in0=beta[:], scalar1=-1.0, scalar2=None, op0=ALU.mult), [beta], [nbeta])
        if stop <= 0.4:
            break
        op("dve", lambda e: e.tensor_tensor(out=gx[:], in0=g4[:, :, 2:4], in1=_bc(prm[:, 0:2].unsqueeze(1), [128, 4, 2]), op=ALU.add), [g4, prm], [gx])
        if stop <= 0.6:
            break
        op("act", lambda e: e.activation(out=gx[:], in_=gx[:], func=AF.Exp), [gx], [gx])
        op("act", lambda e: e.activation(out=gx[:], in_=gx[:], func=AF.Ln, bias=1.0), [gx], [gx])
        if stop <= 0.8:
            break
        op("dve", lambda e: e.tensor_tensor(out=gg[:], in0=gx[:], in1=_bc(prm[:, 2:4].unsqueeze(1), [128, 4, 2]), op=ALU.mult), [gx, prm], [gg])

        if stop <= 1:
            break
        def tile_common(ti, st):
            gcl, eg, ed, egl, be, gch = st["gcl"], st["eg"], st["ed"], st["egl"], st["be"], st["gch"]
            op("dve", lambda e: e.tensor_tensor(out=gch[:].rearrange("p (a b) -> p a b", a=2), in0=_bc(gg[:, ti, :].unsqueeze(1), [128, 2, 2]),
                                                in1=_bc(cst[:, C_CH0, 0:2].unsqueeze(2), [128, 2, 2]), op=ALU.mult), [gg, cst], [gch])
            pg = banks.get()
            op("pe", lambda e: e.matmul(pg[:, 0:2], lhsT=TRI, rhs=gg[:, ti, :], start=True, stop=True), [cst, gg], [pg])
            op("pe", lambda e: e.matmul(pg[:, 2:4], lhsT=BLK, rhs=gg[:, ti, :], start=True, stop=True), [cst, gg], [pg])
            op("pe", lambda e: e.matmul(pg[:, 4:8], lhsT=ONES, rhs=gch[:], start=True, stop=True), [cst, gch], [pg])
            op("dve", lambda e: e.tensor_copy(out=gcl[:], in_=pg[:, 0:8]), [pg], [gcl])
            op("act", lambda e: e.activation(out=eg[:], in_=gcl[:, 0:2], func=AF.Exp), [gcl], [eg])
            op("dve", lambda e: e.tensor_tensor(out=ed[:], in0=gcl[:, 2:4], in1=gcl[:, 0:2], op=ALU.subtract), [gcl], [ed])
            op("act", lambda e: e.activation(out=ed[:], in_=ed[:], func=AF.Exp), [ed], [ed])
            op("act", lambda e: e.activation(out=egl[:], in_=gcl[:, 4:8], func=AF.Exp), [gcl], [egl])
            op("dve", lambda e: e.tensor_tensor(out=be[:], in0=beta[:, ti, :], in1=eg[:], op=ALU.mult), [beta, eg], [be])

        def prep(ti, h, st, B):
            tsl = slice(ti * 128, (ti + 1) * 128)
            qT = nT[h]
            kT = nT[2 + h]
            ps = B["ps"]
            gm, DTm, Dm, AB, Tt, Ttb, attT, bv, kbg, kd, U, WT, dg, qg = (B[n] for n in
                ("gm", "DTm", "Dm", "AB", "Tt", "Ttb", "attT", "bv", "kbg", "kd", "U", "WT", "dg", "qg"))
            eg, ed, be = st["eg"], st["ed"], st["be"]
            A_ = AB[:, 0, :]
            B_ = AB[:, 1, :]
            op("dve", lambda e: e.tensor_scalar(out=gm[:], in0=SU, scalar1=gg[:, ti, h:h + 1], scalar2=None, op0=ALU.mult), [cst, gg], [gm])
            op("pe", lambda e: e.matmul(ps[:, 0:128], lhsT=gm[:], rhs=TRI, start=True, stop=True), [gm, cst], [ps])
            op("pe", lambda e: e.matmul(ps[:, 128:256], lhsT=TRI, rhs=gm[:], start=True, stop=True), [gm, cst], [ps])
            yield
            op("act", lambda e: e.activation(out=DTm[:], in_=ps[:, 0:128], func=AF.Exp), [ps], [DTm])
            op("act", lambda e: e.activation(out=Dm[:], in_=ps[:, 128:256], func=AF.Exp), [ps], [Dm])
            op("pool", lambda e: e.tensor_tensor(out=DTm[:], in0=DTm[:], in1=UT64, op=ALU.mult), [DTm, cst], [DTm])
            op("pool", lambda e: e.tensor_tensor(out=Dm[:], in0=Dm[:], in1=SU, op=ALU.mult), [Dm, cst], [Dm])
            op("pe", lambda e: e.matmul(ps[:, 0:128], lhsT=kT[:, tsl], rhs=kT[:, tsl], start=True, stop=True), [kT], [ps])
            op("pe", lambda e: e.matmul(ps[:, 128:256], lhsT=kT[:, tsl], rhs=qT[:, tsl], start=True, stop=True), [kT, qT], [ps])
            yield
            op("dve", lambda e: e.scalar_tensor_tensor(out=A_, in0=ps[:, 0:128], scalar=nbeta[:, ti, h:h + 1], in1=Dm[:], op0=ALU.mult, op1=ALU.mult),
               [ps, nbeta, Dm], [AB])
            op("dve", lambda e: e.tensor_tensor(out=attT[:], in0=ps[:, 128:256], in1=DTm[:], op=ALU.mult), [ps, DTm], [attT])
            op("pe", lambda e: e.transpose(out=ps[:, 0:128], in_=A_, identity=ident), [AB, cst], [ps])
            yield
            op("act", lambda e: e.activation(out=B_, in_=ps[:, 0:128], func=AF.Copy), [ps], [AB])
            op("pool", lambda e: e.tensor_tensor(out=Tt[:], in0=B_, in1=ident, op=ALU.add), [AB, cst], [Tt])
            for lvl in range(1, 6):
                op("pe", lambda e: e.matmul(ps[:, 0:128], lhsT=B_, rhs=A_, start=True, stop=True), [AB], [ps])
                if lvl < 5:
                    op("pe", lambda e: e.matmul(ps[:, 128:256], lhsT=A_, rhs=B_, start=True, stop=True), [AB], [ps])
                yield
                if lvl < 5:
                    op("act", lambda e: e.activation(out=AB[:].rearrange("p a b -> p (a b)"), in_=ps[:, 0:256], func=AF.Copy), [ps], [AB])
                else:
                    op("act", lambda e: e.activation(out=A_, in_=ps[:, 0:128], func=AF.Copy), [ps], [AB])
                op("pe", lambda e: e.matmul(ps[:, 256:384], lhsT=A_, rhs=Tt[:], start=True, stop=True), [AB, Tt], [ps])
                yield
                op("dve", lambda e: e.tensor_tensor(out=Tt[:], in0=ps[:, 256:384], in1=Tt[:], op=ALU.add), [ps, Tt], [Tt])
            op("act", lambda e: e.activation(out=Ttb[:], in_=Tt[:], func=AF.Copy), [Tt], [Ttb])
            pt_ = B["ptr"]
            op("pe", lambda e: e.transpose(out=pt_[:, 0, :], in_=kT[:, tsl], identity=cstb[:, C_ID, :]), [kT, cstb], [pt_])
            op("pe", lambda e: e.transpose(out=pt_[:, 1, :], in_=vTb[h][:, tsl], identity=cstb[:, C_ID, :]), [vTb[h], cstb], [pt_])
            op("dve", lambda e: e.tensor_scalar(out=kbg[:], in0=pt_[:, 0, :], scalar1=be[:, h:h + 1], scalar2=None, op0=ALU.mult), [pt_, be], [kbg])
            op("dve", lambda e: e.tensor_scalar(out=kd[:], in0=pt_[:, 0, :], scalar1=ed[:, h:h + 1], scalar2=None, op0=ALU.mult), [pt_, ed], [kd])
            op("dve", lambda e: e.tensor_scalar(out=bv[:], in0=pt_[:, 1, :], scalar1=beta[:, ti, h:h + 1], scalar2=None, op0=ALU.mult), [pt_, beta], [bv])
            op("dve", lambda e: e.tensor_scalar(out=dg[:], in0=ident, scalar1=eg[:, h:h + 1], scalar2=None, op0=ALU.mult), [cst, eg], [dg])
            op("pe", lambda e: e.matmul(ps[:, 0:128], lhsT=Ttb[:], rhs=bv[:], start=True, stop=True), [Ttb, bv], [ps])
            op("pe", lambda e: e.matmul(ps[:, 128:256], lhsT=kbg[:], rhs=Ttb[:], start=True, stop=True), [kbg, Ttb], [ps])
            op("pe", lambda e: e.matmul(ps[:, 256:384], lhsT=ONES, rhs=dg[:], start=True, stop=True), [cst, dg], [ps])
            yield
            op("dve", lambda e: e.tensor_copy(out=U[:], in_=ps[:, 0:128]), [ps], [U])
            op("dve", lambda e: e.tensor_copy(out=WT[:], in_=ps[:, 128:256]), [ps], [WT])
            op("dve", lambda e: e.tensor_tensor(out=qg[:], in0=ps[:, 256:384], in1=qT[:, tsl], op=ALU.mult), [ps, qT], [qg])

        def chain(ti, h, st, B, half):
            rows = slice(half * 64, (half + 1) * 64)
            egl = st["egl"]
            U, WT, qg, attT, kd, vnew = B["U"], B["WT"], B["qg"], B["attT"], B["kd"], B["vnew"]
            pw = banks.get()
            op("pe", lambda e: e.matmul(pw[rows, 0:128], lhsT=WT[:, rows], rhs=Sb[h][:], start=True, stop=True), [WT, Sb[h]], [pw])
            yield
            op("dve", lambda e: e.tensor_tensor(out=vnew[rows, :], in0=U[rows, :], in1=pw[rows, 0:128], op=ALU.subtract), [U, pw], [vnew])
            po = banks.get()
            op("pe", lambda e: e.matmul(po[rows, 0:128], lhsT=qg[:, rows], rhs=Sb[h][:], start=True, stop=False), [qg, Sb[h]], [po])
            op("pe", lambda e: e.matmul(po[rows, 0:128], lhsT=attT[rows, rows], rhs=vnew[rows, :], start=False, stop=True), [attT, vnew], [po])
            pk = banks.get()
            op("pe", lambda e: e.matmul(pk[:, 0:128], lhsT=kd[rows, :], rhs=vnew[rows, :], start=True, stop=True), [kd, vnew], [pk])
            yield
            op("dve", lambda e: e.scalar_tensor_tensor(out=Sst[h][:], in0=Sst[h][:], scalar=egl[:, half * 2 + h:half * 2 + h + 1], in1=pk[:, 0:128],
                                                       op0=ALU.mult, op1=ALU.add), [Sst[h], egl, pk], [Sst[h]])
            op("act", lambda e: e.activation(out=Sb[h][:], in_=Sst[h][:], func=AF.Copy), [Sst[h]], [Sb[h]])
            op("act", lambda e: e.activation(out=o_tok[h][rows, ti, :], in_=po[rows, 0:128], func=AF.Copy), [po], [o_tok[h]])

        def run_interleaved(gens):
            gens = list(gens)
            if SEQ_DEBUG:
                for g in gens:
                    for _ in g:
                        pass
                return
            while gens:
                nxt = []
                for g in gens:
                    try:
                        next(g)
                        nxt.append(g)
                    except StopIteration:
                        pass
                gens = nxt

        for tp in range(2):
            tis = (2 * tp, 2 * tp + 1)
            for a, ti in enumerate(tis):
                tile_common(ti, TST[a])
            run_interleaved([prep(ti, h, TST[a], GB[a * 2 + h]) for a, ti in enumerate(tis) for h in range(2)])
            for a, ti in enumerate(tis):
                for half in range(2):
                    run_interleaved([chain(ti, h, TST[a], GB[a * 2 + h], half) for h in range(2)])

        for h in range(2):
            if stop <= 5:
                break
            for ti in range(4):
                op("act", lambda e: e.activation(out=junk[:], in_=o_tok[h][:, ti, :], func=AF.Square, accum_out=ss[:, ti:ti + 1]), [o_tok[h]], [junk, ss])
            rstd_from_ss(k, ss, rstd, 4, 1.0 / 128.0)
            ys = yTs[cnt_y % 2]
            cnt_y += 1
            for ti in range(4):
                op("dve", lambda e: e.scalar_tensor_tensor(out=on[:], in0=o_tok[h][:, ti, :], scalar=rstd[:, ti:ti + 1], in1=dnb[:], op0=ALU.mult, op1=ALU.mult),
                   [o_tok[h], rstd, dnb], [on])
                op("pool", lambda e: e.tensor_tensor(out=ytok[:], in0=on[:], in1=sz[ti][:, h * 128:(h + 1) * 128], op=ALU.mult), [on, sz[ti]], [ytok])
                op("pe", lambda e: e.transpose(out=ptr[:, 0, :], in_=ytok[:], identity=cstb[:, C_ID, :]), [ytok, cstb], [ptr])
                op("act", lambda e: e.activation(out=ys[:, ti * 128:(ti + 1) * 128], in_=ptr[:, 0, :], func=AF.Copy), [ptr], [ys])
            out_toks.append(k.dma("sp", yT[h * 128:(h + 1) * 128, blk * 512:(blk + 1) * 512], ys[:], reads=[ys]))
    k.finish(out_toks)
    k.release(0)
    return nc, k


def phaseA1_inmaps(h, inp):
    maps = []
    consts = make_consts()
    w_in = inp["w_in_c"][0]
    wa = np.ascontiguousarray(inp["w_ada"][1][:, 0:2048])
    ba = np.ascontiguousarray(inp["b_ada"][1][0:2048])
    cw = inp["conv_c_w"][0]
    for core in range(NCORES):
        b, hg = core // 4, core % 4
        hs = [2 * hg, 2 * hg + 1]
        cols = []
        for base in (0, 1024, 2048):
            for hh in hs:
                cols.append(base + np.arange(hh * 128, (hh + 1) * 128))
        for hh in hs:
            cols.append(3072 + np.arange(hh * 128, (hh + 1) * 128))
        cols.append(np.array([4096 + hs[0], 4096 + hs[1], 4104 + hs[0], 4104 + hs[1]]))
        cols = np.concatenate(cols)
        pcol = np.zeros((128, 6, 4), np.float32)
        j = 0
        for base in (0, 1024, 2048):
            for hh in hs:
                pcol[:, j, :] = cw[:, base + hh * 128:base + (hh + 1) * 128].T
                j += 1
        prm = np.zeros((128, 4), np.float32)
        prm[:, 0] = inp["dn_dt_bias"][0][hs[0]]
        prm[:, 1] = inp["dn_dt_bias"][0][hs[1]]
        maps.append({
            "h_in": np.ascontiguousarray(h[b]),
            "condT": np.ascontiguousarray(inp["c"][b].reshape(8, 128).T),
            "w_ada": wa, "b_ada": ba,
            "norm1": np.ascontiguousarray(inp["norm1"][1]),
            "w_in": np.ascontiguousarray(w_in[:, cols]),
            "pcol": np.ascontiguousarray(pcol.reshape(128, 24)),
            "prm": prm,
            "a_log": np.ascontiguousarray(np.tile(inp["dn_a_log"][0][hs][None, :], (128, 1))),
            "dnorm": np.ascontiguousarray(inp["dn_norm"][0]),
            "consts": consts,
        })
    return maps


def assemble_yT_A1(results):
    yT = np.zeros((2, D, S), ml_dtypes.bfloat16)
    for core in range(NCORES):
        b, hg = core // 4, core % 4
        yT[b, hg * 256:(hg + 1) * 256] = results[core]["yT"]
    return yT


_PROGS = {}


def _prog(name):
    if name not in _PROGS:
        if name == "A0":
            _PROGS[name] = build_phaseA0()[0]
        elif name == "A1":
            _PROGS[name] = build_phaseA1()[0]
        elif name == "B0":
            _PROGS[name] = build_phaseB(False)[0]
        else:
            _PROGS[name] = build_phaseB(True)[0]
    return _PROGS[name]


def _gather_B(results):
    return np.stack([np.concatenate([results[b * 4 + q]["out"] for q in range(4)], 0) for b in range(2)])


def kernel(**inputs):
    inp = {k_: np.ascontiguousarray(np.asarray(v, dtype=np.float32)) for k_, v in inputs.items()}
    cores = list(range(NCORES))
    x = inp["x"]
    r = run_bass_kernel_spmd(_prog("A0"), phaseA0_inmaps(x, inp), core_ids=cores)
    yT0 = assemble_yT_A0(r.results)
    r = run_bass_kernel_spmd(_prog("B0"), phaseB_inmaps(0, x, yT0, inp, False), core_ids=cores)
    h0 = _gather_B(r.results)
    r = run_bass_kernel_spmd(_prog("A1"), phaseA1_inmaps(h0, inp), core_ids=cores)
    yT1 = assemble_yT_A1(r.results)
    r = run_bass_kernel_spmd(_prog("B1"), phaseB_inmaps(1, h0, yT1, inp, True), core_ids=cores)
    return _gather_B(r.results).astype(np.float32)
```

```python
import numpy as np
import ml_dtypes
import concourse.bass as bass
import concourse.mybir as mybir
from concourse.bass_utils import run_bass_kernel_spmd

F32 = mybir.dt.float32
BF16 = mybir.dt.bfloat16
AF = mybir.ActivationFunctionType
ALU = mybir.AluOpType
AX = mybir.AxisListType

D = 1024
S = 8192
EPS = 1e-6
NCORES = 8


class T:
    __slots__ = ("h", "w", "r", "name")

    def __init__(self, h, name=""):
        self.h = h
        self.w = None
        self.r = {}
        self.name = name

    def __getitem__(self, idx):
        return self.h[idx]


class KB:
    NDMA_SEM = 6

    def __init__(self, nc):
        self.nc = nc
        self.eng = {"pe": nc.tensor, "act": nc.scalar, "dve": nc.vector, "pool": nc.gpsimd, "sp": nc.sync}
        self.csem = {e: nc.alloc_semaphore("cs_" + e) for e in ("pe", "act", "dve", "pool")}
        self.cnt = {e: 0 for e in self.csem}
        self.pending = {e: False for e in self.csem}
        self.dsem = {}
        self.dcnt = {}
        for q in ("sp", "pool", "act"):
            self.dsem[q] = [nc.alloc_semaphore("ds_%s%d" % (q, i)) for i in range(self.NDMA_SEM)]
            self.dcnt[q] = 0
        self.seen = {e: {} for e in self.eng}
        self.fast_pe = False
        self.ninst = 0
        self.stack = []
        self.tiles = []
        self.freed = {}
        self.uid = 0

    def sb(self, shape, dt=F32, name=None):
        self.uid += 1
        nm = "%s_%d" % (name or "t", self.uid)
        g = self.nc.sbuf_tensor(nm, list(shape), dt)
        h = g.__enter__()
        self.stack.append(g)
        t = T(h, nm)
        t.r = dict(self.freed)
        self.tiles.append(t)
        return t

    def ps(self, shape, dt=F32, name=None):
        self.uid += 1
        nm = "%s_%d" % (name or "p", self.uid)
        g = self.nc.psum_tensor(nm, list(shape), dt)
        h = g.__enter__()
        self.stack.append(g)
        t = T(h, nm)
        t.r = dict(self.freed)
        self.tiles.append(t)
        return t

    def mark(self):
        return len(self.stack)

    def release(self, mark):
        while len(self.stack) > mark:
            g = self.stack.pop()
            t = self.tiles.pop()
            toks = list(t.r.values()) + ([t.w] if t.w is not None else [])
            for tok in toks:
                o = self.freed.get(tok[0])
                if o is None or o[2] < tok[2]:
                    self.freed[tok[0]] = tok
            g.__exit__(None, None, None)

    def _deps(self, e, reads, writes):
        need = {}

        def add(tok):
            if tok is None:
                return
            key, sem, val = tok
            if key == "pe" and e == "pe" and self.fast_pe:
                return
            if self.seen[e].get(key, 0) >= val:
                return
            if key not in need or need[key][1] < val:
                need[key] = (sem, val)

        for t in reads:
            add(t.w)
        for t in writes:
            add(t.w)
            for tok in t.r.values():
                add(tok)
        for key, (sem, val) in need.items():
            self.eng[e].wait_ge(sem, val)
            self.seen[e][key] = val

    def _commit(self, tok, reads, writes):
        key = tok[0]
        for t in reads:
            o = t.r.get(key)
            if o is None or o[2] < tok[2]:
                t.r[key] = tok
        for t in writes:
            t.w = tok
            t.r = {}

    def op(self, e, fn, reads=(), writes=(), inc=True, fast=False):
        inc = True
        self.fast_pe = fast
        self._deps(e, reads, writes)
        self.fast_pe = False
        ins = fn(self.eng[e])
        if inc:
            self.cnt[e] += 1
            ins.then_inc(self.csem[e], 1)
            tok = (e, self.csem[e], self.cnt[e])
            self.pending[e] = False
        else:
            tok = (e, self.csem[e], self.cnt[e] + 1)
            self.pending[e] = True
        self._commit(tok, reads, writes)
        self.ninst += 1
        return tok

    def dma(self, q, out, in_, reads=(), writes=(), **kw):
        i = self.dcnt[q]
        self.dcnt[q] += 1
        slot = i % self.NDMA_SEM
        rnd = i // self.NDMA_SEM
        sem = self.dsem[q][slot]
        key = ("d", q, slot)
        if rnd > 0 and self.seen[q].get(key, 0) < 16 * rnd:
            self.eng[q].wait_ge(sem, 16 * rnd)
            self.seen[q][key] = 16 * rnd
        self._deps(q, reads, writes)
        ins = self.eng[q].dma_start(out=out, in_=in_, **kw)
        ins.then_inc(sem, 16)
        tok = (key, sem, 16 * (rnd + 1))
        self._commit(tok, reads, writes)
        self.ninst += 1
        return tok

    def finish(self, toks):
        for e in self.pending:
            assert not self.pending[e], "engine %s ends with a non-incrementing instruction" % e
        toks = list(toks)
        for e in self.csem:
            if self.cnt[e] > 0:
                toks.append((e, self.csem[e], self.cnt[e]))
        for q in self.dsem:
            n = self.dcnt[q]
            for slot in range(self.NDMA_SEM):
                if n > slot:
                    rounds = (n - slot + self.NDMA_SEM - 1) // self.NDMA_SEM
                    toks.append((("d", q, slot), self.dsem[q][slot], 16 * rounds))
        for tok in toks:
            key, sem, val = tok
            if self.seen["sp"].get(key, 0) < val:
                self.eng["sp"].wait_ge(sem, val)
                self.seen["sp"][key] = val


def _bc(ap, shape):
    return ap.to_broadcast(list(shape))


def load_consts(k, nc, consts_ap):
    c = k.sb([128, NCONST, 128], F32, "consts")
    k.dma("sp", c[:], consts_ap, writes=[c])
    cb = k.sb([128, NCONST, 128], BF16, "consts_bf")
    k.op("dve", lambda e: e.tensor_copy(out=cb[:], in_=c[:]), reads=[c], writes=[cb])
    return c, cb


NCONST = 10
C_ID = 0
C_TRI = 1
C_SU = 2
C_BLK = 3
C_M16 = 4
C_MC1 = 5
C_MC2 = 6
C_ONES = 7
C_UT64 = 8
C_CH0 = 9


def make_consts():
    p = np.arange(128)
    i = p[:, None]
    j = p[None, :]
    c = np.zeros((128, NCONST, 128), np.float32)
    same64 = (i // 64) == (j // 64)
    same32 = (i // 32) == (j // 32)
    same16 = (i // 16) == (j // 16)
    c[:, C_ID] = (i == j)
    c[:, C_TRI] = same64 & (i <= j)
    c[:, C_SU] = same64 & (j < i)
    c[:, C_BLK] = same64
    c[:, C_M16] = same16 & (j < i)
    c[:, C_MC1] = same32 & (~same16) & (j < i)
    c[:, C_MC2] = same64 & (~same32) & (j < i)
    c[:, C_ONES] = 1.0
    c[:, C_UT64] = same64 & (i <= j)
    c[:, C_CH0, 0] = (p < 64)
    c[:, C_CH0, 1] = (p >= 64)
    return c


def compute_mod(k, nc, condT_ap, w_ada_ap, b_ada_ap, ncols, outs, ps_pool):
    m0 = k.mark()
    ct = k.sb([128, 8], F32, "ct")
    k.dma("sp", ct[:], condT_ap, writes=[ct])
    sg = k.sb([128, 8], F32, "sg")
    k.op("act", lambda e: e.activation(out=sg[:], in_=ct[:], func=AF.Sigmoid), reads=[ct], writes=[sg])
    cond = k.sb([128, 8], F32, "cond")
    k.op("dve", lambda e: e.tensor_tensor(out=cond[:], in0=ct[:], in1=sg[:], op=ALU.mult), reads=[ct, sg], writes=[cond])
    cbc = k.sb([128, 8, 128], BF16, "cond_bc")
    k.op("dve", lambda e: e.tensor_copy(out=cbc[:], in_=_bc(cond[:].unsqueeze(2), [128, 8, 128])), reads=[cond], writes=[cbc])
    wv = w_ada_ap.rearrange("(k p) n -> p k n", p=128)
    nch = ncols // 512
    wbuf = [k.sb([128, 8, 512], BF16, "wada%d" % i) for i in range(2)]
    bbuf = [k.sb([128, 512], F32, "bada%d" % i) for i in range(2)]
    for j in range(nch):
        wb = wbuf[j % 2]
        bb = bbuf[j % 2]
        k.dma("pool", wb[:], wv[:, :, j * 512:(j + 1) * 512], writes=[wb])
        k.dma("sp", bb[:], b_ada_ap[j * 512:(j + 1) * 512].partition_broadcast(128), writes=[bb])
        pt = ps_pool[j % len(ps_pool)]
        for kk in range(8):
            k.op("pe", lambda e, kk=kk: e.matmul(pt[:, 0:512], lhsT=cbc[:, kk, :], rhs=wb[:, kk, :], start=(kk == 0), stop=(kk == 7)),
                 reads=[cbc, wb], writes=[pt], fast=True)
        o = outs[j // 2]
        c0 = (j % 2) * 512
        k.op("dve", lambda e: e.tensor_tensor(out=o[:, c0:c0 + 512], in0=pt[:, 0:512], in1=bb[:], op=ALU.add),
             reads=[pt, bb], writes=[o])
    k.release(m0)


def rstd_from_ss(k, ss, rstd, n, scale):
    k.op("dve", lambda e: e.tensor_scalar(out=rstd[:, 0:n], in0=ss[:, 0:n], scalar1=scale, scalar2=EPS, op0=ALU.mult, op1=ALU.add),
         reads=[ss], writes=[rstd])
    k.op("act", lambda e: e.activation(out=rstd[:, 0:n], in_=rstd[:, 0:n], func=AF.Sqrt), reads=[rstd], writes=[rstd])
    k.op("dve", lambda e: e.reciprocal(out=rstd[:, 0:n], in_=rstd[:, 0:n]), reads=[rstd], writes=[rstd])


NTB = 2048
NE = 32


def build_phaseB(final, upto=99, ne=NE):
    nc = bass.Bass("TRN2", target_bir_lowering=False)
    dt = nc.dram_tensor
    h_in = dt("h_in", [NTB, D], F32, kind="ExternalInput").ap()
    yT = dt("yT", [D, NTB], BF16, kind="ExternalInput").ap()
    w_out = dt("w_out", [D, D], F32, kind="ExternalInput").ap()
    condT = dt("condT", [128, 8], F32, kind="ExternalInput").ap()
    w_ada = dt("w_ada", [D, 4096], F32, kind="ExternalInput").ap()
    b_ada = dt("b_ada", [4096], F32, kind="ExternalInput").ap()
    norm2 = dt("norm2", [D], F32, kind="ExternalInput").ap()
    w_r = dt("w_r", [D, 36], F32, kind="ExternalInput").ap()
    b_r = dt("b_r", [36], F32, kind="ExternalInput").ap()
    if upto >= 5:
        w1 = dt("w1", [ne, D, 512], F32, kind="ExternalInput").ap()
        w3 = dt("w3", [ne, D, 512], F32, kind="ExternalInput").ap()
        w2 = dt("w2", [ne, 512, D], F32, kind="ExternalInput").ap()
    fnorm = dt("fnorm", [D], F32, kind="ExternalInput").ap()
    consts = dt("consts", [128, NCONST, 128], F32, kind="ExternalInput").ap()
    out = dt("out", [NTB, D], F32, kind="ExternalOutput").ap()

    k = KB(nc)
    NT = NTB // 128
    cst, cstb = load_consts(k, nc, consts)
    ident = cst

    hres = [k.sb([128, D], F32, "hres%d" % t) for t in range(NT)]
    h_v = h_in.rearrange("(t p) d -> t p d", p=128)
    for t in range(NT):
        k.dma("sp", hres[t][:], h_v[t], writes=[hres[t]])
    gt2 = k.sb([128, D], F32, "gt2")
    u2T = k.sb([128, 8, NTB], BF16, "u2T")
    logits = k.sb([128, NT, 36], F32, "logits")
    Wd = k.sb([128, NT, NE], F32, "Wd")
    m_mod = k.mark()
    gt1 = k.sb([128, D], F32, "gt1")
    sh2 = k.sb([128, D], F32, "sh2")
    sc2 = k.sb([128, D], F32, "sc2")

    if upto < 1:
        return _finB(k, out, hres, NT)
    m1 = k.mark()
    pp = [k.ps([128, 512], F32, "modps%d" % i) for i in range(2)]
    compute_mod(k, nc, condT, w_ada, b_ada, 4096, [gt1, sh2, sc2, gt2], pp)
    k.release(m1)
    g2 = k.sb([128, D], F32, "g2")
    k.dma("sp", g2[:], norm2.partition_broadcast(128), writes=[g2])
    k.op("dve", lambda e: e.scalar_tensor_tensor(out=g2[:], in0=sc2[:], scalar=1.0, in1=g2[:], op0=ALU.add, op1=ALU.mult),
         reads=[sc2, g2], writes=[g2])

    if upto < 2:
        return _finB(k, out, hres, NT)
    m2 = k.mark()
    yTs = k.sb([128, 8, NTB], BF16, "yTs")
    yv = yT.rearrange("(k p) t -> p k t", p=128)
    for kk in range(8):
        k.dma("sp", yTs[:, kk, :], yv[:, kk, :], writes=[yTs])
    wo = k.sb([128, 8, D], BF16, "wo")
    wov = w_out.rearrange("(k p) n -> p k n", p=128)
    for kk in range(0, 8, 2):
        k.dma("pool", wo[:, kk:kk + 2, :], wov[:, kk:kk + 2, :], writes=[wo])
    k.op("pool", lambda e: e.tensor_tensor(out=wo[:], in0=wo[:], in1=_bc(gt1[:].unsqueeze(1), [128, 8, D]), op=ALU.mult),
         reads=[wo, gt1], writes=[wo])
    psy = [k.ps([128, D], F32, "psy%d" % i) for i in range(2)]
    for t in range(NT):
        p = psy[t % 2]
        for half in range(2):
            for kk in range(8):
                k.op("pe", lambda e, kk=kk, half=half: e.matmul(p[:, half * 512:(half + 1) * 512], lhsT=yTs[:, kk, t * 128:(t + 1) * 128],
                                                                 rhs=wo[:, kk, half * 512:(half + 1) * 512], start=(kk == 0), stop=(kk == 7)),
                     reads=[yTs, wo], writes=[p], fast=True)
        k.op("dve", lambda e: e.tensor_tensor(out=hres[t][:], in0=p[:], in1=hres[t][:], op=ALU.add), reads=[p, hres[t]], writes=[hres[t]])
    k.release(m2)

    if upto < 3:
        return _finB(k, out, hres, NT)
    m3 = k.mark()
    ss = k.sb([128, NT], F32, "ss")
    rstd = k.sb([128, NT], F32, "rstd")
    junk = [k.sb([128, D], BF16, "junk%d" % i) for i in range(2)]
    for t in range(NT):
        jk = junk[t % 2]
        k.op("act", lambda e: e.activation(out=jk[:], in_=hres[t][:], func=AF.Square, accum_out=ss[:, t:t + 1]),
             reads=[hres[t]], writes=[jk, ss])
    rstd_from_ss(k, ss, rstd, NT, 1.0 / D)
    wr = k.sb([128, 8, 36], F32, "wr")
    k.dma("sp", wr[:], w_r.rearrange("(k p) n -> p k n", p=128), writes=[wr])
    brb = k.sb([128, 36], F32, "brb")
    k.dma("sp", brb[:], b_r.partition_broadcast(128), writes=[brb])
    t1b = [k.sb([128, D], F32, "t1b%d" % i) for i in range(2)]
    u32 = [k.sb([128, D], F32, "u32_%d" % i) for i in range(2)]
    uT32 = [k.sb([128, 8, 128], F32, "uT32_%d" % i) for i in range(2)]
    pst = [k.ps([128, 8, 128], F32, "pst%d" % i) for i in range(2)]
    psr = [k.ps([128, 36], F32, "psr%d" % i) for i in range(2)]
    for t in range(NT):
        a = t1b[t % 2]
        u = u32[t % 2]
        ut = uT32[t % 2]
        pt = pst[t % 2]
        pr = psr[t % 2]
        k.op("dve", lambda e: e.scalar_tensor_tensor(out=a[:], in0=hres[t][:], scalar=rstd[:, t:t + 1], in1=g2[:], op0=ALU.mult, op1=ALU.mult),
             reads=[hres[t], rstd, g2], writes=[a])
        k.op("pool", lambda e: e.tensor_tensor(out=u[:], in0=a[:], in1=sh2[:], op=ALU.add), reads=[a, sh2], writes=[u])
        for kk in range(8):
            k.op("pe", lambda e, kk=kk: e.transpose(out=pt[:, kk, :], in_=u[:, kk * 128:(kk + 1) * 128], identity=cst[:, C_ID, :]),
                 reads=[u, cst], writes=[pt], fast=True)
        k.op("act", lambda e: e.activation(out=ut[:], in_=pt[:], func=AF.Copy), reads=[pt], writes=[ut])
        k.op("pool", lambda e: e.tensor_copy(out=u2T[:, :, t * 128:(t + 1) * 128], in_=ut[:]), reads=[ut], writes=[u2T])
        for kk in range(8):
            k.op("pe", lambda e, kk=kk: e.matmul(pr[:], lhsT=ut[:, kk, :], rhs=wr[:, kk, :], start=(kk == 0), stop=(kk == 7)),
                 reads=[ut, wr], writes=[pr], fast=True)
        k.op("dve", lambda e: e.tensor_tensor(out=logits[:, t, :], in0=pr[:], in1=brb[:], op=ALU.add), reads=[pr, brb], writes=[logits])
    k.release(m3)

    if upto < 4:
        return _finB(k, out, hres, NT)
    m4 = k.mark()
    BIG = 1.0e30

    def dve(fn, reads, writes):
        k.op("dve", fn, reads=reads, writes=writes)

    lg = logits[:, :, 0:4]
    le = logits[:, :, 4:36]
    gmax = k.sb([128, NT], F32, "gmax")
    dve(lambda e: e.tensor_reduce(out=gmax[:], in_=lg, axis=AX.X, op=ALU.max), [logits], [gmax])
    eg = k.sb([128, NT, 4], F32, "eg")
    dve(lambda e: e.tensor_tensor(out=eg[:], in0=lg, in1=_bc(gmax[:].unsqueeze(2), [128, NT, 4]), op=ALU.subtract), [logits, gmax], [eg])
    k.op("act", lambda e: e.activation(out=eg[:], in_=eg[:], func=AF.Exp), reads=[eg], writes=[eg])
    gsum = k.sb([128, NT], F32, "gsum")
    dve(lambda e: e.tensor_reduce(out=gsum[:], in_=eg[:], axis=AX.X, op=ALU.add), [eg], [gsum])
    pgt = k.sb([128, NT], F32, "pgt")
    dve(lambda e: e.reciprocal(out=pgt[:], in_=gsum[:]), [gsum], [pgt])
    pen = k.sb([128, NT, 4], F32, "pen")
    dve(lambda e: e.tensor_tensor(out=pen[:], in0=lg, in1=_bc(gmax[:].unsqueeze(2), [128, NT, 4]), op=ALU.is_equal), [logits, gmax], [pen])
    dve(lambda e: e.tensor_scalar(out=pen[:], in0=pen[:], scalar1=1.0, scalar2=BIG, op0=ALU.subtract, op1=ALU.mult), [pen], [pen])
    lem = k.sb([128, NT, NE], F32, "lem")
    dve(lambda e: e.tensor_tensor(out=lem[:].rearrange("p t (g x) -> p t g x", g=4), in0=le.rearrange("p t (g x) -> p t g x", g=4),
                                  in1=_bc(pen[:].unsqueeze(3), [128, NT, 4, 8]), op=ALU.add), [logits, pen], [lem])
    mx1 = k.sb([128, NT], F32, "mx1")
    dve(lambda e: e.tensor_reduce(out=mx1[:], in_=lem[:], axis=AX.X, op=ALU.max), [lem], [mx1])
    oh1 = k.sb([128, NT, NE], F32, "oh1")
    dve(lambda e: e.tensor_tensor(out=oh1[:], in0=lem[:], in1=_bc(mx1[:].unsqueeze(2), [128, NT, NE]), op=ALU.is_equal), [lem, mx1], [oh1])
    lem2 = k.sb([128, NT, NE], F32, "lem2")
    dve(lambda e: e.scalar_tensor_tensor(out=lem2[:], in0=oh1[:], scalar=-BIG, in1=lem[:], op0=ALU.mult, op1=ALU.add), [oh1, lem], [lem2])
    mx2 = k.sb([128, NT], F32, "mx2")
    dve(lambda e: e.tensor_reduce(out=mx2[:], in_=lem2[:], axis=AX.X, op=ALU.max), [lem2], [mx2])
    oh2 = k.sb([128, NT, NE], F32, "oh2")
    dve(lambda e: e.tensor_tensor(out=oh2[:], in0=lem2[:], in1=_bc(mx2[:].unsqueeze(2), [128, NT, NE]), op=ALU.is_equal), [lem2, mx2], [oh2])
    rr = k.sb([128, NT], F32, "rr")
    dve(lambda e: e.tensor_tensor(out=rr[:], in0=mx2[:], in1=mx1[:], op=ALU.subtract), [mx2, mx1], [rr])
    k.op("act", lambda e: e.activation(out=rr[:], in_=rr[:], func=AF.Exp), reads=[rr], writes=[rr])
    den = k.sb([128, NT], F32, "den")
    dve(lambda e: e.tensor_scalar(out=den[:], in0=rr[:], scalar1=1.0, scalar2=None, op0=ALU.add), [rr], [den])
    dve(lambda e: e.reciprocal(out=den[:], in_=den[:]), [den], [den])
    wt1 = k.sb([128, NT], F32, "wt1")
    dve(lambda e: e.tensor_tensor(out=wt1[:], in0=pgt[:], in1=den[:], op=ALU.mult), [pgt, den], [wt1])
    wt2 = k.sb([128, NT], F32, "wt2")
    dve(lambda e: e.tensor_tensor(out=wt2[:], in0=wt1[:], in1=rr[:], op=ALU.mult), [wt1, rr], [wt2])
    dve(lambda e: e.tensor_tensor(out=Wd[:], in0=oh1[:], in1=_bc(wt1[:].unsqueeze(2), [128, NT, NE]), op=ALU.mult), [oh1, wt1], [Wd])
    dve(lambda e: e.tensor_tensor(out=oh2[:], in0=oh2[:], in1=_bc(wt2[:].unsqueeze(2), [128, NT, NE]), op=ALU.mult), [oh2, wt2], [oh2])
    dve(lambda e: e.tensor_tensor(out=Wd[:], in0=Wd[:], in1=oh2[:], op=ALU.add), [Wd, oh2], [Wd])
    k.release(m4)

    if upto < 5:
        return _finB(k, out, hres, NT)
    k.release(m_mod)
    m5 = k.mark()
    w1b = [k.sb([128, 8, 512], BF16, "w1b%d" % i) for i in range(2)]
    w3b = [k.sb([128, 8, 512], BF16, "w3b%d" % i) for i in range(2)]
    w2b = [k.sb([128, 4, D], BF16, "w2b%d" % i) for i in range(2)]
    stg = [k.sb([128, 4096], F32, "stg%d" % i) for i in range(2)]
    actT = [[k.sb([128, 512], BF16, "actT%d_%d" % (i, f)) for f in range(4)] for i in range(2)]
    slb = [k.sb([128, 512], F32, "slb%d" % i) for i in range(2)]
    ps1 = [k.ps([128, 512], F32, "ps1_%d" % i) for i in range(2)]
    ps3 = [k.ps([128, 512], F32, "ps3_%d" % i) for i in range(2)]
    psy = [k.ps([128, D], F32, "psye%d" % i) for i in range(2)]
    cnt_f = 0
    cnt_y = 0
    for ex in range(ne):
        b = ex % 2
        s1 = stg[(3 * ex) % 2]
        k.dma("sp", s1[:].rearrange("p (k f) -> p k f", k=8), w1[ex].rearrange("(k p) f -> p k f", p=128), writes=[s1])
        k.op("pool", lambda e: e.tensor_copy(out=w1b[b][:].rearrange("p k f -> p (k f)"), in_=s1[:]), reads=[s1], writes=[w1b[b]])
        s3 = stg[(3 * ex + 1) % 2]
        k.dma("sp", s3[:].rearrange("p (k f) -> p k f", k=8), w3[ex].rearrange("(k p) f -> p k f", p=128), writes=[s3])
        k.op("pool", lambda e: e.tensor_copy(out=w3b[b][:].rearrange("p k f -> p (k f)"), in_=s3[:]), reads=[s3], writes=[w3b[b]])
        s2 = stg[(3 * ex + 2) % 2]
        k.dma("sp", s2[:].rearrange("p (c d) -> p c d", c=4), w2[ex].rearrange("(c p) d -> p c d", p=128), writes=[s2])
        k.op("pool", lambda e: e.tensor_tensor(out=w2b[b][:], in0=s2[:].rearrange("p (c d) -> p c d", c=4), in1=_bc(gt2[:].unsqueeze(1), [128, 4, D]), op=ALU.mult),
             reads=[s2, gt2], writes=[w2b[b]])
        for blk in range(NTB // 512):
            ab = actT[blk % 2]
            for fc in range(4):
                p1 = ps1[cnt_f % 2]
                p3 = ps3[cnt_f % 2]
                sl = slb[cnt_f % 2]
                cnt_f += 1
                for kk in range(8):
                    k.op("pe", lambda e, kk=kk: e.matmul(p1[:], lhsT=w1b[b][:, kk, fc * 128:(fc + 1) * 128], rhs=u2T[:, kk, blk * 512:(blk + 1) * 512],
                                                         start=(kk == 0), stop=(kk == 7)), reads=[w1b[b], u2T], writes=[p1], fast=True)
                for kk in range(8):
                    k.op("pe", lambda e, kk=kk: e.matmul(p3[:], lhsT=w3b[b][:, kk, fc * 128:(fc + 1) * 128], rhs=u2T[:, kk, blk * 512:(blk + 1) * 512],
                                                         start=(kk == 0), stop=(kk == 7)), reads=[w3b[b], u2T], writes=[p3], fast=True)
                k.op("act", lambda e: e.activation(out=sl[:], in_=p1[:], func=AF.Silu), reads=[p1], writes=[sl])
                k.op("dve", lambda e: e.tensor_tensor(out=ab[fc][:], in0=p3[:], in1=sl[:], op=ALU.mult), reads=[p3, sl], writes=[ab[fc]])
            for ti in range(4):
                t = blk * 4 + ti
                py = psy[cnt_y % 2]
                cnt_y += 1
                for half in range(2):
                    for fc in range(4):
                        k.op("pe", lambda e, fc=fc, half=half: e.matmul(py[:, half * 512:(half + 1) * 512], lhsT=ab[fc][:, ti * 128:(ti + 1) * 128],
                                                                         rhs=w2b[b][:, fc, half * 512:(half + 1) * 512], start=(fc == 0), stop=(fc == 3)),
                             reads=[ab[fc], w2b[b]], writes=[py], fast=True)
                k.op("dve", lambda e: e.scalar_tensor_tensor(out=hres[t][:], in0=py[:], scalar=Wd[:, t, ex:ex + 1], in1=hres[t][:],
                                                             op0=ALU.mult, op1=ALU.add), reads=[py, Wd, hres[t]], writes=[hres[t]])
    k.release(m5)

    toks = []
    o_v = out.rearrange("(t p) d -> t p d", p=128)
    if final:
        fn = k.sb([128, D], F32, "fn")
        k.dma("sp", fn[:], fnorm.partition_broadcast(128), writes=[fn])
        ss2 = k.sb([128, NT], F32, "ss2")
        rs2 = k.sb([128, NT], F32, "rs2")
        junk2 = [k.sb([128, D], BF16, "junkf%d" % i) for i in range(2)]
        for t in range(NT):
            jk = junk2[t % 2]
            k.op("act", lambda e: e.activation(out=jk[:], in_=hres[t][:], func=AF.Square, accum_out=ss2[:, t:t + 1]),
                 reads=[hres[t]], writes=[jk, ss2])
        rstd_from_ss(k, ss2, rs2, NT, 1.0 / D)
        for t in range(NT):
            k.op("dve", lambda e: e.scalar_tensor_tensor(out=hres[t][:], in0=hres[t][:], scalar=rs2[:, t:t + 1], in1=fn[:], op0=ALU.mult, op1=ALU.mult),
                 reads=[hres[t], rs2, fn], writes=[hres[t]])
    for t in range(NT):
        toks.append(k.dma("sp", o_v[t], hres[t][:], reads=[hres[t]]))
    k.finish(toks)
    k.release(0)
    return nc, k


def _finB(k, out, hres, NT):
    o_v = out.rearrange("(t p) d -> t p d", p=128)
    toks = [k.dma("sp", o_v[t], hres[t][:], reads=[hres[t]]) for t in range(NT)]
    k.finish(toks)
    k.release(0)
    return k.nc, k


def phaseB_inmaps(layer, h, yT_full, inp, final):
    maps = []
    wa = np.ascontiguousarray(inp["w_ada"][layer][:, 2048:6144])
    ba = np.ascontiguousarray(inp["b_ada"][layer][2048:6144])
    w_out = inp["w_out_ab"][0] if layer == 0 else inp["w_out_c"][0]
    w_r = np.ascontiguousarray(np.concatenate([inp["moe_w_grp"][layer], inp["moe_w_rt"][layer]], axis=1))
    b_r = np.ascontiguousarray(np.concatenate([inp["moe_b_grp"][layer], inp["moe_b_rt"][layer]], axis=0))
    consts = make_consts()
    for core in range(NCORES):
        b, q = core // 4, core % 4
        sl = slice(q * NTB, (q + 1) * NTB)
        maps.append({
            "h_in": np.ascontiguousarray(h[b, sl]),
            "yT": np.ascontiguousarray(yT_full[b][:, sl]),
            "w_out": np.ascontiguousarray(w_out),
            "condT": np.ascontiguousarray(inp["c"][b].reshape(8, 128).T),
            "w_ada": wa, "b_ada": ba,
            "norm2": np.ascontiguousarray(inp["norm2"][layer]),
            "w_r": w_r, "b_r": b_r,
            "w1": inp["moe_w1"][layer], "w3": inp["moe_w3"][layer], "w2": inp["moe_w2"][layer],
            "fnorm": np.ascontiguousarray(inp["final_norm"]),
            "consts": consts,
        })
    return maps


class UMaker:
    def __init__(self, k, nc, h_ap, condT, w_ada, b_ada, norm_ap, cstb):
        self.k = k
        self.cstb = cstb
        self.h_v = h_ap.rearrange("(t p) d -> t p d", p=128)
        self.sh = k.sb([128, D], F32, "sh1")
        self.sc = k.sb([128, D], F32, "sc1")
        m = k.mark()
        pp = [k.ps([128, 512], F32, "modps%d" % i) for i in range(2)]
        compute_mod(k, nc, condT, w_ada, b_ada, 2048, [self.sh, self.sc], pp)
        k.release(m)
        self.g = k.sb([128, D], F32, "g1")
        k.dma("sp", self.g[:], norm_ap.partition_broadcast(128), writes=[self.g])
        k.op("dve", lambda e: e.scalar_tensor_tensor(out=self.g[:], in0=self.sc[:], scalar=1.0, in1=self.g[:], op0=ALU.add, op1=ALU.mult),
             reads=[self.sc, self.g], writes=[self.g])
        self.ht = [k.sb([128, D], F32, "ht%d" % i) for i in range(4)]
        self.junk = k.sb([128, D], BF16, "junk")
        self.ss = k.sb([128, 4], F32, "ss")
        self.rstd = k.sb([128, 4], F32, "rstd")
        self.a = [k.sb([128, D], F32, "ua%d" % i) for i in range(2)]
        self.ub = [k.sb([128, D], BF16, "ub%d" % i) for i in range(2)]
        self.pst = [k.ps([128, 8, 128], BF16, "pst%d" % i) for i in range(1)]

    def block(self, blk, uT):
        k = self.k
        for ti in range(4):
            ht = self.ht[ti]
            k.dma("sp", ht[:], self.h_v[blk * 4 + ti], writes=[ht])
            k.op("act", lambda e: e.activation(out=self.junk[:], in_=ht[:], func=AF.Square, accum_out=self.ss[:, ti:ti + 1]),
                 reads=[ht], writes=[self.junk, self.ss])
        rstd_from_ss(k, self.ss, self.rstd, 4, 1.0 / D)
        for ti in range(4):
            ht = self.ht[ti]
            a = self.a[ti % 2]
            ub = self.ub[ti % 2]
            pt = self.pst[0]
            k.op("dve", lambda e: e.scalar_tensor_tensor(out=a[:], in0=ht[:], scalar=self.rstd[:, ti:ti + 1], in1=self.g[:], op0=ALU.mult, op1=ALU.mult),
                 reads=[ht, self.rstd, self.g], writes=[a])
            k.op("pool", lambda e: e.tensor_tensor(out=ub[:], in0=a[:], in1=self.sh[:], op=ALU.add), reads=[a, self.sh], writes=[ub])
            for kk in range(8):
                k.op("pe", lambda e: e.transpose(out=pt[:, kk, :], in_=ub[:, kk * 128:(kk + 1) * 128], identity=self.cstb[:, C_ID, :]),
                     reads=[ub, self.cstb], writes=[pt], fast=True)
            k.op("act", lambda e: e.activation(out=uT[:, :, ti * 128:(ti + 1) * 128], in_=pt[:], func=AF.Copy), reads=[pt], writes=[uT])


class Banks:
    def __init__(self, k, n):
        self.t = [k.ps([128, 512], F32, "bank%d" % i) for i in range(n)]
        self.i = 0

    def get(self):
        b = self.t[self.i % len(self.t)]
        self.i += 1
        return b


NCOL_A0 = 656


def build_phaseA0(nblk=16):
    nc = bass.Bass("TRN2", target_bir_lowering=False)
    dt = nc.dram_tensor
    h_in = dt("h_in", [S, D], F32, kind="ExternalInput").ap()
    condT = dt("condT", [128, 8], F32, kind="ExternalInput").ap()
    w_ada = dt("w_ada", [D, 2048], F32, kind="ExternalInput").ap()
    b_ada = dt("b_ada", [2048], F32, kind="ExternalInput").ap()
    norm1 = dt("norm1", [D], F32, kind="ExternalInput").ap()
    w_in = dt("w_in", [D, NCOL_A0], F32, kind="ExternalInput").ap()
    WAd = dt("WA", [128, 128], F32, kind="ExternalInput").ap()
    WXd = dt("WX", [128, 128], F32, kind="ExternalInput").ap()
    wg2d = dt("wg2", [16, 64], F32, kind="ExternalInput").ap()
    pcold = dt("pcol", [128, 16], F32, kind="ExternalInput").ap()
    consts = dt("consts", [128, NCONST, 128], F32, kind="ExternalInput").ap()
    yT = dt("yT", [256, S], BF16, kind="ExternalOutput").ap()

    k = KB(nc)
    cst, cstb = load_consts(k, nc, consts)
    um = UMaker(k, nc, h_in, condT, w_ada, b_ada, norm1, cstb)
    win = k.sb([128, 8, NCOL_A0], BF16, "win")
    k.dma("pool", win[:], w_in.rearrange("(k p) n -> p k n", p=128), writes=[win])
    WA = k.sb([128, 128], BF16, "WA")
    WX = k.sb([128, 128], BF16, "WX")
    wg2 = k.sb([16, 64], BF16, "wg2")
    k.dma("pool", WA[:], WAd, writes=[WA])
    k.dma("pool", WX[:], WXd, writes=[WX])
    k.dma("pool", wg2[:], wg2d, writes=[wg2])
    pc = k.sb([128, 16], F32, "pcol")
    k.dma("sp", pc[:], pcold, writes=[pc])
    dc = k.sb([128, 4], F32, "dc")
    tl = k.sb([128, 1], F32, "tl")
    k.op("act", lambda e: e.activation(out=tl[:], in_=pc[:, 7:8], func=AF.Exp, scale=-1.0), reads=[pc], writes=[tl])
    k.op("act", lambda e: e.activation(out=tl[:], in_=tl[:], func=AF.Ln, bias=1.0), reads=[tl], writes=[tl])
    k.op("dve", lambda e: e.tensor_scalar(out=dc[:, 0:1], in0=tl[:], scalar1=-8.0, scalar2=None, op0=ALU.mult), reads=[tl], writes=[dc])
    k.op("dve", lambda e: e.tensor_scalar(out=dc[:, 1:2], in0=tl[:], scalar1=-16.0, scalar2=None, op0=ALU.mult), reads=[tl], writes=[dc])
    k.op("dve", lambda e: e.tensor_scalar(out=dc[:, 2:3], in0=pc[:, 9:10], scalar1=-1.0, scalar2=None, op0=ALU.mult), reads=[pc], writes=[dc])
    rmask = k.sb([64, 8, 64], F32, "rmask")
    k.op("dve", lambda e: e.memset(rmask[:], 1.0), writes=[rmask])
    k.op("dve", lambda e: e.memset(rmask[:, :, 0:1], 0.0), writes=[rmask])
    rmask2 = rmask[:].rearrange("p a b -> p (a b)")

    banks = Banks(k, 5)
    pkg = k.ps([128, 4, 64], BF16, "pkg")
    uT = [k.sb([128, 8, 512], BF16, "uT%d" % i) for i in range(2)]
    xabuf = [k.sb([128, 515], F32, "xabuf%d" % i) for i in range(2)]
    k.op("dve", lambda e: e.memset(xabuf[0][:, 0:3], 0.0), writes=[xabuf[0]])
    hs = [k.sb([128, 512], F32, "hs%d" % i) for i in range(2)]
    Sst = k.sb([64, 128], F32, "Sst")
    k.op("dve", lambda e: e.memset(Sst[:], 0.0), writes=[Sst])
    Sbt = [k.sb([64, 128], BF16, "Sbt%d" % i) for i in range(8)]

    def sbt(shape, dtp, name):
        return k.sb(shape, dtp, name)

    ga = sbt([128, 512], F32, "ga")
    q_sb = sbt([64, 512], F32, "q_sb")
    k_sb = sbt([64, 512], F32, "k_sb")
    sog = sbt([128, 512], F32, "sog")
    gl_bf = sbt([16, 512], BF16, "gl_bf")
    v_tok = sbt([128, 4, 128], BF16, "v_tok")
    xc = sbt([128, 512], F32, "xc")
    xcb = sbt([128, 512], BF16, "xcb")
    r_sb = sbt([128, 512], F32, "r_sb")
    i_sb = sbt([128, 512], F32, "i_sb")
    a_sb = sbt([128, 512], F32, "a_sb")
    a2_sb = sbt([128, 512], F32, "a2_sb")
    t_sb = sbt([128, 512], F32, "t_sb")
    b_sb = sbt([128, 512], F32, "b_sb")
    g2_sb = sbt([128, 512], F32, "g2_sb")
    inner = sbt([128, 512], F32, "inner")
    ge = sbt([128, 512], F32, "ge")
    ya = [sbt([128, 512], BF16, "ya%d" % i) for i in range(2)]
    e1 = sbt([64, 512], F32, "e1")
    sp_ = sbt([64, 512], F32, "sp")
    cum = sbt([64, 512], F32, "cum")
    E1 = sbt([64, 512], F32, "E1")
    E2 = sbt([64, 512], F32, "E2")
    qg = sbt([64, 512], BF16, "qg")
    kg = sbt([64, 512], BF16, "kg")
    kg_tok = sbt([128, 4, 64], BF16, "kg_tok")
    attm = sbt([128, 4, 128], BF16, "attm")
    tS = sbt([64, 128], F32, "tS")
    osb = sbt([128, 512], F32, "osb")
    o2 = sbt([128, 512], F32, "o2")
    rs = sbt([128, 512], F32, "rs")
    t1 = sbt([128, 512], F32, "t1")
    ob = [sbt([128, 512], BF16, "ob%d" % i) for i in range(2)]

    def op(e, fn, r, w):
        return k.op(e, fn, reads=r, writes=w)

    out_toks = []
    for blk in range(nblk):
        u = uT[blk % 2]
        xb = xabuf[blk % 2]
        xbn = xabuf[(blk + 1) % 2]
        um.block(blk, u)

        def proj(c0, c1):
            p = banks.get()
            m = c1 - c0
            for kk in range(8):
                k.op("pe", lambda e: e.matmul(p[0:m, :], lhsT=win[:, kk, c0:c1], rhs=u[:, kk, :], start=(kk == 0), stop=(kk == 7)), reads=[win, u], writes=[p], fast=True)
            return p

        p = proj(0, 128)
        op("act", lambda e: e.activation(out=xb[:, 3:515], in_=p[:], func=AF.Copy), [p], [xb])
        p = proj(128, 256)
        op("act", lambda e: e.activation(out=ga[:], in_=p[:], func=AF.Copy), [p], [ga])
        p = proj(256, 320)
        op("dve", lambda e: e.tensor_scalar(out=q_sb[:], in0=p[0:64, :], scalar1=0.125, scalar2=None, op0=ALU.mult), [p], [q_sb])
        p = proj(320, 384)
        op("act", lambda e: e.activation(out=k_sb[:], in_=p[0:64, :], func=AF.Copy), [p], [k_sb])
        p = proj(512, 640)
        op("act", lambda e: e.activation(out=sog[:], in_=p[:], func=AF.Silu), [p], [sog])
        p = proj(640, 656)
        op("dve", lambda e: e.tensor_copy(out=gl_bf[:], in_=p[0:16, :]), [p], [gl_bf])
        p = banks.get()
        for ti in range(4):
            for kk in range(8):
                op("pe", lambda e: e.matmul(p[:, ti * 128:(ti + 1) * 128], lhsT=u[:, kk, ti * 128:(ti + 1) * 128], rhs=win[:, kk, 384:512],
                                            start=(kk == 0), stop=(kk == 7)), [win, u], [p])
        op("dve", lambda e: e.tensor_copy(out=v_tok[:].rearrange("p a b -> p (a b)"), in_=p[:]), [p], [v_tok])

        op("act", lambda e: e.activation(out=xc[:], in_=xb[:, 3:515], func=AF.Identity, bias=pc[:, 4:5], scale=pc[:, 3:4]), [xb, pc], [xc])
        for w in range(3):
            op("dve", lambda e: e.scalar_tensor_tensor(out=xc[:], in0=xb[:, w:w + 512], scalar=pc[:, w:w + 1], in1=xc[:], op0=ALU.mult, op1=ALU.add),
               [xb, pc, xc], [xc])
        op("pool", lambda e: e.tensor_copy(out=xbn[:, 0:3], in_=xb[:, 512:515]), [xb], [xbn])
        op("act", lambda e: e.activation(out=xcb[:], in_=xc[:], func=AF.Copy), [xc], [xcb])
        p = banks.get()
        op("pe", lambda e: e.matmul(p[:], lhsT=WA[:], rhs=xcb[:], start=True, stop=True), [WA, xcb], [p])
        op("act", lambda e: e.activation(out=r_sb[:], in_=p[:], func=AF.Sigmoid, bias=pc[:, 5:6]), [p, pc], [r_sb])
        p = banks.get()
        op("pe", lambda e: e.matmul(p[:], lhsT=WX[:], rhs=xcb[:], start=True, stop=True), [WX, xcb], [p])
        op("act", lambda e: e.activation(out=i_sb[:], in_=p[:], func=AF.Sigmoid, bias=pc[:, 6:7]), [p, pc], [i_sb])
        op("act", lambda e: e.activation(out=a_sb[:], in_=r_sb[:], func=AF.Exp, scale=dc[:, 0:1]), [r_sb, dc], [a_sb])
        op("act", lambda e: e.activation(out=a2_sb[:], in_=r_sb[:], func=AF.Exp, scale=dc[:, 1:2]), [r_sb, dc], [a2_sb])
        op("act", lambda e: e.activation(out=a2_sb[:], in_=a2_sb[:], func=AF.Sqrt, bias=1.0, scale=-1.0), [a2_sb], [a2_sb])
        op("dve", lambda e: e.tensor_tensor(out=t_sb[:], in0=i_sb[:], in1=xc[:], op=ALU.mult), [i_sb, xc], [t_sb])
        op("pool", lambda e: e.tensor_tensor(out=b_sb[:], in0=t_sb[:], in1=a2_sb[:], op=ALU.mult), [t_sb, a2_sb], [b_sb])
        hcur = hs[blk % 2]
        hprev = hs[(blk + 1) % 2]
        if blk == 0:
            op("dve", lambda e: e.tensor_tensor_scan(out=hcur[:], data0=a_sb[:], data1=b_sb[:], initial=0.0, op0=ALU.mult, op1=ALU.add),
               [a_sb, b_sb], [hcur])
        else:
            op("dve", lambda e: e.tensor_tensor_scan(out=hcur[:], data0=a_sb[:], data1=b_sb[:], initial=hprev[:, 511:512], op0=ALU.mult, op1=ALU.add),
               [a_sb, b_sb, hprev], [hcur])
        op("act", lambda e: e.activation(out=g2_sb[:], in_=ga[:], func=AF.Square), [ga], [g2_sb])
        op("dve", lambda e: e.tensor_scalar(out=g2_sb[:], in0=g2_sb[:], scalar1=0.044715, scalar2=1.0, op0=ALU.mult, op1=ALU.add), [g2_sb], [g2_sb])
        op("pool", lambda e: e.tensor_tensor(out=inner[:], in0=g2_sb[:], in1=ga[:], op=ALU.mult), [g2_sb, ga], [inner])
        op("act", lambda e: e.activation(out=inner[:], in_=inner[:], func=AF.Sigmoid, scale=1.5957691216), [inner], [inner])
        op("dve", lambda e: e.tensor_tensor(out=ge[:], in0=ga[:], in1=inner[:], op=ALU.mult), [ga, inner], [ge])
        yab = ya[blk % 2]
        op("pool", lambda e: e.tensor_tensor(out=yab[:], in0=ge[:], in1=hcur[:], op=ALU.mult), [ge, hcur], [yab])
        out_toks.append(k.dma("sp", yT[0:128, blk * 512:(blk + 1) * 512], yab[:], reads=[yab]))

        p = banks.get()
        op("pe", lambda e: e.matmul(p[0:64, :], lhsT=wg2[:], rhs=gl_bf[:], start=True, stop=True), [wg2, gl_bf], [p])
        op("act", lambda e: e.activation(out=e1[:], in_=p[0:64, :], func=AF.Exp, bias=dc[0:64, 2:3], scale=-1.0), [p, dc], [e1])
        op("act", lambda e: e.activation(out=sp_[:], in_=e1[:], func=AF.Ln, bias=1.0), [e1], [sp_])
        op("dve", lambda e: e.tensor_tensor_scan(out=cum[:], data0=rmask2, data1=sp_[:], initial=0.0, op0=ALU.mult, op1=ALU.add), [rmask, sp_], [cum])
        op("act", lambda e: e.activation(out=E1[:], in_=cum[:], func=AF.Exp, scale=-1.0 / 16.0), [cum], [E1])
        op("act", lambda e: e.activation(out=E2[:], in_=cum[:], func=AF.Exp, scale=1.0 / 16.0), [cum], [E2])
        op("dve", lambda e: e.tensor_tensor(out=qg[:], in0=q_sb[:], in1=E1[:], op=ALU.mult), [q_sb, E1], [qg])
        op("pool", lambda e: e.tensor_tensor(out=kg[:], in0=k_sb[:], in1=E2[:], op=ALU.mult), [k_sb, E2], [kg])
        for pr in range(4):
            op("pe", lambda e: e.transpose(out=pkg[:, pr, :], in_=kg[:, pr * 128:(pr + 1) * 128], identity=cstb[0:64, C_ID, 0:64]), [kg, cstb], [pkg])
        op("act", lambda e: e.activation(out=kg_tok[:], in_=pkg[:], func=AF.Copy), [pkg], [kg_tok])
        p = banks.get()
        for pr in range(4):
            op("pe", lambda e: e.matmul(p[:, pr * 128:(pr + 1) * 128], lhsT=kg[:, pr * 128:(pr + 1) * 128], rhs=qg[:, pr * 128:(pr + 1) * 128],
                                        start=True, stop=True), [kg, qg], [p])
        op("dve", lambda e: e.tensor_tensor(out=attm[:], in0=p[:].rearrange("p (a b) -> p a b", a=4),
                                            in1=_bc(cst[:, C_UT64:C_UT64 + 1, :], [128, 4, 128]), op=ALU.mult), [p, cst], [attm])
        kva = banks.get()
        kvb = banks.get()
        for c in range(8):
            pr, half = c // 2, c % 2
            kvp = kva if c < 4 else kvb
            op("pe", lambda e: e.matmul(kvp[0:64, (c % 4) * 128:(c % 4 + 1) * 128], lhsT=kg_tok[half * 64:(half + 1) * 64, pr, :],
                                        rhs=v_tok[half * 64:(half + 1) * 64, pr, :], start=True, stop=True), [kg_tok, v_tok], [kvp])
        for c in range(8):
            kvp = kva if c < 4 else kvb
            op("act", lambda e: e.activation(out=Sbt[c][:], in_=Sst[:], func=AF.Copy), [Sst], [Sbt[c]])
            op("dve", lambda e: e.tensor_tensor(out=tS[:], in0=kvp[0:64, (c % 4) * 128:(c % 4 + 1) * 128], in1=Sst[:], op=ALU.add), [kvp, Sst], [tS])
            op("dve", lambda e: e.tensor_scalar(out=Sst[:], in0=tS[:], scalar1=E1[:, c * 64 + 63:c * 64 + 64], scalar2=None, op0=ALU.mult),
               [tS, E1], [Sst])
        po = banks.get()
        for pr in range(4):
            op("pe", lambda e: e.matmul(po[:, pr * 128:(pr + 1) * 128], lhsT=v_tok[:, pr, :], rhs=attm[:, pr, :], start=True, stop=False),
               [v_tok, attm], [po])
            for half in range(2):
                c = 2 * pr + half
                op("pe", lambda e: e.matmul(po[:, c * 64:(c + 1) * 64], lhsT=Sbt[c][:], rhs=qg[:, c * 64:(c + 1) * 64], start=False, stop=(half == 1)),
                   [Sbt[c], qg], [po])
        op("act", lambda e: e.activation(out=osb[:], in_=po[:], func=AF.Copy), [po], [osb])
        op("act", lambda e: e.activation(out=o2[:], in_=osb[:], func=AF.Square), [osb], [o2])
        p = banks.get()
        op("pe", lambda e: e.matmul(p[:], lhsT=cst[:, C_ONES, :], rhs=o2[:], start=True, stop=True), [cst, o2], [p])
        op("act", lambda e: e.activation(out=rs[:], in_=p[:], func=AF.Sqrt, bias=EPS, scale=1.0 / 128.0), [p], [rs])
        op("dve", lambda e: e.reciprocal(out=rs[:], in_=rs[:]), [rs], [rs])
        op("dve", lambda e: e.scalar_tensor_tensor(out=t1[:], in0=osb[:], scalar=pc[:, 8:9], in1=rs[:], op0=ALU.mult, op1=ALU.mult), [osb, pc, rs], [t1])
        obb = ob[blk % 2]
        op("pool", lambda e: e.tensor_tensor(out=obb[:], in0=t1[:], in1=sog[:], op=ALU.mult), [t1, sog], [obb])
        out_toks.append(k.dma("sp", yT[128:256, blk * 512:(blk + 1) * 512], obb[:], reads=[obb]))
    k.finish(out_toks)
    k.release(0)
    return nc, k


def phaseA0_inmaps(h, inp):
    maps = []
    consts = make_consts()
    w_in = inp["w_in_ab"][0]
    wa = np.ascontiguousarray(inp["w_ada"][0][:, 0:2048])
    ba = np.ascontiguousarray(inp["b_ada"][0][0:2048])
    for core in range(NCORES):
        b, hg = core // 4, core % 4
        ch = slice(hg * 128, (hg + 1) * 128)
        cols = np.concatenate([
            np.arange(hg * 128, (hg + 1) * 128),
            512 + np.arange(hg * 128, (hg + 1) * 128),
            1024 + np.arange(hg * 64, (hg + 1) * 64),
            1280 + np.arange(hg * 64, (hg + 1) * 64),
            1536 + np.arange(hg * 128, (hg + 1) * 128),
            2048 + np.arange(hg * 128, (hg + 1) * 128),
            2560 + np.arange(16),
        ])
        WA = np.zeros((128, 128), np.float32)
        WX = np.zeros((128, 128), np.float32)
        for j in range(2):
            WA[j * 64:(j + 1) * 64, j * 64:(j + 1) * 64] = inp["rg_wa"][0][hg * 2 + j]
            WX[j * 64:(j + 1) * 64, j * 64:(j + 1) * 64] = inp["rg_wx"][0][hg * 2 + j]
        pcol = np.zeros((128, 16), np.float32)
        pcol[:, 0:4] = inp["conv_a_w"][0][:, ch].T
        pcol[:, 4] = inp["conv_a_b"][0][ch]
        pcol[:, 5] = inp["rg_ba"][0][ch]
        pcol[:, 6] = inp["rg_bx"][0][ch]
        pcol[:, 7] = inp["rg_lam"][0][ch]
        pcol[:, 8] = inp["gla_norm"][0]
        pcol[0:64, 9] = inp["gla_bg2"][0][hg * 64:(hg + 1) * 64]
        maps.append({
            "h_in": np.ascontiguousarray(h[b]),
            "condT": np.ascontiguousarray(inp["c"][b].reshape(8, 128).T),
            "w_ada": wa, "b_ada": ba,
            "norm1": np.ascontiguousarray(inp["norm1"][0]),
            "w_in": np.ascontiguousarray(w_in[:, cols]),
            "WA": WA, "WX": WX,
            "wg2": np.ascontiguousarray(inp["gla_wg2"][0][:, hg * 64:(hg + 1) * 64]),
            "pcol": pcol, "consts": consts,
        })
    return maps


def assemble_yT_A0(results):
    yT = np.zeros((2, D, S), ml_dtypes.bfloat16)
    for core in range(NCORES):
        b, hg = core // 4, core % 4
        r = results[core]["yT"]
        yT[b, hg * 128:(hg + 1) * 128] = r[0:128]
        yT[b, 512 + hg * 128:512 + (hg + 1) * 128] = r[128:256]
    return yT


NCOL_A1 = 1028
import os as _os
SEQ_DEBUG = bool(_os.environ.get('SEQ_DEBUG'))


def build_phaseA1(nblk=16, stop=99):
    nc = bass.Bass("TRN2", target_bir_lowering=False)
    dt = nc.dram_tensor
    h_in = dt("h_in", [S, D], F32, kind="ExternalInput").ap()
    condT = dt("condT", [128, 8], F32, kind="ExternalInput").ap()
    w_ada = dt("w_ada", [D, 2048], F32, kind="ExternalInput").ap()
    b_ada = dt("b_ada", [2048], F32, kind="ExternalInput").ap()
    norm1 = dt("norm1", [D], F32, kind="ExternalInput").ap()
    w_in = dt("w_in", [D, NCOL_A1], F32, kind="ExternalInput").ap()
    pcold = dt("pcol", [128, 24], F32, kind="ExternalInput").ap()
    prmd = dt("prm", [128, 4], F32, kind="ExternalInput").ap()
    dnd = dt("dnorm", [128], F32, kind="ExternalInput").ap()
    alogd = dt("a_log", [128, 2], F32, kind="ExternalInput").ap()
    consts = dt("consts", [128, NCONST, 128], F32, kind="ExternalInput").ap()
    yT = dt("yT", [256, S], BF16, kind="ExternalOutput").ap()

    k = KB(nc)
    cst, cstb = load_consts(k, nc, consts)
    um = UMaker(k, nc, h_in, condT, w_ada, b_ada, norm1, cstb)
    win = k.sb([128, 8, NCOL_A1], BF16, "win")
    wv = w_in.rearrange("(k p) n -> p k n", p=128)
    for kk in range(8):
        k.dma("pool", win[:, kk, :], wv[:, kk, :], writes=[win])
    pc = k.sb([128, 24], F32, "pcol")
    k.dma("sp", pc[:], pcold, writes=[pc])
    prm = k.sb([128, 4], F32, "prm")
    k.dma("sp", prm[:], prmd, writes=[prm])
    dnb = k.sb([128, 128], F32, "dnb")
    k.dma("sp", dnb[:], dnd.partition_broadcast(128), writes=[dnb])
    alg = k.sb([128, 2], F32, "alg")
    k.dma("sp", alg[:], alogd, writes=[alg])
    k.op("act", lambda e: e.activation(out=alg[:], in_=alg[:], func=AF.Exp), reads=[alg], writes=[alg])
    k.op("dve", lambda e: e.tensor_scalar(out=prm[:, 2:4], in0=alg[:], scalar1=-1.0, scalar2=None, op0=ALU.mult), reads=[alg, prm], writes=[prm])

    banks = Banks(k, 6)
    ptr = k.ps([128, 2, 128], BF16, "ptr")
    uT = [k.sb([128, 8, 512], BF16, "uT%d" % i) for i in range(2)]
    cbuf = [[k.sb([128, 515], F32, "cbuf%d_%d" % (j, i)) for i in range(2)] for j in range(6)]
    for j in range(6):
        k.op("dve", lambda e: e.memset(cbuf[j][0][:, 0:3], 0.0), writes=[cbuf[j][0]])
    Sst = [k.sb([128, 128], F32, "Sst%d" % h) for h in range(2)]
    Sb = [k.sb([128, 128], BF16, "Sb%d" % h) for h in range(2)]
    for h in range(2):
        k.op("dve", lambda e: e.memset(Sst[h][:], 0.0), writes=[Sst[h]])
        k.op("dve", lambda e: e.memset(Sb[h][:], 0.0), writes=[Sb[h]])

    sb = k.sb
    sj = [sb([128, 512], F32, "sj%d" % j) for j in range(6)]
    sq = sb([128, 512], F32, "sq")
    rs = sb([128, 512], F32, "rs")
    nT = [sb([128, 512], BF16, "nT%d" % j) for j in range(4)]
    vTb = [sb([128, 512], BF16, "vTb%d" % h) for h in range(2)]
    sz = [sb([128, 256], F32, "sz%d" % t) for t in range(4)]
    g4 = sb([128, 4, 4], F32, "g4")
    beta = sb([128, 4, 2], F32, "beta")
    nbeta = sb([128, 4, 2], F32, "nbeta")
    gx = sb([128, 4, 2], F32, "gx")
    gg = sb([128, 4, 2], F32, "gg")
    TST = []
    for a in range(2):
        TST.append({"gch": sb([128, 4], F32, "gch%d" % a), "gcl": sb([128, 8], F32, "gcl%d" % a), "eg": sb([128, 2], F32, "eg%d" % a),
                    "ed": sb([128, 2], F32, "ed%d" % a), "be": sb([128, 2], F32, "be%d" % a), "egl": sb([128, 4], F32, "egl%d" % a)})
    GB = []
    for g_ in range(4):
        d_ = {"ps": banks.t[g_], "ptr": ptr}
        for nm in ("gm", "DTm", "Dm", "Tt", "U", "dg"):
            d_[nm] = sb([128, 128], F32, "%s%d" % (nm, g_))
        d_["AB"] = sb([128, 2, 128], F32, "AB%d" % g_)
        for nm in ("Ttb", "attT", "bv", "kbg", "kd", "WT", "qg", "vnew"):
            d_[nm] = sb([128, 128], BF16, "%s%d" % (nm, g_))
        GB.append(d_)
    o_tok = [sb([128, 4, 128], F32, "o_tok%d" % h) for h in range(2)]
    ss = sb([128, 4], F32, "oss")
    rstd = sb([128, 4], F32, "orstd")
    junk = sb([128, 128], F32, "ojunk")
    on = sb([128, 128], F32, "on")
    ytok = sb([128, 128], BF16, "ytok")
    yTs = [sb([128, 512], BF16, "yTs%d" % i) for i in range(2)]

    def op(e, fn, r, w):
        return k.op(e, fn, reads=r, writes=w)

    ident = cst[:, C_ID, :]
    TRI = cst[:, C_TRI, :]
    BLK = cst[:, C_BLK, :]
    SU = cst[:, C_SU, :]
    ONES = cst[:, C_ONES, :]
    UT64 = cst[:, C_UT64, :]
    out_toks = []
    cnt_y = 0
    for blk in range(nblk):
        u = uT[blk % 2]
        if stop <= -3:
            break
        um.block(blk, u)
        for j in range(6):
            if stop <= -2:
                break
            cb = cbuf[j][blk % 2]
            cbn = cbuf[j][(blk + 1) % 2]
            p = banks.get()
            for kk in range(8):
                k.op("pe", lambda e: e.matmul(p[:], lhsT=win[:, kk, j * 128:(j + 1) * 128], rhs=u[:, kk, :], start=(kk == 0), stop=(kk == 7)), reads=[win, u], writes=[p], fast=True)
            op("act", lambda e: e.activation(out=cb[:, 3:515], in_=p[:], func=AF.Copy), [p], [cb])
            op("act", lambda e: e.activation(out=sj[j][:], in_=cb[:, 3:515], func=AF.Copy, scale=pc[:, j * 4 + 3:j * 4 + 4]), [cb, pc], [sj[j]])
            for w in range(3):
                op("dve", lambda e: e.scalar_tensor_tensor(out=sj[j][:], in0=cb[:, w:w + 512], scalar=pc[:, j * 4 + w:j * 4 + w + 1], in1=sj[j][:],
                                                           op0=ALU.mult, op1=ALU.add), [cb, pc, sj[j]], [sj[j]])
            op("pool", lambda e: e.tensor_copy(out=cbn[:, 0:3], in_=cb[:, 512:515]), [cb], [cbn])
            op("act", lambda e: e.activation(out=sj[j][:], in_=sj[j][:], func=AF.Silu), [sj[j]], [sj[j]])
        if stop <= -1:
            break
        for j in range(4):
            op("act", lambda e: e.activation(out=sq[:], in_=sj[j][:], func=AF.Square), [sj[j]], [sq])
            p = banks.get()
            op("pe", lambda e: e.matmul(p[:], lhsT=ONES, rhs=sq[:], start=True, stop=True), [cst, sq], [p])
            op("act", lambda e: e.activation(out=rs[:], in_=p[:], func=AF.Sqrt, bias=EPS), [p], [rs])
            op("dve", lambda e: e.reciprocal(out=rs[:], in_=rs[:]), [rs], [rs])
            scl = 128.0 ** -0.5 if j < 2 else 1.0
            op("dve", lambda e: e.scalar_tensor_tensor(out=nT[j][:], in0=sj[j][:], scalar=scl, in1=rs[:], op0=ALU.mult, op1=ALU.mult), [sj[j], rs], [nT[j]])
        for h in range(2):
            op("act", lambda e: e.activation(out=vTb[h][:], in_=sj[4 + h][:], func=AF.Copy), [sj[4 + h]], [vTb[h]])
        if stop <= 0:
            break
        for ti in range(4):
            p = banks.get()
            for kk in range(8):
                op("pe", lambda e: e.matmul(p[:, 0:256], lhsT=u[:, kk, ti * 128:(ti + 1) * 128], rhs=win[:, kk, 768:1024], start=(kk == 0), stop=(kk == 7)),
                   [win, u], [p])
            for kk in range(8):
                op("pe", lambda e: e.matmul(p[:, 256:260], lhsT=u[:, kk, ti * 128:(ti + 1) * 128], rhs=win[:, kk, 1024:1028], start=(kk == 0), stop=(kk == 7)),
                   [win, u], [p])
            op("act", lambda e: e.activation(out=sz[ti][:], in_=p[:, 0:256], func=AF.Silu), [p], [sz[ti]])
            op("act", lambda e: e.activation(out=g4[:, ti, :], in_=p[:, 256:260], func=AF.Copy), [p], [g4])
        if stop <= 0.2:
            break
        op("act", lambda e: e.activation(out=beta[:], in_=g4[:, :, 0:2], func=AF.Sigmoid), [g4], [beta])
        op("dve", lambda e: e.tensor_scalar(out=nbeta[:], in0=beta[:], scalar1=-1.0, scalar2=None, op0=ALU.mult), [beta], [nbeta])
        if stop <= 0.4:
            break
        op("dve", lambda e: e.tensor_tensor(out=gx[:], in0=g4[:, :, 2:4], in1=_bc(prm[:, 0:2].unsqueeze(1), [128, 4, 2]), op=ALU.add), [g4, prm], [gx])
        if stop <= 0.6:
            break
        op("act", lambda e: e.activation(out=gx[:], in_=gx[:], func=AF.Exp), [gx], [gx])
        op("act", lambda e: e.activation(out=gx[:], in_=gx[:], func=AF.Ln, bias=1.0), [gx], [gx])
        if stop <= 0.8:
            break
        op("dve", lambda e: e.tensor_tensor(out=gg[:], in0=gx[:], in1=_bc(prm[:, 2:4].unsqueeze(1), [128, 4, 2]), op=ALU.mult), [gx, prm], [gg])

        if stop <= 1:
            break
        def tile_common(ti, st):
            gcl, eg, ed, egl, be, gch = st["gcl"], st["eg"], st["ed"], st["egl"], st["be"], st["gch"]
            op("dve", lambda e: e.tensor_tensor(out=gch[:].rearrange("p (a b) -> p a b", a=2), in0=_bc(gg[:, ti, :].unsqueeze(1), [128, 2, 2]),
                                                in1=_bc(cst[:, C_CH0, 0:2].unsqueeze(2), [128, 2, 2]), op=ALU.mult), [gg, cst], [gch])
            pg = banks.get()
            op("pe", lambda e: e.matmul(pg[:, 0:2], lhsT=TRI, rhs=gg[:, ti, :], start=True, stop=True), [cst, gg], [pg])
            op("pe", lambda e: e.matmul(pg[:, 2:4], lhsT=BLK, rhs=gg[:, ti, :], start=True, stop=True), [cst, gg], [pg])
            op("pe", lambda e: e.matmul(pg[:, 4:8], lhsT=ONES, rhs=gch[:], start=True, stop=True), [cst, gch], [pg])
            op("dve", lambda e: e.tensor_copy(out=gcl[:], in_=pg[:, 0:8]), [pg], [gcl])
            op("act", lambda e: e.activation(out=eg[:], in_=gcl[:, 0:2], func=AF.Exp), [gcl], [eg])
            op("dve", lambda e: e.tensor_tensor(out=ed[:], in0=gcl[:, 2:4], in1=gcl[:, 0:2], op=ALU.subtract), [gcl], [ed])
            op("act", lambda e: e.activation(out=ed[:], in_=ed[:], func=AF.Exp), [ed], [ed])
            op("act", lambda e: e.activation(out=egl[:], in_=gcl[:, 4:8], func=AF.Exp), [gcl], [egl])
            op("dve", lambda e: e.tensor_tensor(out=be[:], in0=beta[:, ti, :], in1=eg[:], op=ALU.mult), [beta, eg], [be])

        def prep(ti, h, st, B):
            tsl = slice(ti * 128, (ti + 1) * 128)
            qT = nT[h]
            kT = nT[2 + h]
            ps = B["ps"]
            gm, DTm, Dm, AB, Tt, Ttb, attT, bv, kbg, kd, U, WT, dg, qg = (B[n] for n in
                ("gm", "DTm", "Dm", "AB", "Tt", "Ttb", "attT", "bv", "kbg", "kd", "U", "WT", "dg", "qg"))
            eg, ed, be = st["eg"], st["ed"], st["be"]
            A_ = AB[:, 0, :]
            B_ = AB[:, 1, :]
            op("dve", lambda e: e.tensor_scalar(out=gm[:], in0=SU, scalar1=gg[:, ti, h:h + 1], scalar2=None, op0=ALU.mult), [cst, gg], [gm])
            op("pe", lambda e: e.matmul(ps[:, 0:128], lhsT=gm[:], rhs=TRI, start=True, stop=True), [gm, cst], [ps])
            op("pe", lambda e: e.matmul(ps[:, 128:256], lhsT=TRI, rhs=gm[:], start=True, stop=True), [gm, cst], [ps])
            yield
            op("act", lambda e: e.activation(out=DTm[:], in_=ps[:, 0:128], func=AF.Exp), [ps], [DTm])
            op("act", lambda e: e.activation(out=Dm[:], in_=ps[:, 128:256], func=AF.Exp), [ps], [Dm])
            op("pool", lambda e: e.tensor_tensor(out=DTm[:], in0=DTm[:], in1=UT64, op=ALU.mult), [DTm, cst], [DTm])
            op("pool", lambda e: e.tensor_tensor(out=Dm[:], in0=Dm[:], in1=SU, op=ALU.mult), [Dm, cst], [Dm])
            op("pe", lambda e: e.matmul(ps[:, 0:128], lhsT=kT[:, tsl], rhs=kT[:, tsl], start=True, stop=True), [kT], [ps])
            op("pe", lambda e: e.matmul(ps[:, 128:256], lhsT=kT[:, tsl], rhs=qT[:, tsl], start=True, stop=True), [kT, qT], [ps])
            yield
            op("dve", lambda e: e.scalar_tensor_tensor(out=A_, in0=ps[:, 0:128], scalar=nbeta[:, ti, h:h + 1], in1=Dm[:], op0=ALU.mult, op1=ALU.mult),
               [ps, nbeta, Dm], [AB])
            op("dve", lambda e: e.tensor_tensor(out=attT[:], in0=ps[:, 128:256], in1=DTm[:], op=ALU.mult), [ps, DTm], [attT])
            op("pe", lambda e: e.transpose(out=ps[:, 0:128], in_=A_, identity=ident), [AB, cst], [ps])
            yield
            op("act", lambda e: e.activation(out=B_, in_=ps[:, 0:128], func=AF.Copy), [ps], [AB])
            op("pool", lambda e: e.tensor_tensor(out=Tt[:], in0=B_, in1=ident, op=ALU.add), [AB, cst], [Tt])
            for lvl in range(1, 6):
                op("pe", lambda e: e.matmul(ps[:, 0:128], lhsT=B_, rhs=A_, start=True, stop=True), [AB], [ps])
                if lvl < 5:
                    op("pe", lambda e: e.matmul(ps[:, 128:256], lhsT=A_, rhs=B_, start=True, stop=True), [AB], [ps])
                yield
                if lvl < 5:
                    op("act", lambda e: e.activation(out=AB[:].rearrange("p a b -> p (a b)"), in_=ps[:, 0:256], func=AF.Copy), [ps], [AB])
                else:
                    op("act", lambda e: e.activation(out=A_, in_=ps[:, 0:128], func=AF.Copy), [ps], [AB])
                op("pe", lambda e: e.matmul(ps[:, 256:384], lhsT=A_, rhs=Tt[:], start=True, stop=True), [AB, Tt], [ps])
                yield
                op("dve", lambda e: e.tensor_tensor(out=Tt[:], in0=ps[:, 256:384], in1=Tt[:], op=ALU.add), [ps, Tt], [Tt])
            op("act", lambda e: e.activation(out=Ttb[:], in_=Tt[:], func=AF.Copy), [Tt], [Ttb])
            pt_ = B["ptr"]
            op("pe", lambda e: e.transpose(out=pt_[:, 0, :], in_=kT[:, tsl], identity=cstb[:, C_ID, :]), [kT, cstb], [pt_])
            op("pe", lambda e: e.transpose(out=pt_[:, 1, :], in_=vTb[h][:, tsl], identity=cstb[:, C_ID, :]), [vTb[h], cstb], [pt_])
            op("dve", lambda e: e.tensor_scalar(out=kbg[:], in0=pt_[:, 0, :], scalar1=be[:, h:h + 1], scalar2=None, op0=ALU.mult), [pt_, be], [kbg])
            op("dve", lambda e: e.tensor_scalar(out=kd[:], in0=pt_[:, 0, :], scalar1=ed[:, h:h + 1], scalar2=None, op0=ALU.mult), [pt_, ed], [kd])
            op("dve", lambda e: e.tensor_scalar(out=bv[:], in0=pt_[:, 1, :], scalar1=beta[:, ti, h:h + 1], scalar2=None, op0=ALU.mult), [pt_, beta], [bv])
            op("dve", lambda e: e.tensor_scalar(out=dg[:], in0=ident, scalar1=eg[:, h:h + 1], scalar2=None, op0=ALU.mult), [cst, eg], [dg])
            op("pe", lambda e: e.matmul(ps[:, 0:128], lhsT=Ttb[:], rhs=bv[:], start=True, stop=True), [Ttb, bv], [ps])
            op("pe", lambda e: e.matmul(ps[:, 128:256], lhsT=kbg[:], rhs=Ttb[:], start=True, stop=True), [kbg, Ttb], [ps])
            op("pe", lambda e: e.matmul(ps[:, 256:384], lhsT=ONES, rhs=dg[:], start=True, stop=True), [cst, dg], [ps])
            yield
            op("dve", lambda e: e.tensor_copy(out=U[:], in_=ps[:, 0:128]), [ps], [U])
            op("dve", lambda e: e.tensor_copy(out=WT[:], in_=ps[:, 128:256]), [ps], [WT])
            op("dve", lambda e: e.tensor_tensor(out=qg[:], in0=ps[:, 256:384], in1=qT[:, tsl], op=ALU.mult), [ps, qT], [qg])

        def chain(ti, h, st, B, half):
            rows = slice(half * 64, (half + 1) * 64)
            egl = st["egl"]
            U, WT, qg, attT, kd, vnew = B["U"], B["WT"], B["qg"], B["attT"], B["kd"], B["vnew"]
            pw = banks.get()
            op("pe", lambda e: e.matmul(pw[rows, 0:128], lhsT=WT[:, rows], rhs=Sb[h][:], start=True, stop=True), [WT, Sb[h]], [pw])
            yield
            op("dve", lambda e: e.tensor_tensor(out=vnew[rows, :], in0=U[rows, :], in1=pw[rows, 0:128], op=ALU.subtract), [U, pw], [vnew])
            po = banks.get()
            op("pe", lambda e: e.matmul(po[rows, 0:128], lhsT=qg[:, rows], rhs=Sb[h][:], start=True, stop=False), [qg, Sb[h]], [po])
            op("pe", lambda e: e.matmul(po[rows, 0:128], lhsT=attT[rows, rows], rhs=vnew[rows, :], start=False, stop=True), [attT, vnew], [po])
            pk = banks.get()
            op("pe", lambda e: e.matmul(pk[:, 0:128], lhsT=kd[rows, :], rhs=vnew[rows, :], start=True, stop=True), [kd, vnew], [pk])
            yield
            op("dve", lambda e: e.scalar_tensor_tensor(out=Sst[h][:], in0=Sst[h][:], scalar=egl[:, half * 2 + h:half * 2 + h + 1], in1=pk[:, 0:128],
                                                       op0=ALU.mult, op1=ALU.add), [Sst[h], egl, pk], [Sst[h]])
            op("act", lambda e: e.activation(out=Sb[h][:], in_=Sst[h][:], func=AF.Copy), [Sst[h]], [Sb[h]])
            op("act", lambda e: e.activation(out=o_tok[h][rows, ti, :], in_=po[rows, 0:128], func=AF.Copy), [po], [o_tok[h]])

        def run_interleaved(gens):
            gens = list(gens)
            if SEQ_DEBUG:
                for g in gens:
                    for _ in g:
                        pass
                return
            while gens:
                nxt = []
                for g in gens:
                    try:
                        next(g)
                        nxt.append(g)
                    except StopIteration:
                        pass
                gens = nxt

        for tp in range(2):
            tis = (2 * tp, 2 * tp + 1)
            for a, ti in enumerate(tis):
                tile_common(ti, TST[a])
            run_interleaved([prep(ti, h, TST[a], GB[a * 2 + h]) for a, ti in enumerate(tis) for h in range(2)])
            for a, ti in enumerate(tis):
                for half in range(2):
                    run_interleaved([chain(ti, h, TST[a], GB[a * 2 + h], half) for h in range(2)])

        for h in range(2):
            if stop <= 5:
                break
            for ti in range(4):
                op("act", lambda e: e.activation(out=junk[:], in_=o_tok[h][:, ti, :], func=AF.Square, accum_out=ss[:, ti:ti + 1]), [o_tok[h]], [junk, ss])
            rstd_from_ss(k, ss, rstd, 4, 1.0 / 128.0)
            ys = yTs[cnt_y % 2]
            cnt_y += 1
            for ti in range(4):
                op("dve", lambda e: e.scalar_tensor_tensor(out=on[:], in0=o_tok[h][:, ti, :], scalar=rstd[:, ti:ti + 1], in1=dnb[:], op0=ALU.mult, op1=ALU.mult),
                   [o_tok[h], rstd, dnb], [on])
                op("pool", lambda e: e.tensor_tensor(out=ytok[:], in0=on[:], in1=sz[ti][:, h * 128:(h + 1) * 128], op=ALU.mult), [on, sz[ti]], [ytok])
                op("pe", lambda e: e.transpose(out=ptr[:, 0, :], in_=ytok[:], identity=cstb[:, C_ID, :]), [ytok, cstb], [ptr])
                op("act", lambda e: e.activation(out=ys[:, ti * 128:(ti + 1) * 128], in_=ptr[:, 0, :], func=AF.Copy), [ptr], [ys])
            out_toks.append(k.dma("sp", yT[h * 128:(h + 1) * 128, blk * 512:(blk + 1) * 512], ys[:], reads=[ys]))
    k.finish(out_toks)
    k.release(0)
    return nc, k


def phaseA1_inmaps(h, inp):
    maps = []
    consts = make_consts()
    w_in = inp["w_in_c"][0]
    wa = np.ascontiguousarray(inp["w_ada"][1][:, 0:2048])
    ba = np.ascontiguousarray(inp["b_ada"][1][0:2048])
    cw = inp["conv_c_w"][0]
    for core in range(NCORES):
        b, hg = core // 4, core % 4
        hs = [2 * hg, 2 * hg + 1]
        cols = []
        for base in (0, 1024, 2048):
            for hh in hs:
                cols.append(base + np.arange(hh * 128, (hh + 1) * 128))
        for hh in hs:
            cols.append(3072 + np.arange(hh * 128, (hh + 1) * 128))
        cols.append(np.array([4096 + hs[0], 4096 + hs[1], 4104 + hs[0], 4104 + hs[1]]))
        cols = np.concatenate(cols)
        pcol = np.zeros((128, 6, 4), np.float32)
        j = 0
        for base in (0, 1024, 2048):
            for hh in hs:
                pcol[:, j, :] = cw[:, base + hh * 128:base + (hh + 1) * 128].T
                j += 1
        prm = np.zeros((128, 4), np.float32)
        prm[:, 0] = inp["dn_dt_bias"][0][hs[0]]
        prm[:, 1] = inp["dn_dt_bias"][0][hs[1]]
        maps.append({
            "h_in": np.ascontiguousarray(h[b]),
            "condT": np.ascontiguousarray(inp["c"][b].reshape(8, 128).T),
            "w_ada": wa, "b_ada": ba,
            "norm1": np.ascontiguousarray(inp["norm1"][1]),
            "w_in": np.ascontiguousarray(w_in[:, cols]),
            "pcol": np.ascontiguousarray(pcol.reshape(128, 24)),
            "prm": prm,
            "a_log": np.ascontiguousarray(np.tile(inp["dn_a_log"][0][hs][None, :], (128, 1))),
            "dnorm": np.ascontiguousarray(inp["dn_norm"][0]),
            "consts": consts,
        })
    return maps


def assemble_yT_A1(results):
    yT = np.zeros((2, D, S), ml_dtypes.bfloat16)
    for core in range(NCORES):
        b, hg = core // 4, core % 4
        yT[b, hg * 256:(hg + 1) * 256] = results[core]["yT"]
    return yT


_PROGS = {}


def _prog(name):
    if name not in _PROGS:
        if name == "A0":
            _PROGS[name] = build_phaseA0()[0]
        elif name == "A1":
            _PROGS[name] = build_phaseA1()[0]
        elif name == "B0":
            _PROGS[name] = build_phaseB(False)[0]
        else:
            _PROGS[name] = build_phaseB(True)[0]
    return _PROGS[name]


def _gather_B(results):
    return np.stack([np.concatenate([results[b * 4 + q]["out"] for q in range(4)], 0) for b in range(2)])


def kernel(**inputs):
    inp = {k_: np.ascontiguousarray(np.asarray(v, dtype=np.float32)) for k_, v in inputs.items()}
    cores = list(range(NCORES))
    x = inp["x"]
    r = run_bass_kernel_spmd(_prog("A0"), phaseA0_inmaps(x, inp), core_ids=cores)
    yT0 = assemble_yT_A0(r.results)
    r = run_bass_kernel_spmd(_prog("B0"), phaseB_inmaps(0, x, yT0, inp, False), core_ids=cores)
    h0 = _gather_B(r.results)
    r = run_bass_kernel_spmd(_prog("A1"), phaseA1_inmaps(h0, inp), core_ids=cores)
    yT1 = assemble_yT_A1(r.results)
    r = run_bass_kernel_spmd(_prog("B1"), phaseB_inmaps(1, h0, yT1, inp, True), core_ids=cores)
    return _gather_B(r.results).astype(np.float32)
```

```python
import numpy as np
import ml_dtypes
import concourse.bass as bass
import concourse.mybir as mybir
from concourse.bass_utils import run_bass_kernel_spmd

F32 = mybir.dt.float32
BF16 = mybir.dt.bfloat16
AF = mybir.ActivationFunctionType
ALU = mybir.AluOpType
AX = mybir.AxisListType

D = 1024
S = 8192
EPS = 1e-6
NCORES = 8


class T:
    __slots__ = ("h", "w", "r", "name")

    def __init__(self, h, name=""):
        self.h = h
        self.w = None
        self.r = {}
        self.name = name

    def __getitem__(self, idx):
        return self.h[idx]


class KB:
    NDMA_SEM = 6

    def __init__(self, nc):
        self.nc = nc
        self.eng = {"pe": nc.tensor, "act": nc.scalar, "dve": nc.vector, "pool": nc.gpsimd, "sp": nc.sync}
        self.csem = {e: nc.alloc_semaphore("cs_" + e) for e in ("pe", "act", "dve", "pool")}
        self.cnt = {e: 0 for e in self.csem}
        self.pending = {e: False for e in self.csem}
        self.dsem = {}
        self.dcnt = {}
        for q in ("sp", "pool", "act"):
            self.dsem[q] = [nc.alloc_semaphore("ds_%s%d" % (q, i)) for i in range(self.NDMA_SEM)]
            self.dcnt[q] = 0
        self.seen = {e: {} for e in self.eng}
        self.fast_pe = False
        self.ninst = 0
        self.stack = []
        self.tiles = []
        self.freed = {}
        self.uid = 0

    def sb(self, shape, dt=F32, name=None):
        self.uid += 1
        nm = "%s_%d" % (name or "t", self.uid)
        g = self.nc.sbuf_tensor(nm, list(shape), dt)
        h = g.__enter__()
        self.stack.append(g)
        t = T(h, nm)
        t.r = dict(self.freed)
        self.tiles.append(t)
        return t

    def ps(self, shape, dt=F32, name=None):
        self.uid += 1
        nm = "%s_%d" % (name or "p", self.uid)
        g = self.nc.psum_tensor(nm, list(shape), dt)
        h = g.__enter__()
        self.stack.append(g)
        t = T(h, nm)
        t.r = dict(self.freed)
        self.tiles.append(t)
        return t

    def mark(self):
        return len(self.stack)

    def release(self, mark):
        while len(self.stack) > mark:
            g = self.stack.pop()
            t = self.tiles.pop()
            toks = list(t.r.values()) + ([t.w] if t.w is not None else [])
            for tok in toks:
                o = self.freed.get(tok[0])
                if o is None or o[2] < tok[2]:
                    self.freed[tok[0]] = tok
            g.__exit__(None, None, None)

    def _deps(self, e, reads, writes):
        need = {}

        def add(tok):
            if tok is None:
                return
            key, sem, val = tok
            if key == "pe" and e == "pe" and self.fast_pe:
                return
            if self.seen[e].get(key, 0) >= val:
                return
            if key not in need or need[key][1] < val:
                need[key] = (sem, val)

        for t in reads:
            add(t.w)
        for t in writes:
            add(t.w)
            for tok in t.r.values():
                add(tok)
        for key, (sem, val) in need.items():
            self.eng[e].wait_ge(sem, val)
            self.seen[e][key] = val

    def _commit(self, tok, reads, writes):
        key = tok[0]
        for t in reads:
            o = t.r.get(key)
            if o is None or o[2] < tok[2]:
                t.r[key] = tok
        for t in writes:
            t.w = tok
            t.r = {}

    def op(self, e, fn, reads=(), writes=(), inc=True, fast=False):
        inc = True
        self.fast_pe = fast
        self._deps(e, reads, writes)
        self.fast_pe = False
        ins = fn(self.eng[e])
        if inc:
            self.cnt[e] += 1
            ins.then_inc(self.csem[e], 1)
            tok = (e, self.csem[e], self.cnt[e])
            self.pending[e] = False
        else:
            tok = (e, self.csem[e], self.cnt[e] + 1)
            self.pending[e] = True
        self._commit(tok, reads, writes)
        self.ninst += 1
        return tok

    def dma(self, q, out, in_, reads=(), writes=(), **kw):
        i = self.dcnt[q]
        self.dcnt[q] += 1
        slot = i % self.NDMA_SEM
        rnd = i // self.NDMA_SEM
        sem = self.dsem[q][slot]
        key = ("d", q, slot)
        if rnd > 0 and self.seen[q].get(key, 0) < 16 * rnd:
            self.eng[q].wait_ge(sem, 16 * rnd)
            self.seen[q][key] = 16 * rnd
        self._deps(q, reads, writes)
        ins = self.eng[q].dma_start(out=out, in_=in_, **kw)
        ins.then_inc(sem, 16)
        tok = (key, sem, 16 * (rnd + 1))
        self._commit(tok, reads, writes)
        self.ninst += 1
        return tok

    def finish(self, toks):
        for e in self.pending:
            assert not self.pending[e], "engine %s ends with a non-incrementing instruction" % e
        toks = list(toks)
        for e in self.csem:
            if self.cnt[e] > 0:
                toks.append((e, self.csem[e], self.cnt[e]))
        for q in self.dsem:
            n = self.dcnt[q]
            for slot in range(self.NDMA_SEM):
                if n > slot:
                    rounds = (n - slot + self.NDMA_SEM - 1) // self.NDMA_SEM
                    toks.append((("d", q, slot), self.dsem[q][slot], 16 * rounds))
        for tok in toks:
            key, sem, val = tok
            if self.seen["sp"].get(key, 0) < val:
                self.eng["sp"].wait_ge(sem, val)
                self.seen["sp"][key] = val


def _bc(ap, shape):
    return ap.to_broadcast(list(shape))


def load_consts(k, nc, consts_ap):
    c = k.sb([128, NCONST, 128], F32, "consts")
    k.dma("sp", c[:], consts_ap, writes=[c])
    cb = k.sb([128, NCONST, 128], BF16, "consts_bf")
    k.op("dve", lambda e: e.tensor_copy(out=cb[:], in_=c[:]), reads=[c], writes=[cb])
    return c, cb


NCONST = 10
C_ID = 0
C_TRI = 1
C_SU = 2
C_BLK = 3
C_M16 = 4
C_MC1 = 5
C_MC2 = 6
C_ONES = 7
C_UT64 = 8
C_CH0 = 9


def make_consts():
    p = np.arange(128)
    i = p[:, None]
    j = p[None, :]
    c = np.zeros((128, NCONST, 128), np.float32)
    same64 = (i // 64) == (j // 64)
    same32 = (i // 32) == (j // 32)
    same16 = (i // 16) == (j // 16)
    c[:, C_ID] = (i == j)
    c[:, C_TRI] = same64 & (i <= j)
    c[:, C_SU] = same64 & (j < i)
    c[:, C_BLK] = same64
    c[:, C_M16] = same16 & (j < i)
    c[:, C_MC1] = same32 & (~same16) & (j < i)
    c[:, C_MC2] = same64 & (~same32) & (j < i)
    c[:, C_ONES] = 1.0
    c[:, C_UT64] = same64 & (i <= j)
    c[:, C_CH0, 0] = (p < 64)
    c[:, C_CH0, 1] = (p >= 64)
    return c


def compute_mod(k, nc, condT_ap, w_ada_ap, b_ada_ap, ncols, outs, ps_pool):
    m0 = k.mark()
    ct = k.sb([128, 8], F32, "ct")
    k.dma("sp", ct[:], condT_ap, writes=[ct])
    sg = k.sb([128, 8], F32, "sg")
    k.op("act", lambda e: e.activation(out=sg[:], in_=ct[:], func=AF.Sigmoid), reads=[ct], writes=[sg])
    cond = k.sb([128, 8], F32, "cond")
    k.op("dve", lambda e: e.tensor_tensor(out=cond[:], in0=ct[:], in1=sg[:], op=ALU.mult), reads=[ct, sg], writes=[cond])
    cbc = k.sb([128, 8, 128], BF16, "cond_bc")
    k.op("dve", lambda e: e.tensor_copy(out=cbc[:], in_=_bc(cond[:].unsqueeze(2), [128, 8, 128])), reads=[cond], writes=[cbc])
    wv = w_ada_ap.rearrange("(k p) n -> p k n", p=128)
    nch = ncols // 512
    wbuf = [k.sb([128, 8, 512], BF16, "wada%d" % i) for i in range(2)]
    bbuf = [k.sb([128, 512], F32, "bada%d" % i) for i in range(2)]
    for j in range(nch):
        wb = wbuf[j % 2]
        bb = bbuf[j % 2]
        k.dma("pool", wb[:], wv[:, :, j * 512:(j + 1) * 512], writes=[wb])
        k.dma("sp", bb[:], b_ada_ap[j * 512:(j + 1) * 512].partition_broadcast(128), writes=[bb])
        pt = ps_pool[j % len(ps_pool)]
        for kk in range(8):
            k.op("pe", lambda e, kk=kk: e.matmul(pt[:, 0:512], lhsT=cbc[:, kk, :], rhs=wb[:, kk, :], start=(kk == 0), stop=(kk == 7)),
                 reads=[cbc, wb], writes=[pt], fast=True)
        o = outs[j // 2]
        c0 = (j % 2) * 512
        k.op("dve", lambda e: e.tensor_tensor(out=o[:, c0:c0 + 512], in0=pt[:, 0:512], in1=bb[:], op=ALU.add),
             reads=[pt, bb], writes=[o])
    k.release(m0)


def rstd_from_ss(k, ss, rstd, n, scale):
    k.op("dve", lambda e: e.tensor_scalar(out=rstd[:, 0:n], in0=ss[:, 0:n], scalar1=scale, scalar2=EPS, op0=ALU.mult, op1=ALU.add),
         reads=[ss], writes=[rstd])
    k.op("act", lambda e: e.activation(out=rstd[:, 0:n], in_=rstd[:, 0:n], func=AF.Sqrt), reads=[rstd], writes=[rstd])
    k.op("dve", lambda e: e.reciprocal(out=rstd[:, 0:n], in_=rstd[:, 0:n]), reads=[rstd], writes=[rstd])


NTB = 2048
NE = 32


def build_phaseB(final, upto=99, ne=NE):
    nc = bass.Bass("TRN2", target_bir_lowering=False)
    dt = nc.dram_tensor
    h_in = dt("h_in", [NTB, D], F32, kind="ExternalInput").ap()
    yT = dt("yT", [D, NTB], BF16, kind="ExternalInput").ap()
    w_out = dt("w_out", [D, D], F32, kind="ExternalInput").ap()
    condT = dt("condT", [128, 8], F32, kind="ExternalInput").ap()
    w_ada = dt("w_ada", [D, 4096], F32, kind="ExternalInput").ap()
    b_ada = dt("b_ada", [4096], F32, kind="ExternalInput").ap()
    norm2 = dt("norm2", [D], F32, kind="ExternalInput").ap()
    w_r = dt("w_r", [D, 36], F32, kind="ExternalInput").ap()
    b_r = dt("b_r", [36], F32, kind="ExternalInput").ap()
    if upto >= 5:
        w1 = dt("w1", [ne, D, 512], F32, kind="ExternalInput").ap()
        w3 = dt("w3", [ne, D, 512], F32, kind="ExternalInput").ap()
        w2 = dt("w2", [ne, 512, D], F32, kind="ExternalInput").ap()
    fnorm = dt("fnorm", [D], F32, kind="ExternalInput").ap()
    consts = dt("consts", [128, NCONST, 128], F32, kind="ExternalInput").ap()
    out = dt("out", [NTB, D], F32, kind="ExternalOutput").ap()

    k = KB(nc)
    NT = NTB // 128
    cst, cstb = load_consts(k, nc, consts)
    ident = cst

    hres = [k.sb([128, D], F32, "hres%d" % t) for t in range(NT)]
    h_v = h_in.rearrange("(t p) d -> t p d", p=128)
    for t in range(NT):
        k.dma("sp", hres[t][:], h_v[t], writes=[hres[t]])
    gt2 = k.sb([128, D], F32, "gt2")
    u2T = k.sb([128, 8, NTB], BF16, "u2T")
    logits = k.sb([128, NT, 36], F32, "logits")
    Wd = k.sb([128, NT, NE], F32, "Wd")
    m_mod = k.mark()
    gt1 = k.sb([128, D], F32, "gt1")
    sh2 = k.sb([128, D], F32, "sh2")
    sc2 = k.sb([128, D], F32, "sc2")

    if upto < 1:
        return _finB(k, out, hres, NT)
    m1 = k.mark()
    pp = [k.ps([128, 512], F32, "modps%d" % i) for i in range(2)]
    compute_mod(k, nc, condT, w_ada, b_ada, 4096, [gt1, sh2, sc2, gt2], pp)
    k.release(m1)
    g2 = k.sb([128, D], F32, "g2")
    k.dma("sp", g2[:], norm2.partition_broadcast(128), writes=[g2])
    k.op("dve", lambda e: e.scalar_tensor_tensor(out=g2[:], in0=sc2[:], scalar=1.0, in1=g2[:], op0=ALU.add, op1=ALU.mult),
         reads=[sc2, g2], writes=[g2])

    if upto < 2:
        return _finB(k, out, hres, NT)
    m2 = k.mark()
    yTs = k.sb([128, 8, NTB], BF16, "yTs")
    yv = yT.rearrange("(k p) t -> p k t", p=128)
    for kk in range(8):
        k.dma("sp", yTs[:, kk, :], yv[:, kk, :], writes=[yTs])
    wo = k.sb([128, 8, D], BF16, "wo")
    wov = w_out.rearrange("(k p) n -> p k n", p=128)
    for kk in range(0, 8, 2):
        k.dma("pool", wo[:, kk:kk + 2, :], wov[:, kk:kk + 2, :], writes=[wo])
    k.op("pool", lambda e: e.tensor_tensor(out=wo[:], in0=wo[:], in1=_bc(gt1[:].unsqueeze(1), [128, 8, D]), op=ALU.mult),
         reads=[wo, gt1], writes=[wo])
    psy = [k.ps([128, D], F32, "psy%d" % i) for i in range(2)]
    for t in range(NT):
        p = psy[t % 2]
        for half in range(2):
            for kk in range(8):
                k.op("pe", lambda e, kk=kk, half=half: e.matmul(p[:, half * 512:(half + 1) * 512], lhsT=yTs[:, kk, t * 128:(t + 1) * 128],
                                                                 rhs=wo[:, kk, half * 512:(half + 1) * 512], start=(kk == 0), stop=(kk == 7)),
                     reads=[yTs, wo], writes=[p], fast=True)
        k.op("dve", lambda e: e.tensor_tensor(out=hres[t][:], in0=p[:], in1=hres[t][:], op=ALU.add), reads=[p, hres[t]], writes=[hres[t]])
    k.release(m2)

    if upto < 3:
        return _finB(k, out, hres, NT)
    m3 = k.mark()
    ss = k.sb([128, NT], F32, "ss")
    rstd = k.sb([128, NT], F32, "rstd")
    junk = [k.sb([128, D], BF16, "junk%d" % i) for i in range(2)]
    for t in range(NT):
        jk = junk[t % 2]
        k.op("act", lambda e: e.activation(out=jk[:], in_=hres[t][:], func=AF.Square, accum_out=ss[:, t:t + 1]),
             reads=[hres[t]], writes=[jk, ss])
    rstd_from_ss(k, ss, rstd, NT, 1.0 / D)
    wr = k.sb([128, 8, 36], F32, "wr")
    k.dma("sp", wr[:], w_r.rearrange("(k p) n -> p k n", p=128), writes=[wr])
    brb = k.sb([128, 36], F32, "brb")
    k.dma("sp", brb[:], b_r.partition_broadcast(128), writes=[brb])
    t1b = [k.sb([128, D], F32, "t1b%d" % i) for i in range(2)]
    u32 = [k.sb([128, D], F32, "u32_%d" % i) for i in range(2)]
    uT32 = [k.sb([128, 8, 128], F32, "uT32_%d" % i) for i in range(2)]
    pst = [k.ps([128, 8, 128], F32, "pst%d" % i) for i in range(2)]
    psr = [k.ps([128, 36], F32, "psr%d" % i) for i in range(2)]
    for t in range(NT):
        a = t1b[t % 2]
        u = u32[t % 2]
        ut = uT32[t % 2]
        pt = pst[t % 2]
        pr = psr[t % 2]
        k.op("dve", lambda e: e.scalar_tensor_tensor(out=a[:], in0=hres[t][:], scalar=rstd[:, t:t + 1], in1=g2[:], op0=ALU.mult, op1=ALU.mult),
             reads=[hres[t], rstd, g2], writes=[a])
        k.op("pool", lambda e: e.tensor_tensor(out=u[:], in0=a[:], in1=sh2[:], op=ALU.add), reads=[a, sh2], writes=[u])
        for kk in range(8):
            k.op("pe", lambda e, kk=kk: e.transpose(out=pt[:, kk, :], in_=u[:, kk * 128:(kk + 1) * 128], identity=cst[:, C_ID, :]),
                 reads=[u, cst], writes=[pt], fast=True)
        k.op("act", lambda e: e.activation(out=ut[:], in_=pt[:], func=AF.Copy), reads=[pt], writes=[ut])
        k.op("pool", lambda e: e.tensor_copy(out=u2T[:, :, t * 128:(t + 1) * 128], in_=ut[:]), reads=[ut], writes=[u2T])
        for kk in range(8):
            k.op("pe", lambda e, kk=kk: e.matmul(pr[:], lhsT=ut[:, kk, :], rhs=wr[:, kk, :], start=(kk == 0), stop=(kk == 7)),
                 reads=[ut, wr], writes=[pr], fast=True)
        k.op("dve", lambda e: e.tensor_tensor(out=logits[:, t, :], in0=pr[:], in1=brb[:], op=ALU.add), reads=[pr, brb], writes=[logits])
    k.release(m3)

    if upto < 4:
        return _finB(k, out, hres, NT)
    m4 = k.mark()
    BIG = 1.0e30

    def dve(fn, reads, writes):
        k.op("dve", fn, reads=reads, writes=writes)

    lg = logits[:, :, 0:4]
    le = logits[:, :, 4:36]
    gmax = k.sb([128, NT], F32, "gmax")
    dve(lambda e: e.tensor_reduce(out=gmax[:], in_=lg, axis=AX.X, op=ALU.max), [logits], [gmax])
    eg = k.sb([128, NT, 4], F32, "eg")
    dve(lambda e: e.tensor_tensor(out=eg[:], in0=lg, in1=_bc(gmax[:].unsqueeze(2), [128, NT, 4]), op=ALU.subtract), [logits, gmax], [eg])
    k.op("act", lambda e: e.activation(out=eg[:], in_=eg[:], func=AF.Exp), reads=[eg], writes=[eg])
    gsum = k.sb([128, NT], F32, "gsum")
    dve(lambda e: e.tensor_reduce(out=gsum[:], in_=eg[:], axis=AX.X, op=ALU.add), [eg], [gsum])
    pgt = k.sb([128, NT], F32, "pgt")
    dve(lambda e: e.reciprocal(out=pgt[:], in_=gsum[:]), [gsum], [pgt])
    pen = k.sb([128, NT, 4], F32, "pen")
    dve(lambda e: e.tensor_tensor(out=pen[:], in0=lg, in1=_bc(gmax[:].unsqueeze(2), [128, NT, 4]), op=ALU.is_equal), [logits, gmax], [pen])
    dve(lambda e: e.tensor_scalar(out=pen[:], in0=pen[:], scalar1=1.0, scalar2=BIG, op0=ALU.subtract, op1=ALU.mult), [pen], [pen])
    lem = k.sb([128, NT, NE], F32, "lem")
    dve(lambda e: e.tensor_tensor(out=lem[:].rearrange("p t (g x) -> p t g x", g=4), in0=le.rearrange("p t (g x) -> p t g x", g=4),
                                  in1=_bc(pen[:].unsqueeze(3), [128, NT, 4, 8]), op=ALU.add), [logits, pen], [lem])
    mx1 = k.sb([128, NT], F32, "mx1")
    dve(lambda e: e.tensor_reduce(out=mx1[:], in_=lem[:], axis=AX.X, op=ALU.max), [lem], [mx1])
    oh1 = k.sb([128, NT, NE], F32, "oh1")
    dve(lambda e: e.tensor_tensor(out=oh1[:], in0=lem[:], in1=_bc(mx1[:].unsqueeze(2), [128, NT, NE]), op=ALU.is_equal), [lem, mx1], [oh1])
    lem2 = k.sb([128, NT, NE], F32, "lem2")
    dve(lambda e: e.scalar_tensor_tensor(out=lem2[:], in0=oh1[:], scalar=-BIG, in1=lem[:], op0=ALU.mult, op1=ALU.add), [oh1, lem], [lem2])
    mx2 = k.sb([128, NT], F32, "mx2")
    dve(lambda e: e.tensor_reduce(out=mx2[:], in_=lem2[:], axis=AX.X, op=ALU.max), [lem2], [mx2])
    oh2 = k.sb([128, NT, NE], F32, "oh2")
    dve(lambda e: e.tensor_tensor(out=oh2[:], in0=lem2[:], in1=_bc(mx2[:].unsqueeze(2), [128, NT, NE]), op=ALU.is_equal), [lem2, mx2], [oh2])
    rr = k.sb([128, NT], F32, "rr")
    dve(lambda e: e.tensor_tensor(out=rr[:], in0=mx2[:], in1=mx1[:], op=ALU.subtract), [mx2, mx1], [rr])
    k.op("act", lambda e: e.activation(out=rr[:], in_=rr[:], func=AF.Exp), reads=[rr], writes=[rr])
    den = k.sb([128, NT], F32, "den")
    dve(lambda e: e.tensor_scalar(out=den[:], in0=rr[:], scalar1=1.0, scalar2=None, op0=ALU.add), [rr], [den])
    dve(lambda e: e.reciprocal(out=den[:], in_=den[:]), [den], [den])
    wt1 = k.sb([128, NT], F32, "wt1")
    dve(lambda e: e.tensor_tensor(out=wt1[:], in0=pgt[:], in1=den[:], op=ALU.mult), [pgt, den], [wt1])
    wt2 = k.sb([128, NT], F32, "wt2")
    dve(lambda e: e.tensor_tensor(out=wt2[:], in0=wt1[:], in1=rr[:], op=ALU.mult), [wt1, rr], [wt2])
    dve(lambda e: e.tensor_tensor(out=Wd[:], in0=oh1[:], in1=_bc(wt1[:].unsqueeze(2), [128, NT, NE]), op=ALU.mult), [oh1, wt1], [Wd])
    dve(lambda e: e.tensor_tensor(out=oh2[:], in0=oh2[:], in1=_bc(wt2[:].unsqueeze(2), [128, NT, NE]), op=ALU.mult), [oh2, wt2], [oh2])
    dve(lambda e: e.tensor_tensor(out=Wd[:], in0=Wd[:], in1=oh2[:], op=ALU.add), [Wd, oh2], [Wd])
    k.release(m4)

    if upto < 5:
        return _finB(k, out, hres, NT)
    k.release(m_mod)
    m5 = k.mark()
    w1b = [k.sb([128, 8, 512], BF16, "w1b%d" % i) for i in range(2)]
    w3b = [k.sb([128, 8, 512], BF16, "w3b%d" % i) for i in range(2)]
    w2b = [k.sb([128, 4, D], BF16, "w2b%d" % i) for i in range(2)]
    stg = [k.sb([128, 4096], F32, "stg%d" % i) for i in range(2)]
    actT = [[k.sb([128, 512], BF16, "actT%d_%d" % (i, f)) for f in range(4)] for i in range(2)]
    slb = [k.sb([128, 512], F32, "slb%d" % i) for i in range(2)]
    ps1 = [k.ps([128, 512], F32, "ps1_%d" % i) for i in range(2)]
    ps3 = [k.ps([128, 512], F32, "ps3_%d" % i) for i in range(2)]
    psy = [k.ps([128, D], F32, "psye%d" % i) for i in range(2)]
    cnt_f = 0
    cnt_y = 0
    for ex in range(ne):
        b = ex % 2
        s1 = stg[(3 * ex) % 2]
        k.dma("sp", s1[:].rearrange("p (k f) -> p k f", k=8), w1[ex].rearrange("(k p) f -> p k f", p=128), writes=[s1])
        k.op("pool", lambda e: e.tensor_copy(out=w1b[b][:].rearrange("p k f -> p (k f)"), in_=s1[:]), reads=[s1], writes=[w1b[b]])
        s3 = stg[(3 * ex + 1) % 2]
        k.dma("sp", s3[:].rearrange("p (k f) -> p k f", k=8), w3[ex].rearrange("(k p) f -> p k f", p=128), writes=[s3])
        k.op("pool", lambda e: e.tensor_copy(out=w3b[b][:].rearrange("p k f -> p (k f)"), in_=s3[:]), reads=[s3], writes=[w3b[b]])
        s2 = stg[(3 * ex + 2) % 2]
        k.dma("sp", s2[:].rearrange("p (c d) -> p c d", c=4), w2[ex].rearrange("(c p) d -> p c d", p=128), writes=[s2])
        k.op("pool", lambda e: e.tensor_tensor(out=w2b[b][:], in0=s2[:].rearrange("p (c d) -> p c d", c=4), in1=_bc(gt2[:].unsqueeze(1), [128, 4, D]), op=ALU.mult),
             reads=[s2, gt2], writes=[w2b[b]])
        for blk in range(NTB // 512):
            ab = actT[blk % 2]
            for fc in range(4):
                p1 = ps1[cnt_f % 2]
                p3 = ps3[cnt_f % 2]
                sl = slb[cnt_f % 2]
                cnt_f += 1
                for kk in range(8):
                    k.op("pe", lambda e, kk=kk: e.matmul(p1[:], lhsT=w1b[b][:, kk, fc * 128:(fc + 1) * 128], rhs=u2T[:, kk, blk * 512:(blk + 1) * 512],
                                                         start=(kk == 0), stop=(kk == 7)), reads=[w1b[b], u2T], writes=[p1], fast=True)
                for kk in range(8):
                    k.op("pe", lambda e, kk=kk: e.matmul(p3[:], lhsT=w3b[b][:, kk, fc * 128:(fc + 1) * 128], rhs=u2T[:, kk, blk * 512:(blk + 1) * 512],
                                                         start=(kk == 0), stop=(kk == 7)), reads=[w3b[b], u2T], writes=[p3], fast=True)
                k.op("act", lambda e: e.activation(out=sl[:], in_=p1[:], func=AF.Silu), reads=[p1], writes=[sl])
                k.op("dve", lambda e: e.tensor_tensor(out=ab[fc][:], in0=p3[:], in1=sl[:], op=ALU.mult), reads=[p3, sl], writes=[ab[fc]])
            for ti in range(4):
                t = blk * 4 + ti
                py = psy[cnt_y % 2]
                cnt_y += 1
                for half in range(2):
                    for fc in range(4):
                        k.op("pe", lambda e, fc=fc, half=half: e.matmul(py[:, half * 512:(half + 1) * 512], lhsT=ab[fc][:, ti * 128:(ti + 1) * 128],
                                                                         rhs=w2b[b][:, fc, half * 512:(half + 1) * 512], start=(fc == 0), stop=(fc == 3)),
                             reads=[ab[fc], w2b[b]], writes=[py], fast=True)
                k.op("dve", lambda e: e.scalar_tensor_tensor(out=hres[t][:], in0=py[:], scalar=Wd[:, t, ex:ex + 1], in1=hres[t][:],
                                                             op0=ALU.mult, op1=ALU.add), reads=[py, Wd, hres[t]], writes=[hres[t]])
    k.release(m5)

    toks = []
    o_v = out.rearrange("(t p) d -> t p d", p=128)
    if final:
        fn = k.sb([128, D], F32, "fn")
        k.dma("sp", fn[:], fnorm.partition_broadcast(128), writes=[fn])
        ss2 = k.sb([128, NT], F32, "ss2")
        rs2 = k.sb([128, NT], F32, "rs2")
        junk2 = [k.sb([128, D], BF16, "junkf%d" % i) for i in range(2)]
        for t in range(NT):
            jk = junk2[t % 2]
            k.op("act", lambda e: e.activation(out=jk[:], in_=hres[t][:], func=AF.Square, accum_out=ss2[:, t:t + 1]),
                 reads=[hres[t]], writes=[jk, ss2])
        rstd_from_ss(k, ss2, rs2, NT, 1.0 / D)
        for t in range(NT):
            k.op("dve", lambda e: e.scalar_tensor_tensor(out=hres[t][:], in0=hres[t][:], scalar=rs2[:, t:t + 1], in1=fn[:], op0=ALU.mult, op1=ALU.mult),
                 reads=[hres[t], rs2, fn], writes=[hres[t]])
    for t in range(NT):
        toks.append(k.dma("sp", o_v[t], hres[t][:], reads=[hres[t]]))
    k.finish(toks)
    k.release(0)
    return nc, k


def _finB(k, out, hres, NT):
    o_v = out.rearrange("(t p) d -> t p d", p=128)
    toks = [k.dma("sp", o_v[t], hres[t][:], reads=[hres[t]]) for t in range(NT)]
    k.finish(toks)
    k.release(0)
    return k.nc, k


def phaseB_inmaps(layer, h, yT_full, inp, final):
    maps = []
    wa = np.ascontiguousarray(inp["w_ada"][layer][:, 2048:6144])
    ba = np.ascontiguousarray(inp["b_ada"][layer][2048:6144])
    w_out = inp["w_out_ab"][0] if layer == 0 else inp["w_out_c"][0]
    w_r = np.ascontiguousarray(np.concatenate([inp["moe_w_grp"][layer], inp["moe_w_rt"][layer]], axis=1))
    b_r = np.ascontiguousarray(np.concatenate([inp["moe_b_grp"][layer], inp["moe_b_rt"][layer]], axis=0))
    consts = make_consts()
    for core in range(NCORES):
        b, q = core // 4, core % 4
        sl = slice(q * NTB, (q + 1) * NTB)
        maps.append({
            "h_in": np.ascontiguousarray(h[b, sl]),
            "yT": np.ascontiguousarray(yT_full[b][:, sl]),
            "w_out": np.ascontiguousarray(w_out),
            "condT": np.ascontiguousarray(inp["c"][b].reshape(8, 128).T),
            "w_ada": wa, "b_ada": ba,
            "norm2": np.ascontiguousarray(inp["norm2"][layer]),
            "w_r": w_r, "b_r": b_r,
            "w1": inp["moe_w1"][layer], "w3": inp["moe_w3"][layer], "w2": inp["moe_w2"][layer],
            "fnorm": np.ascontiguousarray(inp["final_norm"]),
            "consts": consts,
        })
    return maps


class UMaker:
    def __init__(self, k, nc, h_ap, condT, w_ada, b_ada, norm_ap, cstb):
        self.k = k
        self.cstb = cstb
        self.h_v = h_ap.rearrange("(t p) d -> t p d", p=128)
        self.sh = k.sb([128, D], F32, "sh1")
        self.sc = k.sb([128, D], F32, "sc1")
        m = k.mark()
        pp = [k.ps([128, 512], F32, "modps%d" % i) for i in range(2)]
        compute_mod(k, nc, condT, w_ada, b_ada, 2048, [self.sh, self.sc], pp)
        k.release(m)
        self.g = k.sb([128, D], F32, "g1")
        k.dma("sp", self.g[:], norm_ap.partition_broadcast(128), writes=[self.g])
        k.op("dve", lambda e: e.scalar_tensor_tensor(out=self.g[:], in0=self.sc[:], scalar=1.0, in1=self.g[:], op0=ALU.add, op1=ALU.mult),
             reads=[self.sc, self.g], writes=[self.g])
        self.ht = [k.sb([128, D], F32, "ht%d" % i) for i in range(4)]
        self.junk = k.sb([128, D], BF16, "junk")
        self.ss = k.sb([128, 4], F32, "ss")
        self.rstd = k.sb([128, 4], F32, "rstd")
        self.a = [k.sb([128, D], F32, "ua%d" % i) for i in range(2)]
        self.ub = [k.sb([128, D], BF16, "ub%d" % i) for i in range(2)]
        self.pst = [k.ps([128, 8, 128], BF16, "pst%d" % i) for i in range(1)]

    def block(self, blk, uT):
        k = self.k
        for ti in range(4):
            ht = self.ht[ti]
            k.dma("sp", ht[:], self.h_v[blk * 4 + ti], writes=[ht])
            k.op("act", lambda e: e.activation(out=self.junk[:], in_=ht[:], func=AF.Square, accum_out=self.ss[:, ti:ti + 1]),
                 reads=[ht], writes=[self.junk, self.ss])
        rstd_from_ss(k, self.ss, self.rstd, 4, 1.0 / D)
        for ti in range(4):
            ht = self.ht[ti]
            a = self.a[ti % 2]
            ub = self.ub[ti % 2]
            pt = self.pst[0]
            k.op("dve", lambda e: e.scalar_tensor_tensor(out=a[:], in0=ht[:], scalar=self.rstd[:, ti:ti + 1], in1=self.g[:], op0=ALU.mult, op1=ALU.mult),
                 reads=[ht, self.rstd, self.g], writes=[a])
            k.op("pool", lambda e: e.tensor_tensor(out=ub[:], in0=a[:], in1=self.sh[:], op=ALU.add), reads=[a, self.sh], writes=[ub])
            for kk in range(8):
                k.op("pe", lambda e: e.transpose(out=pt[:, kk, :], in_=ub[:, kk * 128:(kk + 1) * 128], identity=self.cstb[:, C_ID, :]),
                     reads=[ub, self.cstb], writes=[pt], fast=True)
            k.op("act", lambda e: e.activation(out=uT[:, :, ti * 128:(ti + 1) * 128], in_=pt[:], func=AF.Copy), reads=[pt], writes=[uT])


class Banks:
    def __init__(self, k, n):
        self.t = [k.ps([128, 512], F32, "bank%d" % i) for i in range(n)]
        self.i = 0

    def get(self):
        b = self.t[self.i % len(self.t)]
        self.i += 1
        return b


NCOL_A0 = 656


def build_phaseA0(nblk=16):
    nc = bass.Bass("TRN2", target_bir_lowering=False)
    dt = nc.dram_tensor
    h_in = dt("h_in", [S, D], F32, kind="ExternalInput").ap()
    condT = dt("condT", [128, 8], F32, kind="ExternalInput").ap()
    w_ada = dt("w_ada", [D, 2048], F32, kind="ExternalInput").ap()
    b_ada = dt("b_ada", [2048], F32, kind="ExternalInput").ap()
    norm1 = dt("norm1", [D], F32, kind="ExternalInput").ap()
    w_in = dt("w_in", [D, NCOL_A0], F32, kind="ExternalInput").ap()
    WAd = dt("WA", [128, 128], F32, kind="ExternalInput").ap()
    WXd = dt("WX", [128, 128], F32, kind="ExternalInput").ap()
    wg2d = dt("wg2", [16, 64], F32, kind="ExternalInput").ap()
    pcold = dt("pcol", [128, 16], F32, kind="ExternalInput").ap()
    consts = dt("consts", [128, NCONST, 128], F32, kind="ExternalInput").ap()
    yT = dt("yT", [256, S], BF16, kind="ExternalOutput").ap()

    k = KB(nc)
    cst, cstb = load_consts(k, nc, consts)
    um = UMaker(k, nc, h_in, condT, w_ada, b_ada, norm1, cstb)
    win = k.sb([128, 8, NCOL_A0], BF16, "win")
    k.dma("pool", win[:], w_in.rearrange("(k p) n -> p k n", p=128), writes=[win])
    WA = k.sb([128, 128], BF16, "WA")
    WX = k.sb([128, 128], BF16, "WX")
    wg2 = k.sb([16, 64], BF16, "wg2")
    k.dma("pool", WA[:], WAd, writes=[WA])
    k.dma("pool", WX[:], WXd, writes=[WX])
    k.dma("pool", wg2[:], wg2d, writes=[wg2])
    pc = k.sb([128, 16], F32, "pcol")
    k.dma("sp", pc[:], pcold, writes=[pc])
    dc = k.sb([128, 4], F32, "dc")
    tl = k.sb([128, 1], F32, "tl")
    k.op("act", lambda e: e.activation(out=tl[:], in_=pc[:, 7:8], func=AF.Exp, scale=-1.0), reads=[pc], writes=[tl])
    k.op("act", lambda e: e.activation(out=tl[:], in_=tl[:], func=AF.Ln, bias=1.0), reads=[tl], writes=[tl])
    k.op("dve", lambda e: e.tensor_scalar(out=dc[:, 0:1], in0=tl[:], scalar1=-8.0, scalar2=None, op0=ALU.mult), reads=[tl], writes=[dc])
    k.op("dve", lambda e: e.tensor_scalar(out=dc[:, 1:2], in0=tl[:], scalar1=-16.0, scalar2=None, op0=ALU.mult), reads=[tl], writes=[dc])
    k.op("dve", lambda e: e.tensor_scalar(out=dc[:, 2:3], in0=pc[:, 9:10], scalar1=-1.0, scalar2=None, op0=ALU.mult), reads=[pc], writes=[dc])
    rmask = k.sb([64, 8, 64], F32, "rmask")
    k.op("dve", lambda e: e.memset(rmask[:], 1.0), writes=[rmask])
    k.op("dve", lambda e: e.memset(rmask[:, :, 0:1], 0.0), writes=[rmask])
    rmask2 = rmask[:].rearrange("p a b -> p (a b)")

    banks = Banks(k, 5)
    pkg = k.ps([128, 4, 64], BF16, "pkg")
    uT = [k.sb([128, 8, 512], BF16, "uT%d" % i) for i in range(2)]
    xabuf = [k.sb([128, 515], F32, "xabuf%d" % i) for i in range(2)]
    k.op("dve", lambda e: e.memset(xabuf[0][:, 0:3], 0.0), writes=[xabuf[0]])
    hs = [k.sb([128, 512], F32, "hs%d" % i) for i in range(2)]
    Sst = k.sb([64, 128], F32, "Sst")
    k.op("dve", lambda e: e.memset(Sst[:], 0.0), writes=[Sst])
    Sbt = [k.sb([64, 128], BF16, "Sbt%d" % i) for i in range(8)]

    def sbt(shape, dtp, name):
        return k.sb(shape, dtp, name)

    ga = sbt([128, 512], F32, "ga")
    q_sb = sbt([64, 512], F32, "q_sb")
    k_sb = sbt([64, 512], F32, "k_sb")
    sog = sbt([128, 512], F32, "sog")
    gl_bf = sbt([16, 512], BF16, "gl_bf")
    v_tok = sbt([128, 4, 128], BF16, "v_tok")
    xc = sbt([128, 512], F32, "xc")
    xcb = sbt([128, 512], BF16, "xcb")
    r_sb = sbt([128, 512], F32, "r_sb")
    i_sb = sbt([128, 512], F32, "i_sb")
    a_sb = sbt([128, 512], F32, "a_sb")
    a2_sb = sbt([128, 512], F32, "a2_sb")
    t_sb = sbt([128, 512], F32, "t_sb")
    b_sb = sbt([128, 512], F32, "b_sb")
    g2_sb = sbt([128, 512], F32, "g2_sb")
    inner = sbt([128, 512], F32, "inner")
    ge = sbt([128, 512], F32, "ge")
    ya = [sbt([128, 512], BF16, "ya%d" % i) for i in range(2)]
    e1 = sbt([64, 512], F32, "e1")
    sp_ = sbt([64, 512], F32, "sp")
    cum = sbt([64, 512], F32, "cum")
    E1 = sbt([64, 512], F32, "E1")
    E2 = sbt([64, 512], F32, "E2")
    qg = sbt([64, 512], BF16, "qg")
    kg = sbt([64, 512], BF16, "kg")
    kg_tok = sbt([128, 4, 64], BF16, "kg_tok")
    attm = sbt([128, 4, 128], BF16, "attm")
    tS = sbt([64, 128], F32, "tS")
    osb = sbt([128, 512], F32, "osb")
    o2 = sbt([128, 512], F32, "o2")
    rs = sbt([128, 512], F32, "rs")
    t1 = sbt([128, 512], F32, "t1")
    ob = [sbt([128, 512], BF16, "ob%d" % i) for i in range(2)]

    def op(e, fn, r, w):
        return k.op(e, fn, reads=r, writes=w)

    out_toks = []
    for blk in range(nblk):
        u = uT[blk % 2]
        xb = xabuf[blk % 2]
        xbn = xabuf[(blk + 1) % 2]
        um.block(blk, u)

        def proj(c0, c1):
            p = banks.get()
            m = c1 - c0
            for kk in range(8):
                k.op("pe", lambda e: e.matmul(p[0:m, :], lhsT=win[:, kk, c0:c1], rhs=u[:, kk, :], start=(kk == 0), stop=(kk == 7)), reads=[win, u], writes=[p], fast=True)
            return p

        p = proj(0, 128)
        op("act", lambda e: e.activation(out=xb[:, 3:515], in_=p[:], func=AF.Copy), [p], [xb])
        p = proj(128, 256)
        op("act", lambda e: e.activation(out=ga[:], in_=p[:], func=AF.Copy), [p], [ga])
        p = proj(256, 320)
        op("dve", lambda e: e.tensor_scalar(out=q_sb[:], in0=p[0:64, :], scalar1=0.125, scalar2=None, op0=ALU.mult), [p], [q_sb])
        p = proj(320, 384)
        op("act", lambda e: e.activation(out=k_sb[:], in_=p[0:64, :], func=AF.Copy), [p], [k_sb])
        p = proj(512, 640)
        op("act", lambda e: e.activation(out=sog[:], in_=p[:], func=AF.Silu), [p], [sog])
        p = proj(640, 656)
        op("dve", lambda e: e.tensor_copy(out=gl_bf[:], in_=p[0:16, :]), [p], [gl_bf])
        p = banks.get()
        for ti in range(4):
            for kk in range(8):
                op("pe", lambda e: e.matmul(p[:, ti * 128:(ti + 1) * 128], lhsT=u[:, kk, ti * 128:(ti + 1) * 128], rhs=win[:, kk, 384:512],
                                            start=(kk == 0), stop=(kk == 7)), [win, u], [p])
        op("dve", lambda e: e.tensor_copy(out=v_tok[:].rearrange("p a b -> p (a b)"), in_=p[:]), [p], [v_tok])

        def rg_part():
            yield
            op("act", lambda e: e.activation(out=xc[:], in_=xb[:, 3:515], func=AF.Identity, bias=pc[:, 4:5], scale=pc[:, 3:4]), [xb, pc], [xc])
            for w in range(3):
                yield
                op("dve", lambda e: e.scalar_tensor_tensor(out=xc[:], in0=xb[:, w:w + 512], scalar=pc[:, w:w + 1], in1=xc[:], op0=ALU.mult, op1=ALU.add),
                   [xb, pc, xc], [xc])
            yield
            op("pool", lambda e: e.tensor_copy(out=xbn[:, 0:3], in_=xb[:, 512:515]), [xb], [xbn])
            yield
            op("act", lambda e: e.activation(out=xcb[:], in_=xc[:], func=AF.Copy), [xc], [xcb])
            p = banks.get()
            yield
            op("pe", lambda e: e.matmul(p[:], lhsT=WA[:], rhs=xcb[:], start=True, stop=True), [WA, xcb], [p])
            yield
            op("act", lambda e: e.activation(out=r_sb[:], in_=p[:], func=AF.Sigmoid, bias=pc[:, 5:6]), [p, pc], [r_sb])
            p = banks.get()
            yield
            op("pe", lambda e: e.matmul(p[:], lhsT=WX[:], rhs=xcb[:], start=True, stop=True), [WX, xcb], [p])
            yield
            op("act", lambda e: e.activation(out=i_sb[:], in_=p[:], func=AF.Sigmoid, bias=pc[:, 6:7]), [p, pc], [i_sb])
            yield
            op("act", lambda e: e.activation(out=a_sb[:], in_=r_sb[:], func=AF.Exp, scale=dc[:, 0:1]), [r_sb, dc], [a_sb])
            yield
            op("act", lambda e: e.activation(out=a2_sb[:], in_=r_sb[:], func=AF.Exp, scale=dc[:, 1:2]), [r_sb, dc], [a2_sb])
            yield
            op("act", lambda e: e.activation(out=a2_sb[:], in_=a2_sb[:], func=AF.Sqrt, bias=1.0, scale=-1.0), [a2_sb], [a2_sb])
            yield
            op("dve", lambda e: e.tensor_tensor(out=t_sb[:], in0=i_sb[:], in1=xc[:], op=ALU.mult), [i_sb, xc], [t_sb])
            yield
            op("pool", lambda e: e.tensor_tensor(out=b_sb[:], in0=t_sb[:], in1=a2_sb[:], op=ALU.mult), [t_sb, a2_sb], [b_sb])
            hcur = hs[blk % 2]
            hprev = hs[(blk + 1) % 2]
            if blk == 0:
                yield
                op("dve", lambda e: e.tensor_tensor_scan(out=hcur[:], data0=a_sb[:], data1=b_sb[:], initial=0.0, op0=ALU.mult, op1=ALU.add),
                   [a_sb, b_sb], [hcur])
            else:
                yield
                op("dve", lambda e: e.tensor_tensor_scan(out=hcur[:], data0=a_sb[:], data1=b_sb[:], initial=hprev[:, 511:512], op0=ALU.mult, op1=ALU.add),
                   [a_sb, b_sb, hprev], [hcur])
            yield
            op("act", lambda e: e.activation(out=g2_sb[:], in_=ga[:], func=AF.Square), [ga], [g2_sb])
            yield
            op("dve", lambda e: e.tensor_scalar(out=g2_sb[:], in0=g2_sb[:], scalar1=0.044715, scalar2=1.0, op0=ALU.mult, op1=ALU.add), [g2_sb], [g2_sb])
            yield
            op("pool", lambda e: e.tensor_tensor(out=inner[:], in0=g2_sb[:], in1=ga[:], op=ALU.mult), [g2_sb, ga], [inner])
            yield
            op("act", lambda e: e.activation(out=inner[:], in_=inner[:], func=AF.Sigmoid, scale=1.5957691216), [inner], [inner])
            yield
            op("dve", lambda e: e.tensor_tensor(out=ge[:], in0=ga[:], in1=inner[:], op=ALU.mult), [ga, inner], [ge])
            yab = ya[blk % 2]
            yield
            op("pool", lambda e: e.tensor_tensor(out=yab[:], in0=ge[:], in1=hcur[:], op=ALU.mult), [ge, hcur], [yab])
            out_toks.append(k.dma("sp", yT[0:128, blk * 512:(blk + 1) * 512], yab[:], reads=[yab]))

        def gla_part():
            p = banks.get()
            yield
            op("pe", lambda e: e.matmul(p[0:64, :], lhsT=wg2[:], rhs=gl_bf[:], start=True, stop=True), [wg2, gl_bf], [p])
            yield
            op("act", lambda e: e.activation(out=e1[:], in_=p[0:64, :], func=AF.Exp, bias=dc[0:64, 2:3], scale=-1.0), [p, dc], [e1])
            yield
            op("act", lambda e: e.activation(out=sp_[:], in_=e1[:], func=AF.Ln, bias=1.0), [e1], [sp_])
            yield
            op("dve", lambda e: e.tensor_tensor_scan(out=cum[:], data0=rmask2, data1=sp_[:], initial=0.0, op0=ALU.mult, op1=ALU.add), [rmask, sp_], [cum])
            yield
            op("act", lambda e: e.activation(out=E1[:], in_=cum[:], func=AF.Exp, scale=-1.0 / 16.0), [cum], [E1])
            yield
            op("act", lambda e: e.activation(out=E2[:], in_=cum[:], func=AF.Exp, scale=1.0 / 16.0), [cum], [E2])
            yield
            op("dve", lambda e: e.tensor_tensor(out=qg[:], in0=q_sb[:], in1=E1[:], op=ALU.mult), [q_sb, E1], [qg])
            yield
            op("pool", lambda e: e.tensor_tensor(out=kg[:], in0=k_sb[:], in1=E2[:], op=ALU.mult), [k_sb, E2], [kg])
            for pr in range(4):
                yield
                op("pe", lambda e: e.transpose(out=pkg[:, pr, :], in_=kg[:, pr * 128:(pr + 1) * 128], identity=cstb[0:64, C_ID, 0:64]), [kg, cstb], [pkg])
            yield
            op("act", lambda e: e.activation(out=kg_tok[:], in_=pkg[:], func=AF.Copy), [pkg], [kg_tok])
            p = banks.get()
            for pr in range(4):
                yield
                op("pe", lambda e: e.matmul(p[:, pr * 128:(pr + 1) * 128], lhsT=kg[:, pr * 128:(pr + 1) * 128], rhs=qg[:, pr * 128:(pr + 1) * 128],
                                            start=True, stop=True), [kg, qg], [p])
            yield
            op("dve", lambda e: e.tensor_tensor(out=attm[:], in0=p[:].rearrange("p (a b) -> p a b", a=4),
                                                in1=_bc(cst[:, C_UT64:C_UT64 + 1, :], [128, 4, 128]), op=ALU.mult), [p, cst], [attm])
            kva = banks.get()
            kvb = banks.get()
            for c in range(8):
                pr, half = c // 2, c % 2
                kvp = kva if c < 4 else kvb
                yield
                op("pe", lambda e: e.matmul(kvp[0:64, (c % 4) * 128:(c % 4 + 1) * 128], lhsT=kg_tok[half * 64:(half + 1) * 64, pr, :],
                                            rhs=v_tok[half * 64:(half + 1) * 64, pr, :], start=True, stop=True), [kg_tok, v_tok], [kvp])
            for c in range(8):
                kvp = kva if c < 4 else kvb
                yield
                op("act", lambda e: e.activation(out=Sbt[c][:], in_=Sst[:], func=AF.Copy), [Sst], [Sbt[c]])
                yield
                op("dve", lambda e: e.tensor_tensor(out=tS[:], in0=kvp[0:64, (c % 4) * 128:(c % 4 + 1) * 128], in1=Sst[:], op=ALU.add), [kvp, Sst], [tS])
                yield
                op("dve", lambda e: e.tensor_scalar(out=Sst[:], in0=tS[:], scalar1=E1[:, c * 64 + 63:c * 64 + 64], scalar2=None, op0=ALU.mult),
                   [tS, E1], [Sst])
            po = banks.get()
            for pr in range(4):
                yield
                op("pe", lambda e: e.matmul(po[:, pr * 128:(pr + 1) * 128], lhsT=v_tok[:, pr, :], rhs=attm[:, pr, :], start=True, stop=False),
                   [v_tok, attm], [po])
                for half in range(2):
                    c = 2 * pr + half
                    yield
                    op("pe", lambda e: e.matmul(po[:, c * 64:(c + 1) * 64], lhsT=Sbt[c][:], rhs=qg[:, c * 64:(c + 1) * 64], start=False, stop=(half == 1)),
                       [Sbt[c], qg], [po])
            yield
            op("act", lambda e: e.activation(out=osb[:], in_=po[:], func=AF.Copy), [po], [osb])
            yield
            op("act", lambda e: e.activation(out=o2[:], in_=osb[:], func=AF.Square), [osb], [o2])
            p = banks.get()
            yield
            op("pe", lambda e: e.matmul(p[:], lhsT=cst[:, C_ONES, :], rhs=o2[:], start=True, stop=True), [cst, o2], [p])
            yield
            op("act", lambda e: e.activation(out=rs[:], in_=p[:], func=AF.Sqrt, bias=EPS, scale=1.0 / 128.0), [p], [rs])
            yield
            op("dve", lambda e: e.reciprocal(out=rs[:], in_=rs[:]), [rs], [rs])
            yield
            op("dve", lambda e: e.scalar_tensor_tensor(out=t1[:], in0=osb[:], scalar=pc[:, 8:9], in1=rs[:], op0=ALU.mult, op1=ALU.mult), [osb, pc, rs], [t1])
            obb = ob[blk % 2]
            yield
            op("pool", lambda e: e.tensor_tensor(out=obb[:], in0=t1[:], in1=sog[:], op=ALU.mult), [t1, sog], [obb])
            out_toks.append(k.dma("sp", yT[128:256, blk * 512:(blk + 1) * 512], obb[:], reads=[obb]))
        gens = [rg_part(), gla_part()]
        while gens:
            nxt = []
            for g_ in gens:
                try:
                    next(g_)
                    nxt.append(g_)
                except StopIteration:
                    pass
            gens = nxt
    k.finish(out_toks)
    k.release(0)
    return nc, k


def phaseA0_inmaps(h, inp):
    maps = []
    consts = make_consts()
    w_in = inp["w_in_ab"][0]
    wa = np.ascontiguousarray(inp["w_ada"][0][:, 0:2048])
    ba = np.ascontiguousarray(inp["b_ada"][0][0:2048])
    for core in range(NCORES):
        b, hg = core // 4, core % 4
        ch = slice(hg * 128, (hg + 1) * 128)
        cols = np.concatenate([
            np.arange(hg * 128, (hg + 1) * 128),
            512 + np.arange(hg * 128, (hg + 1) * 128),
            1024 + np.arange(hg * 64, (hg + 1) * 64),
            1280 + np.arange(hg * 64, (hg + 1) * 64),
            1536 + np.arange(hg * 128, (hg + 1) * 128),
            2048 + np.arange(hg * 128, (hg + 1) * 128),
            2560 + np.arange(16),
        ])
        WA = np.zeros((128, 128), np.float32)
        WX = np.zeros((128, 128), np.float32)
        for j in range(2):
            WA[j * 64:(j + 1) * 64, j * 64:(j + 1) * 64] = inp["rg_wa"][0][hg * 2 + j]
            WX[j * 64:(j + 1) * 64, j * 64:(j + 1) * 64] = inp["rg_wx"][0][hg * 2 + j]
        pcol = np.zeros((128, 16), np.float32)
        pcol[:, 0:4] = inp["conv_a_w"][0][:, ch].T
        pcol[:, 4] = inp["conv_a_b"][0][ch]
        pcol[:, 5] = inp["rg_ba"][0][ch]
        pcol[:, 6] = inp["rg_bx"][0][ch]
        pcol[:, 7] = inp["rg_lam"][0][ch]
        pcol[:, 8] = inp["gla_norm"][0]
        pcol[0:64, 9] = inp["gla_bg2"][0][hg * 64:(hg + 1) * 64]
        maps.append({
            "h_in": np.ascontiguousarray(h[b]),
            "condT": np.ascontiguousarray(inp["c"][b].reshape(8, 128).T),
            "w_ada": wa, "b_ada": ba,
            "norm1": np.ascontiguousarray(inp["norm1"][0]),
            "w_in": np.ascontiguousarray(w_in[:, cols]),
            "WA": WA, "WX": WX,
            "wg2": np.ascontiguousarray(inp["gla_wg2"][0][:, hg * 64:(hg + 1) * 64]),
            "pcol": pcol, "consts": consts,
        })
    return maps


def assemble_yT_A0(results):
    yT = np.zeros((2, D, S), ml_dtypes.bfloat16)
    for core in range(NCORES):
        b, hg = core // 4, core % 4
        r = results[core]["yT"]
        yT[b, hg * 128:(hg + 1) * 128] = r[0:128]
        yT[b, 512 + hg * 128:512 + (hg + 1) * 128] = r[128:256]
    return yT


NCOL_A1 = 1028
import os as _os
SEQ_DEBUG = bool(_os.environ.get('SEQ_DEBUG'))


def build_phaseA1(nblk=16, stop=99):
    nc = bass.Bass("TRN2", target_bir_lowering=False)
    dt = nc.dram_tensor
    h_in = dt("h_in", [S, D], F32, kind="ExternalInput").ap()
    condT = dt("condT", [128, 8], F32, kind="ExternalInput").ap()
    w_ada = dt("w_ada", [D, 2048], F32, kind="ExternalInput").ap()
    b_ada = dt("b_ada", [2048], F32, kind="ExternalInput").ap()
    norm1 = dt("norm1", [D], F32, kind="ExternalInput").ap()
    w_in = dt("w_in", [D, NCOL_A1], F32, kind="ExternalInput").ap()
    pcold = dt("pcol", [128, 24], F32, kind="ExternalInput").ap()
    prmd = dt("prm", [128, 4], F32, kind="ExternalInput").ap()
    dnd = dt("dnorm", [128], F32, kind="ExternalInput").ap()
    alogd = dt("a_log", [128, 2], F32, kind="ExternalInput").ap()
    consts = dt("consts", [128, NCONST, 128], F32, kind="ExternalInput").ap()
    yT = dt("yT", [256, S], BF16, kind="ExternalOutput").ap()

    k = KB(nc)
    cst, cstb = load_consts(k, nc, consts)
    um = UMaker(k, nc, h_in, condT, w_ada, b_ada, norm1, cstb)
    win = k.sb([128, 8, NCOL_A1], BF16, "win")
    wv = w_in.rearrange("(k p) n -> p k n", p=128)
    for kk in range(8):
        k.dma("pool", win[:, kk, :], wv[:, kk, :], writes=[win])
    pc = k.sb([128, 24], F32, "pcol")
    k.dma("sp", pc[:], pcold, writes=[pc])
    prm = k.sb([128, 4], F32, "prm")
    k.dma("sp", prm[:], prmd, writes=[prm])
    dnb = k.sb([128, 128], F32, "dnb")
    k.dma("sp", dnb[:], dnd.partition_broadcast(128), writes=[dnb])
    alg = k.sb([128, 2], F32, "alg")
    k.dma("sp", alg[:], alogd, writes=[alg])
    k.op("act", lambda e: e.activation(out=alg[:], in_=alg[:], func=AF.Exp), reads=[alg], writes=[alg])
    k.op("dve", lambda e: e.tensor_scalar(out=prm[:, 2:4], in0=alg[:], scalar1=-1.0, scalar2=None, op0=ALU.mult), reads=[alg, prm], writes=[prm])

    banks = Banks(k, 6)
    ptr = k.ps([128, 2, 128], BF16, "ptr")
    uT = [k.sb([128, 8, 512], BF16, "uT%d" % i) for i in range(2)]
    cbuf = [[k.sb([128, 515], F32, "cbuf%d_%d" % (j, i)) for i in range(2)] for j in range(6)]
    for j in range(6):
        k.op("dve", lambda e: e.memset(cbuf[j][0][:, 0:3], 0.0), writes=[cbuf[j][0]])
    Sst = [k.sb([128, 128], F32, "Sst%d" % h) for h in range(2)]
    Sb = [k.sb([128, 128], BF16, "Sb%d" % h) for h in range(2)]
    for h in range(2):
        k.op("dve", lambda e: e.memset(Sst[h][:], 0.0), writes=[Sst[h]])
        k.op("dve", lambda e: e.memset(Sb[h][:], 0.0), writes=[Sb[h]])

    sb = k.sb
    sj = [sb([128, 512], F32, "sj%d" % j) for j in range(6)]
    sq = sb([128, 512], F32, "sq")
    rs = sb([128, 512], F32, "rs")
    nT = [sb([128, 512], BF16, "nT%d" % j) for j in range(4)]
    vTb = [sb([128, 512], BF16, "vTb%d" % h) for h in range(2)]
    sz = [sb([128, 256], F32, "sz%d" % t) for t in range(4)]
    g4 = sb([128, 4, 4], F32, "g4")
    beta = sb([128, 4, 2], F32, "beta")
    nbeta = sb([128, 4, 2], F32, "nbeta")
    gx = sb([128, 4, 2], F32, "gx")
    gg = sb([128, 4, 2], F32, "gg")
    TST = []
    for a in range(2):
        TST.append({"gch": sb([128, 4], F32, "gch%d" % a), "gcl": sb([128, 8], F32, "gcl%d" % a), "eg": sb([128, 2], F32, "eg%d" % a),
                    "ed": sb([128, 2], F32, "ed%d" % a), "be": sb([128, 2], F32, "be%d" % a), "egl": sb([128, 4], F32, "egl%d" % a)})
    GB = []
    for g_ in range(4):
        d_ = {"ps": banks.t[g_], "ptr": ptr}
        for nm in ("gm", "DTm", "Dm", "Tt", "U", "dg"):
            d_[nm] = sb([128, 128], F32, "%s%d" % (nm, g_))
        d_["AB"] = sb([128, 2, 128], F32, "AB%d" % g_)
        for nm in ("Ttb", "attT", "bv", "kbg", "kd", "WT", "qg", "vnew"):
            d_[nm] = sb([128, 128], BF16, "%s%d" % (nm, g_))
        GB.append(d_)
    o_tok = [sb([128, 4, 128], F32, "o_tok%d" % h) for h in range(2)]
    ss = sb([128, 4], F32, "oss")
    rstd = sb([128, 4], F32, "orstd")
    junk = sb([128, 128], F32, "ojunk")
    on = sb([128, 128], F32, "on")
    ytok = sb([128, 128], BF16, "ytok")
    yTs = [sb([128, 512], BF16, "yTs%d" % i) for i in range(2)]

    def op(e, fn, r, w):
        return k.op(e, fn, reads=r, writes=w)

    ident = cst[:, C_ID, :]
    TRI = cst[:, C_TRI, :]
    BLK = cst[:, C_BLK, :]
    SU = cst[:, C_SU, :]
    ONES = cst[:, C_ONES, :]
    UT64 = cst[:, C_UT64, :]
    out_toks = []
    cnt_y = 0
    for blk in range(nblk):
        u = uT[blk % 2]
        if stop <= -3:
            break
        um.block(blk, u)
        for j in range(6):
            if stop <= -2:
                break
            cb = cbuf[j][blk % 2]
            cbn = cbuf[j][(blk + 1) % 2]
            p = banks.get()
            for kk in range(8):
                k.op("pe", lambda e: e.matmul(p[:], lhsT=win[:, kk, j * 128:(j + 1) * 128], rhs=u[:, kk, :], start=(kk == 0), stop=(kk == 7)), reads=[win, u], writes=[p], fast=True)
            op("act", lambda e: e.activation(out=cb[:, 3:515], in_=p[:], func=AF.Copy), [p], [cb])
            op("act", lambda e: e.activation(out=sj[j][:], in_=cb[:, 3:515], func=AF.Copy, scale=pc[:, j * 4 + 3:j * 4 + 4]), [cb, pc], [sj[j]])
            for w in range(3):
                op("dve", lambda e: e.scalar_tensor_tensor(out=sj[j][:], in0=cb[:, w:w + 512], scalar=pc[:, j * 4 + w:j * 4 + w + 1], in1=sj[j][:],
                                                           op0=ALU.mult, op1=ALU.add), [cb, pc, sj[j]], [sj[j]])
            op("pool", lambda e: e.tensor_copy(out=cbn[:, 0:3], in_=cb[:, 512:515]), [cb], [cbn])
            op("act", lambda e: e.activation(out=sj[j][:], in_=sj[j][:], func=AF.Silu), [sj[j]], [sj[j]])
        if stop <= -1:
            break
        for j in range(4):
            op("act", lambda e: e.activation(out=sq[:], in_=sj[j][:], func=AF.Square), [sj[j]], [sq])
            p = banks.get()
            op("pe", lambda e: e.matmul(p[:], lhsT=ONES, rhs=sq[:], start=True, stop=True), [cst, sq], [p])
            op("act", lambda e: e.activation(out=rs[:], in_=p[:], func=AF.Sqrt, bias=EPS), [p], [rs])
            op("dve", lambda e: e.reciprocal(out=rs[:], in_=rs[:]), [rs], [rs])
            scl = 128.0 ** -0.5 if j < 2 else 1.0
            op("dve", lambda e: e.scalar_tensor_tensor(out=nT[j][:], in0=sj[j][:], scalar=scl, in1=rs[:], op0=ALU.mult, op1=ALU.mult), [sj[j], rs], [nT[j]])
        for h in range(2):
            op("act", lambda e: e.activation(out=vTb[h][:], in_=sj[4 + h][:], func=AF.Copy), [sj[4 + h]], [vTb[h]])
        if stop <= 0:
            break
        for ti in range(4):
            p = banks.get()
            for kk in range(8):
                op("pe", lambda e: e.matmul(p[:, 0:256], lhsT=u[:, kk, ti * 128:(ti + 1) * 128], rhs=win[:, kk, 768:1024], start=(kk == 0), stop=(kk == 7)),
                   [win, u], [p])
            for kk in range(8):
                op("pe", lambda e: e.matmul(p[:, 256:260], lhsT=u[:, kk, ti * 128:(ti + 1) * 128], rhs=win[:, kk, 1024:1028], start=(kk == 0), stop=(kk == 7)),
                   [win, u], [p])
            op("act", lambda e: e.activation(out=sz[ti][:], in_=p[:, 0:256], func=AF.Silu), [p], [sz[ti]])
            op("act", lambda e: e.activation(out=g4[:, ti, :], in_=p[:, 256:260], func=AF.Copy), [p], [g4])
        if stop <= 0.2:
            break
        op("act", lambda e: e.activation(out=beta[:], in_=g4[:, :, 0:2], func=AF.Sigmoid), [g4], [beta])
        op("dve", lambda e: e.tensor_scalar(out=nbeta[:], in0=beta[:], scalar1=-1.0, scalar2=None, op0=ALU.mult), [beta], [nbeta])
        if stop <= 0.4:
            break
        op("dve", lambda e: e.tensor_tensor(out=gx[:], in0=g4[:, :, 2:4], in1=_bc(prm[:, 0:2].unsqueeze(1), [128, 4, 2]), op=ALU.add), [g4, prm], [gx])
        if stop <= 0.6:
            break
        op("act", lambda e: e.activation(out=gx[:], in_=gx[:], func=AF.Exp), [gx], [gx])
        op("act", lambda e: e.activation(out=gx[:], in_=gx[:], func=AF.Ln, bias=1.0), [gx], [gx])
        if stop <= 0.8:
            break
        op("dve", lambda e: e.tensor_tensor(out=gg[:], in0=gx[:], in1=_bc(prm[:, 2:4].unsqueeze(1), [128, 4, 2]), op=ALU.mult), [gx, prm], [gg])

        if stop <= 1:
            break
        def tile_common(ti, st):
            gcl, eg, ed, egl, be, gch = st["gcl"], st["eg"], st["ed"], st["egl"], st["be"], st["gch"]
            op("dve", lambda e: e.tensor_tensor(out=gch[:].rearrange("p (a b) -> p a b", a=2), in0=_bc(gg[:, ti, :].unsqueeze(1), [128, 2, 2]),
                                                in1=_bc(cst[:, C_CH0, 0:2].unsqueeze(2), [128, 2, 2]), op=ALU.mult), [gg, cst], [gch])
            pg = banks.get()
            op("pe", lambda e: e.matmul(pg[:, 0:2], lhsT=TRI, rhs=gg[:, ti, :], start=True, stop=True), [cst, gg], [pg])
            op("pe", lambda e: e.matmul(pg[:, 2:4], lhsT=BLK, rhs=gg[:, ti, :], start=True, stop=True), [cst, gg], [pg])
            op("pe", lambda e: e.matmul(pg[:, 4:8], lhsT=ONES, rhs=gch[:], start=True, stop=True), [cst, gch], [pg])
            op("dve", lambda e: e.tensor_copy(out=gcl[:], in_=pg[:, 0:8]), [pg], [gcl])
            op("act", lambda e: e.activation(out=eg[:], in_=gcl[:, 0:2], func=AF.Exp), [gcl], [eg])
            op("dve", lambda e: e.tensor_tensor(out=ed[:], in0=gcl[:, 2:4], in1=gcl[:, 0:2], op=ALU.subtract), [gcl], [ed])
            op("act", lambda e: e.activation(out=ed[:], in_=ed[:], func=AF.Exp), [ed], [ed])
            op("act", lambda e: e.activation(out=egl[:], in_=gcl[:, 4:8], func=AF.Exp), [gcl], [egl])
            op("dve", lambda e: e.tensor_tensor(out=be[:], in0=beta[:, ti, :], in1=eg[:], op=ALU.mult), [beta, eg], [be])

        def prep(ti, h, st, B):
            tsl = slice(ti * 128, (ti + 1) * 128)
            qT = nT[h]
            kT = nT[2 + h]
            ps = B["ps"]
            gm, DTm, Dm, AB, Tt, Ttb, attT, bv, kbg, kd, U, WT, dg, qg = (B[n] for n in
                ("gm", "DTm", "Dm", "AB", "Tt", "Ttb", "attT", "bv", "kbg", "kd", "U", "WT", "dg", "qg"))
            eg, ed, be = st["eg"], st["ed"], st["be"]
            A_ = AB[:, 0, :]
            B_ = AB[:, 1, :]
            op("dve", lambda e: e.tensor_scalar(out=gm[:], in0=SU, scalar1=gg[:, ti, h:h + 1], scalar2=None, op0=ALU.mult), [cst, gg], [gm])
            op("pe", lambda e: e.matmul(ps[:, 0:128], lhsT=gm[:], rhs=TRI, start=True, stop=True), [gm, cst], [ps])
            op("pe", lambda e: e.matmul(ps[:, 128:256], lhsT=TRI, rhs=gm[:], start=True, stop=True), [gm, cst], [ps])
            yield
            op("act", lambda e: e.activation(out=DTm[:], in_=ps[:, 0:128], func=AF.Exp), [ps], [DTm])
            op("act", lambda e: e.activation(out=Dm[:], in_=ps[:, 128:256], func=AF.Exp), [ps], [Dm])
            op("pool", lambda e: e.tensor_tensor(out=DTm[:], in0=DTm[:], in1=UT64, op=ALU.mult), [DTm, cst], [DTm])
            op("pool", lambda e: e.tensor_tensor(out=Dm[:], in0=Dm[:], in1=SU, op=ALU.mult), [Dm, cst], [Dm])
            op("pe", lambda e: e.matmul(ps[:, 0:128], lhsT=kT[:, tsl], rhs=kT[:, tsl], start=True, stop=True), [kT], [ps])
            op("pe", lambda e: e.matmul(ps[:, 128:256], lhsT=kT[:, tsl], rhs=qT[:, tsl], start=True, stop=True), [kT, qT], [ps])
            yield
            op("dve", lambda e: e.scalar_tensor_tensor(out=A_, in0=ps[:, 0:128], scalar=nbeta[:, ti, h:h + 1], in1=Dm[:], op0=ALU.mult, op1=ALU.mult),
               [ps, nbeta, Dm], [AB])
            op("dve", lambda e: e.tensor_tensor(out=attT[:], in0=ps[:, 128:256], in1=DTm[:], op=ALU.mult), [ps, DTm], [attT])
            op("pe", lambda e: e.transpose(out=ps[:, 0:128], in_=A_, identity=ident), [AB, cst], [ps])
            yield
            op("act", lambda e: e.activation(out=B_, in_=ps[:, 0:128], func=AF.Copy), [ps], [AB])
            op("pool", lambda e: e.tensor_tensor(out=Tt[:], in0=B_, in1=ident, op=ALU.add), [AB, cst], [Tt])
            for lvl in range(1, 6):
                op("pe", lambda e: e.matmul(ps[:, 0:128], lhsT=B_, rhs=A_, start=True, stop=True), [AB], [ps])
                if lvl < 5:
                    op("pe", lambda e: e.matmul(ps[:, 128:256], lhsT=A_, rhs=B_, start=True, stop=True), [AB], [ps])
                yield
                if lvl < 5:
                    op("act", lambda e: e.activation(out=AB[:].rearrange("p a b -> p (a b)"), in_=ps[:, 0:256], func=AF.Copy), [ps], [AB])
                else:
                    op("act", lambda e: e.activation(out=A_, in_=ps[:, 0:128], func=AF.Copy), [ps], [AB])
                op("pe", lambda e: e.matmul(ps[:, 256:384], lhsT=A_, rhs=Tt[:], start=True, stop=True), [AB, Tt], [ps])
                yield
                op("dve", lambda e: e.tensor_tensor(out=Tt[:], in0=ps[:, 256:384], in1=Tt[:], op=ALU.add), [ps, Tt], [Tt])
            op("act", lambda e: e.activation(out=Ttb[:], in_=Tt[:], func=AF.Copy), [Tt], [Ttb])
            pt_ = B["ptr"]
            op("pe", lambda e: e.transpose(out=pt_[:, 0, :], in_=kT[:, tsl], identity=cstb[:, C_ID, :]), [kT, cstb], [pt_])
            op("pe", lambda e: e.transpose(out=pt_[:, 1, :], in_=vTb[h][:, tsl], identity=cstb[:, C_ID, :]), [vTb[h], cstb], [pt_])
            op("dve", lambda e: e.tensor_scalar(out=kbg[:], in0=pt_[:, 0, :], scalar1=be[:, h:h + 1], scalar2=None, op0=ALU.mult), [pt_, be], [kbg])
            op("dve", lambda e: e.tensor_scalar(out=kd[:], in0=pt_[:, 0, :], scalar1=ed[:, h:h + 1], scalar2=None, op0=ALU.mult), [pt_, ed], [kd])
            op("dve", lambda e: e.tensor_scalar(out=bv[:], in0=pt_[:, 1, :], scalar1=beta[:, ti, h:h + 1], scalar2=None, op0=ALU.mult), [pt_, beta], [bv])
            op("dve", lambda e: e.tensor_scalar(out=dg[:], in0=ident, scalar1=eg[:, h:h + 1], scalar2=None, op0=ALU.mult), [cst, eg], [dg])
            op("pe", lambda e: e.matmul(ps[:, 0:128], lhsT=Ttb[:], rhs=bv[:], start=True, stop=True), [Ttb, bv], [ps])
            op("pe", lambda e: e.matmul(ps[:, 128:256], lhsT=kbg[:], rhs=Ttb[:], start=True, stop=True), [kbg, Ttb], [ps])
            op("pe", lambda e: e.matmul(ps[:, 256:384], lhsT=ONES, rhs=dg[:], start=True, stop=True), [cst, dg], [ps])
            yield
            op("dve", lambda e: e.tensor_copy(out=U[:], in_=ps[:, 0:128]), [ps], [U])
            op("dve", lambda e: e.tensor_copy(out=WT[:], in_=ps[:, 128:256]), [ps], [WT])
            op("dve", lambda e: e.tensor_tensor(out=qg[:], in0=ps[:, 256:384], in1=qT[:, tsl], op=ALU.mult), [ps, qT], [qg])

        def chain(ti, h, st, B, half):
            rows = slice(half * 64, (half + 1) * 64)
            egl = st["egl"]
            U, WT, qg, attT, kd, vnew = B["U"], B["WT"], B["qg"], B["attT"], B["kd"], B["vnew"]
            pw = banks.get()
            op("pe", lambda e: e.matmul(pw[rows, 0:128], lhsT=WT[:, rows], rhs=Sb[h][:], start=True, stop=True), [WT, Sb[h]], [pw])
            yield
            op("dve", lambda e: e.tensor_tensor(out=vnew[rows, :], in0=U[rows, :], in1=pw[rows, 0:128], op=ALU.subtract), [U, pw], [vnew])
            po = banks.get()
            op("pe", lambda e: e.matmul(po[rows, 0:128], lhsT=qg[:, rows], rhs=Sb[h][:], start=True, stop=False), [qg, Sb[h]], [po])
            op("pe", lambda e: e.matmul(po[rows, 0:128], lhsT=attT[rows, rows], rhs=vnew[rows, :], start=False, stop=True), [attT, vnew], [po])
            pk = banks.get()
            op("pe", lambda e: e.matmul(pk[:, 0:128], lhsT=kd[rows, :], rhs=vnew[rows, :], start=True, stop=True), [kd, vnew], [pk])
            yield
            op("dve", lambda e: e.scalar_tensor_tensor(out=Sst[h][:], in0=Sst[h][:], scalar=egl[:, half * 2 + h:half * 2 + h + 1], in1=pk[:, 0:128],
                                                       op0=ALU.mult, op1=ALU.add), [Sst[h], egl, pk], [Sst[h]])
            op("act", lambda e: e.activation(out=Sb[h][:], in_=Sst[h][:], func=AF.Copy), [Sst[h]], [Sb[h]])
            op("act", lambda e: e.activation(out=o_tok[h][rows, ti, :], in_=po[rows, 0:128], func=AF.Copy), [po], [o_tok[h]])

        def run_interleaved(gens):
            gens = list(gens)
            if SEQ_DEBUG:
                for g in gens:
                    for _ in g:
                        pass
                return
            while gens:
                nxt = []
                for g in gens:
                    try:
                        next(g)
                        nxt.append(g)
                    except StopIteration:
                        pass
                gens = nxt

        for tp in range(2):
            tis = (2 * tp, 2 * tp + 1)
            for a, ti in enumerate(tis):
                tile_common(ti, TST[a])
            run_interleaved([prep(ti, h, TST[a], GB[a * 2 + h]) for a, ti in enumerate(tis) for h in range(2)])
            for a, ti in enumerate(tis):
                for half in range(2):
                    run_interleaved([chain(ti, h, TST[a], GB[a * 2 + h], half) for h in range(2)])

        for h in range(2):
            if stop <= 5:
                break
            for ti in range(4):
                op("act", lambda e: e.activation(out=junk[:], in_=o_tok[h][:, ti, :], func=AF.Square, accum_out=ss[:, ti:ti + 1]), [o_tok[h]], [junk, ss])
            rstd_from_ss(k, ss, rstd, 4, 1.0 / 128.0)
            ys = yTs[cnt_y % 2]
            cnt_y += 1
            for ti in range(4):
                op("dve", lambda e: e.scalar_tensor_tensor(out=on[:], in0=o_tok[h][:, ti, :], scalar=rstd[:, ti:ti + 1], in1=dnb[:], op0=ALU.mult, op1=ALU.mult),
                   [o_tok[h], rstd, dnb], [on])
                op("pool", lambda e: e.tensor_tensor(out=ytok[:], in0=on[:], in1=sz[ti][:, h * 128:(h + 1) * 128], op=ALU.mult), [on, sz[ti]], [ytok])
                op("pe", lambda e: e.transpose(out=ptr[:, 0, :], in_=ytok[:], identity=cstb[:, C_ID, :]), [ytok, cstb], [ptr])
                op("act", lambda e: e.activation(out=ys[:, ti * 128:(ti + 1) * 128], in_=ptr[:, 0, :], func=AF.Copy), [ptr], [ys])
            out_toks.append(k.dma("sp", yT[h * 128:(h + 1) * 128, blk * 512:(blk + 1) * 512], ys[:], reads=[ys]))
    k.finish(out_toks)
    k.release(0)
    return nc, k


def phaseA1_inmaps(h, inp):
    maps = []
    consts = make_consts()
    w_in = inp["w_in_c"][0]
    wa = np.ascontiguousarray(inp["w_ada"][1][:, 0:2048])
    ba = np.ascontiguousarray(inp["b_ada"][1][0:2048])
    cw = inp["conv_c_w"][0]
    for core in range(NCORES):
        b, hg = core // 4, core % 4
        hs = [2 * hg, 2 * hg + 1]
        cols = []
        for base in (0, 1024, 2048):
            for hh in hs:
                cols.append(base + np.arange(hh * 128, (hh + 1) * 128))
        for hh in hs:
            cols.append(3072 + np.arange(hh * 128, (hh + 1) * 128))
        cols.append(np.array([4096 + hs[0], 4096 + hs[1], 4104 + hs[0], 4104 + hs[1]]))
        cols = np.concatenate(cols)
        pcol = np.zeros((128, 6, 4), np.float32)
        j = 0
        for base in (0, 1024, 2048):
            for hh in hs:
                pcol[:, j, :] = cw[:, base + hh * 128:base + (hh + 1) * 128].T
                j += 1
        prm = np.zeros((128, 4), np.float32)
        prm[:, 0] = inp["dn_dt_bias"][0][hs[0]]
        prm[:, 1] = inp["dn_dt_bias"][0][hs[1]]
        maps.append({
            "h_in": np.ascontiguousarray(h[b]),
            "condT": np.ascontiguousarray(inp["c"][b].reshape(8, 128).T),
            "w_ada": wa, "b_ada": ba,
            "norm1": np.ascontiguousarray(inp["norm1"][1]),
            "w_in": np.ascontiguousarray(w_in[:, cols]),
            "pcol": np.ascontiguousarray(pcol.reshape(128, 24)),
            "prm": prm,
            "a_log": np.ascontiguousarray(np.tile(inp["dn_a_log"][0][hs][None, :], (128, 1))),
            "dnorm": np.ascontiguousarray(inp["dn_norm"][0]),
            "consts": consts,
        })
    return maps


def assemble_yT_A1(results):
    yT = np.zeros((2, D, S), ml_dtypes.bfloat16)
    for core in range(NCORES):
        b, hg = core // 4, core % 4
        yT[b, hg * 256:(hg + 1) * 256] = results[core]["yT"]
    return yT


_PROGS = {}


def _prog(name):
    if name not in _PROGS:
        if name == "A0":
            _PROGS[name] = build_phaseA0()[0]
        elif name == "A1":
            _PROGS[name] = build_phaseA1()[0]
        elif name == "B0":
            _PROGS[name] = build_phaseB(False)[0]
        else:
            _PROGS[name] = build_phaseB(True)[0]
    return _PROGS[name]


def _gather_B(results):
    return np.stack([np.concatenate([results[b * 4 + q]["out"] for q in range(4)], 0) for b in range(2)])


def kernel(**inputs):
    inp = {k_: np.ascontiguousarray(np.asarray(v, dtype=np.float32)) for k_, v in inputs.items()}
    cores = list(range(NCORES))
    x = inp["x"]
    r = run_bass_kernel_spmd(_prog("A0"), phaseA0_inmaps(x, inp), core_ids=cores)
    yT0 = assemble_yT_A0(r.results)
    r = run_bass_kernel_spmd(_prog("B0"), phaseB_inmaps(0, x, yT0, inp, False), core_ids=cores)
    h0 = _gather_B(r.results)
    r = run_bass_kernel_spmd(_prog("A1"), phaseA1_inmaps(h0, inp), core_ids=cores)
    yT1 = assemble_yT_A1(r.results)
    r = run_bass_kernel_spmd(_prog("B1"), phaseB_inmaps(1, h0, yT1, inp, True), core_ids=cores)
    return _gather_B(r.results).astype(np.float32)
```
